# Optimizing a Trainium2 kernel written in Bass

```python
import math
import jax
import jax.numpy as jnp
from jax import lax
import numpy as np

D_MODEL = 1024
BATCH = 8
SEQ = 2048
DEPTH = 2

CTX_LEN = 256
GRID_W = 64
HEAD_DIM = 64
N_BRANCH = 4
BRANCH_W = 256
A_HEADS = 4
A_KV_HEADS = 2
A_WINDOW = 128
A_BLOCK = 128
B_HEADS = 4
B_HEAD_DIM = 64
B_WIDTH = B_HEADS * B_HEAD_DIM
B_LORA_W = 32
B_LORA_A = 32
B_LORA_G = 64
B_STREAM = 3 * B_WIDTH + B_LORA_W + B_LORA_A + B_LORA_G
B_LN_EPS = 64e-5
C_HEADS = 4
C_QK_DIM = 32
C_V_DIM = 64
C_BLOCK = 128
D_HEADS = 4
NA_ROWS = 8
NA_COLS = 16
N_EXPERTS = 16
N_GROUPS = 4
TOP_K = 2
GROUP_SCORE_K = 2
D_EXPERT = 512
ROPE_BASE = 10000.0
EPS = 1e-6
NEG_INF = -1e30
F32 = jnp.float32

SPLITS = (A_HEADS * HEAD_DIM, A_KV_HEADS * HEAD_DIM, A_KV_HEADS * HEAD_DIM, B_STREAM,
          C_HEADS * 2 * C_QK_DIM, C_HEADS * 2 * C_QK_DIM, C_HEADS * C_V_DIM,
          D_HEADS * HEAD_DIM, D_HEADS * HEAD_DIM, D_HEADS * HEAD_DIM, N_BRANCH * D_MODEL)
D_IN = sum(SPLITS)

kernel_name = 'hybrid_parallel_mixer_moe_dit'


def split_last(z, sizes):
    out = []
    start = 0
    for size in sizes:
        out.append(z[..., start:start + size])
        start += size
    return out


def heads(u, n):
    return u.reshape(u.shape[:-1] + (n, u.shape[-1] // n))


def rms_norm(x, g, eps=EPS):
    xf = x.astype(F32)
    y = xf * lax.rsqrt(jnp.mean(xf * xf, axis=-1, keepdims=True) + eps)
    return (y * g.astype(F32)).astype(x.dtype)


def modulate(h, shift, scale):
    return h * (1 + scale[..., None, :]) + shift[..., None, :]


def rope_axis(x, pos):
    h = x.shape[-1]
    inv = ROPE_BASE ** (-jnp.arange(0, h, 2, dtype=F32) / h)
    ang = pos.astype(F32)[:, None] * inv[None, :]
    shape = (1, pos.shape[0]) + (1,) * (x.ndim - 3) + (h // 2,)
    cos = jnp.cos(ang).reshape(shape)
    sin = jnp.sin(ang).reshape(shape)
    xf = x.astype(F32)
    x1, x2 = xf[..., :h // 2], xf[..., h // 2:]
    return jnp.concatenate([x1 * cos - x2 * sin, x1 * sin + x2 * cos], -1).astype(x.dtype)


def rope_2d(x, rows, cols):
    h = x.shape[-1] // 2
    return jnp.concatenate([rope_axis(x[..., :h], rows), rope_axis(x[..., h:], cols)], -1)


def dense_attention(q, k, v, sink=None):
    B, T, Hq, d = q.shape
    Hkv = k.shape[2]
    G = Hq // Hkv
    Lk = k.shape[1]
    qg = q.reshape(B, T, Hkv, G, d)
    s = jnp.einsum('bthgd,blhd->bhgtl', qg, k).astype(F32) * (d ** -0.5)
    if sink is not None:
        sk = jnp.broadcast_to(sink.astype(F32).reshape(1, Hkv, G, 1, 1), s.shape[:-1] + (1,))
        s = jnp.concatenate([s, sk], -1)
    p = jax.nn.softmax(s, axis=-1)[..., :Lk]
    o = jnp.einsum('bhgtl,blhd->bthgd', p.astype(v.dtype), v)
    return o.reshape(B, T, Hq * d)


def window_attention(q, k, v, kc, vc, sink):
    B, S, Hq, d = q.shape
    Hkv = k.shape[2]
    G = Hq // Hkv
    L = kc.shape[1]
    nb = S // A_BLOCK
    qb = q.reshape(B, nb, A_BLOCK, Hkv, G, d)

    def band(u):
        up = jnp.pad(u, ((0, 0), (A_BLOCK, A_BLOCK), (0, 0), (0, 0))).reshape(B, nb + 2, A_BLOCK, Hkv, d)
        return jnp.concatenate([up[:, :-2], up[:, 1:-1], up[:, 2:]], axis=2)

    kb, vb = band(k), band(v)
    qpos = jnp.arange(S).reshape(nb, A_BLOCK)
    kpos = jnp.arange(-A_BLOCK, S + A_BLOCK).reshape(nb + 2, A_BLOCK)
    kpos = jnp.concatenate([kpos[:-2], kpos[1:-1], kpos[2:]], axis=1)
    valid = ((jnp.abs(qpos[:, :, None] - kpos[:, None, :]) <= A_WINDOW)
             & (kpos[:, None, :] >= 0) & (kpos[:, None, :] < S))
    scale = d ** -0.5
    s_loc = jnp.einsum('bnqhgd,bnkhd->bnhgqk', qb, kb).astype(F32) * scale
    s_loc = jnp.where(valid[None, :, None, None], s_loc, NEG_INF)
    s_ctx = jnp.einsum('bnqhgd,blhd->bnhgql', qb, kc).astype(F32) * scale
    sk = jnp.broadcast_to(sink.astype(F32).reshape(1, 1, Hkv, G, 1, 1), s_ctx.shape[:-1] + (1,))
    p = jax.nn.softmax(jnp.concatenate([s_loc, s_ctx, sk], -1), axis=-1).astype(v.dtype)
    nk = 3 * A_BLOCK
    o = (jnp.einsum('bnhgqk,bnkhd->bnqhgd', p[..., :nk], vb)
         + jnp.einsum('bnhgql,blhd->bnqhgd', p[..., nk:nk + L], vc))
    return o.reshape(B, S, Hq * d)


def diff_attend(q, k, v, lam):
    s = jnp.einsum('bthmd,bkhmd->bhmtk', q, k).astype(F32) * (q.shape[-1] ** -0.5)
    p = jax.nn.softmax(s, axis=-1)
    w = p[:, :, 0] - lam * p[:, :, 1]
    return jnp.einsum('bhtk,bkhd->bthd', w.astype(v.dtype), v)


def diff_attention(q, k, v, kc, vc, lam):
    B, S = q.shape[:2]
    nb = S // C_BLOCK
    k_all = jnp.concatenate([k, kc], axis=1)
    v_all = jnp.concatenate([v, vc], axis=1)
    q_blocks = jnp.moveaxis(q.reshape((B, nb, C_BLOCK) + q.shape[2:]), 1, 0)
    o = lax.map(lambda qb: diff_attend(qb, k_all, v_all, lam), q_blocks)
    return jnp.moveaxis(o, 0, 1).reshape((B, S) + o.shape[-2:])


def neighbourhood_attention(q, k, v, kc, vc, rpb):
    B, S, H, d = q.shape
    R = S // GRID_W
    kh = min(NA_ROWS, R)
    kw = NA_COLS
    qg = q.reshape(B, R, GRID_W, H, d)
    kg = k.reshape(B, R, GRID_W, H, d)
    vg = v.reshape(B, R, GRID_W, H, d)
    r = jnp.arange(R)
    row_idx = jnp.clip(r - kh // 2, 0, R - kh)[:, None] + jnp.arange(kh)[None, :]
    kn = kg[:, row_idx]
    vn = vg[:, row_idx]
    cidx = jnp.arange(GRID_W)
    c_start = jnp.clip(cidx - kw // 2, 0, GRID_W - kw)
    col_ok = (cidx[None, :] >= c_start[:, None]) & (cidx[None, :] < c_start[:, None] + kw)
    dr = row_idx - r[:, None] + NA_ROWS - 1
    dc = jnp.clip(cidx[None, :] - cidx[:, None] + NA_COLS - 1, 0, 2 * NA_COLS - 2)
    bias = rpb.astype(F32)[:, dr[:, None, :, None], dc[None, :, None, :]]
    bias = jnp.transpose(bias, (1, 0, 2, 3, 4))
    scale = d ** -0.5
    s = jnp.einsum('brqhd,brjkhd->brhqjk', qg, kn).astype(F32) * scale + bias[None]
    s = jnp.where(col_ok[:, None, :], s, NEG_INF)
    nloc = kh * GRID_W
    s = s.reshape(B, R, H, GRID_W, nloc)
    s_ctx = jnp.einsum('brqhd,blhd->brhql', qg, kc).astype(F32) * scale
    p = jax.nn.softmax(jnp.concatenate([s, s_ctx], -1), axis=-1).astype(v.dtype)
    p_loc = p[..., :nloc].reshape(B, R, H, GRID_W, kh, GRID_W)
    o = (jnp.einsum('brhqjk,brjkhd->brqhd', p_loc, vn)
         + jnp.einsum('brhql,blhd->brqhd', p[..., nloc:], vc))
    return o.reshape(B, S, H * d)


def centred_shift(z, mu):
    prev = jnp.pad(z[:, :-1], ((0, 0), (1, 0), (0, 0)))
    nxt = jnp.pad(z[:, 1:], ((0, 0), (0, 1), (0, 0)))
    return z + mu[0] * (prev - z) + mu[1] * (nxt - z)


def rwkv7_prepare(z, b_shift, b_w0, b_w2, b_a0, b_a2, b_g2, b_kk, b_ka):
    z = centred_shift(z, b_shift)
    r, k, v, wl, al, gl = split_last(z, (B_WIDTH, B_WIDTH, B_WIDTH, B_LORA_W, B_LORA_A, B_LORA_G))
    hf = lambda u: heads(u.astype(F32), B_HEADS)
    a = jax.nn.sigmoid((b_a0 + al @ b_a2).astype(F32))
    g = jax.nn.sigmoid(gl) @ b_g2
    kk = hf(k * b_kk)
    kk = kk * lax.rsqrt(jnp.maximum(jnp.sum(kk * kk, -1, keepdims=True), 1e-24))
    k_mod = hf(k.astype(F32) * (1.0 + (a - 1.0) * b_ka.astype(F32)))
    wt = jnp.tanh(wl)
    decays = tuple(
        hf(jnp.exp(-jnp.exp(-jax.nn.softplus(-(b_w0[i] + wt @ b_w2[i]).astype(F32)) - 0.5)))
        for i in range(2))
    return hf(r), k_mod, hf(v), kk, kk * hf(a), decays, g


def rwkv7_scan(state, r, w, k, v, kk, b, reverse):
    def step(S, inp):
        r_t, w_t, k_t, v_t, kk_t, b_t = inp
        sa = jnp.einsum('bhvk,bhk->bhv', S, kk_t)
        S = S * w_t[:, :, None, :] - sa[..., None] * b_t[:, :, None, :] + v_t[..., None] * k_t[:, :, None, :]
        return S, jnp.einsum('bhvk,bhk->bhv', S, r_t)

    xs = tuple(jnp.moveaxis(u, 1, 0) for u in (r, w, k, v, kk, b))
    S, ys = lax.scan(step, state, xs, reverse=reverse)
    return S, jnp.moveaxis(ys, 0, 1)


def rwkv7_readout(y, r, k, v, g, b_rk, b_lnx):
    mu = jnp.mean(y, -1, keepdims=True)
    var = jnp.mean(jnp.square(y - mu), -1, keepdims=True)
    yn = (y - mu) * lax.rsqrt(var + B_LN_EPS)
    bonus = jnp.sum(r * k * heads(b_rk.astype(F32), B_HEADS), -1, keepdims=True) * v
    shp = y.shape[:-2] + (B_WIDTH,)
    out = yn.reshape(shp) * b_lnx[0].astype(F32) + b_lnx[1].astype(F32) + bonus.reshape(shp)
    return (out * g.astype(F32)).astype(g.dtype)


def rwkv7_mixer(z, zc, need_ctx, b_shift, b_w0, b_w2, b_a0, b_a2, b_g2, b_kk, b_ka, b_rk, b_lnx):
    r, k, v, kk, bb, dec, g = rwkv7_prepare(z, b_shift, b_w0, b_w2, b_a0, b_a2, b_g2, b_kk, b_ka)
    rc, kc, vc, kkc, bbc, decc, gc = rwkv7_prepare(zc, b_shift, b_w0, b_w2, b_a0, b_a2, b_g2, b_kk, b_ka)
    zero = jnp.zeros((z.shape[0], B_HEADS, B_HEAD_DIM, B_HEAD_DIM), F32)
    y = 0.0
    yc = 0.0
    for i, rev in enumerate((False, True)):
        s_ctx, yc_i = rwkv7_scan(zero, rc, decc[i], kc, vc, kkc, bbc, rev)
        _, y_i = rwkv7_scan(s_ctx, r, dec[i], k, v, kk, bb, rev)
        y = y + y_i
        yc = yc + yc_i
    out = rwkv7_readout(y, r, k, v, g, b_rk, b_lnx)
    out_c = rwkv7_readout(yc, rc, kc, vc, gc, b_rk, b_lnx) if need_ctx else None
    return out, out_c


def merge(branches, gate_logits, w_branch, w_out):
    ys = jnp.stack(branches, axis=-2)
    proj = jnp.einsum('btnw,nwd->btnd', ys, w_branch)
    g = jax.nn.sigmoid(gate_logits.reshape(gate_logits.shape[:-1] + (N_BRANCH, D_MODEL)).astype(F32))
    return jnp.einsum('btnd,btnd->btd', g.astype(proj.dtype), proj) @ w_out


def token_mixer(h, hc, layer, need_ctx, w_in, a_qk_norm, a_sink, b_shift, b_w0, b_w2, b_a0, b_a2, b_g2,
                b_kk, b_ka, b_rk, b_lnx, c_qk_norm, c_lambda, c_subln, d_qk_norm, d_rpb, w_branch, w_out):
    S = h.shape[1]
    t = jnp.arange(S)
    rows, cols = t // GRID_W, t % GRID_W
    aq, ak, av, bz, cq, ck, cv, dq, dk, dv, gates = split_last(h @ w_in, SPLITS)
    aqc, akc, avc, bzc, cqc, ckc, cvc, dqc, dkc, dvc, gates_c = split_last(hc @ w_in, SPLITS)

    q_a = rope_2d(rms_norm(heads(aq, A_HEADS), a_qk_norm[0]), rows, cols)
    k_a = rope_2d(rms_norm(heads(ak, A_KV_HEADS), a_qk_norm[1]), rows, cols)
    v_a = heads(av, A_KV_HEADS)
    k_ac = rms_norm(heads(akc, A_KV_HEADS), a_qk_norm[1])
    v_ac = heads(avc, A_KV_HEADS)
    y_a = window_attention(q_a, k_a, v_a, k_ac, v_ac, a_sink)

    y_b, yc_b = rwkv7_mixer(bz, bzc, need_ctx, b_shift, b_w0, b_w2, b_a0, b_a2, b_g2, b_kk, b_ka, b_rk, b_lnx)

    lam_init = 0.8 - 0.6 * math.exp(-0.3 * layer)
    lv = c_lambda.astype(F32)
    lam = jnp.exp(jnp.sum(lv[0] * lv[1])) - jnp.exp(jnp.sum(lv[2] * lv[3])) + lam_init
    cheads = lambda u: u.reshape(u.shape[:-1] + (C_HEADS, 2, C_QK_DIM))
    q_c = rope_2d(rms_norm(cheads(cq), c_qk_norm[0]), rows, cols)
    k_c = rope_2d(rms_norm(cheads(ck), c_qk_norm[1]), rows, cols)
    v_c = heads(cv, C_HEADS)
    k_cc = rms_norm(cheads(ckc), c_qk_norm[1])
    v_cc = heads(cvc, C_HEADS)

    def c_out(o):
        o = rms_norm(o, c_subln) * (1.0 - lam_init)
        return o.reshape(o.shape[:-2] + (C_HEADS * C_V_DIM,))

    y_c = c_out(diff_attention(q_c, k_c, v_c, k_cc, v_cc, lam))

    q_d = rms_norm(heads(dq, D_HEADS), d_qk_norm[0])
    k_d = rms_norm(heads(dk, D_HEADS), d_qk_norm[1])
    v_d = heads(dv, D_HEADS)
    k_dc = rms_norm(heads(dkc, D_HEADS), d_qk_norm[1])
    v_dc = heads(dvc, D_HEADS)
    y_d = neighbourhood_attention(q_d, k_d, v_d, k_dc, v_dc, d_rpb)

    y = merge((y_a, y_b, y_c, y_d), gates, w_branch, w_out)
    if not need_ctx:
        return y, None
    yc_a = dense_attention(rms_norm(heads(aqc, A_HEADS), a_qk_norm[0]), k_ac, v_ac, a_sink)
    yc_c = c_out(diff_attend(rms_norm(cheads(cqc), c_qk_norm[0]), k_cc, v_cc, lam))
    yc_d = dense_attention(rms_norm(heads(dqc, D_HEADS), d_qk_norm[0]), k_dc, v_dc)
    yc = merge((yc_a, yc_b, yc_c, yc_d), gates_c, w_branch, w_out)
    return y, yc


def moe(h, router_w, router_b, e_gate, e_up, e_down):
    scores = jax.nn.sigmoid((h @ router_w).astype(F32))
    biased = scores + router_b.astype(F32)
    grouped = biased.reshape(biased.shape[:-1] + (N_GROUPS, N_EXPERTS // N_GROUPS))
    group_score = jnp.sum(lax.top_k(grouped, GROUP_SCORE_K)[0], axis=-1)
    gsel = jnp.argmax(group_score, axis=-1)
    in_group = jnp.arange(N_GROUPS) == gsel[..., None]
    masked = jnp.where(in_group[..., None], grouped, NEG_INF).reshape(biased.shape)
    _, idx = lax.top_k(masked, TOP_K)
    w = jnp.take_along_axis(scores, idx, axis=-1)
    w = w / jnp.sum(w, axis=-1, keepdims=True)
    gate = jnp.sum(jax.nn.one_hot(idx, N_EXPERTS, dtype=F32) * w[..., None], axis=-2)
    y = jnp.zeros_like(h)
    for e in range(N_EXPERTS):
        hid = jax.nn.silu(h @ e_gate[e]) * (h @ e_up[e])
        y = y + gate[..., e:e + 1].astype(h.dtype) * (hid @ e_down[e])
    return y


def setup_inputs(seed: int = 0) -> dict:
    key = jax.random.key(seed)
    keys = jax.random.split(key, 48)
    cnt = [0]

    def nxt():
        k = keys[cnt[0]]
        cnt[0] += 1
        return k

    def nrm(shape, s):
        return s * jax.random.normal(nxt(), shape, F32)

    def unif(shape, lo, hi):
        return jax.random.uniform(nxt(), shape, F32, minval=lo, maxval=hi)

    D = D_MODEL
    return {
        'x': nrm((BATCH, SEQ, D), 1.0),
        'c': nrm((BATCH, D), 1.0),
        'ctx': nrm((BATCH, CTX_LEN, D), 1.0),
        'c_ctx': nrm((D,), 1.0),
        'w_mod': nrm((DEPTH, D, 6 * D), 0.5 * D ** -0.5),
        'b_mod': nrm((DEPTH, 6 * D), 0.02),
        'norm1': 1.0 + nrm((DEPTH, D), 0.02),
        'norm2': 1.0 + nrm((DEPTH, D), 0.02),
        'w_in': nrm((DEPTH, D, D_IN), D ** -0.5),
        'a_qk_norm': 1.0 + nrm((DEPTH, 2, HEAD_DIM), 0.02),
        'a_sink': nrm((DEPTH, A_HEADS), 0.5),
        'b_shift': unif((DEPTH, 2, B_STREAM), 0.0, 0.5),
        'b_w0': unif((DEPTH, 2, B_WIDTH), -5.0, 0.5),
        'b_w2': nrm((DEPTH, 2, B_LORA_W, B_WIDTH), 0.1),
        'b_a0': nrm((DEPTH, B_WIDTH), 0.1),
        'b_a2': nrm((DEPTH, B_LORA_A, B_WIDTH), 0.1),
        'b_g2': nrm((DEPTH, B_LORA_G, B_WIDTH), B_LORA_G ** -0.5),
        'b_kk': 0.85 + nrm((DEPTH, B_WIDTH), 0.02),
        'b_ka': 1.0 + nrm((DEPTH, B_WIDTH), 0.02),
        'b_rk': nrm((DEPTH, B_WIDTH), 0.1),
        'b_lnx': jnp.stack([1.0 + nrm((DEPTH, B_WIDTH), 0.02), nrm((DEPTH, B_WIDTH), 0.02)], axis=1),
        'c_qk_norm': 1.0 + nrm((DEPTH, 2, C_QK_DIM), 0.02),
        'c_lambda': nrm((DEPTH, 4, C_QK_DIM), 0.1),
        'c_subln': 1.0 + nrm((DEPTH, C_V_DIM), 0.02),
        'd_qk_norm': 1.0 + nrm((DEPTH, 2, HEAD_DIM), 0.02),
        'd_rpb': nrm((DEPTH, D_HEADS, 2 * NA_ROWS - 1, 2 * NA_COLS - 1), 0.1),
        'w_branch': nrm((DEPTH, N_BRANCH, BRANCH_W, D), BRANCH_W ** -0.5),
        'w_out': nrm((DEPTH, D, D), D ** -0.5),
        'router_w': nrm((D, N_EXPERTS), D ** -0.5),
        'router_b': nrm((N_EXPERTS,), 0.01),
        'e_gate': nrm((DEPTH, N_EXPERTS, D, D_EXPERT), D ** -0.5),
        'e_up': nrm((DEPTH, N_EXPERTS, D, D_EXPERT), D ** -0.5),
        'e_down': nrm((DEPTH, N_EXPERTS, D_EXPERT, D), D_EXPERT ** -0.5),
    }


def reference(x, c, ctx, c_ctx, w_mod, b_mod, norm1, norm2, w_in, a_qk_norm, a_sink, b_shift, b_w0, b_w2,
              b_a0, b_a2, b_g2, b_kk, b_ka, b_rk, b_lnx, c_qk_norm, c_lambda, c_subln, d_qk_norm, d_rpb,
              w_branch, w_out, router_w, router_b, e_gate, e_up, e_down):
    xc = ctx
    sc = jax.nn.silu(c)
    scc = jax.nn.silu(c_ctx)
    for l in range(DEPTH):
        need_ctx = l < DEPTH - 1
        mod = split_last(sc @ w_mod[l] + b_mod[l], (D_MODEL,) * 6)
        modc = split_last(scc @ w_mod[l] + b_mod[l], (D_MODEL,) * 6)
        h = modulate(rms_norm(x, norm1[l]), mod[0], mod[1])
        hc = modulate(rms_norm(xc, norm1[l]), modc[0], modc[1])
        y, yc = token_mixer(h, hc, l, need_ctx, w_in[l], a_qk_norm[l], a_sink[l], b_shift[l], b_w0[l],
                            b_w2[l], b_a0[l], b_a2[l], b_g2[l], b_kk[l], b_ka[l], b_rk[l], b_lnx[l],
                            c_qk_norm[l], c_lambda[l], c_subln[l], d_qk_norm[l], d_rpb[l], w_branch[l], w_out[l])
        x = x + mod[2][:, None, :] * y
        h2 = modulate(rms_norm(x, norm2[l]), mod[3], mod[4])
        x = x + mod[5][:, None, :] * moe(h2, router_w, router_b, e_gate[l], e_up[l], e_down[l])
        if need_ctx:
            xc = xc + modc[2] * yc
            hc2 = modulate(rms_norm(xc, norm2[l]), modc[3], modc[4])
            xc = xc + modc[5] * moe(hc2, router_w, router_b, e_gate[l], e_up[l], e_down[l])
    return x
```

```python
import math
import numpy as np
from contextlib import ExitStack
import concourse.bass as bass
import concourse.mybir as mybir
from concourse.bass_utils import run_bass_kernel_spmd

F32 = mybir.dt.float32
F32R = mybir.dt.float32r
AF = mybir.ActivationFunctionType
ALU = mybir.AluOpType
AX = mybir.AxisListType
ALLENG = ("tensor", "vector", "scalar", "gpsimd", "sync")

D = 1024
SEQ = 2048
CTX = 256
NT = 18
NTOK = SEQ + CTX
DEPTH = 2
D_IN = 7040
O_AQ, O_AK, O_AV, O_BZ, O_CQ, O_CK, O_CV, O_DQ, O_DK, O_DV, O_G = 0, 256, 384, 512, 1408, 1664, 1920, 2176, 2432, 2688, 2944
EPS = 1e-6
NE = 16
DE = 512


import re as _re
_PSUM_RE = _re.compile(r"(^E_p[GUY]|^M_pr|^pm\d|^ptr\d|^pz\d|_ptr|_pS|_pO|_pl\d|_pd\d|_pag|_pb\d|^pq\d|^M_p)")


def is_psum_key(k):
    return isinstance(k, str) and _PSUM_RE.search(k) is not None


class Sched:
    def __init__(self, nc):
        self.nc = nc
        self.seq = {e: 0 for e in ALLENG}
        self.res = {}
        self.waited = {e: {} for e in ALLENG}
        self.dma_cnt = {}
        self.out_tokens = []
        self.n_dma_sems = 0
        self.dma_sem_of = {}
        self.sems = {}
        self.count = {e: 0 for e in ALLENG}

    def _need(self, eng, tok, waits):
        if tok is None:
            return
        semkey, val, _ = tok
        w = self.waited[eng]
        if w.get(semkey, 0) >= val:
            return
        w[semkey] = val
        waits.append((semkey, val))

    def _deps(self, eng, rd, wr, is_dma):
        waits = []
        for k in rd:
            st = self.res.get(k)
            if st is None:
                continue
            wtok = st[0]
            if wtok is not None:
                self._need(eng, wtok, waits)
        for k in wr:
            st = self.res.get(k)
            if st is None:
                continue
            wtok = st[0]
            if wtok is not None:
                if is_dma or wtok[2] != eng or eng != "tensor":
                    self._need(eng, wtok, waits)
            for rtok in st[1].values():
                if is_dma or rtok[2] != eng or eng != "tensor":
                    self._need(eng, rtok, waits)
        return waits

    def _record(self, tok, rd, wr):
        for k in rd:
            st = self.res.setdefault(k, [None, {}])
            old = st[1].get(tok[0])
            if old is None or old[1] < tok[1]:
                st[1][tok[0]] = tok
        for k in wr:
            self.res[k] = [tok, {}]

    def _sem(self, semkey):
        h = self.sems.get(semkey)
        if h is None:
            h = self.nc.alloc_semaphore("sem_%s_%s" % semkey)
            self.sems[semkey] = h
        return h

    def _emit(self, eng, waits, fn, inc):
        e = getattr(self.nc, eng)
        for semkey, val in waits:
            e.wait_ge(self._sem(semkey), val)
        ins = fn(e)
        ins.then_inc(self._sem(inc[0]), inc[1])
        self.count[eng] += 1

    def op(self, eng, fn, rd=(), wr=()):
        pr = [k for k in rd if is_psum_key(k)]
        if pr:
            rd = [k for k in rd if not is_psum_key(k)]
            wr = list(wr) + pr
        waits = self._deps(eng, rd, wr, False)
        self.seq[eng] += 1
        tok = (("eng", eng), self.seq[eng], eng)
        self._emit(eng, waits, fn, (("eng", eng), 1))
        self._record(tok, rd, wr)
        return tok

    def dma(self, fn, rd=(), wr=(), q="sync", semres=None, is_output=False):
        waits = self._deps(q, rd, wr, True)
        key = semres if semres is not None else (wr[0] if wr else rd[0])
        semkey = self.dma_sem_of.get(key)
        if semkey is None:
            semkey = ("dma", self.n_dma_sems % 94)
            self.n_dma_sems += 1
            self.dma_sem_of[key] = semkey
        self.dma_cnt[semkey] = self.dma_cnt.get(semkey, 0) + 16
        tok = (semkey, self.dma_cnt[semkey], None)
        self._emit(q, waits, fn, (semkey, 16))
        self._record(tok, rd, wr)
        if is_output:
            self.out_tokens.append(tok)
        return tok

    def barrier(self):
        toks = [(("eng", e), self.seq[e], e) for e in ALLENG if self.seq[e] > 0]
        toks += [(k, v, None) for k, v in self.dma_cnt.items()]
        for e in ALLENG:
            waits = []
            for t in toks:
                if t[2] == e:
                    continue
                self._need(e, t, waits)
            eo = getattr(self.nc, e)
            for semkey, val in waits:
                eo.wait_ge(self._sem(semkey), val)

    def finish(self):
        fin = []
        for tok in self.out_tokens:
            self._need("sync", tok, fin)
        for semkey, val in fin:
            self.nc.sync.wait_ge(self._sem(semkey), val)
        return dict(self.count)


class Rot:
    def __init__(self, items):
        self.items = list(items)
        self.i = 0

    def next(self):
        it = self.items[self.i % len(self.items)]
        self.i += 1
        return it


class KB:
    def __init__(self, debug=()):
        self.nc = bass.Bass("TRN2", target_bir_lowering=False)
        self.S = Sched(self.nc)
        self.debug = set(debug)
        self.ins = {}
        self.uid = 0

    def un(self, name):
        self.uid += 1
        return "%s_u%d" % (name, self.uid)

    def inp(self, name, shape):
        t = self.nc.dram_tensor(name, list(shape), F32, kind="ExternalInput").ap()
        self.ins[name] = t
        return t

    def scratch(self, name, shape):
        kind = "ExternalOutput" if name in self.debug else "Internal"
        return self.nc.dram_tensor(name, list(shape), F32, kind=kind).ap()

    def dma(self, out, in_, rd, wr, q="sync", is_output=False, semres=None):
        return self.S.dma(lambda e: e.dma_start(out=out, in_=in_), rd=rd, wr=wr, q=q, is_output=is_output, semres=semres)

    def mm(self, out, lhsT, rhs, start, stop, rd, wr):
        return self.S.op("tensor", lambda e: e.matmul(out=out, lhsT=lhsT, rhs=rhs, start=start, stop=stop), rd=rd, wr=wr)

    def tr(self, out, in_, ident, rd, wr):
        return self.S.op("tensor", lambda e: e.transpose(out=out, in_=in_, identity=ident), rd=list(rd) + ["ident"], wr=wr)

    def act(self, out, in_, func, rd, wr, bias=None, scale=None, accum_out=None):
        kw = {}
        if bias is not None:
            kw["bias"] = bias
        if scale is not None:
            kw["scale"] = scale
        if accum_out is not None:
            kw["accum_out"] = accum_out
        return self.S.op("scalar", lambda e: e.activation(out=out, in_=in_, func=func, **kw), rd=rd, wr=wr)

    def tt(self, out, a, b, op, rd, wr, eng="vector"):
        return self.S.op(eng, lambda e: e.tensor_tensor(out=out, in0=a, in1=b, op=op), rd=rd, wr=wr)

    def ts(self, out, a, s1, s2, op0, op1, rd, wr, eng="vector"):
        if op1 is None:
            return self.S.op(eng, lambda e: e.tensor_scalar(out=out, in0=a, scalar1=s1, scalar2=None, op0=op0), rd=rd, wr=wr)
        return self.S.op(eng, lambda e: e.tensor_scalar(out=out, in0=a, scalar1=s1, scalar2=s2, op0=op0, op1=op1), rd=rd, wr=wr)

    def stt(self, out, a, s, b, op0, op1, rd, wr):
        return self.S.op("vector", lambda e: e.scalar_tensor_tensor(out=out, in0=a, scalar=s, in1=b, op0=op0, op1=op1), rd=rd, wr=wr)

    def red(self, out, in_, op, rd, wr, axis=AX.X):
        return self.S.op("vector", lambda e: e.tensor_reduce(out=out, in_=in_, axis=axis, op=op), rd=rd, wr=wr)

    def cp(self, out, in_, rd, wr, eng="vector"):
        if eng == "scalar":
            return self.act(out, in_, AF.Copy, rd, wr)
        return self.S.op(eng, lambda e: e.tensor_copy(out=out, in_=in_), rd=rd, wr=wr)

    def recip(self, out, in_, rd, wr):
        return self.S.op("vector", lambda e: e.reciprocal(out=out, in_=in_), rd=rd, wr=wr)

    def memset(self, out, val, wr, eng="vector"):
        return self.S.op(eng, lambda e: e.memset(out, val), rd=(), wr=wr)

    def rstd(self, out, ss, n, eps, rd, wr):
        self.ts(out, ss, 1.0 / n, eps, ALU.mult, ALU.add, rd=rd, wr=wr)
        self.act(out, out, AF.Sqrt, rd=wr, wr=wr)
        self.recip(out, out, rd=wr, wr=wr)


def pipeline(stages, items):
    n = len(items)
    for it in range(n + len(stages) - 1):
        for s_, f in enumerate(stages):
            j = it - s_
            if 0 <= j < n:
                f(items[j])


def qk_prep(kb, es, l, Z, ident, zcol0, G, gd, nq_groups, gain_ap, rope_ap, tiles, store):
    nc = kb.nc
    W = G * gd
    NB = 6
    sb = lambda n, s, dt=F32: es.enter_context(nc.sbuf_tensor(kb.un(n), s, dt))
    gain = sb("qk_gain", [128, W])
    kb.dma(gain[:, 0:nq_groups * gd].rearrange("p (g d) -> p g d", d=gd),
           gain_ap[l, 0:1, :].to_broadcast([128, gd]).unsqueeze(1).to_broadcast([128, nq_groups, gd]), rd=(), wr=["qk_gain"])
    kb.dma(gain[:, nq_groups * gd:W].rearrange("p (g d) -> p g d", d=gd),
           gain_ap[l, 1:2, :].to_broadcast([128, gd]).unsqueeze(1).to_broadcast([128, G - nq_groups, gd]), rd=(), wr=["qk_gain"])
    zt = [sb("qk_zt%d" % i, [128, W]) for i in range(NB)]
    sq = [sb("qk_sq%d" % i, [128, W]) for i in range(NB)]
    ssq = [sb("qk_ss%d" % i, [128, G]) for i in range(NB)]
    xn = [sb("qk_xn%d" % i, [128, W]) for i in range(NB)]
    if rope_ap is not None:
        rp = [sb("qk_rp%d" % i, [128, 2, W]) for i in range(NB)]
        t1 = [sb("qk_t1%d" % i, [128, W]) for i in range(NB)]
        t2 = [sb("qk_t2%d" % i, [128, W]) for i in range(NB)]
    q4 = gd // 4
    v3 = lambda ap: ap.rearrange("p (g d) -> p g d", d=gd)

    def s0(i):
        b = i % NB
        zk, sk = "qk_zt%d" % b, "qk_ss%d" % b
        kb.dma(zt[b][:], Z[i * 128:(i + 1) * 128, zcol0:zcol0 + W], rd=(), wr=[zk])
        if rope_ap is not None and i < 16:
            kb.dma(rp[b][:], rope_ap[i * 128:(i + 1) * 128], rd=(), wr=["qk_rp%d" % b])
        kb.tt(sq[b][:], zt[b][:], zt[b][:], ALU.mult, rd=[zk], wr=["qk_sq%d" % b], eng="gpsimd")
        kb.red(ssq[b][:], v3(sq[b][:]), ALU.add, rd=["qk_sq%d" % b], wr=[sk])
        kb.ts(ssq[b][:], ssq[b][:], 1.0 / gd, EPS, ALU.mult, ALU.add, rd=[sk], wr=[sk])
        kb.act(ssq[b][:], ssq[b][:], AF.Sqrt, rd=[sk], wr=[sk])

    def s1(i):
        b = i % NB
        zk, sk, xk = "qk_zt%d" % b, "qk_ss%d" % b, "qk_xn%d" % b
        kb.recip(ssq[b][:], ssq[b][:], rd=[sk], wr=[sk])
        kb.tt(v3(xn[b][:]), v3(zt[b][:]), ssq[b][:].unsqueeze(2).to_broadcast([128, G, gd]), ALU.mult, rd=[zk, sk], wr=[xk])
        kb.tt(xn[b][:], xn[b][:], gain[:], ALU.mult, rd=[xk, "qk_gain"], wr=[xk], eng="gpsimd")

    def s2(i):
        b = i % NB
        xk, rk = "qk_xn%d" % b, "qk_rp%d" % b
        if rope_ap is not None and i < 16:
            kb.tt(t1[b][:], xn[b][:], rp[b][:, 0, :], ALU.mult, rd=[xk, rk], wr=["qk_t1%d" % b])
            xv = xn[b][:].rearrange("p (x h d) -> p x h d", h=2, d=q4)
            sv = rp[b][:, 1, :].rearrange("p (x h d) -> p x h d", h=2, d=q4)
            tv = t2[b][:].rearrange("p (x h d) -> p x h d", h=2, d=q4)
            kb.tt(tv[:, :, 0, :], xv[:, :, 1, :], sv[:, :, 0, :], ALU.mult, rd=[xk, rk], wr=[("qk_t2a", b)], eng="gpsimd")
            kb.tt(tv[:, :, 1, :], xv[:, :, 0, :], sv[:, :, 1, :], ALU.mult, rd=[xk, rk], wr=[("qk_t2b", b)], eng="gpsimd")

    def s3(i):
        b = i % NB
        xk = "qk_xn%d" % b
        if rope_ap is not None and i < 16:
            kb.tt(xn[b][:], t1[b][:], t2[b][:], ALU.add, rd=["qk_t1%d" % b, ("qk_t2a", b), ("qk_t2b", b)], wr=[xk])
        store(i, xn[b], xk)

    pipeline([s0, s1, s2, s3], list(tiles))


def load_v(kb, V1, vkey, Z, col0, H, row0, ntile, raw, rawkey):
    nt_, h_ = V1.shape[1], V1.shape[2]
    kb.cp(V1[:, :, :, 64:66], kb.onez[:, 0:2].unsqueeze(1).unsqueeze(1).to_broadcast([128, nt_, h_, 2]), rd=["onez"], wr=[(vkey, "ones")])
    kb.dma(raw[:, 0:ntile, 0:64 * H], Z[row0:row0 + ntile * 128, col0:col0 + 64 * H].rearrange("(t p) c -> p t c", p=128), rd=(), wr=[rawkey])
    hlf = ntile // 2
    kb.cp(V1[:, 0:hlf, :, 0:64], raw[:, 0:hlf, 0:64 * H].rearrange("p t (h d) -> p t h d", d=64), rd=[rawkey], wr=[(vkey, "a")], eng="vector")
    kb.cp(V1[:, hlf:ntile, :, 0:64], raw[:, hlf:ntile, 0:64 * H].rearrange("p t (h d) -> p t h d", d=64), rd=[rawkey], wr=[(vkey, "b")], eng="scalar")


def attn_finish(kb, po, pok, H, npart, extra_den, yt, ytk, dst, rec, reck):
    if extra_den is not None:
        kb.tt(rec[0:npart, :], po[0:npart, :, 64], extra_den[0:npart, :], ALU.add, rd=[pok, "expsink"], wr=[reck])
    else:
        kb.cp(rec[0:npart, :], po[0:npart, :, 64], rd=[pok], wr=[reck])
    kb.recip(rec[0:npart, :], rec[0:npart, :], rd=[reck], wr=[reck])
    kb.tt(yt[0:npart, :, :], po[0:npart, :, 0:64], rec[0:npart, :].unsqueeze(2).to_broadcast([npart, H, 64]), ALU.mult,
          rd=[pok, reck], wr=[ytk])
    kb.dma(dst, yt[0:npart, :, :].rearrange("p h d -> p (h d)"), rd=[ytk], wr=[("YB", ytk)], semres=ytk)


def attn_A(kb, l, need_ctx, Z, YB, ident, C):
    nc = kb.nc
    with ExitStack() as es:
        sb = lambda n, s, dt=F32: es.enter_context(nc.sbuf_tensor(kb.un(n), s, dt))
        ps = lambda n, s, dt=F32: es.enter_context(nc.psum_tensor(kb.un(n), s, dt))
        qT = sb("A_qT", [64, 4, NTOK], F32R)
        kT = sb("A_kT", [64, 2, NTOK], F32R)
        V1 = sb("A_V1", [128, NT, 2, 66], F32R)
        vraw = sb("A_vraw", [128, NT, 128])
        load_v(kb, V1, "A_V1", Z, O_AV, 2, 0, NT, vraw, "A_vraw")
        bm = sb("A_bm", [128, 2, 128])
        kb.dma(bm[:], C["bandmask"].rearrange("m p q -> p m q"), rd=(), wr=["A_bm"])
        expsink = sb("A_es", [128, 4])
        kb.dma(expsink[:], C["a_sink"][l:l + 1, :].to_broadcast([128, 4]), rd=(), wr=["expsink"])
        kb.act(expsink[:], expsink[:], AF.Exp, rd=["expsink"], wr=["expsink"])
        with ExitStack() as es2:
            ptr = [es2.enter_context(nc.psum_tensor(kb.un("A_ptr%d" % i), [64, 6, 128], F32)) for i in range(2)]

            def store(i, xn, xk):
                p = ptr[i % 2]
                pk = "A_ptr%d" % (i % 2)
                for g in range(6):
                    kb.tr(p[:, g, :], xn[:, g * 64:(g + 1) * 64], ident[:], rd=[xk], wr=[pk])
                kb.cp(qT[:, :, i * 128:(i + 1) * 128], p[:, 0:4, :], rd=[pk], wr=[("A_qT", i)], eng="scalar")
                kb.cp(kT[:, :, i * 128:(i + 1) * 128], p[:, 4:6, :], rd=[pk], wr=[("A_kT", i)], eng="vector")
            qk_prep(kb, es2, l, Z, ident, O_AQ, 6, 64, 4, C["a_qk_norm"], C["ropeA"], range(NT), store)
        kb.S.barrier()
        pS = [ps("A_pS%d" % i, [128, 6, 256]) for i in range(2)]
        pO = [ps("A_pO%d" % i, [128, 4, 66]) for i in range(2)]
        pT = [sb("A_pT%d" % i, [128, 5, 2, 128], F32R) for i in range(2)]
        yt = [sb("A_yt%d" % i, [128, 4, 64]) for i in range(2)]
        rec = [sb("A_rec%d" % i, [128, 4]) for i in range(2)]
        k = 0
        qtiles = list(range(16)) + ([16, 17] if need_ctx else [])
        for n, i in enumerate(qtiles):
            if i < 16:
                ents = [(j, (0 if j == i - 1 else (1 if j == i + 1 else None))) for j in (i - 1, i, i + 1) if 0 <= j < 16]
                ents += [(16, None), (17, None)]
            else:
                ents = [(16, None), (17, None)]
            E = len(ents)
            po = pO[n % 2]
            pok = "A_pO%d" % (n % 2)
            for g in range(2):
                b = k % 2
                k += 1
                psk, ptk = "A_pS%d" % b, "A_pT%d" % b
                for e, (j, mk) in enumerate(ents):
                    kb.mm(pS[b][:, e, :], kT[:, g, j * 128:(j + 1) * 128], qT[:, 2 * g:2 * g + 2, i * 128:(i + 1) * 128], True, True,
                          rd=[("A_kT", j), ("A_qT", i)], wr=[psk])
                kb.act(pT[b][:, 0:E].rearrange("p e h q -> p e (h q)"), pS[b][:, 0:E, :], AF.Exp, rd=[psk], wr=[ptk], scale=0.125)
                for e, (j, mk) in enumerate(ents):
                    if mk is not None:
                        kb.tt(pT[b][:, e], pT[b][:, e], bm[:, mk:mk + 1, :].to_broadcast([128, 2, 128]), ALU.mult, rd=[ptk, "A_bm"], wr=[ptk])
                for hh in range(2):
                    h = 2 * g + hh
                    for e, (j, mk) in enumerate(ents):
                        kb.mm(po[:, h, :], pT[b][:, e, hh, :], V1[:, j, g, :], e == 0, e == E - 1, rd=[ptk, "A_V1"], wr=[pok])
            attn_finish(kb, po, pok, 4, 128, expsink, yt[n % 2], "A_yt%d" % (n % 2), YB[i * 128:(i + 1) * 128, 0:256], rec[n % 2], "A_rec%d" % (n % 2))


def attn_D(kb, l, need_ctx, Z, YB, ident, C):
    nc = kb.nc
    with ExitStack() as es:
        sb = lambda n, s, dt=F32: es.enter_context(nc.sbuf_tensor(kb.un(n), s, dt))
        ps = lambda n, s, dt=F32: es.enter_context(nc.psum_tensor(kb.un(n), s, dt))
        qT = sb("D_qT", [64, 4, NTOK], F32R)
        kT = sb("D_kT", [64, 4, NTOK], F32R)
        V1 = sb("D_V1", [128, NT, 4, 66], F32R)
        V1o = sb("D_V1o", [128, 15, 4, 66], F32R)
        vraw = sb("D_vraw", [128, NT, 256])
        load_v(kb, V1, "D_V1", Z, O_DV, 4, 0, NT, vraw, "D_vraw")
        load_v(kb, V1o, "D_V1o", Z, O_DV, 4, 64, 15, vraw, "D_vraw")
        Eb = sb("D_E", [128, 128, 64])
        mD = sb("D_mD", [128, 64])
        kb.dma(Eb[:].rearrange("p a q -> p (a q)"), C["biasG"][l], rd=(), wr=["D_E"])
        kb.dma(mD[:], C["maskD"], rd=(), wr=["D_mD"])
        kb.act(Eb[:], Eb[:], AF.Exp, rd=["D_E"], wr=["D_E"])
        kb.tt(Eb[:], Eb[:], mD[:].unsqueeze(1).to_broadcast([128, 128, 64]), ALU.mult, rd=["D_E", "D_mD"], wr=["D_E"])
        with ExitStack() as es2:
            ptr = [es2.enter_context(nc.psum_tensor(kb.un("D_ptr%d" % i), [64, 8, 128], F32)) for i in range(2)]

            def store(i, xn, xk):
                p = ptr[i % 2]
                pk = "D_ptr%d" % (i % 2)
                for g in range(8):
                    kb.tr(p[:, g, :], xn[:, g * 64:(g + 1) * 64], ident[:], rd=[xk], wr=[pk])
                kb.cp(qT[:, :, i * 128:(i + 1) * 128], p[:, 0:4, :], rd=[pk], wr=[("D_qT", i)], eng="scalar")
                kb.cp(kT[:, :, i * 128:(i + 1) * 128], p[:, 4:8, :], rd=[pk], wr=[("D_kT", i)], eng="vector")
            qk_prep(kb, es2, l, Z, ident, O_DQ, 8, 64, 4, C["d_qk_norm"], None, range(NT), store)
        kb.S.barrier()
        pS = [ps("D_pS%d" % i, [128, 4, 6, 64]) for i in range(2)]
        pO = [ps("D_pO%d" % i, [128, 4, 66]) for i in range(2)]
        pT = [sb("D_pT%d" % i, [128, 4, 6, 64], F32R) for i in range(2)]
        ptmp = [sb("D_ptmp%d" % i, [128, 4, 4, 64]) for i in range(2)]
        yt = [sb("D_yt%d" % i, [128, 4, 64]) for i in range(2)]
        rec = [sb("D_rec%d" % i, [128, 4]) for i in range(2)]
        for r in range(32):
            start = min(max(r - 4, 0), 24)
            cls = r if r < 4 else (4 if r <= 28 else r - 24)
            b = r % 2
            po = pO[b]
            pok = "D_pO%d" % b
            psk, ptk, tmk = "D_pS%d" % b, "D_pT%d" % b, "D_ptmp%d" % b
            for h in range(4):
                qs = qT[:, h, r * 64:(r + 1) * 64]
                for t in range(4):
                    t0 = (start + 2 * t) * 64
                    kb.mm(pS[b][:, h, t, :], kT[:, h, t0:t0 + 128], qs, True, True, rd=(), wr=[psk])
                for t in range(2):
                    kb.mm(pS[b][:, h, 4 + t, :], kT[:, h, SEQ + t * 128:SEQ + (t + 1) * 128], qs, True, True, rd=(), wr=[psk])
            kb.act(ptmp[b][:], pS[b][:, :, 0:4, :], AF.Exp, rd=[psk], wr=[tmk], scale=0.125)
            kb.act(pT[b][:, :, 4:6, :], pS[b][:, :, 4:6, :], AF.Exp, rd=[psk], wr=[ptk], scale=0.125)
            kb.tt(pT[b][:, :, 0:4, :], ptmp[b][:], Eb[:, cls * 16:(cls + 1) * 16, :].rearrange("p (h t) q -> p h t q", h=4), ALU.mult,
                  rd=[tmk, "D_E"], wr=[ptk])
            for h in range(4):
                for t in range(4):
                    row = start + 2 * t
                    vt = V1[:, row // 2, h, :] if row % 2 == 0 else V1o[:, (row - 1) // 2, h, :]
                    kb.mm(po[0:64, h, :], pT[b][:, h, t, :], vt, t == 0, False, rd=[ptk, "D_V1", "D_V1o"], wr=[pok])
                for t in range(2):
                    kb.mm(po[0:64, h, :], pT[b][:, h, 4 + t, :], V1[:, 16 + t, h, :], False, t == 1, rd=[ptk, "D_V1"], wr=[pok])
            attn_finish(kb, po, pok, 4, 64, None, yt[b], "D_yt%d" % b, YB[r * 64:(r + 1) * 64, 768:1024], rec[b], "D_rec%d" % b)
        if need_ctx:
            pT2 = [sb("D_pTc%d" % i, [128, 2, 128], F32R) for i in range(2)]
            pSc = [pS[i][:, 0, 0:4, :].rearrange("p (a b) d -> p a (b d)", a=2) for i in range(2)]
            k = 0
            for n, i in enumerate((16, 17)):
                po = pO[n % 2]
                pok = "D_pO%d" % (n % 2)
                for h in range(4):
                    b = k % 2
                    k += 1
                    psk, ptk = "D_pS%d" % b, "D_pTc%d" % b
                    for t in range(2):
                        kb.mm(pSc[b][:, t, :], kT[:, h, SEQ + t * 128:SEQ + (t + 1) * 128], qT[:, h, i * 128:(i + 1) * 128], True, True,
                              rd=["D_kTall", "D_qTall"], wr=[psk])
                    kb.act(pT2[b][:], pSc[b], AF.Exp, rd=[psk], wr=[ptk], scale=0.125)
                    for t in range(2):
                        kb.mm(po[:, h, :], pT2[b][:, t, :], V1[:, 16 + t, h, :], t == 0, t == 1, rd=[ptk, "D_V1"], wr=[pok])
                attn_finish(kb, po, pok, 4, 128, None, yt[n % 2], "D_yt%d" % (n % 2), YB[i * 128:(i + 1) * 128, 768:1024], rec[n % 2], "D_rec%d" % (n % 2))


def attn_C(kb, l, need_ctx, Z, YB, ident, C):
    nc = kb.nc
    lam_init = 0.8 - 0.6 * math.exp(-0.3 * l)
    with ExitStack() as es:
        sb = lambda n, s, dt=F32: es.enter_context(nc.sbuf_tensor(kb.un(n), s, dt))
        ps = lambda n, s, dt=F32: es.enter_context(nc.psum_tensor(kb.un(n), s, dt))
        qT = sb("C_qT", [128, 3, NTOK], F32R)
        kT = sb("C_kT", [128, 3, NTOK], F32R)
        V1 = sb("C_V1", [128, NT, 4, 66], F32R)
        lv = sb("C_lv", [128, 4, 32])
        lp = sb("C_lp", [128, 2, 32])
        ls = sb("C_ls", [128, 2])
        nlam = sb("C_nlam", [128, 1])
        kb.dma(lv[:].rearrange("p a d -> p (a d)"), C["c_lambda"][l:l + 1].rearrange("o a d -> o (a d)").to_broadcast([128, 128]), rd=(), wr=["C_lv"])
        lvv = lv[:].rearrange("p (a b) d -> p a b d", b=2)
        kb.tt(lp[:], lvv[:, :, 0, :], lvv[:, :, 1, :], ALU.mult, rd=["C_lv"], wr=["C_lp"])
        kb.red(ls[:], lp[:], ALU.add, rd=["C_lp"], wr=["C_ls"])
        kb.act(ls[:], ls[:], AF.Exp, rd=["C_ls"], wr=["C_ls"])
        kb.tt(nlam[:], ls[:, 1:2], ls[:, 0:1], ALU.subtract, rd=["C_ls"], wr=["C_nlam"])
        kb.ts(nlam[:], nlam[:], -lam_init, None, ALU.add, None, rd=["C_nlam"], wr=["C_nlam"])
        sg = sb("C_sg", [128, 64])
        kb.dma(sg[:], C["c_subln"][l:l + 1, :].to_broadcast([128, 64]), rd=(), wr=["C_sg"])
        kb.ts(sg[:], sg[:], 1.0 - lam_init, None, ALU.mult, None, rd=["C_sg"], wr=["C_sg"])
        with ExitStack() as es2:
            vraw = es2.enter_context(nc.sbuf_tensor(kb.un("C_vraw"), [128, NT, 256], F32))
            load_v(kb, V1, "C_V1", Z, O_CV, 4, 0, NT, vraw, "C_vraw")
            ptrq = [es2.enter_context(nc.psum_tensor(kb.un("C_ptrq%d" % i), [128, 3, 128], F32)) for i in range(2)]
            ptrk = [es2.enter_context(nc.psum_tensor(kb.un("C_ptrk%d" % i), [128, 3, 128], F32)) for i in range(2)]

            def store(i, xn, xk):
                for (pp, pk, dst, dk, c0, eng) in ((ptrq[i % 2], "C_ptrq%d" % (i % 2), qT, "C_qT", 0, "scalar"),
                                                   (ptrk[i % 2], "C_ptrk%d" % (i % 2), kT, "C_kT", 256, "vector")):
                    for idx in range(3):
                        ncol = 96 if idx < 2 else 64
                        kb.tr(pp[0:ncol, idx, :], xn[:, c0 + idx * 96:c0 + idx * 96 + ncol], ident[:], rd=[xk], wr=[pk])
                    kb.cp(dst[0:96, 0:2, i * 128:(i + 1) * 128], pp[0:96, 0:2, :], rd=[pk], wr=[(dk, i)], eng=eng)
                    kb.cp(dst[0:64, 2, i * 128:(i + 1) * 128], pp[0:64, 2, :], rd=[pk], wr=[(dk, i)], eng=eng)
            qk_prep(kb, es2, l, Z, ident, O_CQ, 16, 32, 8, C["c_qk_norm"], C["ropeC"], range(NT), store)
        kb.S.barrier()
        pS = [ps("C_pS%d" % i, [128, 512]) for i in range(4)]
        pOT = [ps("C_pOT%d" % i, [128, 512]) for i in range(2)]
        pO2 = ps("C_pO2", [128, 4, 66])
        oT = [sb("C_oT%d" % i, [66, 512]) for i in range(2)]
        pTs = [sb("C_pT%d" % i, [128, NT, 512], F32R) for i in range(2)]
        ocs = [sb("C_oc%d" % i, [128, 4, 8, 66]) for i in range(2)]
        rec = sb("C_rec", [128, 4, 8])
        on = sb("C_on", [128, 4, 8, 64])
        od = sb("C_od", [128, 4, 4, 64])
        osq = sb("C_osq", [128, 4, 4, 64])
        oss = sb("C_oss", [128, 4, 4])
        yts = [sb("C_yt%d" % i, [128, 4, 4, 64]) for i in range(2)]
        blocks = [(qb * 512, 512, list(range(NT))) for qb in range(4)]
        if need_ctx:
            blocks.append((SEQ, 256, [16, 17]))
        ks = 0
        ko = 0
        scale = 32 ** -0.5
        pending = []

        def c_finish(q0, nq, nqt, bi):
            ob = ocs[bi % 2]
            okk = [("C_oc", bi % 2, qt_) for qt_ in range(nqt)]
            kb.recip(rec[:, 0:nqt, :], ob[:, 0:nqt, :, 64], rd=okk, wr=["C_rec"])
            kb.tt(on[:, 0:nqt], ob[:, 0:nqt, :, 0:64], rec[:, 0:nqt, :].unsqueeze(3).to_broadcast([128, nqt, 8, 64]), ALU.mult, rd=okk + ["C_rec"], wr=["C_on"])
            onv = on[:, 0:nqt].rearrange("p q (h m) d -> p q h m d", m=2)
            kb.stt(od[:, 0:nqt], onv[:, :, :, 1, :], nlam[:, 0:1], onv[:, :, :, 0, :], ALU.mult, ALU.add, rd=["C_on", "C_nlam"], wr=["C_od"])
            kb.tt(osq[:, 0:nqt], od[:, 0:nqt], od[:, 0:nqt], ALU.mult, rd=["C_od"], wr=["C_osq"], eng="gpsimd")
            kb.red(oss[:, 0:nqt, :], osq[:, 0:nqt], ALU.add, rd=["C_osq"], wr=["C_oss"])
            kb.rstd(oss[:, 0:nqt, :], oss[:, 0:nqt, :], 64, EPS, rd=["C_oss"], wr=["C_oss"])
            y = yts[bi % 2]
            yk = "C_yt%d" % (bi % 2)
            kb.tt(y[:, 0:nqt], od[:, 0:nqt], oss[:, 0:nqt, :].unsqueeze(3).to_broadcast([128, nqt, 4, 64]), ALU.mult, rd=["C_od", "C_oss"], wr=[yk])
            kb.tt(y[:, 0:nqt], y[:, 0:nqt], sg[:].unsqueeze(1).unsqueeze(1).to_broadcast([128, nqt, 4, 64]), ALU.mult, rd=[yk, "C_sg"], wr=[yk], eng="gpsimd")
            kb.dma(YB[q0:q0 + nq, 512:768].rearrange("(q p) c -> p q c", p=128), y[:, 0:nqt].rearrange("p q h d -> p q (h d)"), rd=[yk], wr=[("YB", yk)], semres=yk)

        for blk_i, (q0, nq, ktl) in enumerate(blocks):
            nqt = nq // 128
            oc = ocs[blk_i % 2]
            def pv_step(gp, e, j):
                hp = gp // 2
                bp = gp % 2
                kb.mm(pOT[bp][0:66, 0:nq], V1[:, j, hp, :], pTs[bp][:, e, 0:nq], e == 0, e == len(ktl) - 1,
                      rd=[("C_pT", bp, e), "C_V1"], wr=["C_pOT%d" % bp])

            def pv_finish(gp):
                bp = gp % 2
                kb.cp(oT[bp][:, 0:nq], pOT[bp][0:66, 0:nq], rd=["C_pOT%d" % bp], wr=["C_oT%d" % bp], eng="vector")
                for qt in range(nqt):
                    kb.tr(pO2[:, qt, :], oT[bp][:, qt * 128:(qt + 1) * 128], ident[0:66, 0:66], rd=["C_oT%d" % bp], wr=["C_pO2"])
                kb.cp(oc[:, 0:nqt, gp, :], pO2[:, 0:nqt, :], rd=["C_pO2"], wr=[("C_oc", blk_i % 2, qt_) for qt_ in range(nqt)], eng="vector")

            for g in range(8):
                h, s4, hh = g // 2, g % 3, g // 3
                bg = g % 2
                ents_ = list(enumerate(ktl))
                for c0 in range(0, len(ents_), 4):
                    for e, j in ents_[c0:c0 + 4]:
                        b = ks % 4
                        ks += 1
                        kb.mm(pS[b][:, 0:nq], kT[32 * s4:32 * s4 + 32, hh, j * 128:(j + 1) * 128], qT[32 * s4:32 * s4 + 32, hh, q0:q0 + nq], True, True,
                              rd=(), wr=["C_pS%d" % b])
                        kb.act(pTs[bg][:, e, 0:nq], pS[b][:, 0:nq], AF.Exp, rd=["C_pS%d" % b], wr=[("C_pT", bg, e)], scale=scale)
                    if g > 0:
                        for e, j in ents_[c0:c0 + 4]:
                            pv_step(g - 1, e, j)
                if g > 0:
                    pv_finish(g - 1)
                if g == 2 and pending:
                    c_finish(*pending.pop(0))
            for e, j in enumerate(ktl):
                pv_step(7, e, j)
            pv_finish(7)
            pending.append((q0, nq, nqt, blk_i))
        while pending:
            c_finish(*pending.pop(0))


def rwkv(kb, l, need_ctx, Z, YB, RW, ident, C):
    nc = kb.nc
    S = kb.S
    NEG = -math.exp(-0.5)
    with ExitStack() as es:
        sb = lambda n, s, dt=F32: es.enter_context(nc.sbuf_tensor(kb.un(n), s, dt))
        ps = lambda n, s, dt=F32: es.enter_context(nc.psum_tensor(kb.un(n), s, dt))
        mu = sb("R_mu", [128, 3, 896])
        kb.dma(mu[:, 0:2, :], C["b_shift"][l:l + 1].to_broadcast([128, 2, 896]), rd=(), wr=["R_mu"])
        kb.tt(mu[:, 2, :], mu[:, 0, :], mu[:, 1, :], ALU.add, rd=["R_mu"], wr=["R_mu"])
        kb.ts(mu[:, 2, :], mu[:, 2, :], -1.0, 1.0, ALU.mult, ALU.add, rd=["R_mu"], wr=["R_mu"])
        W2t = sb("R_W2t", [32, 512])
        A2t = sb("R_A2t", [32, 256])
        G2t = sb("R_G2t", [64, 256])
        kb.dma(W2t[:].rearrange("p (a n) -> p a n", a=2), C["b_w2"][l].rearrange("a p n -> p a n"), rd=(), wr=["R_Wl"])
        kb.dma(A2t[:], C["b_a2"][l], rd=(), wr=["R_Wl"])
        kb.dma(G2t[:], C["b_g2"][l], rd=(), wr=["R_Wl"])
        w0 = sb("R_w0", [128, 512])
        kb.dma(w0[:], C["b_w0"][l:l + 1].rearrange("o a n -> o (a n)").to_broadcast([128, 512]), rd=(), wr=["R_w0"])
        vb = sb("R_vb", [128, 3, 256])
        kb.dma(vb[:, 0, :], C["b_a0"][l:l + 1, :].to_broadcast([128, 256]), rd=(), wr=["R_vb"])
        kb.dma(vb[:, 1, :], C["b_kk"][l:l + 1, :].to_broadcast([128, 256]), rd=(), wr=["R_vb"])
        kb.dma(vb[:, 2, :], C["b_ka"][l:l + 1, :].to_broadcast([128, 256]), rd=(), wr=["R_vb"])
        NB = 6
        zc = [sb("R_zc%d" % i, [128, 896]) for i in range(2)]
        zp = [sb("R_zp%d" % i, [128, 896]) for i in range(2)]
        zn = [sb("R_zn%d" % i, [128, 896]) for i in range(2)]
        zs = [sb("R_zs%d" % i, [128, 896]) for i in range(NB)]
        lbT = [sb("R_lbT%d" % i, [64, 3, 128]) for i in range(NB)]
        rw = [sb("R_rw%d" % i, [128, 8, 256]) for i in range(NB)]
        tmp = [sb("R_tmp%d" % i, [128, 512]) for i in range(NB)]
        tmp2 = [sb("R_tmp2%d" % i, [128, 256]) for i in range(NB)]
        av = [sb("R_a%d" % i, [128, 256]) for i in range(NB)]
        ssk = [sb("R_ssk%d" % i, [128, 4]) for i in range(NB)]
        pl = [ps("R_pl%d" % i, [64, 3, 128]) for i in range(2)]
        pd = [ps("R_pd%d" % i, [128, 512]) for i in range(2)]
        pag = [ps("R_pag%d" % i, [128, 2, 256]) for i in range(2)]
        h3 = lambda ap: ap.rearrange("p (h d) -> p h d", d=64)

        def s0(i):
            b2, b = i % 2, i % NB
            kc, kp, kn, ks_ = "R_zc%d" % b2, "R_zp%d" % b2, "R_zn%d" % b2, "R_zs%d" % b
            r0 = i * 128
            kb.dma(zc[b2][:], Z[r0:r0 + 128, O_BZ:O_BZ + 896], rd=(), wr=[kc])
            if i in (0, 16):
                kb.memset(zp[b2][:], 0.0, wr=[kp])
                kb.dma(zp[b2][1:128, :], Z[r0:r0 + 127, O_BZ:O_BZ + 896], rd=(), wr=[kp])
            else:
                kb.dma(zp[b2][:], Z[r0 - 1:r0 + 127, O_BZ:O_BZ + 896], rd=(), wr=[kp])
            if i in (15, 17):
                kb.memset(zn[b2][:], 0.0, wr=[kn])
                kb.dma(zn[b2][0:127, :], Z[r0 + 1:r0 + 128, O_BZ:O_BZ + 896], rd=(), wr=[kn])
            else:
                kb.dma(zn[b2][:], Z[r0 + 1:r0 + 129, O_BZ:O_BZ + 896], rd=(), wr=[kn])
            kb.tt(zs[b][:], zc[b2][:], mu[:, 2, :], ALU.mult, rd=[kc, "R_mu"], wr=[ks_])
            kb.tt(zp[b2][:], zp[b2][:], mu[:, 0, :], ALU.mult, rd=[kp, "R_mu"], wr=[kp], eng="gpsimd")
            kb.tt(zn[b2][:], zn[b2][:], mu[:, 1, :], ALU.mult, rd=[kn, "R_mu"], wr=[kn], eng="gpsimd")

        def s1(i):
            b2, b = i % 2, i % NB
            kp, kn, ks_ = "R_zp%d" % b2, "R_zn%d" % b2, "R_zs%d" % b
            kb.tt(zs[b][:], zs[b][:], zp[b2][:], ALU.add, rd=[ks_, kp], wr=[ks_])
            kb.tt(zs[b][:], zs[b][:], zn[b2][:], ALU.add, rd=[ks_, kn], wr=[ks_])

        def s2(i):
            b2, b = i % 2, i % NB
            ks_, kl = "R_zs%d" % b, "R_lbT%d" % b
            z = zs[b]
            kb.tr(pl[b2][0:32, 0, :], z[:, 768:800], ident[:], rd=[ks_], wr=["R_pl%d" % b2])
            kb.tr(pl[b2][0:32, 1, :], z[:, 800:832], ident[:], rd=[ks_], wr=["R_pl%d" % b2])
            kb.tr(pl[b2][0:64, 2, :], z[:, 832:896], ident[:], rd=[ks_], wr=["R_pl%d" % b2])
            kb.act(lbT[b][0:32, 0, :], pl[b2][0:32, 0, :], AF.Tanh, rd=["R_pl%d" % b2], wr=[kl])
            kb.act(lbT[b][0:32, 1, :], pl[b2][0:32, 1, :], AF.Copy, rd=["R_pl%d" % b2], wr=[kl])
            kb.act(lbT[b][0:64, 2, :], pl[b2][0:64, 2, :], AF.Sigmoid, rd=["R_pl%d" % b2], wr=[kl])

        def s3(i):
            b2, b = i % 2, i % NB
            ks_, kl, kr = "R_zs%d" % b, "R_lbT%d" % b, "R_rw%d" % b
            z = zs[b]
            o = rw[b]
            kb.mm(pd[b2][:], lbT[b][0:32, 0, :], W2t[:], True, True, rd=[kl, "R_Wl"], wr=["R_pd%d" % b2])
            kb.mm(pag[b2][:, 0, :], lbT[b][0:32, 1, :], A2t[:], True, True, rd=[kl, "R_Wl"], wr=["R_pag%d" % b2])
            kb.mm(pag[b2][:, 1, :], lbT[b][0:64, 2, :], G2t[:], True, True, rd=[kl, "R_Wl"], wr=["R_pag%d" % b2])
            kb.cp(o[:, 0, :], z[:, 0:256], rd=[ks_], wr=[(kr, 0)], eng="gpsimd")
            kb.cp(o[:, 2, :], z[:, 512:768], rd=[ks_], wr=[(kr, 2)], eng="gpsimd")
            kb.tt(o[:, 3, :], z[:, 256:512], vb[:, 1, :], ALU.mult, rd=[ks_, "R_vb"], wr=[(kr, 3)])
            kb.tt(tmp2[b][:], o[:, 3, :], o[:, 3, :], ALU.mult, rd=[(kr, 3)], wr=["R_tmp2%d" % b])
            kb.red(ssk[b][:], h3(tmp2[b][:]), ALU.add, rd=["R_tmp2%d" % b], wr=["R_ssk%d" % b])
            kb.ts(ssk[b][:], ssk[b][:], 1e-24, None, ALU.max, None, rd=["R_ssk%d" % b], wr=["R_ssk%d" % b])

        def s4(i):
            b2, b = i % 2, i % NB
            kr = "R_rw%d" % b
            o = rw[b]
            kb.act(o[:, 7, :], pag[b2][:, 1, :], AF.Copy, rd=["R_pag%d" % b2], wr=[(kr, 7)])
            kb.tt(av[b][:], pag[b2][:, 0, :], vb[:, 0, :], ALU.add, rd=["R_pag%d" % b2, "R_vb"], wr=["R_a%d" % b])
            kb.tt(tmp[b][:], pd[b2][:], w0[:], ALU.add, rd=["R_pd%d" % b2, "R_w0"], wr=["R_tmp%d" % b])
            kb.act(av[b][:], av[b][:], AF.Sigmoid, rd=["R_a%d" % b], wr=["R_a%d" % b])
            kb.act(tmp[b][:], tmp[b][:], AF.Sigmoid, rd=["R_tmp%d" % b], wr=["R_tmp%d" % b])
            kb.act(ssk[b][:], ssk[b][:], AF.Sqrt, rd=["R_ssk%d" % b], wr=["R_ssk%d" % b])

        def s5(i):
            b2, b = i % 2, i % NB
            ks_, kr = "R_zs%d" % b, "R_rw%d" % b
            z = zs[b]
            o = rw[b]
            kb.ts(o[:, 5:7, :].rearrange("p a n -> p (a n)"), tmp[b][:], NEG, None, ALU.mult, None, rd=["R_tmp%d" % b], wr=[(kr, 5)], eng="gpsimd")
            kb.recip(ssk[b][:], ssk[b][:], rd=["R_ssk%d" % b], wr=["R_ssk%d" % b])
            kb.tt(h3(o[:, 3, :]), h3(o[:, 3, :]), ssk[b][:].unsqueeze(2).to_broadcast([128, 4, 64]), ALU.mult, rd=[(kr, 3), "R_ssk%d" % b], wr=[(kr, 3)])
            kb.tt(o[:, 4, :], o[:, 3, :], av[b][:], ALU.mult, rd=[(kr, 3), "R_a%d" % b], wr=[(kr, 4)], eng="gpsimd")
            kb.stt(tmp2[b][:], av[b][:], -1.0, vb[:, 2, :], ALU.add, ALU.mult, rd=["R_a%d" % b, "R_vb"], wr=["R_tmp2%d" % b])
            kb.stt(o[:, 1, :], tmp2[b][:], 1.0, z[:, 256:512], ALU.add, ALU.mult, rd=["R_tmp2%d" % b, ks_], wr=[(kr, 1)])

        def s6(i):
            b = i % NB
            kr = "R_rw%d" % b
            kb.dma(RW[i * 128:(i + 1) * 128], rw[b][:], rd=[(kr, j_) for j_ in (0, 1, 2, 3, 4, 5, 7)], wr=[("RW", i)], semres=kr)

        pipeline([s0, s1, s2, s3, s4, s5, s6], list(range(NT)))
    S.barrier()
    if "noscan" in kb.debug:
        return
    with ExitStack() as es:
        sb = lambda n, s, dt=F32: es.enter_context(nc.sbuf_tensor(kb.un(n), s, dt))
        ps = lambda n, s, dt=F32: es.enter_context(nc.psum_tensor(kb.un(n), s, dt))
        msk = sb("S_msk", [128, 2, 512])
        kb.dma(msk[:], C["rwkv_masks"].rearrange("d p n -> p d n"), rd=(), wr=["S_msk"])
        ones2 = sb("S_ones", [128, 2])
        kb.memset(ones2[:], 1.0, wr=["S_ones"])
        yacc = sb("S_yacc", [128, NT, 256])
        kb.memset(yacc[:].rearrange("p t c -> p (t c)"), 0.0, wr=[("S_yacc", i_) for i_ in range(NT)])
        Hs = [sb("S_H%d" % d_, [64, 4, 64]) for d_ in range(2)]
        rwt = [sb("S_rw%d" % i_, [128, 8, 256]) for i_ in range(4)]
        exs = [sb("S_ex%d" % i_, [128, 3, 256]) for i_ in range(4)]
        clxs = [sb("S_clx%d" % i_, [128, 256]) for i_ in range(4)]
        q4s = [sb("S_q4%d" % i_, [128, 4, 256]) for i_ in range(4)]
        T4s = [[sb("S_T4_%d_%d" % (i_, h), [64, 4, 128]) for h in range(4)] for i_ in range(4)]
        GMs = [[sb("S_GM%d_%d" % (i_, h), [128, 2, 256]) for h in range(4)] for i_ in range(4)]
        Xss = [[sb("S_X%d_%d" % (d_, i_), [128, 4, 128]) for i_ in range(2)] for d_ in range(2)]
        XTss = [[sb("S_XT%d_%d" % (d_, i_), [128, 4, 128]) for i_ in range(2)] for d_ in range(2)]
        TTss = [[sb("S_TT%d_%d" % (d_, i_), [128, 4, 128]) for i_ in range(2)] for d_ in range(2)]
        pcs = [sb("S_pc%d" % i_, [64, 4]) for i_ in range(4)]
        R1s = [sb("S_R1%d" % d_, [128, 4, 64]) for d_ in range(2)]
        nUs = [sb("S_nU%d" % d_, [128, 4, 64]) for d_ in range(2)]
        pb = [ps("S_pb%d" % i, [128, 512]) for i in range(8)]
        P = lambda i: "S_pb%d" % i
        orders = ([16, 17] + list(range(16)), [17, 16] + list(range(15, -1, -1)))
        for d_ in range(2):
            kb.memset(Hs[d_][:], 0.0, wr=["S_H_d%d" % d_])

        def front(d, n):
            i = orders[d][n]
            sl = "_s%d" % (d * 2 + n % 2)
            dl = "_d%d" % d
            w, ex, clx, q4, T4, GM, pc = rwt[d * 2 + n % 2], exs[d * 2 + n % 2], clxs[d * 2 + n % 2], q4s[d * 2 + n % 2], T4s[d * 2 + n % 2], GMs[d * 2 + n % 2], pcs[d * 2 + n % 2]
            H, Xs, XTs, TTs, R1, nU = Hs[d], Xss[d], XTss[d], TTss[d], R1s[d], nUs[d]
            rk_ = "S_rw" + sl
            kb.dma(w[:], RW[i * 128:(i + 1) * 128], rd=(), wr=[rk_])
            lw = w[:, 5 + d, :]
            kb.mm(pb[0][:, 0:256], msk[:, d, 0:128], lw, True, True, rd=["S_msk", rk_], wr=[P(0)])
            for h in range(4):
                kb.mm(pb[0][0:64, 256 + 2 * h:258 + 2 * h], w[:, 5 + d, h * 64:(h + 1) * 64], ones2[:], True, True, rd=[rk_, "S_ones"], wr=[P(0)])
            kb.act(ex[:, 0, :], pb[0][:, 0:256], AF.Exp, rd=[P(0)], wr=["S_ex" + sl])
            kb.act(ex[:, 2, :], pb[0][:, 0:256], AF.Exp, rd=[P(0)], wr=["S_ex" + sl], scale=-1.0)
            kb.tt(clx[:], pb[0][:, 0:256], lw, ALU.subtract, rd=[P(0), rk_], wr=["S_clx" + sl])
            kb.act(ex[:, 1, :], clx[:], AF.Exp, rd=["S_clx" + sl], wr=["S_ex" + sl])
            kb.act(pc[:], pb[0][0:64, 256:264].rearrange("p (h t) -> p h t", t=2)[:, :, 0], AF.Exp, rd=[P(0)], wr=["S_pc" + sl])
            kb.tt(q4[:, 0, :], w[:, 3, :], ex[:, 1, :], ALU.mult, rd=[rk_, "S_ex" + sl], wr=["S_q4" + sl])
            kb.tt(q4[:, 1, :], w[:, 0, :], ex[:, 0, :], ALU.mult, rd=[rk_, "S_ex" + sl], wr=["S_q4" + sl], eng="gpsimd")
            kb.tt(q4[:, 2, :], w[:, 4, :], ex[:, 2, :], ALU.mult, rd=[rk_, "S_ex" + sl], wr=["S_q4" + sl])
            kb.tt(q4[:, 3, :], w[:, 1, :], ex[:, 2, :], ALU.mult, rd=[rk_, "S_ex" + sl], wr=["S_q4" + sl], eng="gpsimd")
            for h in range(4):
                tb_ = 1 if h % 2 == 0 else 0
                for kd in range(4):
                    kb.tr(pb[tb_][0:64, kd * 128:(kd + 1) * 128], q4[:, kd, h * 64:(h + 1) * 64], ident[:], rd=["S_q4" + sl], wr=[P(tb_)])
                kb.cp(T4[h][:].rearrange("p k t -> p (k t)"), pb[tb_][0:64, :], rd=[P(tb_)], wr=["S_T4_%d" % h + sl], eng="scalar" if h % 2 else "vector")
            for h in range(4):
                t4 = T4[h]
                tk = "S_T4_%d" % h + sl
                gb_ = 2 if h % 2 == 0 else 7
                kb.mm(pb[gb_][:, 0:256], t4[:, 2, :], t4[:, 0:2, :], True, True, rd=[tk], wr=[P(gb_)])
                kb.mm(pb[gb_][:, 256:512], t4[:, 3, :], t4[:, 0:2, :], True, True, rd=[tk], wr=[P(gb_)])
                kb.mm(pb[3][:, h * 128:(h + 1) * 128], t4[:, 0, :], t4[:, 2, :], True, True, rd=[tk], wr=[P(3)])
                kb.tt(GM[h][:], pb[gb_][:].rearrange("p (a n) -> p a n", a=2), msk[:, d, 128:384].unsqueeze(1).to_broadcast([128, 2, 256]), ALU.mult,
                      rd=[P(gb_), "S_msk"], wr=["S_GM%d" % h + sl])
            X, XT, TT = Xs[0], XTs[0], TTs[0]
            XK = lambda c_, p_: ("S_X", c_, p_, d)
            XTK = lambda c_, p_: ("S_XT", c_, p_, d)
            TTK = lambda c_, p_: ("S_TT", c_, p_, d)
            kb.tt(X[:], pb[3][:].rearrange("p (h n) -> p h n", h=4), msk[:, d, 384:512].unsqueeze(1).to_broadcast([128, 4, 128]), ALU.mult,
                  rd=[P(3), "S_msk"], wr=[XK(0, 0), XK(0, 1)])
            for h in range(4):
                kb.cp(XT[:, h, :], GM[h][:, 0, 0:128], rd=["S_GM%d" % h + sl], wr=[XTK(0, h // 2)], eng="gpsimd")
                kb.tt(TT[:, h, :], ident[:], GM[h][:, 0, 0:128], ALU.subtract, rd=["ident", "S_GM%d" % h + sl], wr=[TTK(0, h // 2)])

        def back(d, n):
            i = orders[d][n]
            sl = "_s%d" % (d * 2 + n % 2)
            dl = "_d%d" % d
            w, ex, clx, q4, T4, GM, pc = rwt[d * 2 + n % 2], exs[d * 2 + n % 2], clxs[d * 2 + n % 2], q4s[d * 2 + n % 2], T4s[d * 2 + n % 2], GMs[d * 2 + n % 2], pcs[d * 2 + n % 2]
            H, Xs, XTs, TTs, R1, nU = Hs[d], Xss[d], XTss[d], TTss[d], R1s[d], nUs[d]
            rk_ = "S_rw" + sl
            lw = w[:, 5 + d, :]
            XK = lambda c_, p_: ("S_X", c_, p_, d)
            XTK = lambda c_, p_: ("S_XT", c_, p_, d)
            TTK = lambda c_, p_: ("S_TT", c_, p_, d)
            NB_ = ((4, 5, 6), (2, 7, 1))
            cur = 0
            for kq in range(6):
                nx = 1 - cur
                Xc, XTc, TTc = Xs[cur], XTs[cur], TTs[cur]
                Xn, XTn, TTn = Xs[nx], XTs[nx], TTs[nx]
                for p_ in range(2):
                    bX = NB_[p_][0]
                    for h in (2 * p_, 2 * p_ + 1):
                        kb.mm(pb[bX][:, (h % 2) * 128:(h % 2 + 1) * 128], XTc[:, h, :], Xc[:, h, :], True, True, rd=[XK(cur, p_), XTK(cur, p_)], wr=[P(bX)])
                    kb.cp(Xn[:, 2 * p_:2 * p_ + 2, :].rearrange("p h n -> p (h n)"), pb[bX][:, 0:256], rd=[P(bX)], wr=[XK(nx, p_)], eng="vector")
                if kq < 5:
                    for p_ in range(2):
                        bXT = NB_[p_][1]
                        for h in (2 * p_, 2 * p_ + 1):
                            kb.mm(pb[bXT][:, (h % 2) * 128:(h % 2 + 1) * 128], Xc[:, h, :], XTc[:, h, :], True, True, rd=[XK(cur, p_), XTK(cur, p_)], wr=[P(bXT)])
                        kb.cp(XTn[:, 2 * p_:2 * p_ + 2, :].rearrange("p h n -> p (h n)"), pb[bXT][:, 0:256], rd=[P(bXT)], wr=[XTK(nx, p_)], eng="scalar")
                for p_ in range(2):
                    bT = NB_[p_][2]
                    for h in (2 * p_, 2 * p_ + 1):
                        kb.mm(pb[bT][:, (h % 2) * 128:(h % 2 + 1) * 128], Xn[:, h, :], TTc[:, h, :], True, True, rd=[XK(nx, p_), TTK(cur, p_)], wr=[P(bT)])
                    kb.tt(TTn[:, 2 * p_:2 * p_ + 2, :].rearrange("p h n -> p (h n)"), pb[bT][:, 0:256],
                          TTc[:, 2 * p_:2 * p_ + 2, :].rearrange("p h n -> p (h n)"), ALU.add, rd=[P(bT), TTK(cur, p_)], wr=[TTK(nx, p_)])
                cur = nx
            TTf = TTs[cur]
            ktf = TTK(cur, 0)
            ktf1 = TTK(cur, 1)
            for h in range(4):
                kb.mm(pb[7][:, h * 64:(h + 1) * 64], T4[h][:, 0, :], H[:, h, :], True, False, rd=["S_T4_%d" % h + sl, "S_H" + dl], wr=[P(7)])
                kb.mm(pb[7][:, h * 64:(h + 1) * 64], GM[h][:, 1, 0:128], w[:, 2, h * 64:(h + 1) * 64], False, True, rd=["S_GM%d" % h + sl, rk_], wr=[P(7)])
            kb.cp(R1[:].rearrange("p h v -> p (h v)"), pb[7][:, 0:256], rd=[P(7)], wr=["S_R1" + dl], eng="vector")
            for h in range(4):
                kb.mm(pb[7][:, 256 + h * 64:256 + (h + 1) * 64], TTf[:, h, :], R1[:, h, :], True, True, rd=[ktf, ktf1, "S_R1" + dl], wr=[P(7)])
            kb.ts(nU[:].rearrange("p h v -> p (h v)"), pb[7][:, 256:512], -1.0, None, ALU.mult, None, rd=[P(7)], wr=["S_nU" + dl])
            want_y = (i < 16) or need_ctx
            if want_y:
                for h in range(4):
                    o = pb[3][:, h * 64:(h + 1) * 64]
                    kb.mm(o, T4[h][:, 1, :], H[:, h, :], True, False, rd=["S_T4_%d" % h + sl, "S_H" + dl], wr=[P(3)])
                    kb.mm(o, GM[h][:, 0, 128:256], nU[:, h, :], False, False, rd=["S_GM%d" % h + sl, "S_nU" + dl], wr=[P(3)])
                    kb.mm(o, GM[h][:, 1, 128:256], w[:, 2, h * 64:(h + 1) * 64], False, True, rd=["S_GM%d" % h + sl, rk_], wr=[P(3)])
                kb.tt(yacc[:, i, :], pb[3][:, 0:256], yacc[:, i, :], ALU.add, rd=[P(3), ("S_yacc", i)], wr=[("S_yacc", i)])
            for h in range(4):
                o = pb[2][0:64, h * 64:(h + 1) * 64]
                kb.mm(o, ident[0:64, 0:64], H[:, h, :], True, False, rd=["ident", "S_H" + dl], wr=[P(2)])
                kb.mm(o, q4[:, 2, h * 64:(h + 1) * 64], nU[:, h, :], False, False, rd=["S_q4" + sl, "S_nU" + dl], wr=[P(2)])
                kb.mm(o, q4[:, 3, h * 64:(h + 1) * 64], w[:, 2, h * 64:(h + 1) * 64], False, True, rd=["S_q4" + sl, rk_], wr=[P(2)])
            kb.tt(H[:], pb[2][0:64, 0:256].rearrange("p (h v) -> p h v", h=4), pc[:].unsqueeze(2).to_broadcast([64, 4, 64]), ALU.mult,
                  rd=[P(2), "S_pc" + sl], wr=["S_H" + dl])

        NCH = len(orders[0])
        front(0, 0)
        front(1, 0)
        for n in range(NCH):
            back(0, n)
            if n + 1 < NCH:
                front(0, n + 1)
            back(1, n)
            if n + 1 < NCH:
                front(1, n + 1)
        kb.S.barrier()
        ln = sb("S_ln", [128, 3, 256])
        kb.dma(ln[:, 0:2, :], C["b_lnx"][l:l + 1].to_broadcast([128, 2, 256]), rd=(), wr=["S_ln"])
        kb.dma(ln[:, 2, :], C["b_rk"][l:l + 1, :].to_broadcast([128, 256]), rd=(), wr=["S_ln"])
        NBR = 4
        st = [sb("S_st%d" % i, [128, 4]) for i in range(NBR)]
        st2 = [sb("S_st2%d" % i, [128, 4]) for i in range(NBR)]
        yc_ = [sb("S_yc%d" % i, [128, 256]) for i in range(NBR)]
        t1 = [sb("S_t1%d" % i, [128, 256]) for i in range(NBR)]
        t3 = [sb("S_t3%d" % i, [128, 256]) for i in range(NBR)]
        bo = [sb("S_bo%d" % i, [128, 4]) for i in range(NBR)]
        yo = [sb("S_yo%d" % i, [128, 256]) for i in range(NBR)]
        rwr = rwt
        v3 = lambda ap: ap.rearrange("p (h d) -> p h d", d=64)
        tiles = list(range(NT)) if need_ctx else list(range(16))

        def r0_(i):
            b = i % NBR
            rk_ = "S_rwr%d" % b
            w = rwr[b]
            kb.dma(w[:], RW[i * 128:(i + 1) * 128], rd=(), wr=[rk_])
            y = yacc[:, i, :]
            yk = ("S_yacc", i)
            kb.red(st[b][:], v3(y), ALU.add, rd=[yk], wr=["S_st%d" % b])
            kb.ts(st[b][:], st[b][:], 1.0 / 64, None, ALU.mult, None, rd=["S_st%d" % b], wr=["S_st%d" % b])
            kb.tt(v3(yc_[b][:]), v3(y), st[b][:].unsqueeze(2).to_broadcast([128, 4, 64]), ALU.subtract, rd=[yk, "S_st%d" % b], wr=["S_yc%d" % b])
            kb.tt(t1[b][:], yc_[b][:], yc_[b][:], ALU.mult, rd=["S_yc%d" % b], wr=["S_t1%d" % b], eng="gpsimd")

        def r1_(i):
            b = i % NBR
            rk_ = "S_rwr%d" % b
            w = rwr[b]
            kb.red(st2[b][:], v3(t1[b][:]), ALU.add, rd=["S_t1%d" % b], wr=["S_st2%d" % b])
            kb.ts(st2[b][:], st2[b][:], 1.0 / 64, 64e-5, ALU.mult, ALU.add, rd=["S_st2%d" % b], wr=["S_st2%d" % b])
            kb.act(st2[b][:], st2[b][:], AF.Sqrt, rd=["S_st2%d" % b], wr=["S_st2%d" % b])
            kb.tt(t3[b][:], w[:, 0, :], w[:, 1, :], ALU.mult, rd=[rk_], wr=["S_t3%d" % b], eng="gpsimd")
            kb.tt(t3[b][:], t3[b][:], ln[:, 2, :], ALU.mult, rd=["S_t3%d" % b, "S_ln"], wr=["S_t3%d" % b], eng="gpsimd")

        def r2_(i):
            b = i % NBR
            rk_ = "S_rwr%d" % b
            w = rwr[b]
            kb.recip(st2[b][:], st2[b][:], rd=["S_st2%d" % b], wr=["S_st2%d" % b])
            kb.tt(v3(yc_[b][:]), v3(yc_[b][:]), st2[b][:].unsqueeze(2).to_broadcast([128, 4, 64]), ALU.mult, rd=["S_yc%d" % b, "S_st2%d" % b], wr=["S_yc%d" % b])
            kb.tt(yc_[b][:], yc_[b][:], ln[:, 0, :], ALU.mult, rd=["S_yc%d" % b, "S_ln"], wr=["S_yc%d" % b], eng="gpsimd")
            kb.red(bo[b][:], v3(t3[b][:]), ALU.add, rd=["S_t3%d" % b], wr=["S_bo%d" % b])
            kb.tt(v3(t3[b][:]), v3(w[:, 2, :]), bo[b][:].unsqueeze(2).to_broadcast([128, 4, 64]), ALU.mult, rd=[rk_, "S_bo%d" % b, "S_t3%d" % b], wr=["S_t3%d" % b])

        def r3_(i):
            b = i % NBR
            rk_ = "S_rwr%d" % b
            w = rwr[b]
            kb.tt(t3[b][:], t3[b][:], ln[:, 1, :], ALU.add, rd=["S_t3%d" % b, "S_ln"], wr=["S_t3%d" % b], eng="gpsimd")
            kb.tt(yc_[b][:], yc_[b][:], t3[b][:], ALU.add, rd=["S_yc%d" % b, "S_t3%d" % b], wr=["S_yc%d" % b])
            kb.tt(yo[b][:], yc_[b][:], w[:, 7, :], ALU.mult, rd=["S_yc%d" % b, rk_], wr=["S_yo%d" % b])
            kb.dma(YB[i * 128:(i + 1) * 128, 256:512], yo[b][:], rd=["S_yo%d" % b], wr=[("YB", "S_yo%d" % b)], semres="S_yo%d" % b)

        pipeline([r0_, r1_, r2_, r3_], tiles)


def merge_phase(kb, l, need_ctx, x_in, ctx_in, X, Z, YB, H2T, MOD, ident, C, gate_all):
    nc = kb.nc
    with ExitStack() as es:
        sb = lambda n, s, dt=F32: es.enter_context(nc.sbuf_tensor(kb.un(n), s, dt))
        ps = lambda n, s, dt=F32: es.enter_context(nc.psum_tensor(kb.un(n), s, dt))
        wbr = sb("M_wbr", [128, 8, D], F32R)
        wout = sb("M_wout", [128, 8, D], F32R)
        kb.dma(wbr[:], C["w_branch"][l].rearrange("n (c p) d -> p (n c) d", p=128), rd=(), wr=["M_wbr"], q="gpsimd")
        kb.dma(wout[:], C["w_out"][l].rearrange("(c p) d -> p c d", p=128), rd=(), wr=["M_wout"], q="gpsimd")
        rwt = sb("M_rw", [128, 8, 16])
        kb.dma(rwt[:], C["router_w"].rearrange("(c p) e -> p c e", p=128), rd=(), wr=["M_rw"])
        rb = sb("M_rb", [128, 16])
        kb.dma(rb[:], C["router_b"].unsqueeze(0).to_broadcast([128, 16]), rd=(), wr=["M_rb"])
        gm = [sb("M_gm%d" % s, [128, D]) for s in range(2)]
        A2 = [sb("M_A2%d" % s, [128, D]) for s in range(2)]
        B2 = [sb("M_B2%d" % s, [128, D]) for s in range(2)]
        gbc = sb("M_gbc", [128, D])
        kb.dma(gbc[:], C["norm2"][l:l + 1, :].to_broadcast([128, D]), rd=(), wr=["M_gbc"])
        for s in range(2):
            kb.dma(gm[s][:], MOD[l, s:s + 1, 2 * D:3 * D].to_broadcast([128, D]), rd=(), wr=["M_gm%d" % s])
            kb.dma(A2[s][:], MOD[l, s:s + 1, 4 * D:5 * D].to_broadcast([128, D]), rd=(), wr=["M_A2%d" % s])
            kb.dma(B2[s][:], MOD[l, s:s + 1, 3 * D:4 * D].to_broadcast([128, D]), rd=(), wr=["M_B2%d" % s])
            kb.stt(A2[s][:], A2[s][:], 1.0, gbc[:], ALU.add, ALU.mult, rd=["M_A2%d" % s, "M_gbc"], wr=["M_A2%d" % s])
        yb_ = [sb("M_yb%d" % i_, [128, D]) for i_ in range(2)]
        ybT_ = [sb("M_ybT%d" % i_, [128, 8, 128], F32R) for i_ in range(2)]
        gt_ = [sb("M_gt%d" % i_, [128, 4 * D]) for i_ in range(2)]
        m_ = [sb("M_m%d" % i_, [128, D]) for i_ in range(2)]
        tmp_ = [sb("M_tmp%d" % i_, [128, 512]) for i_ in range(3)]
        mT_ = [sb("M_mT%d" % i_, [128, 8, 128], F32R) for i_ in range(2)]
        xt_ = [sb("M_xt%d" % i_, [128, D]) for i_ in range(2)]
        xn_ = [sb("M_xn%d" % i_, [128, D]) for i_ in range(2)]
        ss_ = [sb("M_ss%d" % i_, [128, 1]) for i_ in range(2)]
        h2_ = [sb("M_h2%d" % i_, [128, D]) for i_ in range(2)]
        h2T_ = [sb("M_h2T%d" % i_, [128, 8, 128]) for i_ in range(2)]
        sc = sb("M_sc", [128, NT, 16])
        bi = sb("M_bi", [128, NT, 16])
        r4 = sb("M_r4", [128, 8, NT, 4])
        msk = sb("M_msk", [128, NT, 16])
        r1 = sb("M_r1", [128, 4, NT])
        M_ptr = [ps("M_ptr%d" % i, [128, 4, 128]) for i in range(2)]
        M_pq = [ps("M_pq%d" % i, [128, 512]) for i in range(3)]
        M_pr = ps("M_pr", [128, 16])
        tiles = list(range(NT)) if need_ctx else list(range(16))
        kqc = [0]

        def transposes(src, srck, dst, dstk):
            for g in range(2):
                for j in range(4):
                    c = g * 4 + j
                    kb.tr(M_ptr[g][:, j, :], src[:, c * 128:(c + 1) * 128], ident[:], rd=srck, wr=["M_ptr%d" % g])
                kb.cp(dst[:, g * 4:(g + 1) * 4, :], M_ptr[g][:], rd=["M_ptr%d" % g], wr=[dstk], eng="scalar" if g == 0 else "vector")

        def s0(i):
            bb_ = i % 2
            kb.dma(yb_[bb_][:], YB[i * 128:(i + 1) * 128, :], rd=(), wr=["M_yb_%d" % bb_])

        def s1(i):
            bb_ = i % 2
            transposes(yb_[bb_], ["M_yb_%d" % bb_], ybT_[bb_], "M_ybT_%d" % bb_)
            kb.dma(gt_[bb_][:], Z[i * 128:(i + 1) * 128, O_G:O_G + 4 * D], rd=(), wr=[("M_gt", bb_, n_) for n_ in range(4)])

        def s2(i):
            bb_ = i % 2
            r0 = i * 128
            gt, m, ybT, xt = gt_[bb_], m_[bb_], ybT_[bb_], xt_[bb_]
            if l == 0:
                src = x_in[r0:r0 + 128, :] if i < 16 else ctx_in[r0 - SEQ:r0 - SEQ + 128, :]
            else:
                src = X[r0:r0 + 128, :]
            kb.dma(xt[:], src, rd=(), wr=["M_xt_%d" % bb_])
            for n in range(4):
                kb.act(gt[:, n * D:(n + 1) * D], gt[:, n * D:(n + 1) * D], AF.Sigmoid, rd=[("M_gt", bb_, n)], wr=[("M_gt", bb_, n)])
            for n in range(4):
                for hf in range(2):
                    b = kqc[0] % 3
                    kqc[0] += 1
                    pk = "M_pq%d" % b
                    for cc in range(2):
                        c = 2 * n + cc
                        kb.mm(M_pq[b][:], ybT[:, c, :], wbr[:, c, hf * 512:(hf + 1) * 512], cc == 0, cc == 1, rd=["M_ybT_%d" % bb_, "M_wbr"], wr=[pk])
                    gsl = gt[:, n * D + hf * 512:n * D + (hf + 1) * 512]
                    mk = ("M_m", hf, bb_)
                    if n == 0:
                        kb.tt(m[:, hf * 512:(hf + 1) * 512], M_pq[b][:], gsl, ALU.mult, rd=[pk, ("M_gt", bb_, n)], wr=[mk])
                    else:
                        tmp = tmp_[b]
                        kb.tt(tmp[:], M_pq[b][:], gsl, ALU.mult, rd=[pk, ("M_gt", bb_, n)], wr=["M_tmp%d" % b])
                        kb.tt(m[:, hf * 512:(hf + 1) * 512], m[:, hf * 512:(hf + 1) * 512], tmp[:], ALU.add, rd=[mk, "M_tmp%d" % b], wr=[mk], eng="gpsimd")
            transposes(m, [("M_m", 0, bb_), ("M_m", 1, bb_)], mT_[bb_], "M_mT_%d" % bb_)

        def s3(i):
            bb_ = i % 2
            s = 0 if i < 16 else 1
            r0 = i * 128
            mT, xt, xn, ss, h2 = mT_[bb_], xt_[bb_], xn_[bb_], ss_[bb_], h2_[bb_]
            xk = [("M_xnh", 0, bb_), ("M_xnh", 1, bb_)]
            for hf in range(2):
                b = kqc[0] % 3
                kqc[0] += 1
                pk = "M_pq%d" % b
                for c in range(8):
                    kb.mm(M_pq[b][:], mT[:, c, :], wout[:, c, hf * 512:(hf + 1) * 512], c == 0, c == 7, rd=["M_mT_%d" % bb_, "M_wout"], wr=[pk])
                tmp = tmp_[b]
                kb.tt(tmp[:], M_pq[b][:], gm[s][:, hf * 512:(hf + 1) * 512], ALU.mult, rd=[pk, "M_gm%d" % s], wr=["M_tmp%d" % b])
                kb.tt(xn[:, hf * 512:(hf + 1) * 512], tmp[:], xt[:, hf * 512:(hf + 1) * 512], ALU.add, rd=["M_tmp%d" % b, "M_xt_%d" % bb_], wr=[xk[hf]], eng="gpsimd")
            kb.dma(X[r0:r0 + 128, :], xn[:], rd=xk, wr=[("X", i)], semres="M_xn_%d" % bb_)
            hk = "M_h2_%d" % bb_
            sk = "M_ss_%d" % bb_
            kb.act(h2[:], xn[:], AF.Square, rd=xk, wr=[hk, sk], accum_out=ss[:])
            kb.rstd(ss[:], ss[:], D, EPS, rd=[sk], wr=[sk])
            kb.stt(h2[:], xn[:], ss[:, 0:1], A2[s][:], ALU.mult, ALU.mult, rd=xk + [sk, "M_A2%d" % s], wr=[hk])
            kb.tt(h2[:], h2[:], B2[s][:], ALU.add, rd=[hk, "M_B2%d" % s], wr=[hk])

        def s4(i):
            bb_ = i % 2
            r0 = i * 128
            h2, h2T = h2_[bb_], h2T_[bb_]
            tk = "M_h2T_%d" % bb_
            transposes(h2, ["M_h2_%d" % bb_], h2T, tk)
            kb.dma(H2T[:, :, r0:r0 + 128], h2T[:], rd=[tk], wr=[("H2T", i)], semres=tk)
            for c in range(8):
                kb.mm(M_pr[:], h2T[:, c, :], rwt[:, c, :], c == 0, c == 7, rd=[tk, "M_rw"], wr=["M_pr"])
            kb.act(sc[:, i, :], M_pr[:], AF.Sigmoid, rd=["M_pr"], wr=[("M_sc", i)])

        pipeline([s0, s1, s2, s3, s4], tiles)
        T_ = len(tiles)
        sck = [("M_sc", i_) for i_ in tiles]
        scv = sc[:, 0:T_, :]
        kb.tt(bi[:, 0:T_, :], scv, rb[:].unsqueeze(1).to_broadcast([128, T_, 16]), ALU.add, rd=sck + ["M_rb"], wr=["M_bi"])
        b4 = bi[:, 0:T_, :].rearrange("p t (g j) -> p t g j", j=4)
        K_ = ["M_r4"]
        R = lambda k_: r4[:, k_, 0:T_, :]
        kb.tt(R(0), b4[:, :, :, 0], b4[:, :, :, 1], ALU.max, rd=["M_bi"], wr=K_)
        kb.tt(R(1), b4[:, :, :, 0], b4[:, :, :, 1], ALU.min, rd=["M_bi"], wr=K_)
        kb.tt(R(2), b4[:, :, :, 2], b4[:, :, :, 3], ALU.max, rd=["M_bi"], wr=K_)
        kb.tt(R(3), b4[:, :, :, 2], b4[:, :, :, 3], ALU.min, rd=["M_bi"], wr=K_)
        kb.tt(R(4), R(0), R(2), ALU.max, rd=K_, wr=K_)
        kb.tt(R(5), R(0), R(2), ALU.min, rd=K_, wr=K_)
        kb.tt(R(6), R(1), R(3), ALU.max, rd=K_, wr=K_)
        kb.tt(R(5), R(5), R(6), ALU.max, rd=K_, wr=K_)
        kb.tt(R(4), R(4), R(5), ALU.add, rd=K_, wr=K_)
        kb.red(r1[:, 0, 0:T_], R(4), ALU.max, rd=K_, wr=["M_r1"])
        kb.tt(R(7), R(4), r1[:, 0, 0:T_].unsqueeze(2).to_broadcast([128, T_, 4]), ALU.is_equal, rd=K_ + ["M_r1"], wr=K_)
        kb.ts(R(6), R(7), 1e30, -1e30, ALU.mult, ALU.add, rd=K_, wr=K_)
        m4 = msk[:, 0:T_, :].rearrange("p t (g j) -> p t g j", j=4)
        kb.tt(m4, b4, R(7).unsqueeze(3).to_broadcast([128, T_, 4, 4]), ALU.mult, rd=["M_bi"] + K_, wr=["M_msk"])
        kb.tt(m4, m4, R(6).unsqueeze(3).to_broadcast([128, T_, 4, 4]), ALU.add, rd=["M_msk"] + K_, wr=["M_msk"])
        mv = msk[:, 0:T_, :]
        bv = bi[:, 0:T_, :]
        kb.red(r1[:, 1, 0:T_], mv, ALU.max, rd=["M_msk"], wr=["M_r1"])
        kb.tt(bv, mv, r1[:, 1, 0:T_].unsqueeze(2).to_broadcast([128, T_, 16]), ALU.is_equal, rd=["M_msk", "M_r1"], wr=["M_bi"])
        kb.stt(mv, bv, -1e30, mv, ALU.mult, ALU.add, rd=["M_bi", "M_msk"], wr=["M_msk"])
        kb.red(r1[:, 2, 0:T_], mv, ALU.max, rd=["M_msk"], wr=["M_r1"])
        kb.tt(mv, mv, r1[:, 2, 0:T_].unsqueeze(2).to_broadcast([128, T_, 16]), ALU.is_equal, rd=["M_msk", "M_r1"], wr=["M_msk"])
        kb.tt(bv, bv, mv, ALU.add, rd=["M_bi", "M_msk"], wr=["M_bi"])
        kb.tt(bv, bv, scv, ALU.mult, rd=["M_bi"] + sck, wr=["M_bi"])
        kb.red(r1[:, 3, 0:T_], bv, ALU.add, rd=["M_bi"], wr=["M_r1"])
        kb.recip(r1[:, 3, 0:T_], r1[:, 3, 0:T_], rd=["M_r1"], wr=["M_r1"])
        t0_ = tiles[0]
        kb.tt(gate_all[:, t0_:t0_ + T_, :], bv, r1[:, 3, 0:T_].unsqueeze(2).to_broadcast([128, T_, 16]), ALU.mult, rd=["M_bi", "M_r1"],
              wr=[("gate", i_) for i_ in tiles])


def moe_phase(kb, l, need_ctx, X, H2T, MOD, C, gate_all, out):
    nc = kb.nc
    last = (l == DEPTH - 1)
    tiles_all = list(range(NT)) if need_ctx else list(range(16))
    passes = [tiles_all[0:9], tiles_all[9:]]
    with ExitStack() as es:
        sb = lambda n, s, dt=F32: es.enter_context(nc.sbuf_tensor(kb.un(n), s, dt))
        ps = lambda n, s, dt=F32: es.enter_context(nc.psum_tensor(kb.un(n), s, dt))
        hT = sb("E_hT", [128, 8, 9 * 128], F32R)
        yacc = sb("E_yacc", [128, 9, D])
        Wg = [sb("E_Wg%d" % i, [128, 8, DE], F32R) for i in range(2)]
        Wu = [sb("E_Wu%d" % i, [128, 8, DE], F32R) for i in range(2)]
        Wd = [sb("E_Wd%d" % i, [128, 4, D], F32R) for i in range(2)]
        sg = [sb("E_sg%d" % i, [128, 512]) for i in range(2)]
        hid = sb("E_hid", [128, 4, 512], F32R)
        gmlp = [sb("E_gmlp%d" % s, [128, D]) for s in range(2)]
        xt = [sb("E_xt%d" % i, [128, D]) for i in range(2)]
        pg = [ps("E_pG%d" % i, [128, 512]) for i in range(2)]
        pu = [ps("E_pU%d" % i, [128, 512]) for i in range(2)]
        py = [ps("E_pY%d" % i, [128, 512]) for i in range(2)]
        for s in range(2):
            kb.dma(gmlp[s][:], MOD[l, s:s + 1, 5 * D:6 * D].to_broadcast([128, D]), rd=(), wr=["E_gmlp%d" % s])
        kg = 0
        ky = 0
        for pi, tl in enumerate(passes):
            ntl = len(tl)
            t0 = tl[0] * 128
            def load_w(e_):
                wb_ = e_ % 2
                kb.dma(Wg[wb_][:], C["e_gate"][l, e_].rearrange("(c p) n -> p c n", p=128), rd=(), wr=["E_Wg%d" % wb_], q="gpsimd")
                kb.dma(Wu[wb_][:], C["e_up"][l, e_].rearrange("(c p) n -> p c n", p=128), rd=(), wr=["E_Wu%d" % wb_], q="gpsimd")
                kb.dma(Wd[wb_][:], C["e_down"][l, e_].rearrange("(c p) n -> p c n", p=128), rd=(), wr=["E_Wd%d" % wb_], q="gpsimd")

            load_w(0)
            for c in range(8):
                kb.dma(hT[:, c, 0:ntl * 128], H2T[:, c, t0:t0 + ntl * 128], rd=(), wr=["E_hT"], q="gpsimd")
            load_w(1)
            groups = []
            ng_ = (ntl + 3) // 4
            a = 0
            for gi_ in range(ng_):
                n_ = (ntl - a + (ng_ - gi_) - 1) // (ng_ - gi_)
                groups.append((a, n_))
                a += n_
            for e in range(NE):
                wb = e % 2
                if e >= 2:
                    load_w(e)
                for (a, n_) in groups:
                    nt = n_ * 128
                    for hc in range(4):
                        b = kg % 2
                        kg += 1
                        for c in range(8):
                            kb.mm(pg[b][:, 0:nt], Wg[wb][:, c, hc * 128:(hc + 1) * 128], hT[:, c, a * 128:a * 128 + nt], c == 0, c == 7,
                                  rd=["E_Wg%d" % wb, "E_hT"], wr=["E_pG%d" % b])
                        for c in range(8):
                            kb.mm(pu[b][:, 0:nt], Wu[wb][:, c, hc * 128:(hc + 1) * 128], hT[:, c, a * 128:a * 128 + nt], c == 0, c == 7,
                                  rd=["E_Wu%d" % wb, "E_hT"], wr=["E_pU%d" % b])
                        kb.act(sg[b][:, 0:nt], pg[b][:, 0:nt], AF.Silu, rd=["E_pG%d" % b], wr=["E_sg%d" % b])
                        kb.tt(hid[:, hc, 0:nt], pu[b][:, 0:nt], sg[b][:, 0:nt], ALU.mult, rd=["E_pU%d" % b, "E_sg%d" % b], wr=[("E_hid", hc)])
                    for tt_ in range(n_):
                        ti = a + tt_
                        gi = tl[ti]
                        for hf in range(2):
                            b = ky % 2
                            ky += 1
                            for hc in range(4):
                                kb.mm(py[b][:], hid[:, hc, tt_ * 128:(tt_ + 1) * 128], Wd[wb][:, hc, hf * 512:(hf + 1) * 512], hc == 0, hc == 3,
                                      rd=[("E_hid", hc), "E_Wd%d" % wb], wr=["E_pY%d" % b])
                            ya = yacc[:, ti, hf * 512:(hf + 1) * 512]
                            yk = ("E_yacc", ti, hf)
                            gcol = gate_all[:, gi, e:e + 1]
                            if e == 0:
                                kb.ts(ya, py[b][:], gcol, None, ALU.mult, None, rd=["E_pY%d" % b, ("gate", gi)], wr=[yk])
                            else:
                                kb.stt(ya, py[b][:], gcol, ya, ALU.mult, ALU.add, rd=["E_pY%d" % b, ("gate", gi), yk], wr=[yk])
            for ti, gi in enumerate(tl):
                s = 0 if gi < 16 else 1
                b = ti % 2
                r0 = gi * 128
                kb.dma(xt[b][:], X[r0:r0 + 128, :], rd=[("X", gi)], wr=["E_xt%d" % b])
                kb.tt(yacc[:, ti, :], yacc[:, ti, :], gmlp[s][:], ALU.mult, rd=[("E_yacc", ti, 0), ("E_yacc", ti, 1), "E_gmlp%d" % s], wr=[("E_yacc", ti, 0), ("E_yacc", ti, 1)], eng="gpsimd")
                kb.tt(xt[b][:], xt[b][:], yacc[:, ti, :], ALU.add, rd=["E_xt%d" % b, ("E_yacc", ti, 0), ("E_yacc", ti, 1)], wr=["E_xt%d" % b])
                if last:
                    kb.dma(out[r0:r0 + 128, :], xt[b][:], rd=["E_xt%d" % b], wr=[("out", gi)], semres="E_xt%d" % b, is_output=True)
                else:
                    kb.dma(X[r0:r0 + 128, :], xt[b][:], rd=["E_xt%d" % b], wr=[("X", gi)], semres="E_xt%d" % b)


def build(debug=(), stop_after=None):
    kb = KB(debug)
    nc, S = kb.nc, kb.S
    x_in = kb.inp("x", [SEQ, D])
    ctx_in = kb.inp("ctx", [CTX, D])
    cmat = kb.inp("cmat", [128, 8, 2])
    w_mod = kb.inp("w_mod", [DEPTH, D, 6 * D])
    b_mod = kb.inp("b_mod", [DEPTH, 6 * D])
    norm1 = kb.inp("norm1", [DEPTH, D])
    norm2 = kb.inp("norm2", [DEPTH, D])
    w_in = kb.inp("w_in", [DEPTH, D, D_IN])
    ident_in = kb.inp("ident", [128, 128])
    out = nc.dram_tensor("out", [SEQ, D], F32, kind="ExternalOutput").ap()
    X = kb.scratch("X", [NTOK, D])
    Z = kb.scratch("Z", [NTOK, D_IN])
    MOD = kb.scratch("MOD", [DEPTH, 2, 6 * D])
    YB = kb.scratch("YB", [NTOK, D])
    RW = kb.scratch("RW", [NTOK, 8, 256])
    H2T = kb.scratch("H2T", [128, 8, NTOK])
    C = {}
    for nm_, shp_ in (("b_shift", [DEPTH, 2, 896]), ("b_w0", [DEPTH, 2, 256]), ("b_w2", [DEPTH, 2, 32, 256]), ("b_a0", [DEPTH, 256]),
                      ("b_a2", [DEPTH, 32, 256]), ("b_g2", [DEPTH, 64, 256]), ("b_kk", [DEPTH, 256]), ("b_ka", [DEPTH, 256]),
                      ("b_rk", [DEPTH, 256]), ("b_lnx", [DEPTH, 2, 256]), ("rwkv_masks", [2, 128, 512])):
        C[nm_] = kb.inp(nm_, shp_)
    for nm_, shp_ in (("w_branch", [DEPTH, 4, 256, D]), ("w_out", [DEPTH, D, D]), ("router_w", [D, NE]), ("router_b", [NE]),
                      ("e_gate", [DEPTH, NE, D, DE]), ("e_up", [DEPTH, NE, D, DE]), ("e_down", [DEPTH, NE, DE, D])):
        C[nm_] = kb.inp(nm_, shp_)
    C["norm2"] = norm2
    C["a_qk_norm"] = kb.inp("a_qk_norm", [DEPTH, 2, 64])
    C["a_sink"] = kb.inp("a_sink", [DEPTH, 4])
    C["c_qk_norm"] = kb.inp("c_qk_norm", [DEPTH, 2, 32])
    C["c_lambda"] = kb.inp("c_lambda", [DEPTH, 4, 32])
    C["c_subln"] = kb.inp("c_subln", [DEPTH, 64])
    C["d_qk_norm"] = kb.inp("d_qk_norm", [DEPTH, 2, 64])
    C["ropeA"] = kb.inp("ropeA", [SEQ, 2, 384])
    C["ropeC"] = kb.inp("ropeC", [SEQ, 2, 512])
    C["bandmask"] = kb.inp("bandmask", [2, 128, 128])
    C["biasG"] = kb.inp("biasG", [DEPTH, 128, 8 * 4 * 4 * 64])
    C["maskD"] = kb.inp("maskD", [128, 64])

    with ExitStack() as es0:
        ident = es0.enter_context(nc.sbuf_tensor("ident_sb", [128, 128], F32))
        kb.dma(ident[:], ident_in, rd=(), wr=["ident"])
        gate_all = es0.enter_context(nc.sbuf_tensor("gate_all", [128, NT, 16], F32))
        kb.onez = es0.enter_context(nc.sbuf_tensor("onez", [128, 2], F32))
        kb.memset(kb.onez[:, 0:1], 1.0, wr=["onez"])
        kb.memset(kb.onez[:, 1:2], 0.0, wr=["onez"])

        for l in range(DEPTH):
            with ExitStack() as es:
                sb = lambda n, s, dt=F32: es.enter_context(nc.sbuf_tensor(kb.un(n), s, dt))
                ps = lambda n, s, dt=F32: es.enter_context(nc.psum_tensor(kb.un(n), s, dt))
                cT = sb("cT", [128, 8, 2])
                scT = sb("scT", [128, 8, 2], F32R)
                bm = sb("bm", [2, 6 * D])
                modsb = sb("modsb", [2, 6 * D])
                wm = [sb("wm%d" % i, [128, 8, 512], F32R) for i in range(2)]
                pm = [ps("pm%d" % i, [2, 512]) for i in range(2)]
                kb.dma(cT[:], cmat, rd=(), wr=["cT"])
                kb.dma(bm[:], b_mod[l:l + 1, :].to_broadcast([2, 6 * D]), rd=(), wr=["bm"])
                kb.act(scT[:], cT[:], AF.Silu, rd=["cT"], wr=["scT"])
                for cg in range(12):
                    w = wm[cg % 2]
                    wk = "wm%d" % (cg % 2)
                    pk = "pm%d" % (cg % 2)
                    kb.dma(w[:], w_mod[l, :, cg * 512:(cg + 1) * 512].rearrange("(c p) n -> p c n", p=128), rd=(), wr=[wk], q="gpsimd")
                    for c in range(8):
                        kb.mm(pm[cg % 2][:], scT[:, c, :], w[:, c, :], c == 0, c == 7, rd=["scT", wk], wr=[pk])
                    kb.tt(modsb[:, cg * 512:(cg + 1) * 512], pm[cg % 2][:], bm[:, cg * 512:(cg + 1) * 512], ALU.add, rd=[pk, "bm"], wr=["modsb"])
                kb.dma(MOD[l], modsb[:], rd=["modsb"], wr=[("MOD", l)])
            S.barrier()

            with ExitStack() as es:
                sb = lambda n, s, dt=F32: es.enter_context(nc.sbuf_tensor(kb.un(n), s, dt))
                ps = lambda n, s, dt=F32: es.enter_context(nc.psum_tensor(kb.un(n), s, dt))
                hT = sb("hT", [128, 8, NTOK], F32R)
                Am = [sb("Am%d" % s, [128, D]) for s in range(2)]
                Bm = [sb("Bm%d" % s, [128, D]) for s in range(2)]
                gbc = sb("gbc", [128, D])
                xt = [sb("xt%d" % i, [128, D]) for i in range(2)]
                sq = sb("sq", [128, D])
                ss = [sb("ss%d" % i, [128, 1]) for i in range(2)]
                hh = [sb("hh%d" % i, [128, D]) for i in range(2)]
                ptr = [ps("ptr%d" % i, [128, 4, 128]) for i in range(2)]
                kb.dma(gbc[:], norm1[l:l + 1, :].to_broadcast([128, D]), rd=(), wr=["gbc"])
                for s in range(2):
                    kb.dma(Am[s][:], MOD[l, s:s + 1, D:2 * D].to_broadcast([128, D]), rd=[("MOD", l)], wr=["Am%d" % s])
                    kb.dma(Bm[s][:], MOD[l, s:s + 1, 0:D].to_broadcast([128, D]), rd=[("MOD", l)], wr=["Bm%d" % s])
                    kb.stt(Am[s][:], Am[s][:], 1.0, gbc[:], ALU.add, ALU.mult, rd=["Am%d" % s, "gbc"], wr=["Am%d" % s])
                for i in range(NT):
                    s = 0 if i < 16 else 1
                    b = i % 2
                    if l == 0:
                        src = x_in[i * 128:(i + 1) * 128, :] if i < 16 else ctx_in[(i - 16) * 128:(i - 15) * 128, :]
                        srck = ()
                    else:
                        src = X[i * 128:(i + 1) * 128, :]
                        srck = [("X", i)]
                    kb.dma(xt[b][:], src, rd=srck, wr=["xt%d" % b])
                    kb.act(sq[:], xt[b][:], AF.Square, rd=["xt%d" % b], wr=["sq", "ss%d" % b], accum_out=ss[b][:])
                    kb.rstd(ss[b][:], ss[b][:], D, EPS, rd=["ss%d" % b], wr=["ss%d" % b])
                    kb.stt(hh[b][:], xt[b][:], ss[b][:, 0:1], Am[s][:], ALU.mult, ALU.mult, rd=["xt%d" % b, "ss%d" % b, "Am%d" % s], wr=["hh%d" % b])
                    kb.tt(hh[b][:], hh[b][:], Bm[s][:], ALU.add, rd=["hh%d" % b, "Bm%d" % s], wr=["hh%d" % b], eng="gpsimd")
                    for g in range(2):
                        for j in range(4):
                            c = g * 4 + j
                            kb.tr(ptr[g][:, j, :], hh[b][:, c * 128:(c + 1) * 128], ident[:], rd=["hh%d" % b], wr=["ptr%d" % g])
                        kb.cp(hT[:, g * 4:(g + 1) * 4, i * 128:(i + 1) * 128], ptr[g][:], rd=["ptr%d" % g], wr=[("hT", i)],
                              eng="scalar" if g == 0 else "vector")
                wz = [sb("wz%d" % i, [128, 8, 512], F32R) for i in range(2)]
                pz = [ps("pz%d" % i, [128, 512]) for i in range(4)]
                zs = [sb("zs%d" % i, [128, 512]) for i in range(4)]
                k = 0
                for cg in range(14):
                    n0 = cg * 512
                    ncol = min(512, D_IN - n0)
                    w = wz[cg % 2]
                    wk = "wz%d" % (cg % 2)
                    kb.dma(w[:, :, 0:ncol], w_in[l, :, n0:n0 + ncol].rearrange("(c p) n -> p c n", p=128), rd=(), wr=[wk], q="gpsimd")
                    for i in range(NT):
                        r = k % 4
                        k += 1
                        for c in range(8):
                            kb.mm(pz[r][:, 0:ncol], hT[:, c, i * 128:(i + 1) * 128], w[:, c, 0:ncol], c == 0, c == 7,
                                  rd=[("hT", i), wk], wr=["pz%d" % r])
                        kb.cp(zs[r][:, 0:ncol], pz[r][:, 0:ncol], rd=["pz%d" % r], wr=["zs%d" % r], eng="scalar" if r % 2 == 0 else "vector")
                        kb.dma(Z[i * 128:(i + 1) * 128, n0:n0 + ncol], zs[r][:, 0:ncol], rd=["zs%d" % r], wr=[("Z", i, cg)], semres="zs%d" % r)
            S.barrier()
            if stop_after == ("proj", l):
                break
            need_ctx = l < DEPTH - 1
            if stop_after != ("C", l):
                rwkv(kb, l, need_ctx, Z, YB, RW, ident, C)
                S.barrier()
            if stop_after == ("B", l):
                break
            attn_A(kb, l, need_ctx, Z, YB, ident, C)
            S.barrier()
            if stop_after == ("A", l):
                break
            attn_D(kb, l, need_ctx, Z, YB, ident, C)
            S.barrier()
            if stop_after == ("D", l):
                break
            attn_C(kb, l, need_ctx, Z, YB, ident, C)
            S.barrier()
            if stop_after == ("C", l):
                break
            merge_phase(kb, l, need_ctx, x_in, ctx_in, X, Z, YB, H2T, MOD, ident, C, gate_all)
            S.barrier()
            if stop_after == ("M", l):
                break
            moe_phase(kb, l, need_ctx, X, H2T, MOD, C, gate_all, out)
            S.barrier()
    S.barrier()
    cnt = S.finish()
    print("instr counts", cnt, "dma sems", S.n_dma_sems)
    return kb


_CONST_CACHE = {}


def host_constants():
    if _CONST_CACHE:
        return _CONST_CACHE
    t = np.arange(SEQ)
    rows, cols = (t // 64).astype(np.float32), (t % 64).astype(np.float32)

    def table(hd, G):
        h = hd // 2
        inv = (np.float32(10000.0) ** (-np.arange(0, h, 2, dtype=np.float32) / np.float32(h))).astype(np.float32)
        ar = rows[:, None] * inv[None, :]
        ac = cols[:, None] * inv[None, :]
        cos = np.concatenate([np.cos(ar), np.cos(ar), np.cos(ac), np.cos(ac)], -1).astype(np.float32)
        sin = np.concatenate([-np.sin(ar), np.sin(ar), -np.sin(ac), np.sin(ac)], -1).astype(np.float32)
        return np.ascontiguousarray(np.stack([np.tile(cos, (1, G)), np.tile(sin, (1, G))], 1))
    _CONST_CACHE["ropeA"] = table(64, 6)
    _CONST_CACHE["ropeC"] = table(32, 16)
    b = np.arange(128)[:, None]
    a = np.arange(128)[None, :]
    _CONST_CACHE["bandmask"] = np.stack([(a <= b), (b <= a)]).astype(np.float32)
    p = np.arange(128)
    kcol = p % 64
    q = np.arange(64)
    cstart = np.clip(q - 8, 0, 48)
    _CONST_CACHE["maskD"] = ((kcol[:, None] >= cstart[None, :]) & (kcol[:, None] < cstart[None, :] + 16)).astype(np.float32)
    _CONST_CACHE["ident"] = np.eye(128, dtype=np.float32)
    s_ = np.arange(128)[:, None]
    t_ = np.arange(128)[None, :]
    mk = []
    for d in range(2):
        le = (s_ <= t_) if d == 0 else (s_ >= t_)
        lt = (s_ < t_) if d == 0 else (s_ > t_)
        mk.append(np.concatenate([le, lt, le, lt.T], 1).astype(np.float32))
    _CONST_CACHE["rwkv_masks"] = np.stack(mk)
    return _CONST_CACHE


def gather_biasG(d_rpb):
    d_rpb = np.asarray(d_rpb, dtype=np.float32)
    p = np.arange(128)
    jl, kcol = p // 64, p % 64
    q = np.arange(64)
    dc = np.clip(kcol[:, None] - q[None, :] + 15, 0, 30)
    out = np.zeros((DEPTH, 128, 8, 4, 4, 64), np.float32)
    for cls in range(8):
        r = cls if cls < 4 else (4 if cls == 4 else 24 + cls)
        start = min(max(r - 4, 0), 24)
        for t in range(4):
            key_row = start + 2 * t + jl
            dr = key_row - r + 7
            for l in range(DEPTH):
                for h in range(4):
                    out[l, :, cls, h, t, :] = d_rpb[l, h][dr[:, None], dc]
    return np.ascontiguousarray(out.reshape(DEPTH, 128, 8 * 4 * 4 * 64))


def make_in_map(inp, b, kb, shared=None):
    f = lambda a: np.ascontiguousarray(np.asarray(a, dtype=np.float32))
    cm = np.stack([np.asarray(inp["c"][b]).reshape(8, 128).T, np.asarray(inp["c_ctx"]).reshape(8, 128).T], axis=-1)
    m = {"x": f(inp["x"][b]), "ctx": f(inp["ctx"][b]), "cmat": f(cm)}
    if shared is None:
        shared = {}
    if "biasG" not in shared:
        shared.update(host_constants())
        shared["biasG"] = gather_biasG(inp["d_rpb"])
    for k in kb.ins:
        if k in m:
            continue
        if k not in shared:
            shared[k] = f(inp[k])
        m[k] = shared[k]
    return m


_KB_CACHE = {}


def kernel(**inputs):
    inp = {k: np.asarray(v) for k, v in inputs.items()}
    if "kb" not in _KB_CACHE:
        _KB_CACHE["kb"] = build()
    kb = _KB_CACHE["kb"]
    shared = {}
    n = 8
    in_maps = [make_in_map(inp, b, kb, shared) for b in range(n)]
    res = run_bass_kernel_spmd(kb.nc, in_maps, core_ids=list(range(n)))
    return np.stack([np.asarray(res.results[b]["out"], dtype=np.float32) for b in range(n)], axis=0)
```

```python
import math
import numpy as np
from contextlib import ExitStack
import concourse.bass as bass
import concourse.mybir as mybir
from concourse.bass_utils import run_bass_kernel_spmd

F32 = mybir.dt.float32
F32R = mybir.dt.float32r
AF = mybir.ActivationFunctionType
ALU = mybir.AluOpType
AX = mybir.AxisListType
ALLENG = ("tensor", "vector", "scalar", "gpsimd", "sync")

D = 1024
SEQ = 2048
CTX = 256
NT = 18
NTOK = SEQ + CTX
DEPTH = 2
D_IN = 7040
O_AQ, O_AK, O_AV, O_BZ, O_CQ, O_CK, O_CV, O_DQ, O_DK, O_DV, O_G = 0, 256, 384, 512, 1408, 1664, 1920, 2176, 2432, 2688, 2944
EPS = 1e-6
NE = 16
DE = 512


import re as _re
_PSUM_RE = _re.compile(r"(^E_p[GUY]|^M_pr|^pm\d|^ptr\d|^pz\d|_ptr|_pS|_pO|_pl\d|_pd\d|_pag|_pb\d|^pq\d|^M_p)")


def is_psum_key(k):
    return isinstance(k, str) and _PSUM_RE.search(k) is not None


class Sched:
    def __init__(self, nc):
        self.nc = nc
        self.seq = {e: 0 for e in ALLENG}
        self.res = {}
        self.waited = {e: {} for e in ALLENG}
        self.dma_cnt = {}
        self.out_tokens = []
        self.n_dma_sems = 0
        self.n_sw_sems = 0
        self.dma_sem_of = {}
        self.sems = {}
        self.count = {e: 0 for e in ALLENG}

    def _need(self, eng, tok, waits):
        if tok is None:
            return
        semkey, val, _ = tok
        w = self.waited[eng]
        if w.get(semkey, 0) >= val:
            return
        w[semkey] = val
        waits.append((semkey, val))

    def _deps(self, eng, rd, wr, is_dma):
        waits = []
        for k in rd:
            st = self.res.get(k)
            if st is None:
                continue
            wtok = st[0]
            if wtok is not None:
                self._need(eng, wtok, waits)
        for k in wr:
            st = self.res.get(k)
            if st is None:
                continue
            wtok = st[0]
            if wtok is not None:
                if is_dma or wtok[2] != eng or eng != "tensor":
                    self._need(eng, wtok, waits)
            for rtok in st[1].values():
                if is_dma or rtok[2] != eng or eng != "tensor":
                    self._need(eng, rtok, waits)
        return waits

    def _record(self, tok, rd, wr):
        for k in rd:
            st = self.res.setdefault(k, [None, {}])
            old = st[1].get(tok[0])
            if old is None or old[1] < tok[1]:
                st[1][tok[0]] = tok
        for k in wr:
            self.res[k] = [tok, {}]

    def _sem(self, semkey):
        h = self.sems.get(semkey)
        if h is None:
            h = self.nc.alloc_semaphore("sem_%s_%s" % semkey)
            self.sems[semkey] = h
        return h

    def _emit(self, eng, waits, fn, inc):
        e = getattr(self.nc, eng)
        for semkey, val in waits:
            e.wait_ge(self._sem(semkey), val)
        ins = fn(e)
        ins.then_inc(self._sem(inc[0]), inc[1])
        self.count[eng] += 1

    def op(self, eng, fn, rd=(), wr=()):
        pr = [k for k in rd if is_psum_key(k)]
        if pr:
            rd = [k for k in rd if not is_psum_key(k)]
            wr = list(wr) + pr
        waits = self._deps(eng, rd, wr, False)
        self.seq[eng] += 1
        tok = (("eng", eng), self.seq[eng], eng)
        self._emit(eng, waits, fn, (("eng", eng), 1))
        self._record(tok, rd, wr)
        return tok

    def dma(self, fn, rd=(), wr=(), q="sync", semres=None, is_output=False):
        waits = self._deps(q, rd, wr, True)
        key = semres if semres is not None else (wr[0] if wr else rd[0])
        semkey = self.dma_sem_of.get((q, key))
        if semkey is None:
            if q == "gpsimd":
                semkey = ("dma", 74 + self.n_sw_sems % 20)
                self.n_sw_sems += 1
            else:
                semkey = ("dma", self.n_dma_sems % 74)
                self.n_dma_sems += 1
            self.dma_sem_of[(q, key)] = semkey
        self.dma_cnt[semkey] = self.dma_cnt.get(semkey, 0) + 16
        tok = (semkey, self.dma_cnt[semkey], None)
        self._emit(q, waits, fn, (semkey, 16))
        self._record(tok, rd, wr)
        if is_output:
            self.out_tokens.append(tok)
        return tok

    def barrier(self):
        toks = [(("eng", e), self.seq[e], e) for e in ALLENG if self.seq[e] > 0]
        toks += [(k, v, None) for k, v in self.dma_cnt.items()]
        for e in ALLENG:
            waits = []
            for t in toks:
                if t[2] == e:
                    continue
                self._need(e, t, waits)
            eo = getattr(self.nc, e)
            for semkey, val in waits:
                eo.wait_ge(self._sem(semkey), val)

    def finish(self):
        fin = []
        for tok in self.out_tokens:
            self._need("sync", tok, fin)
        for semkey, val in fin:
            self.nc.sync.wait_ge(self._sem(semkey), val)
        return dict(self.count)


class Rot:
    def __init__(self, items):
        self.items = list(items)
        self.i = 0

    def next(self):
        it = self.items[self.i % len(self.items)]
        self.i += 1
        return it


class KB:
    def __init__(self, debug=()):
        self.nc = bass.Bass("TRN2", target_bir_lowering=False)
        self.S = Sched(self.nc)
        self.debug = set(debug)
        self.ins = {}
        self.uid = 0

    def un(self, name):
        self.uid += 1
        return "%s_u%d" % (name, self.uid)

    def inp(self, name, shape):
        t = self.nc.dram_tensor(name, list(shape), F32, kind="ExternalInput").ap()
        self.ins[name] = t
        return t

    def scratch(self, name, shape):
        kind = "ExternalOutput" if name in self.debug else "Internal"
        return self.nc.dram_tensor(name, list(shape), F32, kind=kind).ap()

    def dma(self, out, in_, rd, wr, q="sync", is_output=False, semres=None):
        return self.S.dma(lambda e: e.dma_start(out=out, in_=in_), rd=rd, wr=wr, q=q, is_output=is_output, semres=semres)

    def mm(self, out, lhsT, rhs, start, stop, rd, wr):
        return self.S.op("tensor", lambda e: e.matmul(out=out, lhsT=lhsT, rhs=rhs, start=start, stop=stop), rd=rd, wr=wr)

    def tr(self, out, in_, ident, rd, wr):
        return self.S.op("tensor", lambda e: e.transpose(out=out, in_=in_, identity=ident), rd=list(rd) + ["ident"], wr=wr)

    def act(self, out, in_, func, rd, wr, bias=None, scale=None, accum_out=None):
        kw = {}
        if bias is not None:
            kw["bias"] = bias
        if scale is not None:
            kw["scale"] = scale
        if accum_out is not None:
            kw["accum_out"] = accum_out
        return self.S.op("scalar", lambda e: e.activation(out=out, in_=in_, func=func, **kw), rd=rd, wr=wr)

    def tt(self, out, a, b, op, rd, wr, eng="vector"):
        return self.S.op(eng, lambda e: e.tensor_tensor(out=out, in0=a, in1=b, op=op), rd=rd, wr=wr)

    def ts(self, out, a, s1, s2, op0, op1, rd, wr, eng="vector"):
        if op1 is None:
            return self.S.op(eng, lambda e: e.tensor_scalar(out=out, in0=a, scalar1=s1, scalar2=None, op0=op0), rd=rd, wr=wr)
        return self.S.op(eng, lambda e: e.tensor_scalar(out=out, in0=a, scalar1=s1, scalar2=s2, op0=op0, op1=op1), rd=rd, wr=wr)

    def stt(self, out, a, s, b, op0, op1, rd, wr):
        return self.S.op("vector", lambda e: e.scalar_tensor_tensor(out=out, in0=a, scalar=s, in1=b, op0=op0, op1=op1), rd=rd, wr=wr)

    def red(self, out, in_, op, rd, wr, axis=AX.X):
        return self.S.op("vector", lambda e: e.tensor_reduce(out=out, in_=in_, axis=axis, op=op), rd=rd, wr=wr)

    def cp(self, out, in_, rd, wr, eng="vector"):
        if eng == "scalar":
            return self.act(out, in_, AF.Copy, rd, wr)
        return self.S.op(eng, lambda e: e.tensor_copy(out=out, in_=in_), rd=rd, wr=wr)

    def recip(self, out, in_, rd, wr):
        return self.S.op("vector", lambda e: e.reciprocal(out=out, in_=in_), rd=rd, wr=wr)

    def memset(self, out, val, wr, eng="vector"):
        return self.S.op(eng, lambda e: e.memset(out, val), rd=(), wr=wr)

    def rstd(self, out, ss, n, eps, rd, wr):
        self.ts(out, ss, 1.0 / n, eps, ALU.mult, ALU.add, rd=rd, wr=wr)
        self.act(out, out, AF.Sqrt, rd=wr, wr=wr)
        self.recip(out, out, rd=wr, wr=wr)


def pipeline(stages, items):
    n = len(items)
    for it in range(n + len(stages) - 1):
        for s_, f in enumerate(stages):
            j = it - s_
            if 0 <= j < n:
                f(items[j])


def qk_prep(kb, es, l, Z, ident, zcol0, G, gd, nq_groups, gain_ap, rope_ap, tiles, store):
    nc = kb.nc
    W = G * gd
    NB = 6
    sb = lambda n, s, dt=F32: es.enter_context(nc.sbuf_tensor(kb.un(n), s, dt))
    gain = sb("qk_gain", [128, W])
    kb.dma(gain[:, 0:nq_groups * gd].rearrange("p (g d) -> p g d", d=gd),
           gain_ap[l, 0:1, :].to_broadcast([128, gd]).unsqueeze(1).to_broadcast([128, nq_groups, gd]), rd=(), wr=["qk_gain"])
    kb.dma(gain[:, nq_groups * gd:W].rearrange("p (g d) -> p g d", d=gd),
           gain_ap[l, 1:2, :].to_broadcast([128, gd]).unsqueeze(1).to_broadcast([128, G - nq_groups, gd]), rd=(), wr=["qk_gain"])
    zt = [sb("qk_zt%d" % i, [128, W]) for i in range(NB)]
    sq = [sb("qk_sq%d" % i, [128, W]) for i in range(NB)]
    ssq = [sb("qk_ss%d" % i, [128, G]) for i in range(NB)]
    xn = [sb("qk_xn%d" % i, [128, W]) for i in range(NB)]
    if rope_ap is not None:
        rp = [sb("qk_rp%d" % i, [128, 2, W]) for i in range(NB)]
        t1 = [sb("qk_t1%d" % i, [128, W]) for i in range(NB)]
        t2 = [sb("qk_t2%d" % i, [128, W]) for i in range(NB)]
    q4 = gd // 4
    v3 = lambda ap: ap.rearrange("p (g d) -> p g d", d=gd)

    def s0(i):
        b = i % NB
        zk, sk = "qk_zt%d" % b, "qk_ss%d" % b
        kb.dma(zt[b][:], Z[i * 128:(i + 1) * 128, zcol0:zcol0 + W], rd=(), wr=[zk])
        if rope_ap is not None and i < 16:
            kb.dma(rp[b][:], rope_ap[i * 128:(i + 1) * 128], rd=(), wr=["qk_rp%d" % b])
        kb.tt(sq[b][:], zt[b][:], zt[b][:], ALU.mult, rd=[zk], wr=["qk_sq%d" % b], eng="gpsimd")
        kb.red(ssq[b][:], v3(sq[b][:]), ALU.add, rd=["qk_sq%d" % b], wr=[sk])
        kb.ts(ssq[b][:], ssq[b][:], 1.0 / gd, EPS, ALU.mult, ALU.add, rd=[sk], wr=[sk])
        kb.act(ssq[b][:], ssq[b][:], AF.Sqrt, rd=[sk], wr=[sk])

    def s1(i):
        b = i % NB
        zk, sk, xk = "qk_zt%d" % b, "qk_ss%d" % b, "qk_xn%d" % b
        kb.recip(ssq[b][:], ssq[b][:], rd=[sk], wr=[sk])
        kb.tt(v3(xn[b][:]), v3(zt[b][:]), ssq[b][:].unsqueeze(2).to_broadcast([128, G, gd]), ALU.mult, rd=[zk, sk], wr=[xk])
        kb.tt(xn[b][:], xn[b][:], gain[:], ALU.mult, rd=[xk, "qk_gain"], wr=[xk], eng="gpsimd")

    def s2(i):
        b = i % NB
        xk, rk = "qk_xn%d" % b, "qk_rp%d" % b
        if rope_ap is not None and i < 16:
            kb.tt(t1[b][:], xn[b][:], rp[b][:, 0, :], ALU.mult, rd=[xk, rk], wr=["qk_t1%d" % b])
            xv = xn[b][:].rearrange("p (x h d) -> p x h d", h=2, d=q4)
            sv = rp[b][:, 1, :].rearrange("p (x h d) -> p x h d", h=2, d=q4)
            tv = t2[b][:].rearrange("p (x h d) -> p x h d", h=2, d=q4)
            kb.tt(tv[:, :, 0, :], xv[:, :, 1, :], sv[:, :, 0, :], ALU.mult, rd=[xk, rk], wr=[("qk_t2a", b)], eng="gpsimd")
            kb.tt(tv[:, :, 1, :], xv[:, :, 0, :], sv[:, :, 1, :], ALU.mult, rd=[xk, rk], wr=[("qk_t2b", b)], eng="gpsimd")

    def s3(i):
        b = i % NB
        xk = "qk_xn%d" % b
        if rope_ap is not None and i < 16:
            kb.tt(xn[b][:], t1[b][:], t2[b][:], ALU.add, rd=["qk_t1%d" % b, ("qk_t2a", b), ("qk_t2b", b)], wr=[xk])
        store(i, xn[b], xk)

    pipeline([s0, s1, s2, s3], list(tiles))


def load_v(kb, V1, vkey, Z, col0, H, row0, ntile, raw, rawkey):
    nt_, h_ = V1.shape[1], V1.shape[2]
    kb.cp(V1[:, :, :, 64:66], kb.onez[:, 0:2].unsqueeze(1).unsqueeze(1).to_broadcast([128, nt_, h_, 2]), rd=["onez"], wr=[(vkey, "ones")])
    kb.dma(raw[:, 0:ntile, 0:64 * H], Z[row0:row0 + ntile * 128, col0:col0 + 64 * H].rearrange("(t p) c -> p t c", p=128), rd=(), wr=[rawkey])
    hlf = ntile // 2
    kb.cp(V1[:, 0:hlf, :, 0:64], raw[:, 0:hlf, 0:64 * H].rearrange("p t (h d) -> p t h d", d=64), rd=[rawkey], wr=[(vkey, "a")], eng="vector")
    kb.cp(V1[:, hlf:ntile, :, 0:64], raw[:, hlf:ntile, 0:64 * H].rearrange("p t (h d) -> p t h d", d=64), rd=[rawkey], wr=[(vkey, "b")], eng="scalar")


def attn_finish(kb, po, pok, H, npart, extra_den, yt, ytk, dst, rec, reck):
    if extra_den is not None:
        kb.tt(rec[0:npart, :], po[0:npart, :, 64], extra_den[0:npart, :], ALU.add, rd=[pok, "expsink"], wr=[reck])
    else:
        kb.cp(rec[0:npart, :], po[0:npart, :, 64], rd=[pok], wr=[reck])
    kb.recip(rec[0:npart, :], rec[0:npart, :], rd=[reck], wr=[reck])
    kb.tt(yt[0:npart, :, :], po[0:npart, :, 0:64], rec[0:npart, :].unsqueeze(2).to_broadcast([npart, H, 64]), ALU.mult,
          rd=[pok, reck], wr=[ytk])
    kb.dma(dst, yt[0:npart, :, :].rearrange("p h d -> p (h d)"), rd=[ytk], wr=[("YB", ytk)], semres=ytk)


def attn_A(kb, l, need_ctx, Z, YB, ident, C):
    nc = kb.nc
    with ExitStack() as es:
        sb = lambda n, s, dt=F32: es.enter_context(nc.sbuf_tensor(kb.un(n), s, dt))
        ps = lambda n, s, dt=F32: es.enter_context(nc.psum_tensor(kb.un(n), s, dt))
        qT = sb("A_qT", [64, 4, NTOK], F32R)
        kT = sb("A_kT", [64, 2, NTOK], F32R)
        V1 = sb("A_V1", [128, NT, 2, 66], F32R)
        vraw = sb("A_vraw", [128, NT, 128])
        load_v(kb, V1, "A_V1", Z, O_AV, 2, 0, NT, vraw, "A_vraw")
        bm = sb("A_bm", [128, 2, 128])
        kb.dma(bm[:], C["bandmask"].rearrange("m p q -> p m q"), rd=(), wr=["A_bm"])
        expsink = sb("A_es", [128, 4])
        kb.dma(expsink[:], C["a_sink"][l:l + 1, :].to_broadcast([128, 4]), rd=(), wr=["expsink"])
        kb.act(expsink[:], expsink[:], AF.Exp, rd=["expsink"], wr=["expsink"])
        with ExitStack() as es2:
            ptr = [es2.enter_context(nc.psum_tensor(kb.un("A_ptr%d" % i), [64, 6, 128], F32)) for i in range(2)]

            def store(i, xn, xk):
                p = ptr[i % 2]
                pk = "A_ptr%d" % (i % 2)
                for g in range(6):
                    kb.tr(p[:, g, :], xn[:, g * 64:(g + 1) * 64], ident[:], rd=[xk], wr=[pk])
                kb.cp(qT[:, :, i * 128:(i + 1) * 128], p[:, 0:4, :], rd=[pk], wr=[("A_qT", i)], eng="scalar")
                kb.cp(kT[:, :, i * 128:(i + 1) * 128], p[:, 4:6, :], rd=[pk], wr=[("A_kT", i)], eng="vector")
            qk_prep(kb, es2, l, Z, ident, O_AQ, 6, 64, 4, C["a_qk_norm"], C["ropeA"], range(NT), store)
        kb.S.barrier()
        pS = [ps("A_pS%d" % i, [128, 6, 256]) for i in range(2)]
        pO = [ps("A_pO%d" % i, [128, 4, 66]) for i in range(2)]
        pT = [sb("A_pT%d" % i, [128, 5, 2, 128], F32R) for i in range(2)]
        yt = [sb("A_yt%d" % i, [128, 4, 64]) for i in range(2)]
        rec = [sb("A_rec%d" % i, [128, 4]) for i in range(2)]
        k = 0
        qtiles = list(range(16)) + ([16, 17] if need_ctx else [])
        for n, i in enumerate(qtiles):
            if i < 16:
                ents = [(j, (0 if j == i - 1 else (1 if j == i + 1 else None))) for j in (i - 1, i, i + 1) if 0 <= j < 16]
                ents += [(16, None), (17, None)]
            else:
                ents = [(16, None), (17, None)]
            E = len(ents)
            po = pO[n % 2]
            pok = "A_pO%d" % (n % 2)
            for g in range(2):
                b = k % 2
                k += 1
                psk, ptk = "A_pS%d" % b, "A_pT%d" % b
                for e, (j, mk) in enumerate(ents):
                    kb.mm(pS[b][:, e, :], kT[:, g, j * 128:(j + 1) * 128], qT[:, 2 * g:2 * g + 2, i * 128:(i + 1) * 128], True, True,
                          rd=[("A_kT", j), ("A_qT", i)], wr=[psk])
                kb.act(pT[b][:, 0:E].rearrange("p e h q -> p e (h q)"), pS[b][:, 0:E, :], AF.Exp, rd=[psk], wr=[ptk], scale=0.125)
                for e, (j, mk) in enumerate(ents):
                    if mk is not None:
                        kb.tt(pT[b][:, e], pT[b][:, e], bm[:, mk:mk + 1, :].to_broadcast([128, 2, 128]), ALU.mult, rd=[ptk, "A_bm"], wr=[ptk])
                for hh in range(2):
                    h = 2 * g + hh
                    for e, (j, mk) in enumerate(ents):
                        kb.mm(po[:, h, :], pT[b][:, e, hh, :], V1[:, j, g, :], e == 0, e == E - 1, rd=[ptk, "A_V1"], wr=[pok])
            attn_finish(kb, po, pok, 4, 128, expsink, yt[n % 2], "A_yt%d" % (n % 2), YB[i * 128:(i + 1) * 128, 0:256], rec[n % 2], "A_rec%d" % (n % 2))


def attn_D(kb, l, need_ctx, Z, YB, ident, C):
    nc = kb.nc
    with ExitStack() as es:
        sb = lambda n, s, dt=F32: es.enter_context(nc.sbuf_tensor(kb.un(n), s, dt))
        ps = lambda n, s, dt=F32: es.enter_context(nc.psum_tensor(kb.un(n), s, dt))
        qT = sb("D_qT", [64, 4, NTOK], F32R)
        kT = sb("D_kT", [64, 4, NTOK], F32R)
        V1 = sb("D_V1", [128, NT, 4, 66], F32R)
        V1o = sb("D_V1o", [128, 15, 4, 66], F32R)
        vraw = sb("D_vraw", [128, NT, 256])
        load_v(kb, V1, "D_V1", Z, O_DV, 4, 0, NT, vraw, "D_vraw")
        load_v(kb, V1o, "D_V1o", Z, O_DV, 4, 64, 15, vraw, "D_vraw")
        Eb = sb("D_E", [128, 128, 64])
        mD = sb("D_mD", [128, 64])
        kb.dma(Eb[:].rearrange("p a q -> p (a q)"), C["biasG"][l], rd=(), wr=["D_E"])
        kb.dma(mD[:], C["maskD"], rd=(), wr=["D_mD"])
        kb.act(Eb[:], Eb[:], AF.Exp, rd=["D_E"], wr=["D_E"])
        kb.tt(Eb[:], Eb[:], mD[:].unsqueeze(1).to_broadcast([128, 128, 64]), ALU.mult, rd=["D_E", "D_mD"], wr=["D_E"])
        with ExitStack() as es2:
            ptr = [es2.enter_context(nc.psum_tensor(kb.un("D_ptr%d" % i), [64, 8, 128], F32)) for i in range(2)]

            def store(i, xn, xk):
                p = ptr[i % 2]
                pk = "D_ptr%d" % (i % 2)
                for g in range(8):
                    kb.tr(p[:, g, :], xn[:, g * 64:(g + 1) * 64], ident[:], rd=[xk], wr=[pk])
                kb.cp(qT[:, :, i * 128:(i + 1) * 128], p[:, 0:4, :], rd=[pk], wr=[("D_qT", i)], eng="scalar")
                kb.cp(kT[:, :, i * 128:(i + 1) * 128], p[:, 4:8, :], rd=[pk], wr=[("D_kT", i)], eng="vector")
            qk_prep(kb, es2, l, Z, ident, O_DQ, 8, 64, 4, C["d_qk_norm"], None, range(NT), store)
        kb.S.barrier()
        pS = [ps("D_pS%d" % i, [128, 4, 6, 64]) for i in range(2)]
        pO = [ps("D_pO%d" % i, [128, 4, 66]) for i in range(2)]
        pT = [sb("D_pT%d" % i, [128, 4, 6, 64], F32R) for i in range(2)]
        ptmp = [sb("D_ptmp%d" % i, [128, 4, 4, 64]) for i in range(2)]
        yt = [sb("D_yt%d" % i, [128, 4, 64]) for i in range(2)]
        rec = [sb("D_rec%d" % i, [128, 4]) for i in range(2)]
        for r in range(32):
            start = min(max(r - 4, 0), 24)
            cls = r if r < 4 else (4 if r <= 28 else r - 24)
            b = r % 2
            po = pO[b]
            pok = "D_pO%d" % b
            psk, ptk, tmk = "D_pS%d" % b, "D_pT%d" % b, "D_ptmp%d" % b
            for h in range(4):
                qs = qT[:, h, r * 64:(r + 1) * 64]
                for t in range(4):
                    t0 = (start + 2 * t) * 64
                    kb.mm(pS[b][:, h, t, :], kT[:, h, t0:t0 + 128], qs, True, True, rd=(), wr=[psk])
                for t in range(2):
                    kb.mm(pS[b][:, h, 4 + t, :], kT[:, h, SEQ + t * 128:SEQ + (t + 1) * 128], qs, True, True, rd=(), wr=[psk])
            kb.act(ptmp[b][:], pS[b][:, :, 0:4, :], AF.Exp, rd=[psk], wr=[tmk], scale=0.125)
            kb.act(pT[b][:, :, 4:6, :], pS[b][:, :, 4:6, :], AF.Exp, rd=[psk], wr=[ptk], scale=0.125)
            kb.tt(pT[b][:, :, 0:4, :], ptmp[b][:], Eb[:, cls * 16:(cls + 1) * 16, :].rearrange("p (h t) q -> p h t q", h=4), ALU.mult,
                  rd=[tmk, "D_E"], wr=[ptk])
            for h in range(4):
                for t in range(4):
                    row = start + 2 * t
                    vt = V1[:, row // 2, h, :] if row % 2 == 0 else V1o[:, (row - 1) // 2, h, :]
                    kb.mm(po[0:64, h, :], pT[b][:, h, t, :], vt, t == 0, False, rd=[ptk, "D_V1", "D_V1o"], wr=[pok])
                for t in range(2):
                    kb.mm(po[0:64, h, :], pT[b][:, h, 4 + t, :], V1[:, 16 + t, h, :], False, t == 1, rd=[ptk, "D_V1"], wr=[pok])
            attn_finish(kb, po, pok, 4, 64, None, yt[b], "D_yt%d" % b, YB[r * 64:(r + 1) * 64, 768:1024], rec[b], "D_rec%d" % b)
        if need_ctx:
            pT2 = [sb("D_pTc%d" % i, [128, 2, 128], F32R) for i in range(2)]
            pSc = [pS[i][:, 0, 0:4, :].rearrange("p (a b) d -> p a (b d)", a=2) for i in range(2)]
            k = 0
            for n, i in enumerate((16, 17)):
                po = pO[n % 2]
                pok = "D_pO%d" % (n % 2)
                for h in range(4):
                    b = k % 2
                    k += 1
                    psk, ptk = "D_pS%d" % b, "D_pTc%d" % b
                    for t in range(2):
                        kb.mm(pSc[b][:, t, :], kT[:, h, SEQ + t * 128:SEQ + (t + 1) * 128], qT[:, h, i * 128:(i + 1) * 128], True, True,
                              rd=["D_kTall", "D_qTall"], wr=[psk])
                    kb.act(pT2[b][:], pSc[b], AF.Exp, rd=[psk], wr=[ptk], scale=0.125)
                    for t in range(2):
                        kb.mm(po[:, h, :], pT2[b][:, t, :], V1[:, 16 + t, h, :], t == 0, t == 1, rd=[ptk, "D_V1"], wr=[pok])
                attn_finish(kb, po, pok, 4, 128, None, yt[n % 2], "D_yt%d" % (n % 2), YB[i * 128:(i + 1) * 128, 768:1024], rec[n % 2], "D_rec%d" % (n % 2))


def attn_C(kb, l, need_ctx, Z, YB, ident, C):
    nc = kb.nc
    lam_init = 0.8 - 0.6 * math.exp(-0.3 * l)
    with ExitStack() as es:
        sb = lambda n, s, dt=F32: es.enter_context(nc.sbuf_tensor(kb.un(n), s, dt))
        ps = lambda n, s, dt=F32: es.enter_context(nc.psum_tensor(kb.un(n), s, dt))
        qT = sb("C_qT", [128, 3, NTOK], F32R)
        kT = sb("C_kT", [128, 3, NTOK], F32R)
        V1 = sb("C_V1", [128, NT, 4, 66], F32R)
        lv = sb("C_lv", [128, 4, 32])
        lp = sb("C_lp", [128, 2, 32])
        ls = sb("C_ls", [128, 2])
        nlam = sb("C_nlam", [128, 1])
        kb.dma(lv[:].rearrange("p a d -> p (a d)"), C["c_lambda"][l:l + 1].rearrange("o a d -> o (a d)").to_broadcast([128, 128]), rd=(), wr=["C_lv"])
        lvv = lv[:].rearrange("p (a b) d -> p a b d", b=2)
        kb.tt(lp[:], lvv[:, :, 0, :], lvv[:, :, 1, :], ALU.mult, rd=["C_lv"], wr=["C_lp"])
        kb.red(ls[:], lp[:], ALU.add, rd=["C_lp"], wr=["C_ls"])
        kb.act(ls[:], ls[:], AF.Exp, rd=["C_ls"], wr=["C_ls"])
        kb.tt(nlam[:], ls[:, 1:2], ls[:, 0:1], ALU.subtract, rd=["C_ls"], wr=["C_nlam"])
        kb.ts(nlam[:], nlam[:], -lam_init, None, ALU.add, None, rd=["C_nlam"], wr=["C_nlam"])
        sg = sb("C_sg", [128, 64])
        kb.dma(sg[:], C["c_subln"][l:l + 1, :].to_broadcast([128, 64]), rd=(), wr=["C_sg"])
        kb.ts(sg[:], sg[:], 1.0 - lam_init, None, ALU.mult, None, rd=["C_sg"], wr=["C_sg"])
        with ExitStack() as es2:
            vraw = es2.enter_context(nc.sbuf_tensor(kb.un("C_vraw"), [128, NT, 256], F32))
            load_v(kb, V1, "C_V1", Z, O_CV, 4, 0, NT, vraw, "C_vraw")
            ptrq = [es2.enter_context(nc.psum_tensor(kb.un("C_ptrq%d" % i), [128, 3, 128], F32)) for i in range(2)]
            ptrk = [es2.enter_context(nc.psum_tensor(kb.un("C_ptrk%d" % i), [128, 3, 128], F32)) for i in range(2)]

            def store(i, xn, xk):
                for (pp, pk, dst, dk, c0, eng) in ((ptrq[i % 2], "C_ptrq%d" % (i % 2), qT, "C_qT", 0, "scalar"),
                                                   (ptrk[i % 2], "C_ptrk%d" % (i % 2), kT, "C_kT", 256, "vector")):
                    for idx in range(3):
                        ncol = 96 if idx < 2 else 64
                        kb.tr(pp[0:ncol, idx, :], xn[:, c0 + idx * 96:c0 + idx * 96 + ncol], ident[:], rd=[xk], wr=[pk])
                    kb.cp(dst[0:96, 0:2, i * 128:(i + 1) * 128], pp[0:96, 0:2, :], rd=[pk], wr=[(dk, i)], eng=eng)
                    kb.cp(dst[0:64, 2, i * 128:(i + 1) * 128], pp[0:64, 2, :], rd=[pk], wr=[(dk, i)], eng=eng)
            qk_prep(kb, es2, l, Z, ident, O_CQ, 16, 32, 8, C["c_qk_norm"], C["ropeC"], range(NT), store)
        kb.S.barrier()
        pS = [ps("C_pS%d" % i, [128, 512]) for i in range(4)]
        pOT = [ps("C_pOT%d" % i, [128, 512]) for i in range(2)]
        pO2 = ps("C_pO2", [128, 4, 66])
        oT = [sb("C_oT%d" % i, [66, 512]) for i in range(2)]
        pTs = [sb("C_pT%d" % i, [128, NT, 512], F32R) for i in range(2)]
        ocs = [sb("C_oc%d" % i, [128, 4, 8, 66]) for i in range(2)]
        rec = sb("C_rec", [128, 4, 8])
        on = sb("C_on", [128, 4, 8, 64])
        od = sb("C_od", [128, 4, 4, 64])
        osq = sb("C_osq", [128, 4, 4, 64])
        oss = sb("C_oss", [128, 4, 4])
        yts = [sb("C_yt%d" % i, [128, 4, 4, 64]) for i in range(2)]
        blocks = [(qb * 512, 512, list(range(NT))) for qb in range(4)]
        if need_ctx:
            blocks.append((SEQ, 256, [16, 17]))
        ks = 0
        ko = 0
        scale = 32 ** -0.5
        pending = []

        def c_finish(q0, nq, nqt, bi):
            ob = ocs[bi % 2]
            okk = [("C_oc", bi % 2, qt_) for qt_ in range(nqt)]
            kb.recip(rec[:, 0:nqt, :], ob[:, 0:nqt, :, 64], rd=okk, wr=["C_rec"])
            kb.tt(on[:, 0:nqt], ob[:, 0:nqt, :, 0:64], rec[:, 0:nqt, :].unsqueeze(3).to_broadcast([128, nqt, 8, 64]), ALU.mult, rd=okk + ["C_rec"], wr=["C_on"])
            onv = on[:, 0:nqt].rearrange("p q (h m) d -> p q h m d", m=2)
            kb.stt(od[:, 0:nqt], onv[:, :, :, 1, :], nlam[:, 0:1], onv[:, :, :, 0, :], ALU.mult, ALU.add, rd=["C_on", "C_nlam"], wr=["C_od"])
            kb.tt(osq[:, 0:nqt], od[:, 0:nqt], od[:, 0:nqt], ALU.mult, rd=["C_od"], wr=["C_osq"], eng="gpsimd")
            kb.red(oss[:, 0:nqt, :], osq[:, 0:nqt], ALU.add, rd=["C_osq"], wr=["C_oss"])
            kb.rstd(oss[:, 0:nqt, :], oss[:, 0:nqt, :], 64, EPS, rd=["C_oss"], wr=["C_oss"])
            y = yts[bi % 2]
            yk = "C_yt%d" % (bi % 2)
            kb.tt(y[:, 0:nqt], od[:, 0:nqt], oss[:, 0:nqt, :].unsqueeze(3).to_broadcast([128, nqt, 4, 64]), ALU.mult, rd=["C_od", "C_oss"], wr=[yk])
            kb.tt(y[:, 0:nqt], y[:, 0:nqt], sg[:].unsqueeze(1).unsqueeze(1).to_broadcast([128, nqt, 4, 64]), ALU.mult, rd=[yk, "C_sg"], wr=[yk], eng="gpsimd")
            kb.dma(YB[q0:q0 + nq, 512:768].rearrange("(q p) c -> p q c", p=128), y[:, 0:nqt].rearrange("p q h d -> p q (h d)"), rd=[yk], wr=[("YB", yk)], semres=yk)

        for blk_i, (q0, nq, ktl) in enumerate(blocks):
            nqt = nq // 128
            oc = ocs[blk_i % 2]
            def pv_step(gp, e, j):
                hp = gp // 2
                bp = gp % 2
                kb.mm(pOT[bp][0:66, 0:nq], V1[:, j, hp, :], pTs[bp][:, e, 0:nq], e == 0, e == len(ktl) - 1,
                      rd=[("C_pT", bp, e), "C_V1"], wr=["C_pOT%d" % bp])

            def pv_finish(gp):
                bp = gp % 2
                kb.cp(oT[bp][:, 0:nq], pOT[bp][0:66, 0:nq], rd=["C_pOT%d" % bp], wr=["C_oT%d" % bp], eng="vector")
                for qt in range(nqt):
                    kb.tr(pO2[:, qt, :], oT[bp][:, qt * 128:(qt + 1) * 128], ident[0:66, 0:66], rd=["C_oT%d" % bp], wr=["C_pO2"])
                kb.cp(oc[:, 0:nqt, gp, :], pO2[:, 0:nqt, :], rd=["C_pO2"], wr=[("C_oc", blk_i % 2, qt_) for qt_ in range(nqt)], eng="vector")

            for g in range(8):
                h, s4, hh = g // 2, g % 3, g // 3
                bg = g % 2
                ents_ = list(enumerate(ktl))
                for c0 in range(0, len(ents_), 4):
                    for e, j in ents_[c0:c0 + 4]:
                        b = ks % 4
                        ks += 1
                        kb.mm(pS[b][:, 0:nq], kT[32 * s4:32 * s4 + 32, hh, j * 128:(j + 1) * 128], qT[32 * s4:32 * s4 + 32, hh, q0:q0 + nq], True, True,
                              rd=(), wr=["C_pS%d" % b])
                        kb.act(pTs[bg][:, e, 0:nq], pS[b][:, 0:nq], AF.Exp, rd=["C_pS%d" % b], wr=[("C_pT", bg, e)], scale=scale)
                    if g > 0:
                        for e, j in ents_[c0:c0 + 4]:
                            pv_step(g - 1, e, j)
                if g > 0:
                    pv_finish(g - 1)
                if g == 2 and pending:
                    c_finish(*pending.pop(0))
            for e, j in enumerate(ktl):
                pv_step(7, e, j)
            pv_finish(7)
            pending.append((q0, nq, nqt, blk_i))
        while pending:
            c_finish(*pending.pop(0))


def rwkv(kb, l, need_ctx, Z, YB, RW, ident, C):
    nc = kb.nc
    S = kb.S
    NEG = -math.exp(-0.5)
    with ExitStack() as es:
        sb = lambda n, s, dt=F32: es.enter_context(nc.sbuf_tensor(kb.un(n), s, dt))
        ps = lambda n, s, dt=F32: es.enter_context(nc.psum_tensor(kb.un(n), s, dt))
        mu = sb("R_mu", [128, 3, 896])
        kb.dma(mu[:, 0:2, :], C["b_shift"][l:l + 1].to_broadcast([128, 2, 896]), rd=(), wr=["R_mu"])
        kb.tt(mu[:, 2, :], mu[:, 0, :], mu[:, 1, :], ALU.add, rd=["R_mu"], wr=["R_mu"])
        kb.ts(mu[:, 2, :], mu[:, 2, :], -1.0, 1.0, ALU.mult, ALU.add, rd=["R_mu"], wr=["R_mu"])
        W2t = sb("R_W2t", [32, 512])
        A2t = sb("R_A2t", [32, 256])
        G2t = sb("R_G2t", [64, 256])
        kb.dma(W2t[:].rearrange("p (a n) -> p a n", a=2), C["b_w2"][l].rearrange("a p n -> p a n"), rd=(), wr=["R_Wl"])
        kb.dma(A2t[:], C["b_a2"][l], rd=(), wr=["R_Wl"])
        kb.dma(G2t[:], C["b_g2"][l], rd=(), wr=["R_Wl"])
        w0 = sb("R_w0", [128, 512])
        kb.dma(w0[:], C["b_w0"][l:l + 1].rearrange("o a n -> o (a n)").to_broadcast([128, 512]), rd=(), wr=["R_w0"])
        vb = sb("R_vb", [128, 3, 256])
        kb.dma(vb[:, 0, :], C["b_a0"][l:l + 1, :].to_broadcast([128, 256]), rd=(), wr=["R_vb"])
        kb.dma(vb[:, 1, :], C["b_kk"][l:l + 1, :].to_broadcast([128, 256]), rd=(), wr=["R_vb"])
        kb.dma(vb[:, 2, :], C["b_ka"][l:l + 1, :].to_broadcast([128, 256]), rd=(), wr=["R_vb"])
        NB = 6
        zc = [sb("R_zc%d" % i, [128, 896]) for i in range(2)]
        zp = [sb("R_zp%d" % i, [128, 896]) for i in range(2)]
        zn = [sb("R_zn%d" % i, [128, 896]) for i in range(2)]
        zs = [sb("R_zs%d" % i, [128, 896]) for i in range(NB)]
        lbT = [sb("R_lbT%d" % i, [64, 3, 128]) for i in range(NB)]
        rw = [sb("R_rw%d" % i, [128, 8, 256]) for i in range(NB)]
        tmp = [sb("R_tmp%d" % i, [128, 512]) for i in range(NB)]
        tmp2 = [sb("R_tmp2%d" % i, [128, 256]) for i in range(NB)]
        av = [sb("R_a%d" % i, [128, 256]) for i in range(NB)]
        ssk = [sb("R_ssk%d" % i, [128, 4]) for i in range(NB)]
        pl = [ps("R_pl%d" % i, [64, 3, 128]) for i in range(2)]
        pd = [ps("R_pd%d" % i, [128, 512]) for i in range(2)]
        pag = [ps("R_pag%d" % i, [128, 2, 256]) for i in range(2)]
        h3 = lambda ap: ap.rearrange("p (h d) -> p h d", d=64)

        def s0(i):
            b2, b = i % 2, i % NB
            kc, kp, kn, ks_ = "R_zc%d" % b2, "R_zp%d" % b2, "R_zn%d" % b2, "R_zs%d" % b
            r0 = i * 128
            kb.dma(zc[b2][:], Z[r0:r0 + 128, O_BZ:O_BZ + 896], rd=(), wr=[kc])
            if i in (0, 16):
                kb.memset(zp[b2][:], 0.0, wr=[kp])
                kb.dma(zp[b2][1:128, :], Z[r0:r0 + 127, O_BZ:O_BZ + 896], rd=(), wr=[kp])
            else:
                kb.dma(zp[b2][:], Z[r0 - 1:r0 + 127, O_BZ:O_BZ + 896], rd=(), wr=[kp])
            if i in (15, 17):
                kb.memset(zn[b2][:], 0.0, wr=[kn])
                kb.dma(zn[b2][0:127, :], Z[r0 + 1:r0 + 128, O_BZ:O_BZ + 896], rd=(), wr=[kn])
            else:
                kb.dma(zn[b2][:], Z[r0 + 1:r0 + 129, O_BZ:O_BZ + 896], rd=(), wr=[kn])
            kb.tt(zs[b][:], zc[b2][:], mu[:, 2, :], ALU.mult, rd=[kc, "R_mu"], wr=[ks_])
            kb.tt(zp[b2][:], zp[b2][:], mu[:, 0, :], ALU.mult, rd=[kp, "R_mu"], wr=[kp], eng="gpsimd")
            kb.tt(zn[b2][:], zn[b2][:], mu[:, 1, :], ALU.mult, rd=[kn, "R_mu"], wr=[kn], eng="gpsimd")

        def s1(i):
            b2, b = i % 2, i % NB
            kp, kn, ks_ = "R_zp%d" % b2, "R_zn%d" % b2, "R_zs%d" % b
            kb.tt(zs[b][:], zs[b][:], zp[b2][:], ALU.add, rd=[ks_, kp], wr=[ks_])
            kb.tt(zs[b][:], zs[b][:], zn[b2][:], ALU.add, rd=[ks_, kn], wr=[ks_])

        def s2(i):
            b2, b = i % 2, i % NB
            ks_, kl = "R_zs%d" % b, "R_lbT%d" % b
            z = zs[b]
            kb.tr(pl[b2][0:32, 0, :], z[:, 768:800], ident[:], rd=[ks_], wr=["R_pl%d" % b2])
            kb.tr(pl[b2][0:32, 1, :], z[:, 800:832], ident[:], rd=[ks_], wr=["R_pl%d" % b2])
            kb.tr(pl[b2][0:64, 2, :], z[:, 832:896], ident[:], rd=[ks_], wr=["R_pl%d" % b2])
            kb.act(lbT[b][0:32, 0, :], pl[b2][0:32, 0, :], AF.Tanh, rd=["R_pl%d" % b2], wr=[kl])
            kb.act(lbT[b][0:32, 1, :], pl[b2][0:32, 1, :], AF.Copy, rd=["R_pl%d" % b2], wr=[kl])
            kb.act(lbT[b][0:64, 2, :], pl[b2][0:64, 2, :], AF.Sigmoid, rd=["R_pl%d" % b2], wr=[kl])

        def s3(i):
            b2, b = i % 2, i % NB
            ks_, kl, kr = "R_zs%d" % b, "R_lbT%d" % b, "R_rw%d" % b
            z = zs[b]
            o = rw[b]
            kb.mm(pd[b2][:], lbT[b][0:32, 0, :], W2t[:], True, True, rd=[kl, "R_Wl"], wr=["R_pd%d" % b2])
            kb.mm(pag[b2][:, 0, :], lbT[b][0:32, 1, :], A2t[:], True, True, rd=[kl, "R_Wl"], wr=["R_pag%d" % b2])
            kb.mm(pag[b2][:, 1, :], lbT[b][0:64, 2, :], G2t[:], True, True, rd=[kl, "R_Wl"], wr=["R_pag%d" % b2])
            kb.cp(o[:, 0, :], z[:, 0:256], rd=[ks_], wr=[(kr, 0)], eng="gpsimd")
            kb.cp(o[:, 2, :], z[:, 512:768], rd=[ks_], wr=[(kr, 2)], eng="gpsimd")
            kb.tt(o[:, 3, :], z[:, 256:512], vb[:, 1, :], ALU.mult, rd=[ks_, "R_vb"], wr=[(kr, 3)])
            kb.tt(tmp2[b][:], o[:, 3, :], o[:, 3, :], ALU.mult, rd=[(kr, 3)], wr=["R_tmp2%d" % b])
            kb.red(ssk[b][:], h3(tmp2[b][:]), ALU.add, rd=["R_tmp2%d" % b], wr=["R_ssk%d" % b])
            kb.ts(ssk[b][:], ssk[b][:], 1e-24, None, ALU.max, None, rd=["R_ssk%d" % b], wr=["R_ssk%d" % b])

        def s4(i):
            b2, b = i % 2, i % NB
            kr = "R_rw%d" % b
            o = rw[b]
            kb.act(o[:, 7, :], pag[b2][:, 1, :], AF.Copy, rd=["R_pag%d" % b2], wr=[(kr, 7)])
            kb.tt(av[b][:], pag[b2][:, 0, :], vb[:, 0, :], ALU.add, rd=["R_pag%d" % b2, "R_vb"], wr=["R_a%d" % b])
            kb.tt(tmp[b][:], pd[b2][:], w0[:], ALU.add, rd=["R_pd%d" % b2, "R_w0"], wr=["R_tmp%d" % b])
            kb.act(av[b][:], av[b][:], AF.Sigmoid, rd=["R_a%d" % b], wr=["R_a%d" % b])
            kb.act(tmp[b][:], tmp[b][:], AF.Sigmoid, rd=["R_tmp%d" % b], wr=["R_tmp%d" % b])
            kb.act(ssk[b][:], ssk[b][:], AF.Sqrt, rd=["R_ssk%d" % b], wr=["R_ssk%d" % b])

        def s5(i):
            b2, b = i % 2, i % NB
            ks_, kr = "R_zs%d" % b, "R_rw%d" % b
            z = zs[b]
            o = rw[b]
            kb.ts(o[:, 5:7, :].rearrange("p a n -> p (a n)"), tmp[b][:], NEG, None, ALU.mult, None, rd=["R_tmp%d" % b], wr=[(kr, 5)], eng="gpsimd")
            kb.recip(ssk[b][:], ssk[b][:], rd=["R_ssk%d" % b], wr=["R_ssk%d" % b])
            kb.tt(h3(o[:, 3, :]), h3(o[:, 3, :]), ssk[b][:].unsqueeze(2).to_broadcast([128, 4, 64]), ALU.mult, rd=[(kr, 3), "R_ssk%d" % b], wr=[(kr, 3)])
            kb.tt(o[:, 4, :], o[:, 3, :], av[b][:], ALU.mult, rd=[(kr, 3), "R_a%d" % b], wr=[(kr, 4)], eng="gpsimd")
            kb.stt(tmp2[b][:], av[b][:], -1.0, vb[:, 2, :], ALU.add, ALU.mult, rd=["R_a%d" % b, "R_vb"], wr=["R_tmp2%d" % b])
            kb.stt(o[:, 1, :], tmp2[b][:], 1.0, z[:, 256:512], ALU.add, ALU.mult, rd=["R_tmp2%d" % b, ks_], wr=[(kr, 1)])

        def s6(i):
            b = i % NB
            kr = "R_rw%d" % b
            kb.dma(RW[i * 128:(i + 1) * 128], rw[b][:], rd=[(kr, j_) for j_ in (0, 1, 2, 3, 4, 5, 7)], wr=[("RW", i)], semres=kr)

        pipeline([s0, s1, s2, s3, s4, s5, s6], list(range(NT)))
    S.barrier()
    if "noscan" in kb.debug:
        return
    with ExitStack() as es:
        sb = lambda n, s, dt=F32: es.enter_context(nc.sbuf_tensor(kb.un(n), s, dt))
        ps = lambda n, s, dt=F32: es.enter_context(nc.psum_tensor(kb.un(n), s, dt))
        msk = sb("S_msk", [128, 2, 512])
        kb.dma(msk[:], C["rwkv_masks"].rearrange("d p n -> p d n"), rd=(), wr=["S_msk"])
        ones2 = sb("S_ones", [128, 2])
        kb.memset(ones2[:], 1.0, wr=["S_ones"])
        yacc = sb("S_yacc", [128, NT, 256])
        kb.memset(yacc[:].rearrange("p t c -> p (t c)"), 0.0, wr=[("S_yacc", i_) for i_ in range(NT)])
        Hs = [sb("S_H%d" % d_, [64, 4, 64]) for d_ in range(2)]
        rwt = [sb("S_rw%d" % i_, [128, 8, 256]) for i_ in range(4)]
        exs = [sb("S_ex%d" % i_, [128, 3, 256]) for i_ in range(4)]
        clxs = [sb("S_clx%d" % i_, [128, 256]) for i_ in range(4)]
        q4s = [sb("S_q4%d" % i_, [128, 4, 256]) for i_ in range(4)]
        T4s = [[sb("S_T4_%d_%d" % (i_, h), [64, 4, 128]) for h in range(4)] for i_ in range(4)]
        GMs = [[sb("S_GM%d_%d" % (i_, h), [128, 2, 256]) for h in range(4)] for i_ in range(4)]
        Xss = [[sb("S_X%d_%d" % (d_, i_), [128, 4, 128]) for i_ in range(2)] for d_ in range(2)]
        XTss = [[sb("S_XT%d_%d" % (d_, i_), [128, 4, 128]) for i_ in range(2)] for d_ in range(2)]
        TTss = [[sb("S_TT%d_%d" % (d_, i_), [128, 4, 128]) for i_ in range(2)] for d_ in range(2)]
        pcs = [sb("S_pc%d" % i_, [64, 4]) for i_ in range(4)]
        R1s = [sb("S_R1%d" % d_, [128, 4, 64]) for d_ in range(2)]
        nUs = [sb("S_nU%d" % d_, [128, 4, 64]) for d_ in range(2)]
        pb = [ps("S_pb%d" % i, [128, 512]) for i in range(8)]
        P = lambda i: "S_pb%d" % i
        orders = ([16, 17] + list(range(16)), [17, 16] + list(range(15, -1, -1)))
        for d_ in range(2):
            kb.memset(Hs[d_][:], 0.0, wr=["S_H_d%d" % d_])

        def front(d, n):
            i = orders[d][n]
            sl = "_s%d" % (d * 2 + n % 2)
            dl = "_d%d" % d
            w, ex, clx, q4, T4, GM, pc = rwt[d * 2 + n % 2], exs[d * 2 + n % 2], clxs[d * 2 + n % 2], q4s[d * 2 + n % 2], T4s[d * 2 + n % 2], GMs[d * 2 + n % 2], pcs[d * 2 + n % 2]
            H, Xs, XTs, TTs, R1, nU = Hs[d], Xss[d], XTss[d], TTss[d], R1s[d], nUs[d]
            rk_ = "S_rw" + sl
            kb.dma(w[:], RW[i * 128:(i + 1) * 128], rd=(), wr=[rk_])
            lw = w[:, 5 + d, :]
            kb.mm(pb[0][:, 0:256], msk[:, d, 0:128], lw, True, True, rd=["S_msk", rk_], wr=[P(0)])
            for h in range(4):
                kb.mm(pb[0][0:64, 256 + 2 * h:258 + 2 * h], w[:, 5 + d, h * 64:(h + 1) * 64], ones2[:], True, True, rd=[rk_, "S_ones"], wr=[P(0)])
            kb.act(ex[:, 0, :], pb[0][:, 0:256], AF.Exp, rd=[P(0)], wr=["S_ex" + sl])
            kb.act(ex[:, 2, :], pb[0][:, 0:256], AF.Exp, rd=[P(0)], wr=["S_ex" + sl], scale=-1.0)
            kb.tt(clx[:], pb[0][:, 0:256], lw, ALU.subtract, rd=[P(0), rk_], wr=["S_clx" + sl])
            kb.act(ex[:, 1, :], clx[:], AF.Exp, rd=["S_clx" + sl], wr=["S_ex" + sl])
            kb.act(pc[:], pb[0][0:64, 256:264].rearrange("p (h t) -> p h t", t=2)[:, :, 0], AF.Exp, rd=[P(0)], wr=["S_pc" + sl])
            kb.tt(q4[:, 0, :], w[:, 3, :], ex[:, 1, :], ALU.mult, rd=[rk_, "S_ex" + sl], wr=["S_q4" + sl])
            kb.tt(q4[:, 1, :], w[:, 0, :], ex[:, 0, :], ALU.mult, rd=[rk_, "S_ex" + sl], wr=["S_q4" + sl], eng="gpsimd")
            kb.tt(q4[:, 2, :], w[:, 4, :], ex[:, 2, :], ALU.mult, rd=[rk_, "S_ex" + sl], wr=["S_q4" + sl])
            kb.tt(q4[:, 3, :], w[:, 1, :], ex[:, 2, :], ALU.mult, rd=[rk_, "S_ex" + sl], wr=["S_q4" + sl], eng="gpsimd")
            for h in range(4):
                tb_ = 1 if h % 2 == 0 else 0
                for kd in range(4):
                    kb.tr(pb[tb_][0:64, kd * 128:(kd + 1) * 128], q4[:, kd, h * 64:(h + 1) * 64], ident[:], rd=["S_q4" + sl], wr=[P(tb_)])
                kb.cp(T4[h][:].rearrange("p k t -> p (k t)"), pb[tb_][0:64, :], rd=[P(tb_)], wr=["S_T4_%d" % h + sl], eng="scalar" if h % 2 else "vector")
            for h in range(4):
                t4 = T4[h]
                tk = "S_T4_%d" % h + sl
                gb_ = 2 if h % 2 == 0 else 7
                kb.mm(pb[gb_][:, 0:256], t4[:, 2, :], t4[:, 0:2, :], True, True, rd=[tk], wr=[P(gb_)])
                kb.mm(pb[gb_][:, 256:512], t4[:, 3, :], t4[:, 0:2, :], True, True, rd=[tk], wr=[P(gb_)])
                kb.mm(pb[3][:, h * 128:(h + 1) * 128], t4[:, 0, :], t4[:, 2, :], True, True, rd=[tk], wr=[P(3)])
                kb.tt(GM[h][:], pb[gb_][:].rearrange("p (a n) -> p a n", a=2), msk[:, d, 128:384].unsqueeze(1).to_broadcast([128, 2, 256]), ALU.mult,
                      rd=[P(gb_), "S_msk"], wr=["S_GM%d" % h + sl])
            X, XT, TT = Xs[0], XTs[0], TTs[0]
            XK = lambda c_, p_: ("S_X", c_, p_, d)
            XTK = lambda c_, p_: ("S_XT", c_, p_, d)
            TTK = lambda c_, p_: ("S_TT", c_, p_, d)
            kb.tt(X[:], pb[3][:].rearrange("p (h n) -> p h n", h=4), msk[:, d, 384:512].unsqueeze(1).to_broadcast([128, 4, 128]), ALU.mult,
                  rd=[P(3), "S_msk"], wr=[XK(0, 0), XK(0, 1)])
            for h in range(4):
                kb.cp(XT[:, h, :], GM[h][:, 0, 0:128], rd=["S_GM%d" % h + sl], wr=[XTK(0, h // 2)], eng="gpsimd")
                kb.tt(TT[:, h, :], ident[:], GM[h][:, 0, 0:128], ALU.subtract, rd=["ident", "S_GM%d" % h + sl], wr=[TTK(0, h // 2)])

        def back(d, n):
            i = orders[d][n]
            sl = "_s%d" % (d * 2 + n % 2)
            dl = "_d%d" % d
            w, ex, clx, q4, T4, GM, pc = rwt[d * 2 + n % 2], exs[d * 2 + n % 2], clxs[d * 2 + n % 2], q4s[d * 2 + n % 2], T4s[d * 2 + n % 2], GMs[d * 2 + n % 2], pcs[d * 2 + n % 2]
            H, Xs, XTs, TTs, R1, nU = Hs[d], Xss[d], XTss[d], TTss[d], R1s[d], nUs[d]
            rk_ = "S_rw" + sl
            lw = w[:, 5 + d, :]
            XK = lambda c_, p_: ("S_X", c_, p_, d)
            XTK = lambda c_, p_: ("S_XT", c_, p_, d)
            TTK = lambda c_, p_: ("S_TT", c_, p_, d)
            NB_ = ((4, 5, 6), (2, 7, 1))
            cur = 0
            for kq in range(6):
                nx = 1 - cur
                Xc, XTc, TTc = Xs[cur], XTs[cur], TTs[cur]
                Xn, XTn, TTn = Xs[nx], XTs[nx], TTs[nx]
                for p_ in range(2):
                    bX = NB_[p_][0]
                    for h in (2 * p_, 2 * p_ + 1):
                        kb.mm(pb[bX][:, (h % 2) * 128:(h % 2 + 1) * 128], XTc[:, h, :], Xc[:, h, :], True, True, rd=[XK(cur, p_), XTK(cur, p_)], wr=[P(bX)])
                    kb.cp(Xn[:, 2 * p_:2 * p_ + 2, :].rearrange("p h n -> p (h n)"), pb[bX][:, 0:256], rd=[P(bX)], wr=[XK(nx, p_)], eng="vector")
                if kq < 5:
                    for p_ in range(2):
                        bXT = NB_[p_][1]
                        for h in (2 * p_, 2 * p_ + 1):
                            kb.mm(pb[bXT][:, (h % 2) * 128:(h % 2 + 1) * 128], Xc[:, h, :], XTc[:, h, :], True, True, rd=[XK(cur, p_), XTK(cur, p_)], wr=[P(bXT)])
                        kb.cp(XTn[:, 2 * p_:2 * p_ + 2, :].rearrange("p h n -> p (h n)"), pb[bXT][:, 0:256], rd=[P(bXT)], wr=[XTK(nx, p_)], eng="scalar")
                for p_ in range(2):
                    bT = NB_[p_][2]
                    for h in (2 * p_, 2 * p_ + 1):
                        kb.mm(pb[bT][:, (h % 2) * 128:(h % 2 + 1) * 128], Xn[:, h, :], TTc[:, h, :], True, True, rd=[XK(nx, p_), TTK(cur, p_)], wr=[P(bT)])
                    kb.tt(TTn[:, 2 * p_:2 * p_ + 2, :].rearrange("p h n -> p (h n)"), pb[bT][:, 0:256],
                          TTc[:, 2 * p_:2 * p_ + 2, :].rearrange("p h n -> p (h n)"), ALU.add, rd=[P(bT), TTK(cur, p_)], wr=[TTK(nx, p_)])
                cur = nx
            TTf = TTs[cur]
            ktf = TTK(cur, 0)
            ktf1 = TTK(cur, 1)
            for h in range(4):
                kb.mm(pb[7][:, h * 64:(h + 1) * 64], T4[h][:, 0, :], H[:, h, :], True, False, rd=["S_T4_%d" % h + sl, "S_H" + dl], wr=[P(7)])
                kb.mm(pb[7][:, h * 64:(h + 1) * 64], GM[h][:, 1, 0:128], w[:, 2, h * 64:(h + 1) * 64], False, True, rd=["S_GM%d" % h + sl, rk_], wr=[P(7)])
            kb.cp(R1[:].rearrange("p h v -> p (h v)"), pb[7][:, 0:256], rd=[P(7)], wr=["S_R1" + dl], eng="vector")
            for h in range(4):
                kb.mm(pb[7][:, 256 + h * 64:256 + (h + 1) * 64], TTf[:, h, :], R1[:, h, :], True, True, rd=[ktf, ktf1, "S_R1" + dl], wr=[P(7)])
            kb.ts(nU[:].rearrange("p h v -> p (h v)"), pb[7][:, 256:512], -1.0, None, ALU.mult, None, rd=[P(7)], wr=["S_nU" + dl])
            want_y = (i < 16) or need_ctx
            if want_y:
                for h in range(4):
                    o = pb[3][:, h * 64:(h + 1) * 64]
                    kb.mm(o, T4[h][:, 1, :], H[:, h, :], True, False, rd=["S_T4_%d" % h + sl, "S_H" + dl], wr=[P(3)])
                    kb.mm(o, GM[h][:, 0, 128:256], nU[:, h, :], False, False, rd=["S_GM%d" % h + sl, "S_nU" + dl], wr=[P(3)])
                    kb.mm(o, GM[h][:, 1, 128:256], w[:, 2, h * 64:(h + 1) * 64], False, True, rd=["S_GM%d" % h + sl, rk_], wr=[P(3)])
                kb.tt(yacc[:, i, :], pb[3][:, 0:256], yacc[:, i, :], ALU.add, rd=[P(3), ("S_yacc", i)], wr=[("S_yacc", i)])
            for h in range(4):
                o = pb[2][0:64, h * 64:(h + 1) * 64]
                kb.mm(o, ident[0:64, 0:64], H[:, h, :], True, False, rd=["ident", "S_H" + dl], wr=[P(2)])
                kb.mm(o, q4[:, 2, h * 64:(h + 1) * 64], nU[:, h, :], False, False, rd=["S_q4" + sl, "S_nU" + dl], wr=[P(2)])
                kb.mm(o, q4[:, 3, h * 64:(h + 1) * 64], w[:, 2, h * 64:(h + 1) * 64], False, True, rd=["S_q4" + sl, rk_], wr=[P(2)])
            kb.tt(H[:], pb[2][0:64, 0:256].rearrange("p (h v) -> p h v", h=4), pc[:].unsqueeze(2).to_broadcast([64, 4, 64]), ALU.mult,
                  rd=[P(2), "S_pc" + sl], wr=["S_H" + dl])

        NCH = len(orders[0])
        front(0, 0)
        front(1, 0)
        for n in range(NCH):
            back(0, n)
            if n + 1 < NCH:
                front(0, n + 1)
            back(1, n)
            if n + 1 < NCH:
                front(1, n + 1)
        kb.S.barrier()
        ln = sb("S_ln", [128, 3, 256])
        kb.dma(ln[:, 0:2, :], C["b_lnx"][l:l + 1].to_broadcast([128, 2, 256]), rd=(), wr=["S_ln"])
        kb.dma(ln[:, 2, :], C["b_rk"][l:l + 1, :].to_broadcast([128, 256]), rd=(), wr=["S_ln"])
        NBR = 4
        st = [sb("S_st%d" % i, [128, 4]) for i in range(NBR)]
        st2 = [sb("S_st2%d" % i, [128, 4]) for i in range(NBR)]
        yc_ = [sb("S_yc%d" % i, [128, 256]) for i in range(NBR)]
        t1 = [sb("S_t1%d" % i, [128, 256]) for i in range(NBR)]
        t3 = [sb("S_t3%d" % i, [128, 256]) for i in range(NBR)]
        bo = [sb("S_bo%d" % i, [128, 4]) for i in range(NBR)]
        yo = [sb("S_yo%d" % i, [128, 256]) for i in range(NBR)]
        rwr = rwt
        v3 = lambda ap: ap.rearrange("p (h d) -> p h d", d=64)
        tiles = list(range(NT)) if need_ctx else list(range(16))

        def r0_(i):
            b = i % NBR
            rk_ = "S_rwr%d" % b
            w = rwr[b]
            kb.dma(w[:], RW[i * 128:(i + 1) * 128], rd=(), wr=[rk_])
            y = yacc[:, i, :]
            yk = ("S_yacc", i)
            kb.red(st[b][:], v3(y), ALU.add, rd=[yk], wr=["S_st%d" % b])
            kb.ts(st[b][:], st[b][:], 1.0 / 64, None, ALU.mult, None, rd=["S_st%d" % b], wr=["S_st%d" % b])
            kb.tt(v3(yc_[b][:]), v3(y), st[b][:].unsqueeze(2).to_broadcast([128, 4, 64]), ALU.subtract, rd=[yk, "S_st%d" % b], wr=["S_yc%d" % b])
            kb.tt(t1[b][:], yc_[b][:], yc_[b][:], ALU.mult, rd=["S_yc%d" % b], wr=["S_t1%d" % b], eng="gpsimd")

        def r1_(i):
            b = i % NBR
            rk_ = "S_rwr%d" % b
            w = rwr[b]
            kb.red(st2[b][:], v3(t1[b][:]), ALU.add, rd=["S_t1%d" % b], wr=["S_st2%d" % b])
            kb.ts(st2[b][:], st2[b][:], 1.0 / 64, 64e-5, ALU.mult, ALU.add, rd=["S_st2%d" % b], wr=["S_st2%d" % b])
            kb.act(st2[b][:], st2[b][:], AF.Sqrt, rd=["S_st2%d" % b], wr=["S_st2%d" % b])
            kb.tt(t3[b][:], w[:, 0, :], w[:, 1, :], ALU.mult, rd=[rk_], wr=["S_t3%d" % b], eng="gpsimd")
            kb.tt(t3[b][:], t3[b][:], ln[:, 2, :], ALU.mult, rd=["S_t3%d" % b, "S_ln"], wr=["S_t3%d" % b], eng="gpsimd")

        def r2_(i):
            b = i % NBR
            rk_ = "S_rwr%d" % b
            w = rwr[b]
            kb.recip(st2[b][:], st2[b][:], rd=["S_st2%d" % b], wr=["S_st2%d" % b])
            kb.tt(v3(yc_[b][:]), v3(yc_[b][:]), st2[b][:].unsqueeze(2).to_broadcast([128, 4, 64]), ALU.mult, rd=["S_yc%d" % b, "S_st2%d" % b], wr=["S_yc%d" % b])
            kb.tt(yc_[b][:], yc_[b][:], ln[:, 0, :], ALU.mult, rd=["S_yc%d" % b, "S_ln"], wr=["S_yc%d" % b], eng="gpsimd")
            kb.red(bo[b][:], v3(t3[b][:]), ALU.add, rd=["S_t3%d" % b], wr=["S_bo%d" % b])
            kb.tt(v3(t3[b][:]), v3(w[:, 2, :]), bo[b][:].unsqueeze(2).to_broadcast([128, 4, 64]), ALU.mult, rd=[rk_, "S_bo%d" % b, "S_t3%d" % b], wr=["S_t3%d" % b])

        def r3_(i):
            b = i % NBR
            rk_ = "S_rwr%d" % b
            w = rwr[b]
            kb.tt(t3[b][:], t3[b][:], ln[:, 1, :], ALU.add, rd=["S_t3%d" % b, "S_ln"], wr=["S_t3%d" % b], eng="gpsimd")
            kb.tt(yc_[b][:], yc_[b][:], t3[b][:], ALU.add, rd=["S_yc%d" % b, "S_t3%d" % b], wr=["S_yc%d" % b])
            kb.tt(yo[b][:], yc_[b][:], w[:, 7, :], ALU.mult, rd=["S_yc%d" % b, rk_], wr=["S_yo%d" % b])
            kb.dma(YB[i * 128:(i + 1) * 128, 256:512], yo[b][:], rd=["S_yo%d" % b], wr=[("YB", "S_yo%d" % b)], semres="S_yo%d" % b)

        pipeline([r0_, r1_, r2_, r3_], tiles)


def merge_phase(kb, l, need_ctx, x_in, ctx_in, X, Z, YB, H2T, MOD, ident, C, gate_all):
    nc = kb.nc
    with ExitStack() as es:
        sb = lambda n, s, dt=F32: es.enter_context(nc.sbuf_tensor(kb.un(n), s, dt))
        ps = lambda n, s, dt=F32: es.enter_context(nc.psum_tensor(kb.un(n), s, dt))
        wbr = sb("M_wbr", [128, 8, D], F32R)
        wout = sb("M_wout", [128, 8, D], F32R)
        kb.dma(wbr[:], C["w_branch"][l].rearrange("n (c p) d -> p (n c) d", p=128), rd=(), wr=["M_wbr"], q="gpsimd")
        kb.dma(wout[:], C["w_out"][l].rearrange("(c p) d -> p c d", p=128), rd=(), wr=["M_wout"], q="gpsimd")
        rwt = sb("M_rw", [128, 8, 16])
        kb.dma(rwt[:], C["router_w"].rearrange("(c p) e -> p c e", p=128), rd=(), wr=["M_rw"])
        rb = sb("M_rb", [128, 16])
        kb.dma(rb[:], C["router_b"].unsqueeze(0).to_broadcast([128, 16]), rd=(), wr=["M_rb"])
        gm = [sb("M_gm%d" % s, [128, D]) for s in range(2)]
        A2 = [sb("M_A2%d" % s, [128, D]) for s in range(2)]
        B2 = [sb("M_B2%d" % s, [128, D]) for s in range(2)]
        gbc = sb("M_gbc", [128, D])
        kb.dma(gbc[:], C["norm2"][l:l + 1, :].to_broadcast([128, D]), rd=(), wr=["M_gbc"])
        for s in range(2):
            kb.dma(gm[s][:], MOD[l, s:s + 1, 2 * D:3 * D].to_broadcast([128, D]), rd=(), wr=["M_gm%d" % s])
            kb.dma(A2[s][:], MOD[l, s:s + 1, 4 * D:5 * D].to_broadcast([128, D]), rd=(), wr=["M_A2%d" % s])
            kb.dma(B2[s][:], MOD[l, s:s + 1, 3 * D:4 * D].to_broadcast([128, D]), rd=(), wr=["M_B2%d" % s])
            kb.stt(A2[s][:], A2[s][:], 1.0, gbc[:], ALU.add, ALU.mult, rd=["M_A2%d" % s, "M_gbc"], wr=["M_A2%d" % s])
        yb_ = [sb("M_yb%d" % i_, [128, D]) for i_ in range(2)]
        ybT_ = [sb("M_ybT%d" % i_, [128, 8, 128], F32R) for i_ in range(2)]
        gt_ = [sb("M_gt%d" % i_, [128, 4 * D]) for i_ in range(2)]
        m_ = [sb("M_m%d" % i_, [128, D]) for i_ in range(2)]
        tmp_ = [sb("M_tmp%d" % i_, [128, 512]) for i_ in range(3)]
        mT_ = [sb("M_mT%d" % i_, [128, 8, 128], F32R) for i_ in range(2)]
        xt_ = [sb("M_xt%d" % i_, [128, D]) for i_ in range(2)]
        xn_ = [sb("M_xn%d" % i_, [128, D]) for i_ in range(2)]
        ss_ = [sb("M_ss%d" % i_, [128, 1]) for i_ in range(2)]
        h2_ = [sb("M_h2%d" % i_, [128, D]) for i_ in range(2)]
        h2T_ = [sb("M_h2T%d" % i_, [128, 8, 128]) for i_ in range(2)]
        sc = sb("M_sc", [128, NT, 16])
        bi = sb("M_bi", [128, NT, 16])
        r4 = sb("M_r4", [128, 8, NT, 4])
        msk = sb("M_msk", [128, NT, 16])
        r1 = sb("M_r1", [128, 4, NT])
        M_ptr = [ps("M_ptr%d" % i, [128, 4, 128]) for i in range(2)]
        M_pq = [ps("M_pq%d" % i, [128, 512]) for i in range(3)]
        M_pr = ps("M_pr", [128, 16])
        tiles = list(range(NT)) if need_ctx else list(range(16))
        kqc = [0]

        def transposes(src, srck, dst, dstk):
            for g in range(2):
                for j in range(4):
                    c = g * 4 + j
                    kb.tr(M_ptr[g][:, j, :], src[:, c * 128:(c + 1) * 128], ident[:], rd=srck, wr=["M_ptr%d" % g])
                kb.cp(dst[:, g * 4:(g + 1) * 4, :], M_ptr[g][:], rd=["M_ptr%d" % g], wr=[dstk], eng="scalar" if g == 0 else "vector")

        def s0(i):
            bb_ = i % 2
            kb.dma(yb_[bb_][:], YB[i * 128:(i + 1) * 128, :], rd=(), wr=["M_yb_%d" % bb_])

        def s1(i):
            bb_ = i % 2
            transposes(yb_[bb_], ["M_yb_%d" % bb_], ybT_[bb_], "M_ybT_%d" % bb_)
            kb.dma(gt_[bb_][:], Z[i * 128:(i + 1) * 128, O_G:O_G + 4 * D], rd=(), wr=[("M_gt", bb_, n_) for n_ in range(4)])

        def s2(i):
            bb_ = i % 2
            r0 = i * 128
            gt, m, ybT, xt = gt_[bb_], m_[bb_], ybT_[bb_], xt_[bb_]
            if l == 0:
                src = x_in[r0:r0 + 128, :] if i < 16 else ctx_in[r0 - SEQ:r0 - SEQ + 128, :]
            else:
                src = X[r0:r0 + 128, :]
            kb.dma(xt[:], src, rd=(), wr=["M_xt_%d" % bb_])
            for n in range(4):
                kb.act(gt[:, n * D:(n + 1) * D], gt[:, n * D:(n + 1) * D], AF.Sigmoid, rd=[("M_gt", bb_, n)], wr=[("M_gt", bb_, n)])
            for n in range(4):
                for hf in range(2):
                    b = kqc[0] % 3
                    kqc[0] += 1
                    pk = "M_pq%d" % b
                    for cc in range(2):
                        c = 2 * n + cc
                        kb.mm(M_pq[b][:], ybT[:, c, :], wbr[:, c, hf * 512:(hf + 1) * 512], cc == 0, cc == 1, rd=["M_ybT_%d" % bb_, "M_wbr"], wr=[pk])
                    gsl = gt[:, n * D + hf * 512:n * D + (hf + 1) * 512]
                    mk = ("M_m", hf, bb_)
                    if n == 0:
                        kb.tt(m[:, hf * 512:(hf + 1) * 512], M_pq[b][:], gsl, ALU.mult, rd=[pk, ("M_gt", bb_, n)], wr=[mk])
                    else:
                        tmp = tmp_[b]
                        kb.tt(tmp[:], M_pq[b][:], gsl, ALU.mult, rd=[pk, ("M_gt", bb_, n)], wr=["M_tmp%d" % b])
                        kb.tt(m[:, hf * 512:(hf + 1) * 512], m[:, hf * 512:(hf + 1) * 512], tmp[:], ALU.add, rd=[mk, "M_tmp%d" % b], wr=[mk], eng="gpsimd")
            transposes(m, [("M_m", 0, bb_), ("M_m", 1, bb_)], mT_[bb_], "M_mT_%d" % bb_)

        def s3(i):
            bb_ = i % 2
            s = 0 if i < 16 else 1
            r0 = i * 128
            mT, xt, xn, ss, h2 = mT_[bb_], xt_[bb_], xn_[bb_], ss_[bb_], h2_[bb_]
            xk = [("M_xnh", 0, bb_), ("M_xnh", 1, bb_)]
            for hf in range(2):
                b = kqc[0] % 3
                kqc[0] += 1
                pk = "M_pq%d" % b
                for c in range(8):
                    kb.mm(M_pq[b][:], mT[:, c, :], wout[:, c, hf * 512:(hf + 1) * 512], c == 0, c == 7, rd=["M_mT_%d" % bb_, "M_wout"], wr=[pk])
                tmp = tmp_[b]
                kb.tt(tmp[:], M_pq[b][:], gm[s][:, hf * 512:(hf + 1) * 512], ALU.mult, rd=[pk, "M_gm%d" % s], wr=["M_tmp%d" % b])
                kb.tt(xn[:, hf * 512:(hf + 1) * 512], tmp[:], xt[:, hf * 512:(hf + 1) * 512], ALU.add, rd=["M_tmp%d" % b, "M_xt_%d" % bb_], wr=[xk[hf]], eng="gpsimd")
            kb.dma(X[r0:r0 + 128, :], xn[:], rd=xk, wr=[("X", i)], semres="M_xn_%d" % bb_)
            hk = "M_h2_%d" % bb_
            sk = "M_ss_%d" % bb_
            kb.act(h2[:], xn[:], AF.Square, rd=xk, wr=[hk, sk], accum_out=ss[:])
            kb.rstd(ss[:], ss[:], D, EPS, rd=[sk], wr=[sk])
            kb.stt(h2[:], xn[:], ss[:, 0:1], A2[s][:], ALU.mult, ALU.mult, rd=xk + [sk, "M_A2%d" % s], wr=[hk])
            kb.tt(h2[:], h2[:], B2[s][:], ALU.add, rd=[hk, "M_B2%d" % s], wr=[hk])

        def s4(i):
            bb_ = i % 2
            r0 = i * 128
            h2, h2T = h2_[bb_], h2T_[bb_]
            tk = "M_h2T_%d" % bb_
            transposes(h2, ["M_h2_%d" % bb_], h2T, tk)
            kb.dma(H2T[:, :, r0:r0 + 128], h2T[:], rd=[tk], wr=[("H2T", i)], semres=tk)
            for c in range(8):
                kb.mm(M_pr[:], h2T[:, c, :], rwt[:, c, :], c == 0, c == 7, rd=[tk, "M_rw"], wr=["M_pr"])
            kb.act(sc[:, i, :], M_pr[:], AF.Sigmoid, rd=["M_pr"], wr=[("M_sc", i)])

        pipeline([s0, s1, s2, s3, s4], tiles)
        T_ = len(tiles)
        sck = [("M_sc", i_) for i_ in tiles]
        scv = sc[:, 0:T_, :]
        kb.tt(bi[:, 0:T_, :], scv, rb[:].unsqueeze(1).to_broadcast([128, T_, 16]), ALU.add, rd=sck + ["M_rb"], wr=["M_bi"])
        b4 = bi[:, 0:T_, :].rearrange("p t (g j) -> p t g j", j=4)
        K_ = ["M_r4"]
        R = lambda k_: r4[:, k_, 0:T_, :]
        kb.tt(R(0), b4[:, :, :, 0], b4[:, :, :, 1], ALU.max, rd=["M_bi"], wr=K_)
        kb.tt(R(1), b4[:, :, :, 0], b4[:, :, :, 1], ALU.min, rd=["M_bi"], wr=K_)
        kb.tt(R(2), b4[:, :, :, 2], b4[:, :, :, 3], ALU.max, rd=["M_bi"], wr=K_)
        kb.tt(R(3), b4[:, :, :, 2], b4[:, :, :, 3], ALU.min, rd=["M_bi"], wr=K_)
        kb.tt(R(4), R(0), R(2), ALU.max, rd=K_, wr=K_)
        kb.tt(R(5), R(0), R(2), ALU.min, rd=K_, wr=K_)
        kb.tt(R(6), R(1), R(3), ALU.max, rd=K_, wr=K_)
        kb.tt(R(5), R(5), R(6), ALU.max, rd=K_, wr=K_)
        kb.tt(R(4), R(4), R(5), ALU.add, rd=K_, wr=K_)
        kb.red(r1[:, 0, 0:T_], R(4), ALU.max, rd=K_, wr=["M_r1"])
        kb.tt(R(7), R(4), r1[:, 0, 0:T_].unsqueeze(2).to_broadcast([128, T_, 4]), ALU.is_equal, rd=K_ + ["M_r1"], wr=K_)
        kb.ts(R(6), R(7), 1e30, -1e30, ALU.mult, ALU.add, rd=K_, wr=K_)
        m4 = msk[:, 0:T_, :].rearrange("p t (g j) -> p t g j", j=4)
        kb.tt(m4, b4, R(7).unsqueeze(3).to_broadcast([128, T_, 4, 4]), ALU.mult, rd=["M_bi"] + K_, wr=["M_msk"])
        kb.tt(m4, m4, R(6).unsqueeze(3).to_broadcast([128, T_, 4, 4]), ALU.add, rd=["M_msk"] + K_, wr=["M_msk"])
        mv = msk[:, 0:T_, :]
        bv = bi[:, 0:T_, :]
        kb.red(r1[:, 1, 0:T_], mv, ALU.max, rd=["M_msk"], wr=["M_r1"])
        kb.tt(bv, mv, r1[:, 1, 0:T_].unsqueeze(2).to_broadcast([128, T_, 16]), ALU.is_equal, rd=["M_msk", "M_r1"], wr=["M_bi"])
        kb.stt(mv, bv, -1e30, mv, ALU.mult, ALU.add, rd=["M_bi", "M_msk"], wr=["M_msk"])
        kb.red(r1[:, 2, 0:T_], mv, ALU.max, rd=["M_msk"], wr=["M_r1"])
        kb.tt(mv, mv, r1[:, 2, 0:T_].unsqueeze(2).to_broadcast([128, T_, 16]), ALU.is_equal, rd=["M_msk", "M_r1"], wr=["M_msk"])
        kb.tt(bv, bv, mv, ALU.add, rd=["M_bi", "M_msk"], wr=["M_bi"])
        kb.tt(bv, bv, scv, ALU.mult, rd=["M_bi"] + sck, wr=["M_bi"])
        kb.red(r1[:, 3, 0:T_], bv, ALU.add, rd=["M_bi"], wr=["M_r1"])
        kb.recip(r1[:, 3, 0:T_], r1[:, 3, 0:T_], rd=["M_r1"], wr=["M_r1"])
        t0_ = tiles[0]
        kb.tt(gate_all[:, t0_:t0_ + T_, :], bv, r1[:, 3, 0:T_].unsqueeze(2).to_broadcast([128, T_, 16]), ALU.mult, rd=["M_bi", "M_r1"],
              wr=[("gate", i_) for i_ in tiles])


def moe_phase(kb, l, need_ctx, X, H2T, MOD, C, gate_all, out):
    nc = kb.nc
    last = (l == DEPTH - 1)
    tiles_all = list(range(NT)) if need_ctx else list(range(16))
    passes = [tiles_all[0:9], tiles_all[9:]]
    with ExitStack() as es:
        sb = lambda n, s, dt=F32: es.enter_context(nc.sbuf_tensor(kb.un(n), s, dt))
        ps = lambda n, s, dt=F32: es.enter_context(nc.psum_tensor(kb.un(n), s, dt))
        hT = sb("E_hT", [128, 8, 9 * 128], F32R)
        yacc = sb("E_yacc", [128, 9, D])
        Wg = [sb("E_Wg%d" % i, [128, 8, DE], F32R) for i in range(2)]
        Wu = [sb("E_Wu%d" % i, [128, 8, DE], F32R) for i in range(2)]
        Wd = [sb("E_Wd%d" % i, [128, 4, D], F32R) for i in range(2)]
        sg = [sb("E_sg%d" % i, [128, 512]) for i in range(2)]
        hid = sb("E_hid", [128, 4, 512], F32R)
        gmlp = [sb("E_gmlp%d" % s, [128, D]) for s in range(2)]
        xt = [sb("E_xt%d" % i, [128, D]) for i in range(2)]
        pg = [ps("E_pG%d" % i, [128, 512]) for i in range(2)]
        pu = [ps("E_pU%d" % i, [128, 512]) for i in range(2)]
        py = [ps("E_pY%d" % i, [128, 512]) for i in range(2)]
        for s in range(2):
            kb.dma(gmlp[s][:], MOD[l, s:s + 1, 5 * D:6 * D].to_broadcast([128, D]), rd=(), wr=["E_gmlp%d" % s])
        kg = 0
        ky = 0
        for pi, tl in enumerate(passes):
            ntl = len(tl)
            t0 = tl[0] * 128
            def load_w(e_):
                wb_ = e_ % 2
                kb.dma(Wg[wb_][:], C["e_gate"][l, e_].rearrange("(c p) n -> p c n", p=128), rd=(), wr=["E_Wg%d" % wb_], q="gpsimd")
                kb.dma(Wu[wb_][:], C["e_up"][l, e_].rearrange("(c p) n -> p c n", p=128), rd=(), wr=["E_Wu%d" % wb_], q="gpsimd")
                kb.dma(Wd[wb_][:], C["e_down"][l, e_].rearrange("(c p) n -> p c n", p=128), rd=(), wr=["E_Wd%d" % wb_], q="gpsimd")

            load_w(0)
            for c in range(8):
                kb.dma(hT[:, c, 0:ntl * 128], H2T[:, c, t0:t0 + ntl * 128], rd=(), wr=["E_hT"], q="gpsimd")
            load_w(1)
            groups = []
            ng_ = (ntl + 3) // 4
            a = 0
            for gi_ in range(ng_):
                n_ = (ntl - a + (ng_ - gi_) - 1) // (ng_ - gi_)
                groups.append((a, n_))
                a += n_
            for e in range(NE):
                wb = e % 2
                if e >= 2:
                    load_w(e)
                for (a, n_) in groups:
                    nt = n_ * 128
                    for hc in range(4):
                        b = kg % 2
                        kg += 1
                        for c in range(8):
                            kb.mm(pg[b][:, 0:nt], Wg[wb][:, c, hc * 128:(hc + 1) * 128], hT[:, c, a * 128:a * 128 + nt], c == 0, c == 7,
                                  rd=["E_Wg%d" % wb, "E_hT"], wr=["E_pG%d" % b])
                        for c in range(8):
                            kb.mm(pu[b][:, 0:nt], Wu[wb][:, c, hc * 128:(hc + 1) * 128], hT[:, c, a * 128:a * 128 + nt], c == 0, c == 7,
                                  rd=["E_Wu%d" % wb, "E_hT"], wr=["E_pU%d" % b])
                        kb.act(sg[b][:, 0:nt], pg[b][:, 0:nt], AF.Silu, rd=["E_pG%d" % b], wr=["E_sg%d" % b])
                        kb.tt(hid[:, hc, 0:nt], pu[b][:, 0:nt], sg[b][:, 0:nt], ALU.mult, rd=["E_pU%d" % b, "E_sg%d" % b], wr=[("E_hid", hc)])
                    for tt_ in range(n_):
                        ti = a + tt_
                        gi = tl[ti]
                        for hf in range(2):
                            b = ky % 2
                            ky += 1
                            for hc in range(4):
                                kb.mm(py[b][:], hid[:, hc, tt_ * 128:(tt_ + 1) * 128], Wd[wb][:, hc, hf * 512:(hf + 1) * 512], hc == 0, hc == 3,
                                      rd=[("E_hid", hc), "E_Wd%d" % wb], wr=["E_pY%d" % b])
                            ya = yacc[:, ti, hf * 512:(hf + 1) * 512]
                            yk = ("E_yacc", ti, hf)
                            gcol = gate_all[:, gi, e:e + 1]
                            if e == 0:
                                kb.ts(ya, py[b][:], gcol, None, ALU.mult, None, rd=["E_pY%d" % b, ("gate", gi)], wr=[yk])
                            else:
                                kb.stt(ya, py[b][:], gcol, ya, ALU.mult, ALU.add, rd=["E_pY%d" % b, ("gate", gi), yk], wr=[yk])
            for ti, gi in enumerate(tl):
                s = 0 if gi < 16 else 1
                b = ti % 2
                r0 = gi * 128
                kb.dma(xt[b][:], X[r0:r0 + 128, :], rd=[("X", gi)], wr=["E_xt%d" % b])
                kb.tt(yacc[:, ti, :], yacc[:, ti, :], gmlp[s][:], ALU.mult, rd=[("E_yacc", ti, 0), ("E_yacc", ti, 1), "E_gmlp%d" % s], wr=[("E_yacc", ti, 0), ("E_yacc", ti, 1)], eng="gpsimd")
                kb.tt(xt[b][:], xt[b][:], yacc[:, ti, :], ALU.add, rd=["E_xt%d" % b, ("E_yacc", ti, 0), ("E_yacc", ti, 1)], wr=["E_xt%d" % b])
                if last:
                    kb.dma(out[r0:r0 + 128, :], xt[b][:], rd=["E_xt%d" % b], wr=[("out", gi)], semres="E_xt%d" % b, is_output=True)
                else:
                    kb.dma(X[r0:r0 + 128, :], xt[b][:], rd=["E_xt%d" % b], wr=[("X", gi)], semres="E_xt%d" % b)


def build(debug=(), stop_after=None):
    kb = KB(debug)
    nc, S = kb.nc, kb.S
    x_in = kb.inp("x", [SEQ, D])
    ctx_in = kb.inp("ctx", [CTX, D])
    cmat = kb.inp("cmat", [128, 8, 2])
    w_mod = kb.inp("w_mod", [DEPTH, D, 6 * D])
    b_mod = kb.inp("b_mod", [DEPTH, 6 * D])
    norm1 = kb.inp("norm1", [DEPTH, D])
    norm2 = kb.inp("norm2", [DEPTH, D])
    w_in = kb.inp("w_in", [DEPTH, D, D_IN])
    ident_in = kb.inp("ident", [128, 128])
    out = nc.dram_tensor("out", [SEQ, D], F32, kind="ExternalOutput").ap()
    X = kb.scratch("X", [NTOK, D])
    Z = kb.scratch("Z", [NTOK, D_IN])
    MOD = kb.scratch("MOD", [DEPTH, 2, 6 * D])
    YB = kb.scratch("YB", [NTOK, D])
    RW = kb.scratch("RW", [NTOK, 8, 256])
    H2T = kb.scratch("H2T", [128, 8, NTOK])
    C = {}
    for nm_, shp_ in (("b_shift", [DEPTH, 2, 896]), ("b_w0", [DEPTH, 2, 256]), ("b_w2", [DEPTH, 2, 32, 256]), ("b_a0", [DEPTH, 256]),
                      ("b_a2", [DEPTH, 32, 256]), ("b_g2", [DEPTH, 64, 256]), ("b_kk", [DEPTH, 256]), ("b_ka", [DEPTH, 256]),
                      ("b_rk", [DEPTH, 256]), ("b_lnx", [DEPTH, 2, 256]), ("rwkv_masks", [2, 128, 512])):
        C[nm_] = kb.inp(nm_, shp_)
    for nm_, shp_ in (("w_branch", [DEPTH, 4, 256, D]), ("w_out", [DEPTH, D, D]), ("router_w", [D, NE]), ("router_b", [NE]),
                      ("e_gate", [DEPTH, NE, D, DE]), ("e_up", [DEPTH, NE, D, DE]), ("e_down", [DEPTH, NE, DE, D])):
        C[nm_] = kb.inp(nm_, shp_)
    C["norm2"] = norm2
    C["a_qk_norm"] = kb.inp("a_qk_norm", [DEPTH, 2, 64])
    C["a_sink"] = kb.inp("a_sink", [DEPTH, 4])
    C["c_qk_norm"] = kb.inp("c_qk_norm", [DEPTH, 2, 32])
    C["c_lambda"] = kb.inp("c_lambda", [DEPTH, 4, 32])
    C["c_subln"] = kb.inp("c_subln", [DEPTH, 64])
    C["d_qk_norm"] = kb.inp("d_qk_norm", [DEPTH, 2, 64])
    C["ropeA"] = kb.inp("ropeA", [SEQ, 2, 384])
    C["ropeC"] = kb.inp("ropeC", [SEQ, 2, 512])
    C["bandmask"] = kb.inp("bandmask", [2, 128, 128])
    C["biasG"] = kb.inp("biasG", [DEPTH, 128, 8 * 4 * 4 * 64])
    C["maskD"] = kb.inp("maskD", [128, 64])

    with ExitStack() as es0:
        ident = es0.enter_context(nc.sbuf_tensor("ident_sb", [128, 128], F32))
        kb.dma(ident[:], ident_in, rd=(), wr=["ident"])
        gate_all = es0.enter_context(nc.sbuf_tensor("gate_all", [128, NT, 16], F32))
        kb.onez = es0.enter_context(nc.sbuf_tensor("onez", [128, 2], F32))
        kb.memset(kb.onez[:, 0:1], 1.0, wr=["onez"])
        kb.memset(kb.onez[:, 1:2], 0.0, wr=["onez"])

        for l in range(DEPTH):
            with ExitStack() as es:
                sb = lambda n, s, dt=F32: es.enter_context(nc.sbuf_tensor(kb.un(n), s, dt))
                ps = lambda n, s, dt=F32: es.enter_context(nc.psum_tensor(kb.un(n), s, dt))
                cT = sb("cT", [128, 8, 2])
                scT = sb("scT", [128, 8, 2], F32R)
                bm = sb("bm", [2, 6 * D])
                modsb = sb("modsb", [2, 6 * D])
                wm = [sb("wm%d" % i, [128, 8, 512], F32R) for i in range(2)]
                pm = [ps("pm%d" % i, [2, 512]) for i in range(2)]
                kb.dma(cT[:], cmat, rd=(), wr=["cT"])
                kb.dma(bm[:], b_mod[l:l + 1, :].to_broadcast([2, 6 * D]), rd=(), wr=["bm"])
                kb.act(scT[:], cT[:], AF.Silu, rd=["cT"], wr=["scT"])
                for cg in range(12):
                    w = wm[cg % 2]
                    wk = "wm%d" % (cg % 2)
                    pk = "pm%d" % (cg % 2)
                    kb.dma(w[:], w_mod[l, :, cg * 512:(cg + 1) * 512].rearrange("(c p) n -> p c n", p=128), rd=(), wr=[wk], q="gpsimd")
                    for c in range(8):
                        kb.mm(pm[cg % 2][:], scT[:, c, :], w[:, c, :], c == 0, c == 7, rd=["scT", wk], wr=[pk])
                    kb.tt(modsb[:, cg * 512:(cg + 1) * 512], pm[cg % 2][:], bm[:, cg * 512:(cg + 1) * 512], ALU.add, rd=[pk, "bm"], wr=["modsb"])
                kb.dma(MOD[l], modsb[:], rd=["modsb"], wr=[("MOD", l)])
            S.barrier()

            with ExitStack() as es:
                sb = lambda n, s, dt=F32: es.enter_context(nc.sbuf_tensor(kb.un(n), s, dt))
                ps = lambda n, s, dt=F32: es.enter_context(nc.psum_tensor(kb.un(n), s, dt))
                hT = sb("hT", [128, 8, NTOK], F32R)
                Am = [sb("Am%d" % s, [128, D]) for s in range(2)]
                Bm = [sb("Bm%d" % s, [128, D]) for s in range(2)]
                gbc = sb("gbc", [128, D])
                xt = [sb("xt%d" % i, [128, D]) for i in range(2)]
                sq = sb("sq", [128, D])
                ss = [sb("ss%d" % i, [128, 1]) for i in range(2)]
                hh = [sb("hh%d" % i, [128, D]) for i in range(2)]
                ptr = [ps("ptr%d" % i, [128, 4, 128]) for i in range(2)]
                kb.dma(gbc[:], norm1[l:l + 1, :].to_broadcast([128, D]), rd=(), wr=["gbc"])
                for s in range(2):
                    kb.dma(Am[s][:], MOD[l, s:s + 1, D:2 * D].to_broadcast([128, D]), rd=[("MOD", l)], wr=["Am%d" % s])
                    kb.dma(Bm[s][:], MOD[l, s:s + 1, 0:D].to_broadcast([128, D]), rd=[("MOD", l)], wr=["Bm%d" % s])
                    kb.stt(Am[s][:], Am[s][:], 1.0, gbc[:], ALU.add, ALU.mult, rd=["Am%d" % s, "gbc"], wr=["Am%d" % s])
                for i in range(NT):
                    s = 0 if i < 16 else 1
                    b = i % 2
                    if l == 0:
                        src = x_in[i * 128:(i + 1) * 128, :] if i < 16 else ctx_in[(i - 16) * 128:(i - 15) * 128, :]
                        srck = ()
                    else:
                        src = X[i * 128:(i + 1) * 128, :]
                        srck = [("X", i)]
                    kb.dma(xt[b][:], src, rd=srck, wr=["xt%d" % b])
                    kb.act(sq[:], xt[b][:], AF.Square, rd=["xt%d" % b], wr=["sq", "ss%d" % b], accum_out=ss[b][:])
                    kb.rstd(ss[b][:], ss[b][:], D, EPS, rd=["ss%d" % b], wr=["ss%d" % b])
                    kb.stt(hh[b][:], xt[b][:], ss[b][:, 0:1], Am[s][:], ALU.mult, ALU.mult, rd=["xt%d" % b, "ss%d" % b, "Am%d" % s], wr=["hh%d" % b])
                    kb.tt(hh[b][:], hh[b][:], Bm[s][:], ALU.add, rd=["hh%d" % b, "Bm%d" % s], wr=["hh%d" % b], eng="gpsimd")
                    for g in range(2):
                        for j in range(4):
                            c = g * 4 + j
                            kb.tr(ptr[g][:, j, :], hh[b][:, c * 128:(c + 1) * 128], ident[:], rd=["hh%d" % b], wr=["ptr%d" % g])
                        kb.cp(hT[:, g * 4:(g + 1) * 4, i * 128:(i + 1) * 128], ptr[g][:], rd=["ptr%d" % g], wr=[("hT", i)],
                              eng="scalar" if g == 0 else "vector")
                wz = [sb("wz%d" % i, [128, 8, 512], F32R) for i in range(2)]
                pz = [ps("pz%d" % i, [128, 512]) for i in range(4)]
                zs = [sb("zs%d" % i, [128, 512]) for i in range(4)]
                k = 0
                for cg in range(14):
                    n0 = cg * 512
                    ncol = min(512, D_IN - n0)
                    w = wz[cg % 2]
                    wk = "wz%d" % (cg % 2)
                    kb.dma(w[:, :, 0:ncol], w_in[l, :, n0:n0 + ncol].rearrange("(c p) n -> p c n", p=128), rd=(), wr=[wk], q="gpsimd")
                    for i in range(NT):
                        r = k % 4
                        k += 1
                        for c in range(8):
                            kb.mm(pz[r][:, 0:ncol], hT[:, c, i * 128:(i + 1) * 128], w[:, c, 0:ncol], c == 0, c == 7,
                                  rd=[("hT", i), wk], wr=["pz%d" % r])
                        kb.cp(zs[r][:, 0:ncol], pz[r][:, 0:ncol], rd=["pz%d" % r], wr=["zs%d" % r], eng="scalar" if r % 2 == 0 else "vector")
                        kb.dma(Z[i * 128:(i + 1) * 128, n0:n0 + ncol], zs[r][:, 0:ncol], rd=["zs%d" % r], wr=[("Z", i, cg)], semres="zs%d" % r)
            S.barrier()
            if stop_after == ("proj", l):
                break
            need_ctx = l < DEPTH - 1
            if stop_after != ("C", l):
                rwkv(kb, l, need_ctx, Z, YB, RW, ident, C)
                S.barrier()
            if stop_after == ("B", l):
                break
            attn_A(kb, l, need_ctx, Z, YB, ident, C)
            S.barrier()
            if stop_after == ("A", l):
                break
            attn_D(kb, l, need_ctx, Z, YB, ident, C)
            S.barrier()
            if stop_after == ("D", l):
                break
            attn_C(kb, l, need_ctx, Z, YB, ident, C)
            S.barrier()
            if stop_after == ("C", l):
                break
            merge_phase(kb, l, need_ctx, x_in, ctx_in, X, Z, YB, H2T, MOD, ident, C, gate_all)
            S.barrier()
            if stop_after == ("M", l):
                break
            moe_phase(kb, l, need_ctx, X, H2T, MOD, C, gate_all, out)
            S.barrier()
    S.barrier()
    cnt = S.finish()
    print("instr counts", cnt, "dma sems", S.n_dma_sems)
    return kb


_CONST_CACHE = {}


def host_constants():
    if _CONST_CACHE:
        return _CONST_CACHE
    t = np.arange(SEQ)
    rows, cols = (t // 64).astype(np.float32), (t % 64).astype(np.float32)

    def table(hd, G):
        h = hd // 2
        inv = (np.float32(10000.0) ** (-np.arange(0, h, 2, dtype=np.float32) / np.float32(h))).astype(np.float32)
        ar = rows[:, None] * inv[None, :]
        ac = cols[:, None] * inv[None, :]
        cos = np.concatenate([np.cos(ar), np.cos(ar), np.cos(ac), np.cos(ac)], -1).astype(np.float32)
        sin = np.concatenate([-np.sin(ar), np.sin(ar), -np.sin(ac), np.sin(ac)], -1).astype(np.float32)
        return np.ascontiguousarray(np.stack([np.tile(cos, (1, G)), np.tile(sin, (1, G))], 1))
    _CONST_CACHE["ropeA"] = table(64, 6)
    _CONST_CACHE["ropeC"] = table(32, 16)
    b = np.arange(128)[:, None]
    a = np.arange(128)[None, :]
    _CONST_CACHE["bandmask"] = np.stack([(a <= b), (b <= a)]).astype(np.float32)
    p = np.arange(128)
    kcol = p % 64
    q = np.arange(64)
    cstart = np.clip(q - 8, 0, 48)
    _CONST_CACHE["maskD"] = ((kcol[:, None] >= cstart[None, :]) & (kcol[:, None] < cstart[None, :] + 16)).astype(np.float32)
    _CONST_CACHE["ident"] = np.eye(128, dtype=np.float32)
    s_ = np.arange(128)[:, None]
    t_ = np.arange(128)[None, :]
    mk = []
    for d in range(2):
        le = (s_ <= t_) if d == 0 else (s_ >= t_)
        lt = (s_ < t_) if d == 0 else (s_ > t_)
        mk.append(np.concatenate([le, lt, le, lt.T], 1).astype(np.float32))
    _CONST_CACHE["rwkv_masks"] = np.stack(mk)
    return _CONST_CACHE


def gather_biasG(d_rpb):
    d_rpb = np.asarray(d_rpb, dtype=np.float32)
    p = np.arange(128)
    jl, kcol = p // 64, p % 64
    q = np.arange(64)
    dc = np.clip(kcol[:, None] - q[None, :] + 15, 0, 30)
    out = np.zeros((DEPTH, 128, 8, 4, 4, 64), np.float32)
    for cls in range(8):
        r = cls if cls < 4 else (4 if cls == 4 else 24 + cls)
        start = min(max(r - 4, 0), 24)
        for t in range(4):
            key_row = start + 2 * t + jl
            dr = key_row - r + 7
            for l in range(DEPTH):
                for h in range(4):
                    out[l, :, cls, h, t, :] = d_rpb[l, h][dr[:, None], dc]
    return np.ascontiguousarray(out.reshape(DEPTH, 128, 8 * 4 * 4 * 64))


def make_in_map(inp, b, kb, shared=None):
    f = lambda a: np.ascontiguousarray(np.asarray(a, dtype=np.float32))
    cm = np.stack([np.asarray(inp["c"][b]).reshape(8, 128).T, np.asarray(inp["c_ctx"]).reshape(8, 128).T], axis=-1)
    m = {"x": f(inp["x"][b]), "ctx": f(inp["ctx"][b]), "cmat": f(cm)}
    if shared is None:
        shared = {}
    if "biasG" not in shared:
        shared.update(host_constants())
        shared["biasG"] = gather_biasG(inp["d_rpb"])
    for k in kb.ins:
        if k in m:
            continue
        if k not in shared:
            shared[k] = f(inp[k])
        m[k] = shared[k]
    return m


_KB_CACHE = {}


def kernel(**inputs):
    inp = {k: np.asarray(v) for k, v in inputs.items()}
    if "kb" not in _KB_CACHE:
        _KB_CACHE["kb"] = build()
    kb = _KB_CACHE["kb"]
    shared = {}
    n = 8
    in_maps = [make_in_map(inp, b, kb, shared) for b in range(n)]
    res = run_bass_kernel_spmd(kb.nc, in_maps, core_ids=list(range(n)))
    return np.stack([np.asarray(res.results[b]["out"], dtype=np.float32) for b in range(n)], axis=0)
```

```python
import math
import numpy as np
from contextlib import ExitStack
import concourse.bass as bass
import concourse.mybir as mybir
from concourse.bass_utils import run_bass_kernel_spmd

F32 = mybir.dt.float32
F32R = mybir.dt.float32r
AF = mybir.ActivationFunctionType
ALU = mybir.AluOpType
AX = mybir.AxisListType
ALLENG = ("tensor", "vector", "scalar", "gpsimd", "sync")

D = 1024
SEQ = 2048
CTX = 256
NT = 18
NTOK = SEQ + CTX
DEPTH = 2
D_IN = 7040
O_AQ, O_AK, O_AV, O_BZ, O_CQ, O_CK, O_CV, O_DQ, O_DK, O_DV, O_G = 0, 256, 384, 512, 1408, 1664, 1920, 2176, 2432, 2688, 2944
EPS = 1e-6
NE = 16
DE = 512


import re as _re
_PSUM_RE = _re.compile(r"(^E_p[GUY]|^M_pr|^pm\d|^ptr\d|^pz\d|_ptr|_pS|_pO|_pl\d|_pd\d|_pag|_pb\d|^pq\d|^M_p)")


def is_psum_key(k):
    return isinstance(k, str) and _PSUM_RE.search(k) is not None


class Sched:
    def __init__(self, nc):
        self.nc = nc
        self.seq = {e: 0 for e in ALLENG}
        self.res = {}
        self.waited = {e: {} for e in ALLENG}
        self.dma_cnt = {}
        self.out_tokens = []
        self.n_dma_sems = 0
        self.n_sw_sems = 0
        self.dma_sem_of = {}
        self.sems = {}
        self.count = {e: 0 for e in ALLENG}

    def _need(self, eng, tok, waits):
        if tok is None:
            return
        semkey, val, _ = tok
        w = self.waited[eng]
        if w.get(semkey, 0) >= val:
            return
        w[semkey] = val
        waits.append((semkey, val))

    def _deps(self, eng, rd, wr, is_dma):
        waits = []
        for k in rd:
            st = self.res.get(k)
            if st is None:
                continue
            wtok = st[0]
            if wtok is not None:
                self._need(eng, wtok, waits)
        for k in wr:
            st = self.res.get(k)
            if st is None:
                continue
            wtok = st[0]
            if wtok is not None:
                if is_dma or wtok[2] != eng or eng != "tensor":
                    self._need(eng, wtok, waits)
            for rtok in st[1].values():
                if is_dma or rtok[2] != eng or eng != "tensor":
                    self._need(eng, rtok, waits)
        return waits

    def _record(self, tok, rd, wr):
        for k in rd:
            st = self.res.setdefault(k, [None, {}])
            old = st[1].get(tok[0])
            if old is None or old[1] < tok[1]:
                st[1][tok[0]] = tok
        for k in wr:
            self.res[k] = [tok, {}]

    def _sem(self, semkey):
        h = self.sems.get(semkey)
        if h is None:
            h = self.nc.alloc_semaphore("sem_%s_%s" % semkey)
            self.sems[semkey] = h
        return h

    def _emit(self, eng, waits, fn, inc):
        e = getattr(self.nc, eng)
        for semkey, val in waits:
            e.wait_ge(self._sem(semkey), val)
        ins = fn(e)
        ins.then_inc(self._sem(inc[0]), inc[1])
        self.count[eng] += 1

    def op(self, eng, fn, rd=(), wr=()):
        pr = [k for k in rd if is_psum_key(k)]
        if pr:
            rd = [k for k in rd if not is_psum_key(k)]
            wr = list(wr) + pr
        waits = self._deps(eng, rd, wr, False)
        self.seq[eng] += 1
        tok = (("eng", eng), self.seq[eng], eng)
        self._emit(eng, waits, fn, (("eng", eng), 1))
        self._record(tok, rd, wr)
        return tok

    def dma(self, fn, rd=(), wr=(), q="sync", semres=None, is_output=False):
        waits = self._deps(q, rd, wr, True)
        key = semres if semres is not None else (wr[0] if wr else rd[0])
        semkey = self.dma_sem_of.get((q, key))
        if semkey is None:
            if q == "gpsimd":
                semkey = ("dma", 74 + self.n_sw_sems % 20)
                self.n_sw_sems += 1
            else:
                semkey = ("dma", self.n_dma_sems % 74)
                self.n_dma_sems += 1
            self.dma_sem_of[(q, key)] = semkey
        self.dma_cnt[semkey] = self.dma_cnt.get(semkey, 0) + 16
        tok = (semkey, self.dma_cnt[semkey], None)
        self._emit(q, waits, fn, (semkey, 16))
        self._record(tok, rd, wr)
        if is_output:
            self.out_tokens.append(tok)
        return tok

    def barrier(self):
        toks = [(("eng", e), self.seq[e], e) for e in ALLENG if self.seq[e] > 0]
        toks += [(k, v, None) for k, v in self.dma_cnt.items()]
        for e in ALLENG:
            waits = []
            for t in toks:
                if t[2] == e:
                    continue
                self._need(e, t, waits)
            eo = getattr(self.nc, e)
            for semkey, val in waits:
                eo.wait_ge(self._sem(semkey), val)

    def finish(self):
        fin = []
        for tok in self.out_tokens:
            self._need("sync", tok, fin)
        for semkey, val in fin:
            self.nc.sync.wait_ge(self._sem(semkey), val)
        return dict(self.count)


class Rot:
    def __init__(self, items):
        self.items = list(items)
        self.i = 0

    def next(self):
        it = self.items[self.i % len(self.items)]
        self.i += 1
        return it


class KB:
    def __init__(self, debug=()):
        self.nc = bass.Bass("TRN2", target_bir_lowering=False)
        self.S = Sched(self.nc)
        self.debug = set(debug)
        self.ins = {}
        self.uid = 0

    def un(self, name):
        self.uid += 1
        return "%s_u%d" % (name, self.uid)

    def inp(self, name, shape):
        t = self.nc.dram_tensor(name, list(shape), F32, kind="ExternalInput").ap()
        self.ins[name] = t
        return t

    def scratch(self, name, shape):
        kind = "ExternalOutput" if name in self.debug else "Internal"
        return self.nc.dram_tensor(name, list(shape), F32, kind=kind).ap()

    def dma(self, out, in_, rd, wr, q="sync", is_output=False, semres=None):
        return self.S.dma(lambda e: e.dma_start(out=out, in_=in_), rd=rd, wr=wr, q=q, is_output=is_output, semres=semres)

    def mm(self, out, lhsT, rhs, start, stop, rd, wr):
        return self.S.op("tensor", lambda e: e.matmul(out=out, lhsT=lhsT, rhs=rhs, start=start, stop=stop), rd=rd, wr=wr)

    def tr(self, out, in_, ident, rd, wr):
        return self.S.op("tensor", lambda e: e.transpose(out=out, in_=in_, identity=ident), rd=list(rd) + ["ident"], wr=wr)

    def act(self, out, in_, func, rd, wr, bias=None, scale=None, accum_out=None):
        kw = {}
        if bias is not None:
            kw["bias"] = bias
        if scale is not None:
            kw["scale"] = scale
        if accum_out is not None:
            kw["accum_out"] = accum_out
        return self.S.op("scalar", lambda e: e.activation(out=out, in_=in_, func=func, **kw), rd=rd, wr=wr)

    def tt(self, out, a, b, op, rd, wr, eng="vector"):
        return self.S.op(eng, lambda e: e.tensor_tensor(out=out, in0=a, in1=b, op=op), rd=rd, wr=wr)

    def ts(self, out, a, s1, s2, op0, op1, rd, wr, eng="vector"):
        if op1 is None:
            return self.S.op(eng, lambda e: e.tensor_scalar(out=out, in0=a, scalar1=s1, scalar2=None, op0=op0), rd=rd, wr=wr)
        return self.S.op(eng, lambda e: e.tensor_scalar(out=out, in0=a, scalar1=s1, scalar2=s2, op0=op0, op1=op1), rd=rd, wr=wr)

    def stt(self, out, a, s, b, op0, op1, rd, wr):
        return self.S.op("vector", lambda e: e.scalar_tensor_tensor(out=out, in0=a, scalar=s, in1=b, op0=op0, op1=op1), rd=rd, wr=wr)

    def red(self, out, in_, op, rd, wr, axis=AX.X):
        return self.S.op("vector", lambda e: e.tensor_reduce(out=out, in_=in_, axis=axis, op=op), rd=rd, wr=wr)

    def cp(self, out, in_, rd, wr, eng="vector"):
        if eng == "scalar":
            return self.act(out, in_, AF.Copy, rd, wr)
        return self.S.op(eng, lambda e: e.tensor_copy(out=out, in_=in_), rd=rd, wr=wr)

    def recip(self, out, in_, rd, wr):
        return self.S.op("vector", lambda e: e.reciprocal(out=out, in_=in_), rd=rd, wr=wr)

    def memset(self, out, val, wr, eng="vector"):
        return self.S.op(eng, lambda e: e.memset(out, val), rd=(), wr=wr)

    def rstd(self, out, ss, n, eps, rd, wr):
        self.ts(out, ss, 1.0 / n, eps, ALU.mult, ALU.add, rd=rd, wr=wr)
        self.act(out, out, AF.Sqrt, rd=wr, wr=wr)
        self.recip(out, out, rd=wr, wr=wr)


def pipeline(stages, items):
    n = len(items)
    for it in range(n + len(stages) - 1):
        for s_, f in enumerate(stages):
            j = it - s_
            if 0 <= j < n:
                f(items[j])


def qk_prep(kb, es, l, Z, ident, zcol0, G, gd, nq_groups, gain_ap, rope_ap, tiles, store):
    nc = kb.nc
    W = G * gd
    NB = 6
    sb = lambda n, s, dt=F32: es.enter_context(nc.sbuf_tensor(kb.un(n), s, dt))
    gain = sb("qk_gain", [128, W])
    kb.dma(gain[:, 0:nq_groups * gd].rearrange("p (g d) -> p g d", d=gd),
           gain_ap[l, 0:1, :].to_broadcast([128, gd]).unsqueeze(1).to_broadcast([128, nq_groups, gd]), rd=(), wr=["qk_gain"])
    kb.dma(gain[:, nq_groups * gd:W].rearrange("p (g d) -> p g d", d=gd),
           gain_ap[l, 1:2, :].to_broadcast([128, gd]).unsqueeze(1).to_broadcast([128, G - nq_groups, gd]), rd=(), wr=["qk_gain"])
    zt = [sb("qk_zt%d" % i, [128, W]) for i in range(NB)]
    sq = [sb("qk_sq%d" % i, [128, W]) for i in range(NB)]
    ssq = [sb("qk_ss%d" % i, [128, G]) for i in range(NB)]
    xn = [sb("qk_xn%d" % i, [128, W]) for i in range(NB)]
    if rope_ap is not None:
        rp = [sb("qk_rp%d" % i, [128, 2, W]) for i in range(NB)]
        t1 = [sb("qk_t1%d" % i, [128, W]) for i in range(NB)]
        t2 = [sb("qk_t2%d" % i, [128, W]) for i in range(NB)]
    q4 = gd // 4
    v3 = lambda ap: ap.rearrange("p (g d) -> p g d", d=gd)

    def s0(i):
        b = i % NB
        zk, sk = "qk_zt%d" % b, "qk_ss%d" % b
        kb.dma(zt[b][:], Z[i * 128:(i + 1) * 128, zcol0:zcol0 + W], rd=(), wr=[zk])
        if rope_ap is not None and i < 16:
            kb.dma(rp[b][:], rope_ap[i * 128:(i + 1) * 128], rd=(), wr=["qk_rp%d" % b])
        kb.tt(sq[b][:], zt[b][:], zt[b][:], ALU.mult, rd=[zk], wr=["qk_sq%d" % b], eng="gpsimd")
        kb.red(ssq[b][:], v3(sq[b][:]), ALU.add, rd=["qk_sq%d" % b], wr=[sk])
        kb.ts(ssq[b][:], ssq[b][:], 1.0 / gd, EPS, ALU.mult, ALU.add, rd=[sk], wr=[sk])
        kb.act(ssq[b][:], ssq[b][:], AF.Sqrt, rd=[sk], wr=[sk])

    def s1(i):
        b = i % NB
        zk, sk, xk = "qk_zt%d" % b, "qk_ss%d" % b, "qk_xn%d" % b
        kb.recip(ssq[b][:], ssq[b][:], rd=[sk], wr=[sk])
        kb.tt(v3(xn[b][:]), v3(zt[b][:]), ssq[b][:].unsqueeze(2).to_broadcast([128, G, gd]), ALU.mult, rd=[zk, sk], wr=[xk])
        kb.tt(xn[b][:], xn[b][:], gain[:], ALU.mult, rd=[xk, "qk_gain"], wr=[xk], eng="gpsimd")

    def s2(i):
        b = i % NB
        xk, rk = "qk_xn%d" % b, "qk_rp%d" % b
        if rope_ap is not None and i < 16:
            kb.tt(t1[b][:], xn[b][:], rp[b][:, 0, :], ALU.mult, rd=[xk, rk], wr=["qk_t1%d" % b])
            xv = xn[b][:].rearrange("p (x h d) -> p x h d", h=2, d=q4)
            sv = rp[b][:, 1, :].rearrange("p (x h d) -> p x h d", h=2, d=q4)
            tv = t2[b][:].rearrange("p (x h d) -> p x h d", h=2, d=q4)
            kb.tt(tv[:, :, 0, :], xv[:, :, 1, :], sv[:, :, 0, :], ALU.mult, rd=[xk, rk], wr=[("qk_t2a", b)], eng="gpsimd")
            kb.tt(tv[:, :, 1, :], xv[:, :, 0, :], sv[:, :, 1, :], ALU.mult, rd=[xk, rk], wr=[("qk_t2b", b)], eng="gpsimd")

    def s3(i):
        b = i % NB
        xk = "qk_xn%d" % b
        if rope_ap is not None and i < 16:
            kb.tt(xn[b][:], t1[b][:], t2[b][:], ALU.add, rd=["qk_t1%d" % b, ("qk_t2a", b), ("qk_t2b", b)], wr=[xk])
        store(i, xn[b], xk)

    pipeline([s0, s1, s2, s3], list(tiles))


def load_v(kb, V1, vkey, Z, col0, H, row0, ntile, raw, rawkey):
    nt_, h_ = V1.shape[1], V1.shape[2]
    kb.cp(V1[:, :, :, 64:66], kb.onez[:, 0:2].unsqueeze(1).unsqueeze(1).to_broadcast([128, nt_, h_, 2]), rd=["onez"], wr=[(vkey, "ones")])
    kb.dma(raw[:, 0:ntile, 0:64 * H], Z[row0:row0 + ntile * 128, col0:col0 + 64 * H].rearrange("(t p) c -> p t c", p=128), rd=(), wr=[rawkey])
    hlf = ntile // 2
    kb.cp(V1[:, 0:hlf, :, 0:64], raw[:, 0:hlf, 0:64 * H].rearrange("p t (h d) -> p t h d", d=64), rd=[rawkey], wr=[(vkey, "a")], eng="vector")
    kb.cp(V1[:, hlf:ntile, :, 0:64], raw[:, hlf:ntile, 0:64 * H].rearrange("p t (h d) -> p t h d", d=64), rd=[rawkey], wr=[(vkey, "b")], eng="scalar")


def attn_finish(kb, po, pok, H, npart, extra_den, yt, ytk, dst, rec, reck):
    if extra_den is not None:
        kb.tt(rec[0:npart, :], po[0:npart, :, 64], extra_den[0:npart, :], ALU.add, rd=[pok, "expsink"], wr=[reck])
    else:
        kb.cp(rec[0:npart, :], po[0:npart, :, 64], rd=[pok], wr=[reck])
    kb.recip(rec[0:npart, :], rec[0:npart, :], rd=[reck], wr=[reck])
    kb.tt(yt[0:npart, :, :], po[0:npart, :, 0:64], rec[0:npart, :].unsqueeze(2).to_broadcast([npart, H, 64]), ALU.mult,
          rd=[pok, reck], wr=[ytk])
    kb.dma(dst, yt[0:npart, :, :].rearrange("p h d -> p (h d)"), rd=[ytk], wr=[("YB", ytk)], semres=ytk)


def attn_A(kb, l, need_ctx, Z, YB, ident, C):
    nc = kb.nc
    with ExitStack() as es:
        sb = lambda n, s, dt=F32: es.enter_context(nc.sbuf_tensor(kb.un(n), s, dt))
        ps = lambda n, s, dt=F32: es.enter_context(nc.psum_tensor(kb.un(n), s, dt))
        qT = sb("A_qT", [64, 4, NTOK], F32R)
        kT = sb("A_kT", [64, 2, NTOK], F32R)
        V1 = sb("A_V1", [128, NT, 2, 66], F32R)
        vraw = sb("A_vraw", [128, NT, 128])
        load_v(kb, V1, "A_V1", Z, O_AV, 2, 0, NT, vraw, "A_vraw")
        bm = sb("A_bm", [128, 2, 128])
        kb.dma(bm[:], C["bandmask"].rearrange("m p q -> p m q"), rd=(), wr=["A_bm"])
        expsink = sb("A_es", [128, 4])
        kb.dma(expsink[:], C["a_sink"][l:l + 1, :].to_broadcast([128, 4]), rd=(), wr=["expsink"])
        kb.act(expsink[:], expsink[:], AF.Exp, rd=["expsink"], wr=["expsink"])
        with ExitStack() as es2:
            ptr = [es2.enter_context(nc.psum_tensor(kb.un("A_ptr%d" % i), [64, 6, 128], F32)) for i in range(2)]

            def store(i, xn, xk):
                p = ptr[i % 2]
                pk = "A_ptr%d" % (i % 2)
                for g in range(6):
                    kb.tr(p[:, g, :], xn[:, g * 64:(g + 1) * 64], ident[:], rd=[xk], wr=[pk])
                kb.cp(qT[:, :, i * 128:(i + 1) * 128], p[:, 0:4, :], rd=[pk], wr=[("A_qT", i)], eng="scalar")
                kb.cp(kT[:, :, i * 128:(i + 1) * 128], p[:, 4:6, :], rd=[pk], wr=[("A_kT", i)], eng="vector")
            qk_prep(kb, es2, l, Z, ident, O_AQ, 6, 64, 4, C["a_qk_norm"], C["ropeA"], range(NT), store)
        kb.S.barrier()
        pS = [ps("A_pS%d" % i, [128, 6, 256]) for i in range(2)]
        pO = [ps("A_pO%d" % i, [128, 4, 66]) for i in range(2)]
        pT = [sb("A_pT%d" % i, [128, 5, 2, 128], F32R) for i in range(2)]
        yt = [sb("A_yt%d" % i, [128, 4, 64]) for i in range(2)]
        rec = [sb("A_rec%d" % i, [128, 4]) for i in range(2)]
        qtiles = list(range(16)) + ([16, 17] if need_ctx else [])
        items = [(n, i, g) for n, i in enumerate(qtiles) for g in range(2)]

        def ents_of(i):
            if i < 16:
                e_ = [(j, (0 if j == i - 1 else (1 if j == i + 1 else None))) for j in (i - 1, i, i + 1) if 0 <= j < 16]
                return e_ + [(16, None), (17, None)]
            return [(16, None), (17, None)]

        def qk(it):
            n, i, g = it
            b = (2 * n + g) % 2
            ents = ents_of(i)
            E = len(ents)
            psk, ptk = "A_pS%d" % b, "A_pT%d" % b
            for e, (j, mk) in enumerate(ents):
                kb.mm(pS[b][:, e, :], kT[:, g, j * 128:(j + 1) * 128], qT[:, 2 * g:2 * g + 2, i * 128:(i + 1) * 128], True, True,
                      rd=[("A_kT", j), ("A_qT", i)], wr=[psk])
            kb.act(pT[b][:, 0:E].rearrange("p e h q -> p e (h q)"), pS[b][:, 0:E, :], AF.Exp, rd=[psk], wr=[ptk], scale=0.125)
            for e, (j, mk) in enumerate(ents):
                if mk is not None:
                    kb.tt(pT[b][:, e], pT[b][:, e], bm[:, mk:mk + 1, :].to_broadcast([128, 2, 128]), ALU.mult, rd=[ptk, "A_bm"], wr=[ptk])

        def pv(it):
            n, i, g = it
            b = (2 * n + g) % 2
            ents = ents_of(i)
            E = len(ents)
            ptk = "A_pT%d" % b
            po = pO[n % 2]
            pok = "A_pO%d" % (n % 2)
            for hh in range(2):
                h = 2 * g + hh
                for e, (j, mk) in enumerate(ents):
                    kb.mm(po[:, h, :], pT[b][:, e, hh, :], V1[:, j, g, :], e == 0, e == E - 1, rd=[ptk, "A_V1"], wr=[pok])
            if g == 1:
                attn_finish(kb, po, pok, 4, 128, expsink, yt[n % 2], "A_yt%d" % (n % 2), YB[i * 128:(i + 1) * 128, 0:256], rec[n % 2], "A_rec%d" % (n % 2))

        pipeline([qk, pv], items)


def attn_D(kb, l, need_ctx, Z, YB, ident, C):
    nc = kb.nc
    with ExitStack() as es:
        sb = lambda n, s, dt=F32: es.enter_context(nc.sbuf_tensor(kb.un(n), s, dt))
        ps = lambda n, s, dt=F32: es.enter_context(nc.psum_tensor(kb.un(n), s, dt))
        qT = sb("D_qT", [64, 4, NTOK], F32R)
        kT = sb("D_kT", [64, 4, NTOK], F32R)
        V1 = sb("D_V1", [128, NT, 4, 66], F32R)
        V1o = sb("D_V1o", [128, 15, 4, 66], F32R)
        vraw = sb("D_vraw", [128, NT, 256])
        load_v(kb, V1, "D_V1", Z, O_DV, 4, 0, NT, vraw, "D_vraw")
        load_v(kb, V1o, "D_V1o", Z, O_DV, 4, 64, 15, vraw, "D_vraw")
        Eb = sb("D_E", [128, 128, 64])
        mD = sb("D_mD", [128, 64])
        kb.dma(Eb[:].rearrange("p a q -> p (a q)"), C["biasG"][l], rd=(), wr=["D_E"])
        kb.dma(mD[:], C["maskD"], rd=(), wr=["D_mD"])
        kb.act(Eb[:], Eb[:], AF.Exp, rd=["D_E"], wr=["D_E"])
        kb.tt(Eb[:], Eb[:], mD[:].unsqueeze(1).to_broadcast([128, 128, 64]), ALU.mult, rd=["D_E", "D_mD"], wr=["D_E"])
        with ExitStack() as es2:
            ptr = [es2.enter_context(nc.psum_tensor(kb.un("D_ptr%d" % i), [64, 8, 128], F32)) for i in range(2)]

            def store(i, xn, xk):
                p = ptr[i % 2]
                pk = "D_ptr%d" % (i % 2)
                for g in range(8):
                    kb.tr(p[:, g, :], xn[:, g * 64:(g + 1) * 64], ident[:], rd=[xk], wr=[pk])
                kb.cp(qT[:, :, i * 128:(i + 1) * 128], p[:, 0:4, :], rd=[pk], wr=[("D_qT", i)], eng="scalar")
                kb.cp(kT[:, :, i * 128:(i + 1) * 128], p[:, 4:8, :], rd=[pk], wr=[("D_kT", i)], eng="vector")
            qk_prep(kb, es2, l, Z, ident, O_DQ, 8, 64, 4, C["d_qk_norm"], None, range(NT), store)
        kb.S.barrier()
        pS = [ps("D_pS%d" % i, [128, 4, 6, 64]) for i in range(2)]
        pO = [ps("D_pO%d" % i, [128, 4, 66]) for i in range(2)]
        pT = [sb("D_pT%d" % i, [128, 4, 6, 64], F32R) for i in range(2)]
        ptmp = [sb("D_ptmp%d" % i, [128, 4, 4, 64]) for i in range(2)]
        yt = [sb("D_yt%d" % i, [128, 4, 64]) for i in range(2)]
        rec = [sb("D_rec%d" % i, [128, 4]) for i in range(2)]
        def d_qk(r):
            start = min(max(r - 4, 0), 24)
            cls = r if r < 4 else (4 if r <= 28 else r - 24)
            b = r % 2
            psk, ptk, tmk = "D_pS%d" % b, "D_pT%d" % b, "D_ptmp%d" % b
            for h in range(4):
                qs = qT[:, h, r * 64:(r + 1) * 64]
                for t in range(4):
                    t0 = (start + 2 * t) * 64
                    kb.mm(pS[b][:, h, t, :], kT[:, h, t0:t0 + 128], qs, True, True, rd=(), wr=[psk])
                for t in range(2):
                    kb.mm(pS[b][:, h, 4 + t, :], kT[:, h, SEQ + t * 128:SEQ + (t + 1) * 128], qs, True, True, rd=(), wr=[psk])
            kb.act(ptmp[b][:], pS[b][:, :, 0:4, :], AF.Exp, rd=[psk], wr=[tmk], scale=0.125)
            kb.act(pT[b][:, :, 4:6, :], pS[b][:, :, 4:6, :], AF.Exp, rd=[psk], wr=[(ptk, "c")], scale=0.125)
            kb.tt(pT[b][:, :, 0:4, :], ptmp[b][:], Eb[:, cls * 16:(cls + 1) * 16, :].rearrange("p (h t) q -> p h t q", h=4), ALU.mult,
                  rd=[tmk, "D_E"], wr=[(ptk, "l")])

        def d_pv(r):
            start = min(max(r - 4, 0), 24)
            b = r % 2
            po = pO[b]
            pok = "D_pO%d" % b
            ptk = "D_pT%d" % b
            for h in range(4):
                for t in range(4):
                    row = start + 2 * t
                    vt = V1[:, row // 2, h, :] if row % 2 == 0 else V1o[:, (row - 1) // 2, h, :]
                    kb.mm(po[0:64, h, :], pT[b][:, h, t, :], vt, t == 0, False, rd=[(ptk, "l")], wr=[pok])
                for t in range(2):
                    kb.mm(po[0:64, h, :], pT[b][:, h, 4 + t, :], V1[:, 16 + t, h, :], False, t == 1, rd=[(ptk, "c")], wr=[pok])
            attn_finish(kb, po, pok, 4, 64, None, yt[b], "D_yt%d" % b, YB[r * 64:(r + 1) * 64, 768:1024], rec[b], "D_rec%d" % b)

        pipeline([d_qk, d_pv], list(range(32)))
        kb.S.barrier()
        if need_ctx:
            pT2 = [sb("D_pTc%d" % i, [128, 2, 128], F32R) for i in range(2)]
            pSc = [pS[i][:, 0, 0:4, :].rearrange("p (a b) d -> p a (b d)", a=2) for i in range(2)]
            k = 0
            for n, i in enumerate((16, 17)):
                po = pO[n % 2]
                pok = "D_pO%d" % (n % 2)
                for h in range(4):
                    b = k % 2
                    k += 1
                    psk, ptk = "D_pS%d" % b, "D_pTc%d" % b
                    for t in range(2):
                        kb.mm(pSc[b][:, t, :], kT[:, h, SEQ + t * 128:SEQ + (t + 1) * 128], qT[:, h, i * 128:(i + 1) * 128], True, True,
                              rd=["D_kTall", "D_qTall"], wr=[psk])
                    kb.act(pT2[b][:], pSc[b], AF.Exp, rd=[psk], wr=[ptk], scale=0.125)
                    for t in range(2):
                        kb.mm(po[:, h, :], pT2[b][:, t, :], V1[:, 16 + t, h, :], t == 0, t == 1, rd=[ptk, "D_V1"], wr=[pok])
                attn_finish(kb, po, pok, 4, 128, None, yt[n % 2], "D_yt%d" % (n % 2), YB[i * 128:(i + 1) * 128, 768:1024], rec[n % 2], "D_rec%d" % (n % 2))


def attn_C(kb, l, need_ctx, Z, YB, ident, C):
    nc = kb.nc
    lam_init = 0.8 - 0.6 * math.exp(-0.3 * l)
    with ExitStack() as es:
        sb = lambda n, s, dt=F32: es.enter_context(nc.sbuf_tensor(kb.un(n), s, dt))
        ps = lambda n, s, dt=F32: es.enter_context(nc.psum_tensor(kb.un(n), s, dt))
        qT = sb("C_qT", [128, 3, NTOK], F32R)
        kT = sb("C_kT", [128, 3, NTOK], F32R)
        V1 = sb("C_V1", [128, NT, 4, 66], F32R)
        lv = sb("C_lv", [128, 4, 32])
        lp = sb("C_lp", [128, 2, 32])
        ls = sb("C_ls", [128, 2])
        nlam = sb("C_nlam", [128, 1])
        kb.dma(lv[:].rearrange("p a d -> p (a d)"), C["c_lambda"][l:l + 1].rearrange("o a d -> o (a d)").to_broadcast([128, 128]), rd=(), wr=["C_lv"])
        lvv = lv[:].rearrange("p (a b) d -> p a b d", b=2)
        kb.tt(lp[:], lvv[:, :, 0, :], lvv[:, :, 1, :], ALU.mult, rd=["C_lv"], wr=["C_lp"])
        kb.red(ls[:], lp[:], ALU.add, rd=["C_lp"], wr=["C_ls"])
        kb.act(ls[:], ls[:], AF.Exp, rd=["C_ls"], wr=["C_ls"])
        kb.tt(nlam[:], ls[:, 1:2], ls[:, 0:1], ALU.subtract, rd=["C_ls"], wr=["C_nlam"])
        kb.ts(nlam[:], nlam[:], -lam_init, None, ALU.add, None, rd=["C_nlam"], wr=["C_nlam"])
        sg = sb("C_sg", [128, 64])
        kb.dma(sg[:], C["c_subln"][l:l + 1, :].to_broadcast([128, 64]), rd=(), wr=["C_sg"])
        kb.ts(sg[:], sg[:], 1.0 - lam_init, None, ALU.mult, None, rd=["C_sg"], wr=["C_sg"])
        with ExitStack() as es2:
            vraw = es2.enter_context(nc.sbuf_tensor(kb.un("C_vraw"), [128, NT, 256], F32))
            load_v(kb, V1, "C_V1", Z, O_CV, 4, 0, NT, vraw, "C_vraw")
            ptrq = [es2.enter_context(nc.psum_tensor(kb.un("C_ptrq%d" % i), [128, 3, 128], F32)) for i in range(2)]
            ptrk = [es2.enter_context(nc.psum_tensor(kb.un("C_ptrk%d" % i), [128, 3, 128], F32)) for i in range(2)]

            def store(i, xn, xk):
                for (pp, pk, dst, dk, c0, eng) in ((ptrq[i % 2], "C_ptrq%d" % (i % 2), qT, "C_qT", 0, "scalar"),
                                                   (ptrk[i % 2], "C_ptrk%d" % (i % 2), kT, "C_kT", 256, "vector")):
                    for idx in range(3):
                        ncol = 96 if idx < 2 else 64
                        kb.tr(pp[0:ncol, idx, :], xn[:, c0 + idx * 96:c0 + idx * 96 + ncol], ident[:], rd=[xk], wr=[pk])
                    kb.cp(dst[0:96, 0:2, i * 128:(i + 1) * 128], pp[0:96, 0:2, :], rd=[pk], wr=[(dk, i)], eng=eng)
                    kb.cp(dst[0:64, 2, i * 128:(i + 1) * 128], pp[0:64, 2, :], rd=[pk], wr=[(dk, i)], eng=eng)
            qk_prep(kb, es2, l, Z, ident, O_CQ, 16, 32, 8, C["c_qk_norm"], C["ropeC"], range(NT), store)
        kb.S.barrier()
        pS = [ps("C_pS%d" % i, [128, 512]) for i in range(4)]
        pOT = [ps("C_pOT%d" % i, [128, 512]) for i in range(2)]
        pO2 = ps("C_pO2", [128, 4, 66])
        oT = [sb("C_oT%d" % i, [66, 512]) for i in range(2)]
        pTs = [sb("C_pT%d" % i, [128, NT, 512], F32R) for i in range(2)]
        ocs = [sb("C_oc%d" % i, [128, 4, 8, 66]) for i in range(2)]
        rec = sb("C_rec", [128, 4, 8])
        on = sb("C_on", [128, 4, 8, 64])
        od = sb("C_od", [128, 4, 4, 64])
        osq = sb("C_osq", [128, 4, 4, 64])
        oss = sb("C_oss", [128, 4, 4])
        yts = [sb("C_yt%d" % i, [128, 4, 4, 64]) for i in range(2)]
        blocks = [(qb * 512, 512, list(range(NT))) for qb in range(4)]
        if need_ctx:
            blocks.append((SEQ, 256, [16, 17]))
        ks = 0
        ko = 0
        scale = 32 ** -0.5
        pending = []

        def c_finish(q0, nq, nqt, bi):
            ob = ocs[bi % 2]
            okk = [("C_oc", bi % 2, qt_) for qt_ in range(nqt)]
            kb.recip(rec[:, 0:nqt, :], ob[:, 0:nqt, :, 64], rd=okk, wr=["C_rec"])
            kb.tt(on[:, 0:nqt], ob[:, 0:nqt, :, 0:64], rec[:, 0:nqt, :].unsqueeze(3).to_broadcast([128, nqt, 8, 64]), ALU.mult, rd=okk + ["C_rec"], wr=["C_on"])
            onv = on[:, 0:nqt].rearrange("p q (h m) d -> p q h m d", m=2)
            kb.stt(od[:, 0:nqt], onv[:, :, :, 1, :], nlam[:, 0:1], onv[:, :, :, 0, :], ALU.mult, ALU.add, rd=["C_on", "C_nlam"], wr=["C_od"])
            kb.tt(osq[:, 0:nqt], od[:, 0:nqt], od[:, 0:nqt], ALU.mult, rd=["C_od"], wr=["C_osq"], eng="gpsimd")
            kb.red(oss[:, 0:nqt, :], osq[:, 0:nqt], ALU.add, rd=["C_osq"], wr=["C_oss"])
            kb.rstd(oss[:, 0:nqt, :], oss[:, 0:nqt, :], 64, EPS, rd=["C_oss"], wr=["C_oss"])
            y = yts[bi % 2]
            yk = "C_yt%d" % (bi % 2)
            kb.tt(y[:, 0:nqt], od[:, 0:nqt], oss[:, 0:nqt, :].unsqueeze(3).to_broadcast([128, nqt, 4, 64]), ALU.mult, rd=["C_od", "C_oss"], wr=[yk])
            kb.tt(y[:, 0:nqt], y[:, 0:nqt], sg[:].unsqueeze(1).unsqueeze(1).to_broadcast([128, nqt, 4, 64]), ALU.mult, rd=[yk, "C_sg"], wr=[yk], eng="gpsimd")
            kb.dma(YB[q0:q0 + nq, 512:768].rearrange("(q p) c -> p q c", p=128), y[:, 0:nqt].rearrange("p q h d -> p q (h d)"), rd=[yk], wr=[("YB", yk)], semres=yk)

        for blk_i, (q0, nq, ktl) in enumerate(blocks):
            nqt = nq // 128
            oc = ocs[blk_i % 2]
            def pv_step(gp, e, j):
                hp = gp // 2
                bp = gp % 2
                kb.mm(pOT[bp][0:66, 0:nq], V1[:, j, hp, :], pTs[bp][:, e, 0:nq], e == 0, e == len(ktl) - 1,
                      rd=[("C_pT", bp, e), "C_V1"], wr=["C_pOT%d" % bp])

            def pv_finish(gp):
                bp = gp % 2
                kb.cp(oT[bp][:, 0:nq], pOT[bp][0:66, 0:nq], rd=["C_pOT%d" % bp], wr=["C_oT%d" % bp], eng="vector")
                for qt in range(nqt):
                    kb.tr(pO2[:, qt, :], oT[bp][:, qt * 128:(qt + 1) * 128], ident[0:66, 0:66], rd=["C_oT%d" % bp], wr=["C_pO2"])
                kb.cp(oc[:, 0:nqt, gp, :], pO2[:, 0:nqt, :], rd=["C_pO2"], wr=[("C_oc", blk_i % 2, qt_) for qt_ in range(nqt)], eng="vector")

            for g in range(8):
                h, s4, hh = g // 2, g % 3, g // 3
                bg = g % 2
                ents_ = list(enumerate(ktl))
                for c0 in range(0, len(ents_), 4):
                    for e, j in ents_[c0:c0 + 4]:
                        b = ks % 4
                        ks += 1
                        kb.mm(pS[b][:, 0:nq], kT[32 * s4:32 * s4 + 32, hh, j * 128:(j + 1) * 128], qT[32 * s4:32 * s4 + 32, hh, q0:q0 + nq], True, True,
                              rd=(), wr=["C_pS%d" % b])
                        kb.act(pTs[bg][:, e, 0:nq], pS[b][:, 0:nq], AF.Exp, rd=["C_pS%d" % b], wr=[("C_pT", bg, e)], scale=scale)
                    if g > 0:
                        for e, j in ents_[c0:c0 + 4]:
                            pv_step(g - 1, e, j)
                if g > 0:
                    pv_finish(g - 1)
                if g == 2 and pending:
                    c_finish(*pending.pop(0))
            for e, j in enumerate(ktl):
                pv_step(7, e, j)
            pv_finish(7)
            pending.append((q0, nq, nqt, blk_i))
        while pending:
            c_finish(*pending.pop(0))


def rwkv(kb, l, need_ctx, Z, YB, RW, ident, C):
    nc = kb.nc
    S = kb.S
    NEG = -math.exp(-0.5)
    with ExitStack() as es:
        sb = lambda n, s, dt=F32: es.enter_context(nc.sbuf_tensor(kb.un(n), s, dt))
        ps = lambda n, s, dt=F32: es.enter_context(nc.psum_tensor(kb.un(n), s, dt))
        mu = sb("R_mu", [128, 3, 896])
        kb.dma(mu[:, 0:2, :], C["b_shift"][l:l + 1].to_broadcast([128, 2, 896]), rd=(), wr=["R_mu"])
        kb.tt(mu[:, 2, :], mu[:, 0, :], mu[:, 1, :], ALU.add, rd=["R_mu"], wr=["R_mu"])
        kb.ts(mu[:, 2, :], mu[:, 2, :], -1.0, 1.0, ALU.mult, ALU.add, rd=["R_mu"], wr=["R_mu"])
        W2t = sb("R_W2t", [32, 512])
        A2t = sb("R_A2t", [32, 256])
        G2t = sb("R_G2t", [64, 256])
        kb.dma(W2t[:].rearrange("p (a n) -> p a n", a=2), C["b_w2"][l].rearrange("a p n -> p a n"), rd=(), wr=["R_Wl"])
        kb.dma(A2t[:], C["b_a2"][l], rd=(), wr=["R_Wl"])
        kb.dma(G2t[:], C["b_g2"][l], rd=(), wr=["R_Wl"])
        w0 = sb("R_w0", [128, 512])
        kb.dma(w0[:], C["b_w0"][l:l + 1].rearrange("o a n -> o (a n)").to_broadcast([128, 512]), rd=(), wr=["R_w0"])
        vb = sb("R_vb", [128, 3, 256])
        kb.dma(vb[:, 0, :], C["b_a0"][l:l + 1, :].to_broadcast([128, 256]), rd=(), wr=["R_vb"])
        kb.dma(vb[:, 1, :], C["b_kk"][l:l + 1, :].to_broadcast([128, 256]), rd=(), wr=["R_vb"])
        kb.dma(vb[:, 2, :], C["b_ka"][l:l + 1, :].to_broadcast([128, 256]), rd=(), wr=["R_vb"])
        NB = 6
        zc = [sb("R_zc%d" % i, [128, 896]) for i in range(2)]
        zp = [sb("R_zp%d" % i, [128, 896]) for i in range(2)]
        zn = [sb("R_zn%d" % i, [128, 896]) for i in range(2)]
        zs = [sb("R_zs%d" % i, [128, 896]) for i in range(NB)]
        lbT = [sb("R_lbT%d" % i, [64, 3, 128]) for i in range(NB)]
        rw = [sb("R_rw%d" % i, [128, 8, 256]) for i in range(NB)]
        tmp = [sb("R_tmp%d" % i, [128, 512]) for i in range(NB)]
        tmp2 = [sb("R_tmp2%d" % i, [128, 256]) for i in range(NB)]
        av = [sb("R_a%d" % i, [128, 256]) for i in range(NB)]
        ssk = [sb("R_ssk%d" % i, [128, 4]) for i in range(NB)]
        pl = [ps("R_pl%d" % i, [64, 3, 128]) for i in range(2)]
        pd = [ps("R_pd%d" % i, [128, 512]) for i in range(2)]
        pag = [ps("R_pag%d" % i, [128, 2, 256]) for i in range(2)]
        h3 = lambda ap: ap.rearrange("p (h d) -> p h d", d=64)

        def s0(i):
            b2, b = i % 2, i % NB
            kc, kp, kn, ks_ = "R_zc%d" % b2, "R_zp%d" % b2, "R_zn%d" % b2, "R_zs%d" % b
            r0 = i * 128
            kb.dma(zc[b2][:], Z[r0:r0 + 128, O_BZ:O_BZ + 896], rd=(), wr=[kc])
            if i in (0, 16):
                kb.memset(zp[b2][:], 0.0, wr=[kp])
                kb.dma(zp[b2][1:128, :], Z[r0:r0 + 127, O_BZ:O_BZ + 896], rd=(), wr=[kp])
            else:
                kb.dma(zp[b2][:], Z[r0 - 1:r0 + 127, O_BZ:O_BZ + 896], rd=(), wr=[kp])
            if i in (15, 17):
                kb.memset(zn[b2][:], 0.0, wr=[kn])
                kb.dma(zn[b2][0:127, :], Z[r0 + 1:r0 + 128, O_BZ:O_BZ + 896], rd=(), wr=[kn])
            else:
                kb.dma(zn[b2][:], Z[r0 + 1:r0 + 129, O_BZ:O_BZ + 896], rd=(), wr=[kn])
            kb.tt(zs[b][:], zc[b2][:], mu[:, 2, :], ALU.mult, rd=[kc, "R_mu"], wr=[ks_])
            kb.tt(zp[b2][:], zp[b2][:], mu[:, 0, :], ALU.mult, rd=[kp, "R_mu"], wr=[kp], eng="gpsimd")
            kb.tt(zn[b2][:], zn[b2][:], mu[:, 1, :], ALU.mult, rd=[kn, "R_mu"], wr=[kn], eng="gpsimd")

        def s1(i):
            b2, b = i % 2, i % NB
            kp, kn, ks_ = "R_zp%d" % b2, "R_zn%d" % b2, "R_zs%d" % b
            kb.tt(zs[b][:], zs[b][:], zp[b2][:], ALU.add, rd=[ks_, kp], wr=[ks_])
            kb.tt(zs[b][:], zs[b][:], zn[b2][:], ALU.add, rd=[ks_, kn], wr=[ks_])

        def s2(i):
            b2, b = i % 2, i % NB
            ks_, kl = "R_zs%d" % b, "R_lbT%d" % b
            z = zs[b]
            kb.tr(pl[b2][0:32, 0, :], z[:, 768:800], ident[:], rd=[ks_], wr=["R_pl%d" % b2])
            kb.tr(pl[b2][0:32, 1, :], z[:, 800:832], ident[:], rd=[ks_], wr=["R_pl%d" % b2])
            kb.tr(pl[b2][0:64, 2, :], z[:, 832:896], ident[:], rd=[ks_], wr=["R_pl%d" % b2])
            kb.act(lbT[b][0:32, 0, :], pl[b2][0:32, 0, :], AF.Tanh, rd=["R_pl%d" % b2], wr=[kl])
            kb.act(lbT[b][0:32, 1, :], pl[b2][0:32, 1, :], AF.Copy, rd=["R_pl%d" % b2], wr=[kl])
            kb.act(lbT[b][0:64, 2, :], pl[b2][0:64, 2, :], AF.Sigmoid, rd=["R_pl%d" % b2], wr=[kl])

        def s3(i):
            b2, b = i % 2, i % NB
            ks_, kl, kr = "R_zs%d" % b, "R_lbT%d" % b, "R_rw%d" % b
            z = zs[b]
            o = rw[b]
            kb.mm(pd[b2][:], lbT[b][0:32, 0, :], W2t[:], True, True, rd=[kl, "R_Wl"], wr=["R_pd%d" % b2])
            kb.mm(pag[b2][:, 0, :], lbT[b][0:32, 1, :], A2t[:], True, True, rd=[kl, "R_Wl"], wr=["R_pag%d" % b2])
            kb.mm(pag[b2][:, 1, :], lbT[b][0:64, 2, :], G2t[:], True, True, rd=[kl, "R_Wl"], wr=["R_pag%d" % b2])
            kb.cp(o[:, 0, :], z[:, 0:256], rd=[ks_], wr=[(kr, 0)], eng="gpsimd")
            kb.cp(o[:, 2, :], z[:, 512:768], rd=[ks_], wr=[(kr, 2)], eng="gpsimd")
            kb.tt(o[:, 3, :], z[:, 256:512], vb[:, 1, :], ALU.mult, rd=[ks_, "R_vb"], wr=[(kr, 3)])
            kb.tt(tmp2[b][:], o[:, 3, :], o[:, 3, :], ALU.mult, rd=[(kr, 3)], wr=["R_tmp2%d" % b])
            kb.red(ssk[b][:], h3(tmp2[b][:]), ALU.add, rd=["R_tmp2%d" % b], wr=["R_ssk%d" % b])
            kb.ts(ssk[b][:], ssk[b][:], 1e-24, None, ALU.max, None, rd=["R_ssk%d" % b], wr=["R_ssk%d" % b])

        def s4(i):
            b2, b = i % 2, i % NB
            kr = "R_rw%d" % b
            o = rw[b]
            kb.act(o[:, 7, :], pag[b2][:, 1, :], AF.Copy, rd=["R_pag%d" % b2], wr=[(kr, 7)])
            kb.tt(av[b][:], pag[b2][:, 0, :], vb[:, 0, :], ALU.add, rd=["R_pag%d" % b2, "R_vb"], wr=["R_a%d" % b])
            kb.tt(tmp[b][:], pd[b2][:], w0[:], ALU.add, rd=["R_pd%d" % b2, "R_w0"], wr=["R_tmp%d" % b])
            kb.act(av[b][:], av[b][:], AF.Sigmoid, rd=["R_a%d" % b], wr=["R_a%d" % b])
            kb.act(tmp[b][:], tmp[b][:], AF.Sigmoid, rd=["R_tmp%d" % b], wr=["R_tmp%d" % b])
            kb.act(ssk[b][:], ssk[b][:], AF.Sqrt, rd=["R_ssk%d" % b], wr=["R_ssk%d" % b])

        def s5(i):
            b2, b = i % 2, i % NB
            ks_, kr = "R_zs%d" % b, "R_rw%d" % b
            z = zs[b]
            o = rw[b]
            kb.ts(o[:, 5:7, :].rearrange("p a n -> p (a n)"), tmp[b][:], NEG, None, ALU.mult, None, rd=["R_tmp%d" % b], wr=[(kr, 5)], eng="gpsimd")
            kb.recip(ssk[b][:], ssk[b][:], rd=["R_ssk%d" % b], wr=["R_ssk%d" % b])
            kb.tt(h3(o[:, 3, :]), h3(o[:, 3, :]), ssk[b][:].unsqueeze(2).to_broadcast([128, 4, 64]), ALU.mult, rd=[(kr, 3), "R_ssk%d" % b], wr=[(kr, 3)])
            kb.tt(o[:, 4, :], o[:, 3, :], av[b][:], ALU.mult, rd=[(kr, 3), "R_a%d" % b], wr=[(kr, 4)], eng="gpsimd")
            kb.stt(tmp2[b][:], av[b][:], -1.0, vb[:, 2, :], ALU.add, ALU.mult, rd=["R_a%d" % b, "R_vb"], wr=["R_tmp2%d" % b])
            kb.stt(o[:, 1, :], tmp2[b][:], 1.0, z[:, 256:512], ALU.add, ALU.mult, rd=["R_tmp2%d" % b, ks_], wr=[(kr, 1)])

        def s6(i):
            b = i % NB
            kr = "R_rw%d" % b
            kb.dma(RW[i * 128:(i + 1) * 128], rw[b][:], rd=[(kr, j_) for j_ in (0, 1, 2, 3, 4, 5, 7)], wr=[("RW", i)], semres=kr)

        pipeline([s0, s1, s2, s3, s4, s5, s6], list(range(NT)))
    S.barrier()
    if "noscan" in kb.debug:
        return
    with ExitStack() as es:
        sb = lambda n, s, dt=F32: es.enter_context(nc.sbuf_tensor(kb.un(n), s, dt))
        ps = lambda n, s, dt=F32: es.enter_context(nc.psum_tensor(kb.un(n), s, dt))
        msk = sb("S_msk", [128, 2, 512])
        kb.dma(msk[:], C["rwkv_masks"].rearrange("d p n -> p d n"), rd=(), wr=["S_msk"])
        ones2 = sb("S_ones", [128, 2])
        kb.memset(ones2[:], 1.0, wr=["S_ones"])
        yacc = sb("S_yacc", [128, NT, 256])
        kb.memset(yacc[:].rearrange("p t c -> p (t c)"), 0.0, wr=[("S_yacc", i_) for i_ in range(NT)])
        Hs = [sb("S_H%d" % d_, [64, 4, 64]) for d_ in range(2)]
        rwt = [sb("S_rw%d" % i_, [128, 8, 256]) for i_ in range(4)]
        exs = [sb("S_ex%d" % i_, [128, 3, 256]) for i_ in range(4)]
        clxs = [sb("S_clx%d" % i_, [128, 256]) for i_ in range(4)]
        q4s = [sb("S_q4%d" % i_, [128, 4, 256]) for i_ in range(4)]
        T4s = [[sb("S_T4_%d_%d" % (i_, h), [64, 4, 128]) for h in range(4)] for i_ in range(4)]
        GMs = [[sb("S_GM%d_%d" % (i_, h), [128, 2, 256]) for h in range(4)] for i_ in range(4)]
        Xss = [[sb("S_X%d_%d" % (d_, i_), [128, 4, 128]) for i_ in range(2)] for d_ in range(2)]
        XTss = [[sb("S_XT%d_%d" % (d_, i_), [128, 4, 128]) for i_ in range(2)] for d_ in range(2)]
        TTss = [[sb("S_TT%d_%d" % (d_, i_), [128, 4, 128]) for i_ in range(2)] for d_ in range(2)]
        pcs = [sb("S_pc%d" % i_, [64, 4]) for i_ in range(4)]
        R1s = [sb("S_R1%d" % d_, [128, 4, 64]) for d_ in range(2)]
        nUs = [sb("S_nU%d" % d_, [128, 4, 64]) for d_ in range(2)]
        pb = [ps("S_pb%d" % i, [128, 512]) for i in range(8)]
        P = lambda i: "S_pb%d" % i
        orders = ([16, 17] + list(range(16)), [17, 16] + list(range(15, -1, -1)))
        for d_ in range(2):
            kb.memset(Hs[d_][:], 0.0, wr=["S_H_d%d" % d_])

        def front(d, n):
            i = orders[d][n]
            sl = "_s%d" % (d * 2 + n % 2)
            dl = "_d%d" % d
            w, ex, clx, q4, T4, GM, pc = rwt[d * 2 + n % 2], exs[d * 2 + n % 2], clxs[d * 2 + n % 2], q4s[d * 2 + n % 2], T4s[d * 2 + n % 2], GMs[d * 2 + n % 2], pcs[d * 2 + n % 2]
            H, Xs, XTs, TTs, R1, nU = Hs[d], Xss[d], XTss[d], TTss[d], R1s[d], nUs[d]
            rk_ = "S_rw" + sl
            kb.dma(w[:], RW[i * 128:(i + 1) * 128], rd=(), wr=[rk_])
            lw = w[:, 5 + d, :]
            kb.mm(pb[0][:, 0:256], msk[:, d, 0:128], lw, True, True, rd=["S_msk", rk_], wr=[P(0)])
            for h in range(4):
                kb.mm(pb[0][0:64, 256 + 2 * h:258 + 2 * h], w[:, 5 + d, h * 64:(h + 1) * 64], ones2[:], True, True, rd=[rk_, "S_ones"], wr=[P(0)])
            kb.act(ex[:, 0, :], pb[0][:, 0:256], AF.Exp, rd=[P(0)], wr=["S_ex" + sl])
            kb.act(ex[:, 2, :], pb[0][:, 0:256], AF.Exp, rd=[P(0)], wr=["S_ex" + sl], scale=-1.0)
            kb.tt(clx[:], pb[0][:, 0:256], lw, ALU.subtract, rd=[P(0), rk_], wr=["S_clx" + sl])
            kb.act(ex[:, 1, :], clx[:], AF.Exp, rd=["S_clx" + sl], wr=["S_ex" + sl])
            kb.act(pc[:], pb[0][0:64, 256:264].rearrange("p (h t) -> p h t", t=2)[:, :, 0], AF.Exp, rd=[P(0)], wr=["S_pc" + sl])
            kb.tt(q4[:, 0, :], w[:, 3, :], ex[:, 1, :], ALU.mult, rd=[rk_, "S_ex" + sl], wr=["S_q4" + sl])
            kb.tt(q4[:, 1, :], w[:, 0, :], ex[:, 0, :], ALU.mult, rd=[rk_, "S_ex" + sl], wr=["S_q4" + sl], eng="gpsimd")
            kb.tt(q4[:, 2, :], w[:, 4, :], ex[:, 2, :], ALU.mult, rd=[rk_, "S_ex" + sl], wr=["S_q4" + sl])
            kb.tt(q4[:, 3, :], w[:, 1, :], ex[:, 2, :], ALU.mult, rd=[rk_, "S_ex" + sl], wr=["S_q4" + sl], eng="gpsimd")
            for h in range(4):
                tb_ = 1 if h % 2 == 0 else 0
                for kd in range(4):
                    kb.tr(pb[tb_][0:64, kd * 128:(kd + 1) * 128], q4[:, kd, h * 64:(h + 1) * 64], ident[:], rd=["S_q4" + sl], wr=[P(tb_)])
                kb.cp(T4[h][:].rearrange("p k t -> p (k t)"), pb[tb_][0:64, :], rd=[P(tb_)], wr=["S_T4_%d" % h + sl], eng="scalar" if h % 2 else "vector")
            for h in range(4):
                t4 = T4[h]
                tk = "S_T4_%d" % h + sl
                gb_ = 2 if h % 2 == 0 else 7
                kb.mm(pb[gb_][:, 0:256], t4[:, 2, :], t4[:, 0:2, :], True, True, rd=[tk], wr=[P(gb_)])
                kb.mm(pb[gb_][:, 256:512], t4[:, 3, :], t4[:, 0:2, :], True, True, rd=[tk], wr=[P(gb_)])
                kb.mm(pb[3][:, h * 128:(h + 1) * 128], t4[:, 0, :], t4[:, 2, :], True, True, rd=[tk], wr=[P(3)])
                kb.tt(GM[h][:], pb[gb_][:].rearrange("p (a n) -> p a n", a=2), msk[:, d, 128:384].unsqueeze(1).to_broadcast([128, 2, 256]), ALU.mult,
                      rd=[P(gb_), "S_msk"], wr=["S_GM%d" % h + sl])
            X, XT, TT = Xs[0], XTs[0], TTs[0]
            XK = lambda c_, p_: ("S_X", c_, p_, d)
            XTK = lambda c_, p_: ("S_XT", c_, p_, d)
            TTK = lambda c_, p_: ("S_TT", c_, p_, d)
            kb.tt(X[:], pb[3][:].rearrange("p (h n) -> p h n", h=4), msk[:, d, 384:512].unsqueeze(1).to_broadcast([128, 4, 128]), ALU.mult,
                  rd=[P(3), "S_msk"], wr=[XK(0, 0), XK(0, 1)])
            for h in range(4):
                kb.cp(XT[:, h, :], GM[h][:, 0, 0:128], rd=["S_GM%d" % h + sl], wr=[XTK(0, h // 2)], eng="gpsimd")
                kb.tt(TT[:, h, :], ident[:], GM[h][:, 0, 0:128], ALU.subtract, rd=["ident", "S_GM%d" % h + sl], wr=[TTK(0, h // 2)])

        def back(d, n):
            i = orders[d][n]
            sl = "_s%d" % (d * 2 + n % 2)
            dl = "_d%d" % d
            w, ex, clx, q4, T4, GM, pc = rwt[d * 2 + n % 2], exs[d * 2 + n % 2], clxs[d * 2 + n % 2], q4s[d * 2 + n % 2], T4s[d * 2 + n % 2], GMs[d * 2 + n % 2], pcs[d * 2 + n % 2]
            H, Xs, XTs, TTs, R1, nU = Hs[d], Xss[d], XTss[d], TTss[d], R1s[d], nUs[d]
            rk_ = "S_rw" + sl
            lw = w[:, 5 + d, :]
            XK = lambda c_, p_: ("S_X", c_, p_, d)
            XTK = lambda c_, p_: ("S_XT", c_, p_, d)
            TTK = lambda c_, p_: ("S_TT", c_, p_, d)
            NB_ = ((4, 5, 6), (2, 7, 1))
            cur = 0
            for kq in range(6):
                nx = 1 - cur
                Xc, XTc, TTc = Xs[cur], XTs[cur], TTs[cur]
                Xn, XTn, TTn = Xs[nx], XTs[nx], TTs[nx]
                for p_ in range(2):
                    bX = NB_[p_][0]
                    for h in (2 * p_, 2 * p_ + 1):
                        kb.mm(pb[bX][:, (h % 2) * 128:(h % 2 + 1) * 128], XTc[:, h, :], Xc[:, h, :], True, True, rd=[XK(cur, p_), XTK(cur, p_)], wr=[P(bX)])
                    kb.cp(Xn[:, 2 * p_:2 * p_ + 2, :].rearrange("p h n -> p (h n)"), pb[bX][:, 0:256], rd=[P(bX)], wr=[XK(nx, p_)], eng="vector")
                if kq < 5:
                    for p_ in range(2):
                        bXT = NB_[p_][1]
                        for h in (2 * p_, 2 * p_ + 1):
                            kb.mm(pb[bXT][:, (h % 2) * 128:(h % 2 + 1) * 128], Xc[:, h, :], XTc[:, h, :], True, True, rd=[XK(cur, p_), XTK(cur, p_)], wr=[P(bXT)])
                        kb.cp(XTn[:, 2 * p_:2 * p_ + 2, :].rearrange("p h n -> p (h n)"), pb[bXT][:, 0:256], rd=[P(bXT)], wr=[XTK(nx, p_)], eng="scalar")
                for p_ in range(2):
                    bT = NB_[p_][2]
                    for h in (2 * p_, 2 * p_ + 1):
                        kb.mm(pb[bT][:, (h % 2) * 128:(h % 2 + 1) * 128], Xn[:, h, :], TTc[:, h, :], True, True, rd=[XK(nx, p_), TTK(cur, p_)], wr=[P(bT)])
                    kb.tt(TTn[:, 2 * p_:2 * p_ + 2, :].rearrange("p h n -> p (h n)"), pb[bT][:, 0:256],
                          TTc[:, 2 * p_:2 * p_ + 2, :].rearrange("p h n -> p (h n)"), ALU.add, rd=[P(bT), TTK(cur, p_)], wr=[TTK(nx, p_)])
                cur = nx
            TTf = TTs[cur]
            ktf = TTK(cur, 0)
            ktf1 = TTK(cur, 1)
            for h in range(4):
                kb.mm(pb[7][:, h * 64:(h + 1) * 64], T4[h][:, 0, :], H[:, h, :], True, False, rd=["S_T4_%d" % h + sl, "S_H" + dl], wr=[P(7)])
                kb.mm(pb[7][:, h * 64:(h + 1) * 64], GM[h][:, 1, 0:128], w[:, 2, h * 64:(h + 1) * 64], False, True, rd=["S_GM%d" % h + sl, rk_], wr=[P(7)])
            kb.cp(R1[:].rearrange("p h v -> p (h v)"), pb[7][:, 0:256], rd=[P(7)], wr=["S_R1" + dl], eng="vector")
            for h in range(4):
                kb.mm(pb[7][:, 256 + h * 64:256 + (h + 1) * 64], TTf[:, h, :], R1[:, h, :], True, True, rd=[ktf, ktf1, "S_R1" + dl], wr=[P(7)])
            kb.ts(nU[:].rearrange("p h v -> p (h v)"), pb[7][:, 256:512], -1.0, None, ALU.mult, None, rd=[P(7)], wr=["S_nU" + dl])
            want_y = (i < 16) or need_ctx
            if want_y:
                for h in range(4):
                    o = pb[3][:, h * 64:(h + 1) * 64]
                    kb.mm(o, T4[h][:, 1, :], H[:, h, :], True, False, rd=["S_T4_%d" % h + sl, "S_H" + dl], wr=[P(3)])
                    kb.mm(o, GM[h][:, 0, 128:256], nU[:, h, :], False, False, rd=["S_GM%d" % h + sl, "S_nU" + dl], wr=[P(3)])
                    kb.mm(o, GM[h][:, 1, 128:256], w[:, 2, h * 64:(h + 1) * 64], False, True, rd=["S_GM%d" % h + sl, rk_], wr=[P(3)])
                kb.tt(yacc[:, i, :], pb[3][:, 0:256], yacc[:, i, :], ALU.add, rd=[P(3), ("S_yacc", i)], wr=[("S_yacc", i)])
            for h in range(4):
                o = pb[2][0:64, h * 64:(h + 1) * 64]
                kb.mm(o, ident[0:64, 0:64], H[:, h, :], True, False, rd=["ident", "S_H" + dl], wr=[P(2)])
                kb.mm(o, q4[:, 2, h * 64:(h + 1) * 64], nU[:, h, :], False, False, rd=["S_q4" + sl, "S_nU" + dl], wr=[P(2)])
                kb.mm(o, q4[:, 3, h * 64:(h + 1) * 64], w[:, 2, h * 64:(h + 1) * 64], False, True, rd=["S_q4" + sl, rk_], wr=[P(2)])
            kb.tt(H[:], pb[2][0:64, 0:256].rearrange("p (h v) -> p h v", h=4), pc[:].unsqueeze(2).to_broadcast([64, 4, 64]), ALU.mult,
                  rd=[P(2), "S_pc" + sl], wr=["S_H" + dl])

        NCH = len(orders[0])
        front(0, 0)
        front(1, 0)
        for n in range(NCH):
            back(0, n)
            if n + 1 < NCH:
                front(0, n + 1)
            back(1, n)
            if n + 1 < NCH:
                front(1, n + 1)
        kb.S.barrier()
        ln = sb("S_ln", [128, 3, 256])
        kb.dma(ln[:, 0:2, :], C["b_lnx"][l:l + 1].to_broadcast([128, 2, 256]), rd=(), wr=["S_ln"])
        kb.dma(ln[:, 2, :], C["b_rk"][l:l + 1, :].to_broadcast([128, 256]), rd=(), wr=["S_ln"])
        NBR = 4
        st = [sb("S_st%d" % i, [128, 4]) for i in range(NBR)]
        st2 = [sb("S_st2%d" % i, [128, 4]) for i in range(NBR)]
        yc_ = [sb("S_yc%d" % i, [128, 256]) for i in range(NBR)]
        t1 = [sb("S_t1%d" % i, [128, 256]) for i in range(NBR)]
        t3 = [sb("S_t3%d" % i, [128, 256]) for i in range(NBR)]
        bo = [sb("S_bo%d" % i, [128, 4]) for i in range(NBR)]
        yo = [sb("S_yo%d" % i, [128, 256]) for i in range(NBR)]
        rwr = rwt
        v3 = lambda ap: ap.rearrange("p (h d) -> p h d", d=64)
        tiles = list(range(NT)) if need_ctx else list(range(16))

        def r0_(i):
            b = i % NBR
            rk_ = "S_rwr%d" % b
            w = rwr[b]
            kb.dma(w[:], RW[i * 128:(i + 1) * 128], rd=(), wr=[rk_])
            y = yacc[:, i, :]
            yk = ("S_yacc", i)
            kb.red(st[b][:], v3(y), ALU.add, rd=[yk], wr=["S_st%d" % b])
            kb.ts(st[b][:], st[b][:], 1.0 / 64, None, ALU.mult, None, rd=["S_st%d" % b], wr=["S_st%d" % b])
            kb.tt(v3(yc_[b][:]), v3(y), st[b][:].unsqueeze(2).to_broadcast([128, 4, 64]), ALU.subtract, rd=[yk, "S_st%d" % b], wr=["S_yc%d" % b])
            kb.tt(t1[b][:], yc_[b][:], yc_[b][:], ALU.mult, rd=["S_yc%d" % b], wr=["S_t1%d" % b], eng="gpsimd")

        def r1_(i):
            b = i % NBR
            rk_ = "S_rwr%d" % b
            w = rwr[b]
            kb.red(st2[b][:], v3(t1[b][:]), ALU.add, rd=["S_t1%d" % b], wr=["S_st2%d" % b])
            kb.ts(st2[b][:], st2[b][:], 1.0 / 64, 64e-5, ALU.mult, ALU.add, rd=["S_st2%d" % b], wr=["S_st2%d" % b])
            kb.act(st2[b][:], st2[b][:], AF.Sqrt, rd=["S_st2%d" % b], wr=["S_st2%d" % b])
            kb.tt(t3[b][:], w[:, 0, :], w[:, 1, :], ALU.mult, rd=[rk_], wr=["S_t3%d" % b], eng="gpsimd")
            kb.tt(t3[b][:], t3[b][:], ln[:, 2, :], ALU.mult, rd=["S_t3%d" % b, "S_ln"], wr=["S_t3%d" % b], eng="gpsimd")

        def r2_(i):
            b = i % NBR
            rk_ = "S_rwr%d" % b
            w = rwr[b]
            kb.recip(st2[b][:], st2[b][:], rd=["S_st2%d" % b], wr=["S_st2%d" % b])
            kb.tt(v3(yc_[b][:]), v3(yc_[b][:]), st2[b][:].unsqueeze(2).to_broadcast([128, 4, 64]), ALU.mult, rd=["S_yc%d" % b, "S_st2%d" % b], wr=["S_yc%d" % b])
            kb.tt(yc_[b][:], yc_[b][:], ln[:, 0, :], ALU.mult, rd=["S_yc%d" % b, "S_ln"], wr=["S_yc%d" % b], eng="gpsimd")
            kb.red(bo[b][:], v3(t3[b][:]), ALU.add, rd=["S_t3%d" % b], wr=["S_bo%d" % b])
            kb.tt(v3(t3[b][:]), v3(w[:, 2, :]), bo[b][:].unsqueeze(2).to_broadcast([128, 4, 64]), ALU.mult, rd=[rk_, "S_bo%d" % b, "S_t3%d" % b], wr=["S_t3%d" % b])

        def r3_(i):
            b = i % NBR
            rk_ = "S_rwr%d" % b
            w = rwr[b]
            kb.tt(t3[b][:], t3[b][:], ln[:, 1, :], ALU.add, rd=["S_t3%d" % b, "S_ln"], wr=["S_t3%d" % b], eng="gpsimd")
            kb.tt(yc_[b][:], yc_[b][:], t3[b][:], ALU.add, rd=["S_yc%d" % b, "S_t3%d" % b], wr=["S_yc%d" % b])
            kb.tt(yo[b][:], yc_[b][:], w[:, 7, :], ALU.mult, rd=["S_yc%d" % b, rk_], wr=["S_yo%d" % b])
            kb.dma(YB[i * 128:(i + 1) * 128, 256:512], yo[b][:], rd=["S_yo%d" % b], wr=[("YB", "S_yo%d" % b)], semres="S_yo%d" % b)

        pipeline([r0_, r1_, r2_, r3_], tiles)


def merge_phase(kb, l, need_ctx, x_in, ctx_in, X, Z, YB, H2T, MOD, ident, C, gate_all):
    nc = kb.nc
    with ExitStack() as es:
        sb = lambda n, s, dt=F32: es.enter_context(nc.sbuf_tensor(kb.un(n), s, dt))
        ps = lambda n, s, dt=F32: es.enter_context(nc.psum_tensor(kb.un(n), s, dt))
        wbr = sb("M_wbr", [128, 8, D], F32R)
        wout = sb("M_wout", [128, 8, D], F32R)
        kb.dma(wbr[:], C["w_branch"][l].rearrange("n (c p) d -> p (n c) d", p=128), rd=(), wr=["M_wbr"], q="gpsimd")
        kb.dma(wout[:], C["w_out"][l].rearrange("(c p) d -> p c d", p=128), rd=(), wr=["M_wout"], q="gpsimd")
        rwt = sb("M_rw", [128, 8, 16])
        kb.dma(rwt[:], C["router_w"].rearrange("(c p) e -> p c e", p=128), rd=(), wr=["M_rw"])
        rb = sb("M_rb", [128, 16])
        kb.dma(rb[:], C["router_b"].unsqueeze(0).to_broadcast([128, 16]), rd=(), wr=["M_rb"])
        gm = [sb("M_gm%d" % s, [128, D]) for s in range(2)]
        A2 = [sb("M_A2%d" % s, [128, D]) for s in range(2)]
        B2 = [sb("M_B2%d" % s, [128, D]) for s in range(2)]
        gbc = sb("M_gbc", [128, D])
        kb.dma(gbc[:], C["norm2"][l:l + 1, :].to_broadcast([128, D]), rd=(), wr=["M_gbc"])
        for s in range(2):
            kb.dma(gm[s][:], MOD[l, s:s + 1, 2 * D:3 * D].to_broadcast([128, D]), rd=(), wr=["M_gm%d" % s])
            kb.dma(A2[s][:], MOD[l, s:s + 1, 4 * D:5 * D].to_broadcast([128, D]), rd=(), wr=["M_A2%d" % s])
            kb.dma(B2[s][:], MOD[l, s:s + 1, 3 * D:4 * D].to_broadcast([128, D]), rd=(), wr=["M_B2%d" % s])
            kb.stt(A2[s][:], A2[s][:], 1.0, gbc[:], ALU.add, ALU.mult, rd=["M_A2%d" % s, "M_gbc"], wr=["M_A2%d" % s])
        yb_ = [sb("M_yb%d" % i_, [128, D]) for i_ in range(2)]
        ybT_ = [sb("M_ybT%d" % i_, [128, 8, 128], F32R) for i_ in range(2)]
        gt_ = [sb("M_gt%d" % i_, [128, 4 * D]) for i_ in range(2)]
        m_ = [sb("M_m%d" % i_, [128, D]) for i_ in range(2)]
        tmp_ = [sb("M_tmp%d" % i_, [128, 512]) for i_ in range(3)]
        mT_ = [sb("M_mT%d" % i_, [128, 8, 128], F32R) for i_ in range(2)]
        xt_ = [sb("M_xt%d" % i_, [128, D]) for i_ in range(2)]
        xn_ = [sb("M_xn%d" % i_, [128, D]) for i_ in range(2)]
        ss_ = [sb("M_ss%d" % i_, [128, 1]) for i_ in range(2)]
        h2_ = [sb("M_h2%d" % i_, [128, D]) for i_ in range(2)]
        h2T_ = [sb("M_h2T%d" % i_, [128, 8, 128]) for i_ in range(2)]
        sc = sb("M_sc", [128, NT, 16])
        bi = sb("M_bi", [128, NT, 16])
        r4 = sb("M_r4", [128, 8, NT, 4])
        msk = sb("M_msk", [128, NT, 16])
        r1 = sb("M_r1", [128, 4, NT])
        M_ptr = [ps("M_ptr%d" % i, [128, 4, 128]) for i in range(2)]
        M_pq = [ps("M_pq%d" % i, [128, 512]) for i in range(3)]
        M_pr = ps("M_pr", [128, 16])
        tiles = list(range(NT)) if need_ctx else list(range(16))
        kqc = [0]

        def transposes(src, srck, dst, dstk):
            for g in range(2):
                for j in range(4):
                    c = g * 4 + j
                    kb.tr(M_ptr[g][:, j, :], src[:, c * 128:(c + 1) * 128], ident[:], rd=srck, wr=["M_ptr%d" % g])
                kb.cp(dst[:, g * 4:(g + 1) * 4, :], M_ptr[g][:], rd=["M_ptr%d" % g], wr=[dstk], eng="scalar" if g == 0 else "vector")

        def s0(i):
            bb_ = i % 2
            kb.dma(yb_[bb_][:], YB[i * 128:(i + 1) * 128, :], rd=(), wr=["M_yb_%d" % bb_])

        def s1(i):
            bb_ = i % 2
            transposes(yb_[bb_], ["M_yb_%d" % bb_], ybT_[bb_], "M_ybT_%d" % bb_)
            kb.dma(gt_[bb_][:], Z[i * 128:(i + 1) * 128, O_G:O_G + 4 * D], rd=(), wr=[("M_gt", bb_, n_) for n_ in range(4)])

        def s2(i):
            bb_ = i % 2
            r0 = i * 128
            gt, m, ybT, xt = gt_[bb_], m_[bb_], ybT_[bb_], xt_[bb_]
            if l == 0:
                src = x_in[r0:r0 + 128, :] if i < 16 else ctx_in[r0 - SEQ:r0 - SEQ + 128, :]
            else:
                src = X[r0:r0 + 128, :]
            kb.dma(xt[:], src, rd=(), wr=["M_xt_%d" % bb_])
            for n in range(4):
                kb.act(gt[:, n * D:(n + 1) * D], gt[:, n * D:(n + 1) * D], AF.Sigmoid, rd=[("M_gt", bb_, n)], wr=[("M_gt", bb_, n)])
            for n in range(4):
                for hf in range(2):
                    b = kqc[0] % 3
                    kqc[0] += 1
                    pk = "M_pq%d" % b
                    for cc in range(2):
                        c = 2 * n + cc
                        kb.mm(M_pq[b][:], ybT[:, c, :], wbr[:, c, hf * 512:(hf + 1) * 512], cc == 0, cc == 1, rd=["M_ybT_%d" % bb_, "M_wbr"], wr=[pk])
                    gsl = gt[:, n * D + hf * 512:n * D + (hf + 1) * 512]
                    mk = ("M_m", hf, bb_)
                    if n == 0:
                        kb.tt(m[:, hf * 512:(hf + 1) * 512], M_pq[b][:], gsl, ALU.mult, rd=[pk, ("M_gt", bb_, n)], wr=[mk])
                    else:
                        tmp = tmp_[b]
                        kb.tt(tmp[:], M_pq[b][:], gsl, ALU.mult, rd=[pk, ("M_gt", bb_, n)], wr=["M_tmp%d" % b])
                        kb.tt(m[:, hf * 512:(hf + 1) * 512], m[:, hf * 512:(hf + 1) * 512], tmp[:], ALU.add, rd=[mk, "M_tmp%d" % b], wr=[mk], eng="gpsimd")
            transposes(m, [("M_m", 0, bb_), ("M_m", 1, bb_)], mT_[bb_], "M_mT_%d" % bb_)

        def s3(i):
            bb_ = i % 2
            s = 0 if i < 16 else 1
            r0 = i * 128
            mT, xt, xn, ss, h2 = mT_[bb_], xt_[bb_], xn_[bb_], ss_[bb_], h2_[bb_]
            xk = [("M_xnh", 0, bb_), ("M_xnh", 1, bb_)]
            for hf in range(2):
                b = kqc[0] % 3
                kqc[0] += 1
                pk = "M_pq%d" % b
                for c in range(8):
                    kb.mm(M_pq[b][:], mT[:, c, :], wout[:, c, hf * 512:(hf + 1) * 512], c == 0, c == 7, rd=["M_mT_%d" % bb_, "M_wout"], wr=[pk])
                tmp = tmp_[b]
                kb.tt(tmp[:], M_pq[b][:], gm[s][:, hf * 512:(hf + 1) * 512], ALU.mult, rd=[pk, "M_gm%d" % s], wr=["M_tmp%d" % b])
                kb.tt(xn[:, hf * 512:(hf + 1) * 512], tmp[:], xt[:, hf * 512:(hf + 1) * 512], ALU.add, rd=["M_tmp%d" % b, "M_xt_%d" % bb_], wr=[xk[hf]], eng="gpsimd")
            kb.dma(X[r0:r0 + 128, :], xn[:], rd=xk, wr=[("X", i)], semres="M_xn_%d" % bb_)
            hk = "M_h2_%d" % bb_
            sk = "M_ss_%d" % bb_
            kb.act(h2[:], xn[:], AF.Square, rd=xk, wr=[hk, sk], accum_out=ss[:])
            kb.rstd(ss[:], ss[:], D, EPS, rd=[sk], wr=[sk])
            kb.stt(h2[:], xn[:], ss[:, 0:1], A2[s][:], ALU.mult, ALU.mult, rd=xk + [sk, "M_A2%d" % s], wr=[hk])
            kb.tt(h2[:], h2[:], B2[s][:], ALU.add, rd=[hk, "M_B2%d" % s], wr=[hk])

        def s4(i):
            bb_ = i % 2
            r0 = i * 128
            h2, h2T = h2_[bb_], h2T_[bb_]
            tk = "M_h2T_%d" % bb_
            transposes(h2, ["M_h2_%d" % bb_], h2T, tk)
            kb.dma(H2T[:, :, r0:r0 + 128], h2T[:], rd=[tk], wr=[("H2T", i)], semres=tk)
            for c in range(8):
                kb.mm(M_pr[:], h2T[:, c, :], rwt[:, c, :], c == 0, c == 7, rd=[tk, "M_rw"], wr=["M_pr"])
            kb.act(sc[:, i, :], M_pr[:], AF.Sigmoid, rd=["M_pr"], wr=[("M_sc", i)])

        pipeline([s0, s1, s2, s3, s4], tiles)
        T_ = len(tiles)
        sck = [("M_sc", i_) for i_ in tiles]
        scv = sc[:, 0:T_, :]
        kb.tt(bi[:, 0:T_, :], scv, rb[:].unsqueeze(1).to_broadcast([128, T_, 16]), ALU.add, rd=sck + ["M_rb"], wr=["M_bi"])
        b4 = bi[:, 0:T_, :].rearrange("p t (g j) -> p t g j", j=4)
        K_ = ["M_r4"]
        R = lambda k_: r4[:, k_, 0:T_, :]
        kb.tt(R(0), b4[:, :, :, 0], b4[:, :, :, 1], ALU.max, rd=["M_bi"], wr=K_)
        kb.tt(R(1), b4[:, :, :, 0], b4[:, :, :, 1], ALU.min, rd=["M_bi"], wr=K_)
        kb.tt(R(2), b4[:, :, :, 2], b4[:, :, :, 3], ALU.max, rd=["M_bi"], wr=K_)
        kb.tt(R(3), b4[:, :, :, 2], b4[:, :, :, 3], ALU.min, rd=["M_bi"], wr=K_)
        kb.tt(R(4), R(0), R(2), ALU.max, rd=K_, wr=K_)
        kb.tt(R(5), R(0), R(2), ALU.min, rd=K_, wr=K_)
        kb.tt(R(6), R(1), R(3), ALU.max, rd=K_, wr=K_)
        kb.tt(R(5), R(5), R(6), ALU.max, rd=K_, wr=K_)
        kb.tt(R(4), R(4), R(5), ALU.add, rd=K_, wr=K_)
        kb.red(r1[:, 0, 0:T_], R(4), ALU.max, rd=K_, wr=["M_r1"])
        kb.tt(R(7), R(4), r1[:, 0, 0:T_].unsqueeze(2).to_broadcast([128, T_, 4]), ALU.is_equal, rd=K_ + ["M_r1"], wr=K_)
        kb.ts(R(6), R(7), 1e30, -1e30, ALU.mult, ALU.add, rd=K_, wr=K_)
        m4 = msk[:, 0:T_, :].rearrange("p t (g j) -> p t g j", j=4)
        kb.tt(m4, b4, R(7).unsqueeze(3).to_broadcast([128, T_, 4, 4]), ALU.mult, rd=["M_bi"] + K_, wr=["M_msk"])
        kb.tt(m4, m4, R(6).unsqueeze(3).to_broadcast([128, T_, 4, 4]), ALU.add, rd=["M_msk"] + K_, wr=["M_msk"])
        mv = msk[:, 0:T_, :]
        bv = bi[:, 0:T_, :]
        kb.red(r1[:, 1, 0:T_], mv, ALU.max, rd=["M_msk"], wr=["M_r1"])
        kb.tt(bv, mv, r1[:, 1, 0:T_].unsqueeze(2).to_broadcast([128, T_, 16]), ALU.is_equal, rd=["M_msk", "M_r1"], wr=["M_bi"])
        kb.stt(mv, bv, -1e30, mv, ALU.mult, ALU.add, rd=["M_bi", "M_msk"], wr=["M_msk"])
        kb.red(r1[:, 2, 0:T_], mv, ALU.max, rd=["M_msk"], wr=["M_r1"])
        kb.tt(mv, mv, r1[:, 2, 0:T_].unsqueeze(2).to_broadcast([128, T_, 16]), ALU.is_equal, rd=["M_msk", "M_r1"], wr=["M_msk"])
        kb.tt(bv, bv, mv, ALU.add, rd=["M_bi", "M_msk"], wr=["M_bi"])
        kb.tt(bv, bv, scv, ALU.mult, rd=["M_bi"] + sck, wr=["M_bi"])
        kb.red(r1[:, 3, 0:T_], bv, ALU.add, rd=["M_bi"], wr=["M_r1"])
        kb.recip(r1[:, 3, 0:T_], r1[:, 3, 0:T_], rd=["M_r1"], wr=["M_r1"])
        t0_ = tiles[0]
        kb.tt(gate_all[:, t0_:t0_ + T_, :], bv, r1[:, 3, 0:T_].unsqueeze(2).to_broadcast([128, T_, 16]), ALU.mult, rd=["M_bi", "M_r1"],
              wr=[("gate", i_) for i_ in tiles])


def moe_phase(kb, l, need_ctx, X, H2T, MOD, C, gate_all, out):
    nc = kb.nc
    last = (l == DEPTH - 1)
    tiles_all = list(range(NT)) if need_ctx else list(range(16))
    passes = [tiles_all[0:9], tiles_all[9:]]
    with ExitStack() as es:
        sb = lambda n, s, dt=F32: es.enter_context(nc.sbuf_tensor(kb.un(n), s, dt))
        ps = lambda n, s, dt=F32: es.enter_context(nc.psum_tensor(kb.un(n), s, dt))
        hT = sb("E_hT", [128, 8, 9 * 128], F32R)
        yacc = sb("E_yacc", [128, 9, D])
        Wg = [sb("E_Wg%d" % i, [128, 8, DE], F32R) for i in range(2)]
        Wu = [sb("E_Wu%d" % i, [128, 8, DE], F32R) for i in range(2)]
        Wd = [sb("E_Wd%d" % i, [128, 4, D], F32R) for i in range(2)]
        sg = [sb("E_sg%d" % i, [128, 512]) for i in range(2)]
        hid = sb("E_hid", [128, 4, 512], F32R)
        gmlp = [sb("E_gmlp%d" % s, [128, D]) for s in range(2)]
        xt = [sb("E_xt%d" % i, [128, D]) for i in range(2)]
        pg = [ps("E_pG%d" % i, [128, 512]) for i in range(2)]
        pu = [ps("E_pU%d" % i, [128, 512]) for i in range(2)]
        py = [ps("E_pY%d" % i, [128, 512]) for i in range(2)]
        for s in range(2):
            kb.dma(gmlp[s][:], MOD[l, s:s + 1, 5 * D:6 * D].to_broadcast([128, D]), rd=(), wr=["E_gmlp%d" % s])
        kg = 0
        ky = 0
        for pi, tl in enumerate(passes):
            ntl = len(tl)
            t0 = tl[0] * 128
            def load_w(e_):
                wb_ = e_ % 2
                kb.dma(Wg[wb_][:], C["e_gate"][l, e_].rearrange("(c p) n -> p c n", p=128), rd=(), wr=["E_Wg%d" % wb_], q="gpsimd")
                kb.dma(Wu[wb_][:], C["e_up"][l, e_].rearrange("(c p) n -> p c n", p=128), rd=(), wr=["E_Wu%d" % wb_], q="gpsimd")
                kb.dma(Wd[wb_][:], C["e_down"][l, e_].rearrange("(c p) n -> p c n", p=128), rd=(), wr=["E_Wd%d" % wb_], q="gpsimd")

            load_w(0)
            for c in range(8):
                kb.dma(hT[:, c, 0:ntl * 128], H2T[:, c, t0:t0 + ntl * 128], rd=(), wr=["E_hT"], q="gpsimd")
            load_w(1)
            groups = []
            ng_ = (ntl + 3) // 4
            a = 0
            for gi_ in range(ng_):
                n_ = (ntl - a + (ng_ - gi_) - 1) // (ng_ - gi_)
                groups.append((a, n_))
                a += n_
            for e in range(NE):
                wb = e % 2
                if e >= 2:
                    load_w(e)
                for (a, n_) in groups:
                    nt = n_ * 128
                    for hc in range(4):
                        b = kg % 2
                        kg += 1
                        for c in range(8):
                            kb.mm(pg[b][:, 0:nt], Wg[wb][:, c, hc * 128:(hc + 1) * 128], hT[:, c, a * 128:a * 128 + nt], c == 0, c == 7,
                                  rd=["E_Wg%d" % wb, "E_hT"], wr=["E_pG%d" % b])
                        for c in range(8):
                            kb.mm(pu[b][:, 0:nt], Wu[wb][:, c, hc * 128:(hc + 1) * 128], hT[:, c, a * 128:a * 128 + nt], c == 0, c == 7,
                                  rd=["E_Wu%d" % wb, "E_hT"], wr=["E_pU%d" % b])
                        kb.act(sg[b][:, 0:nt], pg[b][:, 0:nt], AF.Silu, rd=["E_pG%d" % b], wr=["E_sg%d" % b])
                        kb.tt(hid[:, hc, 0:nt], pu[b][:, 0:nt], sg[b][:, 0:nt], ALU.mult, rd=["E_pU%d" % b, "E_sg%d" % b], wr=[("E_hid", hc)])
                    for tt_ in range(n_):
                        ti = a + tt_
                        gi = tl[ti]
                        for hf in range(2):
                            b = ky % 2
                            ky += 1
                            for hc in range(4):
                                kb.mm(py[b][:], hid[:, hc, tt_ * 128:(tt_ + 1) * 128], Wd[wb][:, hc, hf * 512:(hf + 1) * 512], hc == 0, hc == 3,
                                      rd=[("E_hid", hc), "E_Wd%d" % wb], wr=["E_pY%d" % b])
                            ya = yacc[:, ti, hf * 512:(hf + 1) * 512]
                            yk = ("E_yacc", ti, hf)
                            gcol = gate_all[:, gi, e:e + 1]
                            if e == 0:
                                kb.ts(ya, py[b][:], gcol, None, ALU.mult, None, rd=["E_pY%d" % b, ("gate", gi)], wr=[yk])
                            else:
                                kb.stt(ya, py[b][:], gcol, ya, ALU.mult, ALU.add, rd=["E_pY%d" % b, ("gate", gi), yk], wr=[yk])
            for ti, gi in enumerate(tl):
                s = 0 if gi < 16 else 1
                b = ti % 2
                r0 = gi * 128
                kb.dma(xt[b][:], X[r0:r0 + 128, :], rd=[("X", gi)], wr=["E_xt%d" % b])
                kb.tt(yacc[:, ti, :], yacc[:, ti, :], gmlp[s][:], ALU.mult, rd=[("E_yacc", ti, 0), ("E_yacc", ti, 1), "E_gmlp%d" % s], wr=[("E_yacc", ti, 0), ("E_yacc", ti, 1)], eng="gpsimd")
                kb.tt(xt[b][:], xt[b][:], yacc[:, ti, :], ALU.add, rd=["E_xt%d" % b, ("E_yacc", ti, 0), ("E_yacc", ti, 1)], wr=["E_xt%d" % b])
                if last:
                    kb.dma(out[r0:r0 + 128, :], xt[b][:], rd=["E_xt%d" % b], wr=[("out", gi)], semres="E_xt%d" % b, is_output=True)
                else:
                    kb.dma(X[r0:r0 + 128, :], xt[b][:], rd=["E_xt%d" % b], wr=[("X", gi)], semres="E_xt%d" % b)


def build(debug=(), stop_after=None):
    kb = KB(debug)
    nc, S = kb.nc, kb.S
    x_in = kb.inp("x", [SEQ, D])
    ctx_in = kb.inp("ctx", [CTX, D])
    cmat = kb.inp("cmat", [128, 8, 2])
    w_mod = kb.inp("w_mod", [DEPTH, D, 6 * D])
    b_mod = kb.inp("b_mod", [DEPTH, 6 * D])
    norm1 = kb.inp("norm1", [DEPTH, D])
    norm2 = kb.inp("norm2", [DEPTH, D])
    w_in = kb.inp("w_in", [DEPTH, D, D_IN])
    ident_in = kb.inp("ident", [128, 128])
    out = nc.dram_tensor("out", [SEQ, D], F32, kind="ExternalOutput").ap()
    X = kb.scratch("X", [NTOK, D])
    Z = kb.scratch("Z", [NTOK, D_IN])
    MOD = kb.scratch("MOD", [DEPTH, 2, 6 * D])
    YB = kb.scratch("YB", [NTOK, D])
    RW = kb.scratch("RW", [NTOK, 8, 256])
    H2T = kb.scratch("H2T", [128, 8, NTOK])
    C = {}
    for nm_, shp_ in (("b_shift", [DEPTH, 2, 896]), ("b_w0", [DEPTH, 2, 256]), ("b_w2", [DEPTH, 2, 32, 256]), ("b_a0", [DEPTH, 256]),
                      ("b_a2", [DEPTH, 32, 256]), ("b_g2", [DEPTH, 64, 256]), ("b_kk", [DEPTH, 256]), ("b_ka", [DEPTH, 256]),
                      ("b_rk", [DEPTH, 256]), ("b_lnx", [DEPTH, 2, 256]), ("rwkv_masks", [2, 128, 512])):
        C[nm_] = kb.inp(nm_, shp_)
    for nm_, shp_ in (("w_branch", [DEPTH, 4, 256, D]), ("w_out", [DEPTH, D, D]), ("router_w", [D, NE]), ("router_b", [NE]),
                      ("e_gate", [DEPTH, NE, D, DE]), ("e_up", [DEPTH, NE, D, DE]), ("e_down", [DEPTH, NE, DE, D])):
        C[nm_] = kb.inp(nm_, shp_)
    C["norm2"] = norm2
    C["a_qk_norm"] = kb.inp("a_qk_norm", [DEPTH, 2, 64])
    C["a_sink"] = kb.inp("a_sink", [DEPTH, 4])
    C["c_qk_norm"] = kb.inp("c_qk_norm", [DEPTH, 2, 32])
    C["c_lambda"] = kb.inp("c_lambda", [DEPTH, 4, 32])
    C["c_subln"] = kb.inp("c_subln", [DEPTH, 64])
    C["d_qk_norm"] = kb.inp("d_qk_norm", [DEPTH, 2, 64])
    C["ropeA"] = kb.inp("ropeA", [SEQ, 2, 384])
    C["ropeC"] = kb.inp("ropeC", [SEQ, 2, 512])
    C["bandmask"] = kb.inp("bandmask", [2, 128, 128])
    C["biasG"] = kb.inp("biasG", [DEPTH, 128, 8 * 4 * 4 * 64])
    C["maskD"] = kb.inp("maskD", [128, 64])

    with ExitStack() as es0:
        ident = es0.enter_context(nc.sbuf_tensor("ident_sb", [128, 128], F32))
        kb.dma(ident[:], ident_in, rd=(), wr=["ident"])
        gate_all = es0.enter_context(nc.sbuf_tensor("gate_all", [128, NT, 16], F32))
        kb.onez = es0.enter_context(nc.sbuf_tensor("onez", [128, 2], F32))
        kb.memset(kb.onez[:, 0:1], 1.0, wr=["onez"])
        kb.memset(kb.onez[:, 1:2], 0.0, wr=["onez"])

        for l in range(DEPTH):
            with ExitStack() as es:
                sb = lambda n, s, dt=F32: es.enter_context(nc.sbuf_tensor(kb.un(n), s, dt))
                ps = lambda n, s, dt=F32: es.enter_context(nc.psum_tensor(kb.un(n), s, dt))
                cT = sb("cT", [128, 8, 2])
                scT = sb("scT", [128, 8, 2], F32R)
                bm = sb("bm", [2, 6 * D])
                modsb = sb("modsb", [2, 6 * D])
                wm = [sb("wm%d" % i, [128, 8, 512], F32R) for i in range(2)]
                pm = [ps("pm%d" % i, [2, 512]) for i in range(2)]
                kb.dma(cT[:], cmat, rd=(), wr=["cT"])
                kb.dma(bm[:], b_mod[l:l + 1, :].to_broadcast([2, 6 * D]), rd=(), wr=["bm"])
                kb.act(scT[:], cT[:], AF.Silu, rd=["cT"], wr=["scT"])
                for cg in range(12):
                    w = wm[cg % 2]
                    wk = "wm%d" % (cg % 2)
                    pk = "pm%d" % (cg % 2)
                    kb.dma(w[:], w_mod[l, :, cg * 512:(cg + 1) * 512].rearrange("(c p) n -> p c n", p=128), rd=(), wr=[wk], q="gpsimd")
                    for c in range(8):
                        kb.mm(pm[cg % 2][:], scT[:, c, :], w[:, c, :], c == 0, c == 7, rd=["scT", wk], wr=[pk])
                    kb.tt(modsb[:, cg * 512:(cg + 1) * 512], pm[cg % 2][:], bm[:, cg * 512:(cg + 1) * 512], ALU.add, rd=[pk, "bm"], wr=["modsb"])
                kb.dma(MOD[l], modsb[:], rd=["modsb"], wr=[("MOD", l)])
            S.barrier()

            with ExitStack() as es:
                sb = lambda n, s, dt=F32: es.enter_context(nc.sbuf_tensor(kb.un(n), s, dt))
                ps = lambda n, s, dt=F32: es.enter_context(nc.psum_tensor(kb.un(n), s, dt))
                hT = sb("hT", [128, 8, NTOK], F32R)
                Am = [sb("Am%d" % s, [128, D]) for s in range(2)]
                Bm = [sb("Bm%d" % s, [128, D]) for s in range(2)]
                gbc = sb("gbc", [128, D])
                xt = [sb("xt%d" % i, [128, D]) for i in range(3)]
                sq = sb("sq", [128, D])
                ss = [sb("ss%d" % i, [128, 1]) for i in range(3)]
                hh = [sb("hh%d" % i, [128, D]) for i in range(3)]
                ptr = [ps("ptr%d" % i, [128, 4, 128]) for i in range(2)]
                kb.dma(gbc[:], norm1[l:l + 1, :].to_broadcast([128, D]), rd=(), wr=["gbc"])
                for s in range(2):
                    kb.dma(Am[s][:], MOD[l, s:s + 1, D:2 * D].to_broadcast([128, D]), rd=[("MOD", l)], wr=["Am%d" % s])
                    kb.dma(Bm[s][:], MOD[l, s:s + 1, 0:D].to_broadcast([128, D]), rd=[("MOD", l)], wr=["Bm%d" % s])
                    kb.stt(Am[s][:], Am[s][:], 1.0, gbc[:], ALU.add, ALU.mult, rd=["Am%d" % s, "gbc"], wr=["Am%d" % s])

                def h0(i):
                    b = i % 3
                    if l == 0:
                        src = x_in[i * 128:(i + 1) * 128, :] if i < 16 else ctx_in[(i - 16) * 128:(i - 15) * 128, :]
                        srck = ()
                    else:
                        src = X[i * 128:(i + 1) * 128, :]
                        srck = [("X", i)]
                    kb.dma(xt[b][:], src, rd=srck, wr=["xt%d" % b])
                    kb.act(sq[:], xt[b][:], AF.Square, rd=["xt%d" % b], wr=["sq", "ss%d" % b], accum_out=ss[b][:])
                    kb.ts(ss[b][:], ss[b][:], 1.0 / D, EPS, ALU.mult, ALU.add, rd=["ss%d" % b], wr=["ss%d" % b])
                    kb.act(ss[b][:], ss[b][:], AF.Sqrt, rd=["ss%d" % b], wr=["ss%d" % b])

                def h1(i):
                    b = i % 3
                    s = 0 if i < 16 else 1
                    kb.recip(ss[b][:], ss[b][:], rd=["ss%d" % b], wr=["ss%d" % b])
                    kb.stt(hh[b][:], xt[b][:], ss[b][:, 0:1], Am[s][:], ALU.mult, ALU.mult, rd=["xt%d" % b, "ss%d" % b, "Am%d" % s], wr=["hh%d" % b])
                    kb.tt(hh[b][:], hh[b][:], Bm[s][:], ALU.add, rd=["hh%d" % b, "Bm%d" % s], wr=["hh%d" % b], eng="gpsimd")

                def h2_(i):
                    b = i % 3
                    for g in range(2):
                        for j in range(4):
                            c = g * 4 + j
                            kb.tr(ptr[g][:, j, :], hh[b][:, c * 128:(c + 1) * 128], ident[:], rd=["hh%d" % b], wr=["ptr%d" % g])
                        kb.cp(hT[:, g * 4:(g + 1) * 4, i * 128:(i + 1) * 128], ptr[g][:], rd=["ptr%d" % g], wr=[("hT", i)],
                              eng="scalar" if g == 0 else "vector")

                pipeline([h0, h1, h2_], list(range(NT)))
                wz = [sb("wz%d" % i, [128, 8, 512], F32R) for i in range(2)]
                pz = [ps("pz%d" % i, [128, 512]) for i in range(4)]
                zs = [sb("zs%d" % i, [128, 512]) for i in range(4)]
                k = 0
                for cg in range(14):
                    n0 = cg * 512
                    ncol = min(512, D_IN - n0)
                    w = wz[cg % 2]
                    wk = "wz%d" % (cg % 2)
                    kb.dma(w[:, :, 0:ncol], w_in[l, :, n0:n0 + ncol].rearrange("(c p) n -> p c n", p=128), rd=(), wr=[wk], q="gpsimd")
                    for i in range(NT):
                        r = k % 4
                        k += 1
                        for c in range(8):
                            kb.mm(pz[r][:, 0:ncol], hT[:, c, i * 128:(i + 1) * 128], w[:, c, 0:ncol], c == 0, c == 7,
                                  rd=[("hT", i), wk], wr=["pz%d" % r])
                        kb.cp(zs[r][:, 0:ncol], pz[r][:, 0:ncol], rd=["pz%d" % r], wr=["zs%d" % r], eng="scalar" if r % 2 == 0 else "vector")
                        kb.dma(Z[i * 128:(i + 1) * 128, n0:n0 + ncol], zs[r][:, 0:ncol], rd=["zs%d" % r], wr=[("Z", i, cg)], semres="zs%d" % r)
            S.barrier()
            if stop_after == ("proj", l):
                break
            need_ctx = l < DEPTH - 1
            if stop_after != ("C", l):
                rwkv(kb, l, need_ctx, Z, YB, RW, ident, C)
                S.barrier()
            if stop_after == ("B", l):
                break
            attn_A(kb, l, need_ctx, Z, YB, ident, C)
            S.barrier()
            if stop_after == ("A", l):
                break
            attn_D(kb, l, need_ctx, Z, YB, ident, C)
            S.barrier()
            if stop_after == ("D", l):
                break
            attn_C(kb, l, need_ctx, Z, YB, ident, C)
            S.barrier()
            if stop_after == ("C", l):
                break
            merge_phase(kb, l, need_ctx, x_in, ctx_in, X, Z, YB, H2T, MOD, ident, C, gate_all)
            S.barrier()
            if stop_after == ("M", l):
                break
            moe_phase(kb, l, need_ctx, X, H2T, MOD, C, gate_all, out)
            S.barrier()
    S.barrier()
    cnt = S.finish()
    print("instr counts", cnt, "dma sems", S.n_dma_sems)
    return kb


_CONST_CACHE = {}


def host_constants():
    if _CONST_CACHE:
        return _CONST_CACHE
    t = np.arange(SEQ)
    rows, cols = (t // 64).astype(np.float32), (t % 64).astype(np.float32)

    def table(hd, G):
        h = hd // 2
        inv = (np.float32(10000.0) ** (-np.arange(0, h, 2, dtype=np.float32) / np.float32(h))).astype(np.float32)
        ar = rows[:, None] * inv[None, :]
        ac = cols[:, None] * inv[None, :]
        cos = np.concatenate([np.cos(ar), np.cos(ar), np.cos(ac), np.cos(ac)], -1).astype(np.float32)
        sin = np.concatenate([-np.sin(ar), np.sin(ar), -np.sin(ac), np.sin(ac)], -1).astype(np.float32)
        return np.ascontiguousarray(np.stack([np.tile(cos, (1, G)), np.tile(sin, (1, G))], 1))
    _CONST_CACHE["ropeA"] = table(64, 6)
    _CONST_CACHE["ropeC"] = table(32, 16)
    b = np.arange(128)[:, None]
    a = np.arange(128)[None, :]
    _CONST_CACHE["bandmask"] = np.stack([(a <= b), (b <= a)]).astype(np.float32)
    p = np.arange(128)
    kcol = p % 64
    q = np.arange(64)
    cstart = np.clip(q - 8, 0, 48)
    _CONST_CACHE["maskD"] = ((kcol[:, None] >= cstart[None, :]) & (kcol[:, None] < cstart[None, :] + 16)).astype(np.float32)
    _CONST_CACHE["ident"] = np.eye(128, dtype=np.float32)
    s_ = np.arange(128)[:, None]
    t_ = np.arange(128)[None, :]
    mk = []
    for d in range(2):
        le = (s_ <= t_) if d == 0 else (s_ >= t_)
        lt = (s_ < t_) if d == 0 else (s_ > t_)
        mk.append(np.concatenate([le, lt, le, lt.T], 1).astype(np.float32))
    _CONST_CACHE["rwkv_masks"] = np.stack(mk)
    return _CONST_CACHE


def gather_biasG(d_rpb):
    d_rpb = np.asarray(d_rpb, dtype=np.float32)
    p = np.arange(128)
    jl, kcol = p // 64, p % 64
    q = np.arange(64)
    dc = np.clip(kcol[:, None] - q[None, :] + 15, 0, 30)
    out = np.zeros((DEPTH, 128, 8, 4, 4, 64), np.float32)
    for cls in range(8):
        r = cls if cls < 4 else (4 if cls == 4 else 24 + cls)
        start = min(max(r - 4, 0), 24)
        for t in range(4):
            key_row = start + 2 * t + jl
            dr = key_row - r + 7
            for l in range(DEPTH):
                for h in range(4):
                    out[l, :, cls, h, t, :] = d_rpb[l, h][dr[:, None], dc]
    return np.ascontiguousarray(out.reshape(DEPTH, 128, 8 * 4 * 4 * 64))


def make_in_map(inp, b, kb, shared=None):
    f = lambda a: np.ascontiguousarray(np.asarray(a, dtype=np.float32))
    cm = np.stack([np.asarray(inp["c"][b]).reshape(8, 128).T, np.asarray(inp["c_ctx"]).reshape(8, 128).T], axis=-1)
    m = {"x": f(inp["x"][b]), "ctx": f(inp["ctx"][b]), "cmat": f(cm)}
    if shared is None:
        shared = {}
    if "biasG" not in shared:
        shared.update(host_constants())
        shared["biasG"] = gather_biasG(inp["d_rpb"])
    for k in kb.ins:
        if k in m:
            continue
        if k not in shared:
            shared[k] = f(inp[k])
        m[k] = shared[k]
    return m


_KB_CACHE = {}


def kernel(**inputs):
    inp = {k: np.asarray(v) for k, v in inputs.items()}
    if "kb" not in _KB_CACHE:
        _KB_CACHE["kb"] = build()
    kb = _KB_CACHE["kb"]
    shared = {}
    n = 8
    in_maps = [make_in_map(inp, b, kb, shared) for b in range(n)]
    res = run_bass_kernel_spmd(kb.nc, in_maps, core_ids=list(range(n)))
    return np.stack([np.asarray(res.results[b]["out"], dtype=np.float32) for b in range(n)], axis=0)
```

```python
import math
import numpy as np
from contextlib import ExitStack
import concourse.bass as bass
import concourse.mybir as mybir
from concourse.bass_utils import run_bass_kernel_spmd

F32 = mybir.dt.float32
F32R = mybir.dt.float32r
AF = mybir.ActivationFunctionType
ALU = mybir.AluOpType
AX = mybir.AxisListType
ALLENG = ("tensor", "vector", "scalar", "gpsimd", "sync")

D = 1024
SEQ = 2048
CTX = 256
NT = 18
NTOK = SEQ + CTX
DEPTH = 2
D_IN = 7040
O_AQ, O_AK, O_AV, O_BZ, O_CQ, O_CK, O_CV, O_DQ, O_DK, O_DV, O_G = 0, 256, 384, 512, 1408, 1664, 1920, 2176, 2432, 2688, 2944
EPS = 1e-6
NE = 16
DE = 512


import re as _re
_PSUM_RE = _re.compile(r"(^E_p[GUY]|^M_pr|^pm\d|^ptr\d|^pz\d|_ptr|_pS|_pO|_pl\d|_pd\d|_pag|_pb\d|^pq\d|^M_p)")


def is_psum_key(k):
    return isinstance(k, str) and _PSUM_RE.search(k) is not None


class Sched:
    def __init__(self, nc):
        self.nc = nc
        self.seq = {e: 0 for e in ALLENG}
        self.res = {}
        self.waited = {e: {} for e in ALLENG}
        self.dma_cnt = {}
        self.out_tokens = []
        self.n_dma_sems = 0
        self.n_sw_sems = 0
        self.dma_sem_of = {}
        self.sems = {}
        self.count = {e: 0 for e in ALLENG}

    def _need(self, eng, tok, waits):
        if tok is None:
            return
        semkey, val, _ = tok
        w = self.waited[eng]
        if w.get(semkey, 0) >= val:
            return
        w[semkey] = val
        waits.append((semkey, val))

    def _deps(self, eng, rd, wr, is_dma):
        waits = []
        for k in rd:
            st = self.res.get(k)
            if st is None:
                continue
            wtok = st[0]
            if wtok is not None:
                self._need(eng, wtok, waits)
        for k in wr:
            st = self.res.get(k)
            if st is None:
                continue
            wtok = st[0]
            if wtok is not None:
                if is_dma or wtok[2] != eng or eng != "tensor":
                    self._need(eng, wtok, waits)
            for rtok in st[1].values():
                if is_dma or rtok[2] != eng or eng != "tensor":
                    self._need(eng, rtok, waits)
        return waits

    def _record(self, tok, rd, wr):
        for k in rd:
            st = self.res.setdefault(k, [None, {}])
            old = st[1].get(tok[0])
            if old is None or old[1] < tok[1]:
                st[1][tok[0]] = tok
        for k in wr:
            self.res[k] = [tok, {}]

    def _sem(self, semkey):
        h = self.sems.get(semkey)
        if h is None:
            h = self.nc.alloc_semaphore("sem_%s_%s" % semkey)
            self.sems[semkey] = h
        return h

    def _emit(self, eng, waits, fn, inc):
        e = getattr(self.nc, eng)
        for semkey, val in waits:
            e.wait_ge(self._sem(semkey), val)
        ins = fn(e)
        ins.then_inc(self._sem(inc[0]), inc[1])
        self.count[eng] += 1

    def op(self, eng, fn, rd=(), wr=()):
        pr = [k for k in rd if is_psum_key(k)]
        if pr:
            rd = [k for k in rd if not is_psum_key(k)]
            wr = list(wr) + pr
        waits = self._deps(eng, rd, wr, False)
        self.seq[eng] += 1
        tok = (("eng", eng), self.seq[eng], eng)
        self._emit(eng, waits, fn, (("eng", eng), 1))
        self._record(tok, rd, wr)
        return tok

    def dma(self, fn, rd=(), wr=(), q="sync", semres=None, is_output=False):
        waits = self._deps(q, rd, wr, True)
        key = semres if semres is not None else (wr[0] if wr else rd[0])
        semkey = self.dma_sem_of.get((q, key))
        if semkey is None:
            if q == "gpsimd":
                semkey = ("dma", 74 + self.n_sw_sems % 20)
                self.n_sw_sems += 1
            else:
                semkey = ("dma", self.n_dma_sems % 74)
                self.n_dma_sems += 1
            self.dma_sem_of[(q, key)] = semkey
        self.dma_cnt[semkey] = self.dma_cnt.get(semkey, 0) + 16
        tok = (semkey, self.dma_cnt[semkey], None)
        self._emit(q, waits, fn, (semkey, 16))
        self._record(tok, rd, wr)
        if is_output:
            self.out_tokens.append(tok)
        return tok

    def barrier(self):
        toks = [(("eng", e), self.seq[e], e) for e in ALLENG if self.seq[e] > 0]
        toks += [(k, v, None) for k, v in self.dma_cnt.items()]
        for e in ALLENG:
            waits = []
            for t in toks:
                if t[2] == e:
                    continue
                self._need(e, t, waits)
            eo = getattr(self.nc, e)
            for semkey, val in waits:
                eo.wait_ge(self._sem(semkey), val)

    def finish(self):
        fin = []
        for tok in self.out_tokens:
            self._need("sync", tok, fin)
        for semkey, val in fin:
            self.nc.sync.wait_ge(self._sem(semkey), val)
        return dict(self.count)


class Rot:
    def __init__(self, items):
        self.items = list(items)
        self.i = 0

    def next(self):
        it = self.items[self.i % len(self.items)]
        self.i += 1
        return it


class KB:
    def __init__(self, debug=()):
        self.nc = bass.Bass("TRN2", target_bir_lowering=False)
        self.S = Sched(self.nc)
        self.debug = set(debug)
        self.ins = {}
        self.uid = 0

    def un(self, name):
        self.uid += 1
        return "%s_u%d" % (name, self.uid)

    def inp(self, name, shape):
        t = self.nc.dram_tensor(name, list(shape), F32, kind="ExternalInput").ap()
        self.ins[name] = t
        return t

    def scratch(self, name, shape):
        kind = "ExternalOutput" if name in self.debug else "Internal"
        return self.nc.dram_tensor(name, list(shape), F32, kind=kind).ap()

    def dma(self, out, in_, rd, wr, q="sync", is_output=False, semres=None):
        return self.S.dma(lambda e: e.dma_start(out=out, in_=in_), rd=rd, wr=wr, q=q, is_output=is_output, semres=semres)

    def mm(self, out, lhsT, rhs, start, stop, rd, wr):
        return self.S.op("tensor", lambda e: e.matmul(out=out, lhsT=lhsT, rhs=rhs, start=start, stop=stop), rd=rd, wr=wr)

    def tr(self, out, in_, ident, rd, wr):
        return self.S.op("tensor", lambda e: e.transpose(out=out, in_=in_, identity=ident), rd=list(rd) + ["ident"], wr=wr)

    def act(self, out, in_, func, rd, wr, bias=None, scale=None, accum_out=None):
        kw = {}
        if bias is not None:
            kw["bias"] = bias
        if scale is not None:
            kw["scale"] = scale
        if accum_out is not None:
            kw["accum_out"] = accum_out
        return self.S.op("scalar", lambda e: e.activation(out=out, in_=in_, func=func, **kw), rd=rd, wr=wr)

    def tt(self, out, a, b, op, rd, wr, eng="vector"):
        return self.S.op(eng, lambda e: e.tensor_tensor(out=out, in0=a, in1=b, op=op), rd=rd, wr=wr)

    def ts(self, out, a, s1, s2, op0, op1, rd, wr, eng="vector"):
        if op1 is None:
            return self.S.op(eng, lambda e: e.tensor_scalar(out=out, in0=a, scalar1=s1, scalar2=None, op0=op0), rd=rd, wr=wr)
        return self.S.op(eng, lambda e: e.tensor_scalar(out=out, in0=a, scalar1=s1, scalar2=s2, op0=op0, op1=op1), rd=rd, wr=wr)

    def stt(self, out, a, s, b, op0, op1, rd, wr):
        return self.S.op("vector", lambda e: e.scalar_tensor_tensor(out=out, in0=a, scalar=s, in1=b, op0=op0, op1=op1), rd=rd, wr=wr)

    def red(self, out, in_, op, rd, wr, axis=AX.X):
        return self.S.op("vector", lambda e: e.tensor_reduce(out=out, in_=in_, axis=axis, op=op), rd=rd, wr=wr)

    def cp(self, out, in_, rd, wr, eng="vector"):
        if eng == "scalar":
            return self.act(out, in_, AF.Copy, rd, wr)
        return self.S.op(eng, lambda e: e.tensor_copy(out=out, in_=in_), rd=rd, wr=wr)

    def recip(self, out, in_, rd, wr):
        return self.S.op("vector", lambda e: e.reciprocal(out=out, in_=in_), rd=rd, wr=wr)

    def memset(self, out, val, wr, eng="vector"):
        return self.S.op(eng, lambda e: e.memset(out, val), rd=(), wr=wr)

    def rstd(self, out, ss, n, eps, rd, wr):
        self.ts(out, ss, 1.0 / n, eps, ALU.mult, ALU.add, rd=rd, wr=wr)
        self.act(out, out, AF.Sqrt, rd=wr, wr=wr)
        self.recip(out, out, rd=wr, wr=wr)


def pipeline(stages, items):
    n = len(items)
    for it in range(n + len(stages) - 1):
        for s_, f in enumerate(stages):
            j = it - s_
            if 0 <= j < n:
                f(items[j])


def qk_prep(kb, es, l, Z, ident, zcol0, G, gd, nq_groups, gain_ap, rope_ap, tiles, store):
    nc = kb.nc
    W = G * gd
    NB = 6
    sb = lambda n, s, dt=F32: es.enter_context(nc.sbuf_tensor(kb.un(n), s, dt))
    gain = sb("qk_gain", [128, W])
    kb.dma(gain[:, 0:nq_groups * gd].rearrange("p (g d) -> p g d", d=gd),
           gain_ap[l, 0:1, :].to_broadcast([128, gd]).unsqueeze(1).to_broadcast([128, nq_groups, gd]), rd=(), wr=["qk_gain"])
    kb.dma(gain[:, nq_groups * gd:W].rearrange("p (g d) -> p g d", d=gd),
           gain_ap[l, 1:2, :].to_broadcast([128, gd]).unsqueeze(1).to_broadcast([128, G - nq_groups, gd]), rd=(), wr=["qk_gain"])
    zt = [sb("qk_zt%d" % i, [128, W]) for i in range(NB)]
    sq = [sb("qk_sq%d" % i, [128, W]) for i in range(NB)]
    ssq = [sb("qk_ss%d" % i, [128, G]) for i in range(NB)]
    xn = [sb("qk_xn%d" % i, [128, W]) for i in range(NB)]
    if rope_ap is not None:
        rp = [sb("qk_rp%d" % i, [128, 2, W]) for i in range(NB)]
        t1 = [sb("qk_t1%d" % i, [128, W]) for i in range(NB)]
        t2 = [sb("qk_t2%d" % i, [128, W]) for i in range(NB)]
    q4 = gd // 4
    v3 = lambda ap: ap.rearrange("p (g d) -> p g d", d=gd)

    def s0(i):
        b = i % NB
        zk, sk = "qk_zt%d" % b, "qk_ss%d" % b
        kb.dma(zt[b][:], Z[i * 128:(i + 1) * 128, zcol0:zcol0 + W], rd=(), wr=[zk])
        if rope_ap is not None and i < 16:
            kb.dma(rp[b][:], rope_ap[i * 128:(i + 1) * 128], rd=(), wr=["qk_rp%d" % b])
        kb.tt(sq[b][:], zt[b][:], zt[b][:], ALU.mult, rd=[zk], wr=["qk_sq%d" % b], eng="gpsimd")
        kb.red(ssq[b][:], v3(sq[b][:]), ALU.add, rd=["qk_sq%d" % b], wr=[sk])
        kb.ts(ssq[b][:], ssq[b][:], 1.0 / gd, EPS, ALU.mult, ALU.add, rd=[sk], wr=[sk])
        kb.act(ssq[b][:], ssq[b][:], AF.Sqrt, rd=[sk], wr=[sk])

    def s1(i):
        b = i % NB
        zk, sk, xk = "qk_zt%d" % b, "qk_ss%d" % b, "qk_xn%d" % b
        kb.recip(ssq[b][:], ssq[b][:], rd=[sk], wr=[sk])
        kb.tt(v3(xn[b][:]), v3(zt[b][:]), ssq[b][:].unsqueeze(2).to_broadcast([128, G, gd]), ALU.mult, rd=[zk, sk], wr=[xk])
        kb.tt(xn[b][:], xn[b][:], gain[:], ALU.mult, rd=[xk, "qk_gain"], wr=[xk], eng="gpsimd")

    def s2(i):
        b = i % NB
        xk, rk = "qk_xn%d" % b, "qk_rp%d" % b
        if rope_ap is not None and i < 16:
            kb.tt(t1[b][:], xn[b][:], rp[b][:, 0, :], ALU.mult, rd=[xk, rk], wr=["qk_t1%d" % b])
            xv = xn[b][:].rearrange("p (x h d) -> p x h d", h=2, d=q4)
            sv = rp[b][:, 1, :].rearrange("p (x h d) -> p x h d", h=2, d=q4)
            tv = t2[b][:].rearrange("p (x h d) -> p x h d", h=2, d=q4)
            kb.tt(tv[:, :, 0, :], xv[:, :, 1, :], sv[:, :, 0, :], ALU.mult, rd=[xk, rk], wr=[("qk_t2a", b)], eng="gpsimd")
            kb.tt(tv[:, :, 1, :], xv[:, :, 0, :], sv[:, :, 1, :], ALU.mult, rd=[xk, rk], wr=[("qk_t2b", b)], eng="gpsimd")

    def s3(i):
        b = i % NB
        xk = "qk_xn%d" % b
        if rope_ap is not None and i < 16:
            kb.tt(xn[b][:], t1[b][:], t2[b][:], ALU.add, rd=["qk_t1%d" % b, ("qk_t2a", b), ("qk_t2b", b)], wr=[xk])
        store(i, xn[b], xk)

    pipeline([s0, s1, s2, s3], list(tiles))


def load_v(kb, V1, vkey, Z, col0, H, row0, ntile, raw, rawkey):
    nt_, h_ = V1.shape[1], V1.shape[2]
    kb.cp(V1[:, :, :, 64:66], kb.onez[:, 0:2].unsqueeze(1).unsqueeze(1).to_broadcast([128, nt_, h_, 2]), rd=["onez"], wr=[(vkey, "ones")])
    kb.dma(raw[:, 0:ntile, 0:64 * H], Z[row0:row0 + ntile * 128, col0:col0 + 64 * H].rearrange("(t p) c -> p t c", p=128), rd=(), wr=[rawkey])
    hlf = ntile // 2
    kb.cp(V1[:, 0:hlf, :, 0:64], raw[:, 0:hlf, 0:64 * H].rearrange("p t (h d) -> p t h d", d=64), rd=[rawkey], wr=[(vkey, "a")], eng="vector")
    kb.cp(V1[:, hlf:ntile, :, 0:64], raw[:, hlf:ntile, 0:64 * H].rearrange("p t (h d) -> p t h d", d=64), rd=[rawkey], wr=[(vkey, "b")], eng="scalar")


def attn_finish(kb, po, pok, H, npart, extra_den, yt, ytk, dst, rec, reck):
    if extra_den is not None:
        kb.tt(rec[0:npart, :], po[0:npart, :, 64], extra_den[0:npart, :], ALU.add, rd=[pok, "expsink"], wr=[reck])
    else:
        kb.cp(rec[0:npart, :], po[0:npart, :, 64], rd=[pok], wr=[reck])
    kb.recip(rec[0:npart, :], rec[0:npart, :], rd=[reck], wr=[reck])
    kb.tt(yt[0:npart, :, :], po[0:npart, :, 0:64], rec[0:npart, :].unsqueeze(2).to_broadcast([npart, H, 64]), ALU.mult,
          rd=[pok, reck], wr=[ytk])
    kb.dma(dst, yt[0:npart, :, :].rearrange("p h d -> p (h d)"), rd=[ytk], wr=[("YB", ytk)], semres=ytk)


def attn_A(kb, l, need_ctx, Z, YB, ident, C):
    nc = kb.nc
    with ExitStack() as es:
        sb = lambda n, s, dt=F32: es.enter_context(nc.sbuf_tensor(kb.un(n), s, dt))
        ps = lambda n, s, dt=F32: es.enter_context(nc.psum_tensor(kb.un(n), s, dt))
        qT = sb("A_qT", [64, 4, NTOK], F32R)
        kT = sb("A_kT", [64, 2, NTOK], F32R)
        V1 = sb("A_V1", [128, NT, 2, 66], F32R)
        vraw = sb("A_vraw", [128, NT, 128])
        load_v(kb, V1, "A_V1", Z, O_AV, 2, 0, NT, vraw, "A_vraw")
        bm = sb("A_bm", [128, 2, 128])
        kb.dma(bm[:], C["bandmask"].rearrange("m p q -> p m q"), rd=(), wr=["A_bm"])
        expsink = sb("A_es", [128, 4])
        kb.dma(expsink[:], C["a_sink"][l:l + 1, :].to_broadcast([128, 4]), rd=(), wr=["expsink"])
        kb.act(expsink[:], expsink[:], AF.Exp, rd=["expsink"], wr=["expsink"])
        with ExitStack() as es2:
            ptr = [es2.enter_context(nc.psum_tensor(kb.un("A_ptr%d" % i), [64, 6, 128], F32)) for i in range(2)]

            def store(i, xn, xk):
                p = ptr[i % 2]
                pk = "A_ptr%d" % (i % 2)
                for g in range(6):
                    kb.tr(p[:, g, :], xn[:, g * 64:(g + 1) * 64], ident[:], rd=[xk], wr=[pk])
                kb.cp(qT[:, :, i * 128:(i + 1) * 128], p[:, 0:4, :], rd=[pk], wr=[("A_qT", i)], eng="scalar")
                kb.cp(kT[:, :, i * 128:(i + 1) * 128], p[:, 4:6, :], rd=[pk], wr=[("A_kT", i)], eng="vector")
            qk_prep(kb, es2, l, Z, ident, O_AQ, 6, 64, 4, C["a_qk_norm"], C["ropeA"], range(NT), store)
        kb.S.barrier()
        pS = [ps("A_pS%d" % i, [128, 6, 256]) for i in range(2)]
        pO = [ps("A_pO%d" % i, [128, 4, 66]) for i in range(2)]
        pT = [sb("A_pT%d" % i, [128, 5, 2, 128], F32R) for i in range(2)]
        yt = [sb("A_yt%d" % i, [128, 4, 64]) for i in range(2)]
        rec = [sb("A_rec%d" % i, [128, 4]) for i in range(2)]
        qtiles = list(range(16)) + ([16, 17] if need_ctx else [])
        items = [(n, i, g) for n, i in enumerate(qtiles) for g in range(2)]

        def ents_of(i):
            if i < 16:
                e_ = [(j, (0 if j == i - 1 else (1 if j == i + 1 else None))) for j in (i - 1, i, i + 1) if 0 <= j < 16]
                return e_ + [(16, None), (17, None)]
            return [(16, None), (17, None)]

        def qk(it):
            n, i, g = it
            b = (2 * n + g) % 2
            ents = ents_of(i)
            E = len(ents)
            psk, ptk = "A_pS%d" % b, "A_pT%d" % b
            for e, (j, mk) in enumerate(ents):
                kb.mm(pS[b][:, e, :], kT[:, g, j * 128:(j + 1) * 128], qT[:, 2 * g:2 * g + 2, i * 128:(i + 1) * 128], True, True,
                      rd=[("A_kT", j), ("A_qT", i)], wr=[psk])
            kb.act(pT[b][:, 0:E].rearrange("p e h q -> p e (h q)"), pS[b][:, 0:E, :], AF.Exp, rd=[psk], wr=[ptk], scale=0.125)
            for e, (j, mk) in enumerate(ents):
                if mk is not None:
                    kb.tt(pT[b][:, e], pT[b][:, e], bm[:, mk:mk + 1, :].to_broadcast([128, 2, 128]), ALU.mult, rd=[ptk, "A_bm"], wr=[ptk])

        def pv(it):
            n, i, g = it
            b = (2 * n + g) % 2
            ents = ents_of(i)
            E = len(ents)
            ptk = "A_pT%d" % b
            po = pO[n % 2]
            pok = "A_pO%d" % (n % 2)
            for hh in range(2):
                h = 2 * g + hh
                for e, (j, mk) in enumerate(ents):
                    kb.mm(po[:, h, :], pT[b][:, e, hh, :], V1[:, j, g, :], e == 0, e == E - 1, rd=[ptk, "A_V1"], wr=[pok])
            if g == 1:
                attn_finish(kb, po, pok, 4, 128, expsink, yt[n % 2], "A_yt%d" % (n % 2), YB[i * 128:(i + 1) * 128, 0:256], rec[n % 2], "A_rec%d" % (n % 2))

        pipeline([qk, pv], items)


def attn_D(kb, l, need_ctx, Z, YB, ident, C):
    nc = kb.nc
    with ExitStack() as es:
        sb = lambda n, s, dt=F32: es.enter_context(nc.sbuf_tensor(kb.un(n), s, dt))
        ps = lambda n, s, dt=F32: es.enter_context(nc.psum_tensor(kb.un(n), s, dt))
        qT = sb("D_qT", [64, 4, NTOK], F32R)
        kT = sb("D_kT", [64, 4, NTOK], F32R)
        V1 = sb("D_V1", [128, NT, 4, 66], F32R)
        V1o = sb("D_V1o", [128, 15, 4, 66], F32R)
        vraw = sb("D_vraw", [128, NT, 256])
        load_v(kb, V1, "D_V1", Z, O_DV, 4, 0, NT, vraw, "D_vraw")
        load_v(kb, V1o, "D_V1o", Z, O_DV, 4, 64, 15, vraw, "D_vraw")
        Eb = sb("D_E", [128, 128, 64])
        mD = sb("D_mD", [128, 64])
        kb.dma(Eb[:].rearrange("p a q -> p (a q)"), C["biasG"][l], rd=(), wr=["D_E"])
        kb.dma(mD[:], C["maskD"], rd=(), wr=["D_mD"])
        kb.act(Eb[:], Eb[:], AF.Exp, rd=["D_E"], wr=["D_E"])
        kb.tt(Eb[:], Eb[:], mD[:].unsqueeze(1).to_broadcast([128, 128, 64]), ALU.mult, rd=["D_E", "D_mD"], wr=["D_E"])
        with ExitStack() as es2:
            ptr = [es2.enter_context(nc.psum_tensor(kb.un("D_ptr%d" % i), [64, 8, 128], F32)) for i in range(2)]

            def store(i, xn, xk):
                p = ptr[i % 2]
                pk = "D_ptr%d" % (i % 2)
                for g in range(8):
                    kb.tr(p[:, g, :], xn[:, g * 64:(g + 1) * 64], ident[:], rd=[xk], wr=[pk])
                kb.cp(qT[:, :, i * 128:(i + 1) * 128], p[:, 0:4, :], rd=[pk], wr=[("D_qT", i)], eng="scalar")
                kb.cp(kT[:, :, i * 128:(i + 1) * 128], p[:, 4:8, :], rd=[pk], wr=[("D_kT", i)], eng="vector")
            qk_prep(kb, es2, l, Z, ident, O_DQ, 8, 64, 4, C["d_qk_norm"], None, range(NT), store)
        kb.S.barrier()
        pS = [ps("D_pS%d" % i, [128, 4, 6, 64]) for i in range(2)]
        pO = [ps("D_pO%d" % i, [128, 4, 66]) for i in range(2)]
        pT = [sb("D_pT%d" % i, [128, 4, 6, 64], F32R) for i in range(2)]
        ptmp = [sb("D_ptmp%d" % i, [128, 4, 4, 64]) for i in range(2)]
        yt = [sb("D_yt%d" % i, [128, 4, 64]) for i in range(2)]
        rec = [sb("D_rec%d" % i, [128, 4]) for i in range(2)]
        def d_qk(r):
            start = min(max(r - 4, 0), 24)
            cls = r if r < 4 else (4 if r <= 28 else r - 24)
            b = r % 2
            psk, ptk, tmk = "D_pS%d" % b, "D_pT%d" % b, "D_ptmp%d" % b
            for h in range(4):
                qs = qT[:, h, r * 64:(r + 1) * 64]
                for t in range(4):
                    t0 = (start + 2 * t) * 64
                    kb.mm(pS[b][:, h, t, :], kT[:, h, t0:t0 + 128], qs, True, True, rd=(), wr=[psk])
                for t in range(2):
                    kb.mm(pS[b][:, h, 4 + t, :], kT[:, h, SEQ + t * 128:SEQ + (t + 1) * 128], qs, True, True, rd=(), wr=[psk])
            kb.act(ptmp[b][:], pS[b][:, :, 0:4, :], AF.Exp, rd=[psk], wr=[tmk], scale=0.125)
            kb.act(pT[b][:, :, 4:6, :], pS[b][:, :, 4:6, :], AF.Exp, rd=[psk], wr=[(ptk, "c")], scale=0.125)
            kb.tt(pT[b][:, :, 0:4, :], ptmp[b][:], Eb[:, cls * 16:(cls + 1) * 16, :].rearrange("p (h t) q -> p h t q", h=4), ALU.mult,
                  rd=[tmk, "D_E"], wr=[(ptk, "l")])

        def d_pv(r):
            start = min(max(r - 4, 0), 24)
            b = r % 2
            po = pO[b]
            pok = "D_pO%d" % b
            ptk = "D_pT%d" % b
            for h in range(4):
                for t in range(4):
                    row = start + 2 * t
                    vt = V1[:, row // 2, h, :] if row % 2 == 0 else V1o[:, (row - 1) // 2, h, :]
                    kb.mm(po[0:64, h, :], pT[b][:, h, t, :], vt, t == 0, False, rd=[(ptk, "l")], wr=[pok])
                for t in range(2):
                    kb.mm(po[0:64, h, :], pT[b][:, h, 4 + t, :], V1[:, 16 + t, h, :], False, t == 1, rd=[(ptk, "c")], wr=[pok])
            attn_finish(kb, po, pok, 4, 64, None, yt[b], "D_yt%d" % b, YB[r * 64:(r + 1) * 64, 768:1024], rec[b], "D_rec%d" % b)

        pipeline([d_qk, d_pv], list(range(32)))
        kb.S.barrier()
        if need_ctx:
            pT2 = [sb("D_pTc%d" % i, [128, 2, 128], F32R) for i in range(2)]
            pSc = [pS[i][:, 0, 0:4, :].rearrange("p (a b) d -> p a (b d)", a=2) for i in range(2)]
            k = 0
            for n, i in enumerate((16, 17)):
                po = pO[n % 2]
                pok = "D_pO%d" % (n % 2)
                for h in range(4):
                    b = k % 2
                    k += 1
                    psk, ptk = "D_pS%d" % b, "D_pTc%d" % b
                    for t in range(2):
                        kb.mm(pSc[b][:, t, :], kT[:, h, SEQ + t * 128:SEQ + (t + 1) * 128], qT[:, h, i * 128:(i + 1) * 128], True, True,
                              rd=["D_kTall", "D_qTall"], wr=[psk])
                    kb.act(pT2[b][:], pSc[b], AF.Exp, rd=[psk], wr=[ptk], scale=0.125)
                    for t in range(2):
                        kb.mm(po[:, h, :], pT2[b][:, t, :], V1[:, 16 + t, h, :], t == 0, t == 1, rd=[ptk, "D_V1"], wr=[pok])
                attn_finish(kb, po, pok, 4, 128, None, yt[n % 2], "D_yt%d" % (n % 2), YB[i * 128:(i + 1) * 128, 768:1024], rec[n % 2], "D_rec%d" % (n % 2))


def attn_C(kb, l, need_ctx, Z, YB, ident, C):
    nc = kb.nc
    lam_init = 0.8 - 0.6 * math.exp(-0.3 * l)
    with ExitStack() as es:
        sb = lambda n, s, dt=F32: es.enter_context(nc.sbuf_tensor(kb.un(n), s, dt))
        ps = lambda n, s, dt=F32: es.enter_context(nc.psum_tensor(kb.un(n), s, dt))
        qT = sb("C_qT", [128, 3, NTOK], F32R)
        kT = sb("C_kT", [128, 3, NTOK], F32R)
        V1 = sb("C_V1", [128, NT, 4, 66], F32R)
        lv = sb("C_lv", [128, 4, 32])
        lp = sb("C_lp", [128, 2, 32])
        ls = sb("C_ls", [128, 2])
        nlam = sb("C_nlam", [128, 1])
        kb.dma(lv[:].rearrange("p a d -> p (a d)"), C["c_lambda"][l:l + 1].rearrange("o a d -> o (a d)").to_broadcast([128, 128]), rd=(), wr=["C_lv"])
        lvv = lv[:].rearrange("p (a b) d -> p a b d", b=2)
        kb.tt(lp[:], lvv[:, :, 0, :], lvv[:, :, 1, :], ALU.mult, rd=["C_lv"], wr=["C_lp"])
        kb.red(ls[:], lp[:], ALU.add, rd=["C_lp"], wr=["C_ls"])
        kb.act(ls[:], ls[:], AF.Exp, rd=["C_ls"], wr=["C_ls"])
        kb.tt(nlam[:], ls[:, 1:2], ls[:, 0:1], ALU.subtract, rd=["C_ls"], wr=["C_nlam"])
        kb.ts(nlam[:], nlam[:], -lam_init, None, ALU.add, None, rd=["C_nlam"], wr=["C_nlam"])
        sg = sb("C_sg", [128, 64])
        kb.dma(sg[:], C["c_subln"][l:l + 1, :].to_broadcast([128, 64]), rd=(), wr=["C_sg"])
        kb.ts(sg[:], sg[:], 1.0 - lam_init, None, ALU.mult, None, rd=["C_sg"], wr=["C_sg"])
        with ExitStack() as es2:
            vraw = es2.enter_context(nc.sbuf_tensor(kb.un("C_vraw"), [128, NT, 256], F32))
            load_v(kb, V1, "C_V1", Z, O_CV, 4, 0, NT, vraw, "C_vraw")
            ptrq = [es2.enter_context(nc.psum_tensor(kb.un("C_ptrq%d" % i), [128, 3, 128], F32)) for i in range(2)]
            ptrk = [es2.enter_context(nc.psum_tensor(kb.un("C_ptrk%d" % i), [128, 3, 128], F32)) for i in range(2)]

            def store(i, xn, xk):
                for (pp, pk, dst, dk, c0, eng) in ((ptrq[i % 2], "C_ptrq%d" % (i % 2), qT, "C_qT", 0, "scalar"),
                                                   (ptrk[i % 2], "C_ptrk%d" % (i % 2), kT, "C_kT", 256, "vector")):
                    for idx in range(3):
                        ncol = 96 if idx < 2 else 64
                        kb.tr(pp[0:ncol, idx, :], xn[:, c0 + idx * 96:c0 + idx * 96 + ncol], ident[:], rd=[xk], wr=[pk])
                    kb.cp(dst[0:96, 0:2, i * 128:(i + 1) * 128], pp[0:96, 0:2, :], rd=[pk], wr=[(dk, i)], eng=eng)
                    kb.cp(dst[0:64, 2, i * 128:(i + 1) * 128], pp[0:64, 2, :], rd=[pk], wr=[(dk, i)], eng=eng)
            qk_prep(kb, es2, l, Z, ident, O_CQ, 16, 32, 8, C["c_qk_norm"], C["ropeC"], range(NT), store)
        kb.S.barrier()
        pS = [ps("C_pS%d" % i, [128, 512]) for i in range(4)]
        pOT = [ps("C_pOT%d" % i, [128, 512]) for i in range(2)]
        pO2 = ps("C_pO2", [128, 4, 66])
        oT = [sb("C_oT%d" % i, [66, 512]) for i in range(2)]
        pTs = [sb("C_pT%d" % i, [128, NT, 512], F32R) for i in range(2)]
        ocs = [sb("C_oc%d" % i, [128, 4, 8, 66]) for i in range(2)]
        rec = sb("C_rec", [128, 4, 8])
        on = sb("C_on", [128, 4, 8, 64])
        od = sb("C_od", [128, 4, 4, 64])
        osq = sb("C_osq", [128, 4, 4, 64])
        oss = sb("C_oss", [128, 4, 4])
        yts = [sb("C_yt%d" % i, [128, 4, 4, 64]) for i in range(2)]
        blocks = [(qb * 512, 512, list(range(NT))) for qb in range(4)]
        if need_ctx:
            blocks.append((SEQ, 256, [16, 17]))
        ks = 0
        ko = 0
        scale = 32 ** -0.5
        pending = []

        def c_finish(q0, nq, nqt, bi):
            ob = ocs[bi % 2]
            okk = [("C_oc", bi % 2, qt_) for qt_ in range(nqt)]
            kb.recip(rec[:, 0:nqt, :], ob[:, 0:nqt, :, 64], rd=okk, wr=["C_rec"])
            kb.tt(on[:, 0:nqt], ob[:, 0:nqt, :, 0:64], rec[:, 0:nqt, :].unsqueeze(3).to_broadcast([128, nqt, 8, 64]), ALU.mult, rd=okk + ["C_rec"], wr=["C_on"])
            onv = on[:, 0:nqt].rearrange("p q (h m) d -> p q h m d", m=2)
            kb.stt(od[:, 0:nqt], onv[:, :, :, 1, :], nlam[:, 0:1], onv[:, :, :, 0, :], ALU.mult, ALU.add, rd=["C_on", "C_nlam"], wr=["C_od"])
            kb.tt(osq[:, 0:nqt], od[:, 0:nqt], od[:, 0:nqt], ALU.mult, rd=["C_od"], wr=["C_osq"], eng="gpsimd")
            kb.red(oss[:, 0:nqt, :], osq[:, 0:nqt], ALU.add, rd=["C_osq"], wr=["C_oss"])
            kb.rstd(oss[:, 0:nqt, :], oss[:, 0:nqt, :], 64, EPS, rd=["C_oss"], wr=["C_oss"])
            y = yts[bi % 2]
            yk = "C_yt%d" % (bi % 2)
            kb.tt(y[:, 0:nqt], od[:, 0:nqt], oss[:, 0:nqt, :].unsqueeze(3).to_broadcast([128, nqt, 4, 64]), ALU.mult, rd=["C_od", "C_oss"], wr=[yk])
            kb.tt(y[:, 0:nqt], y[:, 0:nqt], sg[:].unsqueeze(1).unsqueeze(1).to_broadcast([128, nqt, 4, 64]), ALU.mult, rd=[yk, "C_sg"], wr=[yk], eng="gpsimd")
            kb.dma(YB[q0:q0 + nq, 512:768].rearrange("(q p) c -> p q c", p=128), y[:, 0:nqt].rearrange("p q h d -> p q (h d)"), rd=[yk], wr=[("YB", yk)], semres=yk)

        for blk_i, (q0, nq, ktl) in enumerate(blocks):
            nqt = nq // 128
            oc = ocs[blk_i % 2]
            def pv_step(gp, e, j):
                hp = gp // 2
                bp = gp % 2
                kb.mm(pOT[bp][0:66, 0:nq], V1[:, j, hp, :], pTs[bp][:, e, 0:nq], e == 0, e == len(ktl) - 1,
                      rd=[("C_pT", bp, e), "C_V1"], wr=["C_pOT%d" % bp])

            def pv_finish(gp):
                bp = gp % 2
                kb.cp(oT[bp][:, 0:nq], pOT[bp][0:66, 0:nq], rd=["C_pOT%d" % bp], wr=["C_oT%d" % bp], eng="vector")
                for qt in range(nqt):
                    kb.tr(pO2[:, qt, :], oT[bp][:, qt * 128:(qt + 1) * 128], ident[0:66, 0:66], rd=["C_oT%d" % bp], wr=["C_pO2"])
                kb.cp(oc[:, 0:nqt, gp, :], pO2[:, 0:nqt, :], rd=["C_pO2"], wr=[("C_oc", blk_i % 2, qt_) for qt_ in range(nqt)], eng="vector")

            for g in range(8):
                h, s4, hh = g // 2, g % 3, g // 3
                bg = g % 2
                ents_ = list(enumerate(ktl))
                for c0 in range(0, len(ents_), 4):
                    for e, j in ents_[c0:c0 + 4]:
                        b = ks % 4
                        ks += 1
                        kb.mm(pS[b][:, 0:nq], kT[32 * s4:32 * s4 + 32, hh, j * 128:(j + 1) * 128], qT[32 * s4:32 * s4 + 32, hh, q0:q0 + nq], True, True,
                              rd=(), wr=["C_pS%d" % b])
                        kb.act(pTs[bg][:, e, 0:nq], pS[b][:, 0:nq], AF.Exp, rd=["C_pS%d" % b], wr=[("C_pT", bg, e)], scale=scale)
                    if g > 0:
                        for e, j in ents_[c0:c0 + 4]:
                            pv_step(g - 1, e, j)
                if g > 0:
                    pv_finish(g - 1)
                if g == 2 and pending:
                    c_finish(*pending.pop(0))
            for e, j in enumerate(ktl):
                pv_step(7, e, j)
            pv_finish(7)
            pending.append((q0, nq, nqt, blk_i))
        while pending:
            c_finish(*pending.pop(0))


def rwkv(kb, l, need_ctx, Z, YB, RW, ident, C):
    nc = kb.nc
    S = kb.S
    NEG = -math.exp(-0.5)
    with ExitStack() as es:
        sb = lambda n, s, dt=F32: es.enter_context(nc.sbuf_tensor(kb.un(n), s, dt))
        ps = lambda n, s, dt=F32: es.enter_context(nc.psum_tensor(kb.un(n), s, dt))
        mu = sb("R_mu", [128, 3, 896])
        kb.dma(mu[:, 0:2, :], C["b_shift"][l:l + 1].to_broadcast([128, 2, 896]), rd=(), wr=["R_mu"])
        kb.tt(mu[:, 2, :], mu[:, 0, :], mu[:, 1, :], ALU.add, rd=["R_mu"], wr=["R_mu"])
        kb.ts(mu[:, 2, :], mu[:, 2, :], -1.0, 1.0, ALU.mult, ALU.add, rd=["R_mu"], wr=["R_mu"])
        W2t = sb("R_W2t", [32, 512])
        A2t = sb("R_A2t", [32, 256])
        G2t = sb("R_G2t", [64, 256])
        kb.dma(W2t[:].rearrange("p (a n) -> p a n", a=2), C["b_w2"][l].rearrange("a p n -> p a n"), rd=(), wr=["R_Wl"])
        kb.dma(A2t[:], C["b_a2"][l], rd=(), wr=["R_Wl"])
        kb.dma(G2t[:], C["b_g2"][l], rd=(), wr=["R_Wl"])
        w0 = sb("R_w0", [128, 512])
        kb.dma(w0[:], C["b_w0"][l:l + 1].rearrange("o a n -> o (a n)").to_broadcast([128, 512]), rd=(), wr=["R_w0"])
        vb = sb("R_vb", [128, 3, 256])
        kb.dma(vb[:, 0, :], C["b_a0"][l:l + 1, :].to_broadcast([128, 256]), rd=(), wr=["R_vb"])
        kb.dma(vb[:, 1, :], C["b_kk"][l:l + 1, :].to_broadcast([128, 256]), rd=(), wr=["R_vb"])
        kb.dma(vb[:, 2, :], C["b_ka"][l:l + 1, :].to_broadcast([128, 256]), rd=(), wr=["R_vb"])
        NB = 6
        zc = [sb("R_zc%d" % i, [128, 896]) for i in range(2)]
        zp = [sb("R_zp%d" % i, [128, 896]) for i in range(2)]
        zn = [sb("R_zn%d" % i, [128, 896]) for i in range(2)]
        zs = [sb("R_zs%d" % i, [128, 896]) for i in range(NB)]
        lbT = [sb("R_lbT%d" % i, [64, 3, 128]) for i in range(NB)]
        rw = [sb("R_rw%d" % i, [128, 8, 256]) for i in range(NB)]
        tmp = [sb("R_tmp%d" % i, [128, 512]) for i in range(NB)]
        tmp2 = [sb("R_tmp2%d" % i, [128, 256]) for i in range(NB)]
        av = [sb("R_a%d" % i, [128, 256]) for i in range(NB)]
        ssk = [sb("R_ssk%d" % i, [128, 4]) for i in range(NB)]
        pl = [ps("R_pl%d" % i, [64, 3, 128]) for i in range(2)]
        pd = [ps("R_pd%d" % i, [128, 512]) for i in range(2)]
        pag = [ps("R_pag%d" % i, [128, 2, 256]) for i in range(2)]
        h3 = lambda ap: ap.rearrange("p (h d) -> p h d", d=64)

        def s0(i):
            b2, b = i % 2, i % NB
            kc, kp, kn, ks_ = "R_zc%d" % b2, "R_zp%d" % b2, "R_zn%d" % b2, "R_zs%d" % b
            r0 = i * 128
            kb.dma(zc[b2][:], Z[r0:r0 + 128, O_BZ:O_BZ + 896], rd=(), wr=[kc])
            if i in (0, 16):
                kb.memset(zp[b2][:], 0.0, wr=[kp])
                kb.dma(zp[b2][1:128, :], Z[r0:r0 + 127, O_BZ:O_BZ + 896], rd=(), wr=[kp])
            else:
                kb.dma(zp[b2][:], Z[r0 - 1:r0 + 127, O_BZ:O_BZ + 896], rd=(), wr=[kp])
            if i in (15, 17):
                kb.memset(zn[b2][:], 0.0, wr=[kn])
                kb.dma(zn[b2][0:127, :], Z[r0 + 1:r0 + 128, O_BZ:O_BZ + 896], rd=(), wr=[kn])
            else:
                kb.dma(zn[b2][:], Z[r0 + 1:r0 + 129, O_BZ:O_BZ + 896], rd=(), wr=[kn])
            kb.tt(zs[b][:], zc[b2][:], mu[:, 2, :], ALU.mult, rd=[kc, "R_mu"], wr=[ks_])
            kb.tt(zp[b2][:], zp[b2][:], mu[:, 0, :], ALU.mult, rd=[kp, "R_mu"], wr=[kp], eng="gpsimd")
            kb.tt(zn[b2][:], zn[b2][:], mu[:, 1, :], ALU.mult, rd=[kn, "R_mu"], wr=[kn], eng="gpsimd")

        def s1(i):
            b2, b = i % 2, i % NB
            kp, kn, ks_ = "R_zp%d" % b2, "R_zn%d" % b2, "R_zs%d" % b
            kb.tt(zs[b][:], zs[b][:], zp[b2][:], ALU.add, rd=[ks_, kp], wr=[ks_])
            kb.tt(zs[b][:], zs[b][:], zn[b2][:], ALU.add, rd=[ks_, kn], wr=[ks_])

        def s2(i):
            b2, b = i % 2, i % NB
            ks_, kl = "R_zs%d" % b, "R_lbT%d" % b
            z = zs[b]
            kb.tr(pl[b2][0:32, 0, :], z[:, 768:800], ident[:], rd=[ks_], wr=["R_pl%d" % b2])
            kb.tr(pl[b2][0:32, 1, :], z[:, 800:832], ident[:], rd=[ks_], wr=["R_pl%d" % b2])
            kb.tr(pl[b2][0:64, 2, :], z[:, 832:896], ident[:], rd=[ks_], wr=["R_pl%d" % b2])
            kb.act(lbT[b][0:32, 0, :], pl[b2][0:32, 0, :], AF.Tanh, rd=["R_pl%d" % b2], wr=[kl])
            kb.act(lbT[b][0:32, 1, :], pl[b2][0:32, 1, :], AF.Copy, rd=["R_pl%d" % b2], wr=[kl])
            kb.act(lbT[b][0:64, 2, :], pl[b2][0:64, 2, :], AF.Sigmoid, rd=["R_pl%d" % b2], wr=[kl])

        def s3(i):
            b2, b = i % 2, i % NB
            ks_, kl, kr = "R_zs%d" % b, "R_lbT%d" % b, "R_rw%d" % b
            z = zs[b]
            o = rw[b]
            kb.mm(pd[b2][:], lbT[b][0:32, 0, :], W2t[:], True, True, rd=[kl, "R_Wl"], wr=["R_pd%d" % b2])
            kb.mm(pag[b2][:, 0, :], lbT[b][0:32, 1, :], A2t[:], True, True, rd=[kl, "R_Wl"], wr=["R_pag%d" % b2])
            kb.mm(pag[b2][:, 1, :], lbT[b][0:64, 2, :], G2t[:], True, True, rd=[kl, "R_Wl"], wr=["R_pag%d" % b2])
            kb.cp(o[:, 0, :], z[:, 0:256], rd=[ks_], wr=[(kr, 0)], eng="gpsimd")
            kb.cp(o[:, 2, :], z[:, 512:768], rd=[ks_], wr=[(kr, 2)], eng="gpsimd")
            kb.tt(o[:, 3, :], z[:, 256:512], vb[:, 1, :], ALU.mult, rd=[ks_, "R_vb"], wr=[(kr, 3)])
            kb.tt(tmp2[b][:], o[:, 3, :], o[:, 3, :], ALU.mult, rd=[(kr, 3)], wr=["R_tmp2%d" % b])
            kb.red(ssk[b][:], h3(tmp2[b][:]), ALU.add, rd=["R_tmp2%d" % b], wr=["R_ssk%d" % b])
            kb.ts(ssk[b][:], ssk[b][:], 1e-24, None, ALU.max, None, rd=["R_ssk%d" % b], wr=["R_ssk%d" % b])

        def s4(i):
            b2, b = i % 2, i % NB
            kr = "R_rw%d" % b
            o = rw[b]
            kb.act(o[:, 7, :], pag[b2][:, 1, :], AF.Copy, rd=["R_pag%d" % b2], wr=[(kr, 7)])
            kb.tt(av[b][:], pag[b2][:, 0, :], vb[:, 0, :], ALU.add, rd=["R_pag%d" % b2, "R_vb"], wr=["R_a%d" % b])
            kb.tt(tmp[b][:], pd[b2][:], w0[:], ALU.add, rd=["R_pd%d" % b2, "R_w0"], wr=["R_tmp%d" % b])
            kb.act(av[b][:], av[b][:], AF.Sigmoid, rd=["R_a%d" % b], wr=["R_a%d" % b])
            kb.act(tmp[b][:], tmp[b][:], AF.Sigmoid, rd=["R_tmp%d" % b], wr=["R_tmp%d" % b])
            kb.act(ssk[b][:], ssk[b][:], AF.Sqrt, rd=["R_ssk%d" % b], wr=["R_ssk%d" % b])

        def s5(i):
            b2, b = i % 2, i % NB
            ks_, kr = "R_zs%d" % b, "R_rw%d" % b
            z = zs[b]
            o = rw[b]
            kb.ts(o[:, 5:7, :].rearrange("p a n -> p (a n)"), tmp[b][:], NEG, None, ALU.mult, None, rd=["R_tmp%d" % b], wr=[(kr, 5)], eng="gpsimd")
            kb.recip(ssk[b][:], ssk[b][:], rd=["R_ssk%d" % b], wr=["R_ssk%d" % b])
            kb.tt(h3(o[:, 3, :]), h3(o[:, 3, :]), ssk[b][:].unsqueeze(2).to_broadcast([128, 4, 64]), ALU.mult, rd=[(kr, 3), "R_ssk%d" % b], wr=[(kr, 3)])
            kb.tt(o[:, 4, :], o[:, 3, :], av[b][:], ALU.mult, rd=[(kr, 3), "R_a%d" % b], wr=[(kr, 4)], eng="gpsimd")
            kb.stt(tmp2[b][:], av[b][:], -1.0, vb[:, 2, :], ALU.add, ALU.mult, rd=["R_a%d" % b, "R_vb"], wr=["R_tmp2%d" % b])
            kb.stt(o[:, 1, :], tmp2[b][:], 1.0, z[:, 256:512], ALU.add, ALU.mult, rd=["R_tmp2%d" % b, ks_], wr=[(kr, 1)])

        def s6(i):
            b = i % NB
            kr = "R_rw%d" % b
            kb.dma(RW[i * 128:(i + 1) * 128], rw[b][:], rd=[(kr, j_) for j_ in (0, 1, 2, 3, 4, 5, 7)], wr=[("RW", i)], semres=kr)

        pipeline([s0, s1, s2, s3, s4, s5, s6], list(range(NT)))
    S.barrier()
    if "noscan" in kb.debug:
        return
    with ExitStack() as es:
        sb = lambda n, s, dt=F32: es.enter_context(nc.sbuf_tensor(kb.un(n), s, dt))
        ps = lambda n, s, dt=F32: es.enter_context(nc.psum_tensor(kb.un(n), s, dt))
        msk = sb("S_msk", [128, 2, 512])
        kb.dma(msk[:], C["rwkv_masks"].rearrange("d p n -> p d n"), rd=(), wr=["S_msk"])
        ones2 = sb("S_ones", [128, 2])
        kb.memset(ones2[:], 1.0, wr=["S_ones"])
        yacc = sb("S_yacc", [128, NT, 256])
        kb.memset(yacc[:].rearrange("p t c -> p (t c)"), 0.0, wr=[("S_yacc", i_) for i_ in range(NT)])
        Hs = [sb("S_H%d" % d_, [64, 4, 64]) for d_ in range(2)]
        rwt = [sb("S_rw%d" % i_, [128, 8, 256]) for i_ in range(4)]
        exs = [sb("S_ex%d" % i_, [128, 3, 256]) for i_ in range(4)]
        clxs = [sb("S_clx%d" % i_, [128, 256]) for i_ in range(4)]
        q4s = [sb("S_q4%d" % i_, [128, 4, 256]) for i_ in range(4)]
        T4s = [[sb("S_T4_%d_%d" % (i_, h), [64, 4, 128]) for h in range(4)] for i_ in range(4)]
        GMs = [[sb("S_GM%d_%d" % (i_, h), [128, 2, 256]) for h in range(4)] for i_ in range(4)]
        Xss = [[sb("S_X%d_%d" % (d_, i_), [128, 4, 128]) for i_ in range(2)] for d_ in range(2)]
        XTss = [[sb("S_XT%d_%d" % (d_, i_), [128, 4, 128]) for i_ in range(2)] for d_ in range(2)]
        TTss = [[sb("S_TT%d_%d" % (d_, i_), [128, 4, 128]) for i_ in range(2)] for d_ in range(2)]
        pcs = [sb("S_pc%d" % i_, [64, 4]) for i_ in range(4)]
        R1s = [sb("S_R1%d" % d_, [128, 4, 64]) for d_ in range(2)]
        nUs = [sb("S_nU%d" % d_, [128, 4, 64]) for d_ in range(2)]
        pb = [ps("S_pb%d" % i, [128, 512]) for i in range(8)]
        P = lambda i: "S_pb%d" % i
        orders = ([16, 17] + list(range(16)), [17, 16] + list(range(15, -1, -1)))
        for d_ in range(2):
            kb.memset(Hs[d_][:], 0.0, wr=["S_H_d%d" % d_])

        def front(d, n):
            i = orders[d][n]
            sl = "_s%d" % (d * 2 + n % 2)
            dl = "_d%d" % d
            w, ex, clx, q4, T4, GM, pc = rwt[d * 2 + n % 2], exs[d * 2 + n % 2], clxs[d * 2 + n % 2], q4s[d * 2 + n % 2], T4s[d * 2 + n % 2], GMs[d * 2 + n % 2], pcs[d * 2 + n % 2]
            H, Xs, XTs, TTs, R1, nU = Hs[d], Xss[d], XTss[d], TTss[d], R1s[d], nUs[d]
            rk_ = "S_rw" + sl
            kb.dma(w[:], RW[i * 128:(i + 1) * 128], rd=(), wr=[rk_])
            lw = w[:, 5 + d, :]
            kb.mm(pb[0][:, 0:256], msk[:, d, 0:128], lw, True, True, rd=["S_msk", rk_], wr=[P(0)])
            for h in range(4):
                kb.mm(pb[0][0:64, 256 + 2 * h:258 + 2 * h], w[:, 5 + d, h * 64:(h + 1) * 64], ones2[:], True, True, rd=[rk_, "S_ones"], wr=[P(0)])
            kb.act(ex[:, 0, :], pb[0][:, 0:256], AF.Exp, rd=[P(0)], wr=["S_ex" + sl])
            kb.act(ex[:, 2, :], pb[0][:, 0:256], AF.Exp, rd=[P(0)], wr=["S_ex" + sl], scale=-1.0)
            kb.tt(clx[:], pb[0][:, 0:256], lw, ALU.subtract, rd=[P(0), rk_], wr=["S_clx" + sl])
            kb.act(ex[:, 1, :], clx[:], AF.Exp, rd=["S_clx" + sl], wr=["S_ex" + sl])
            kb.act(pc[:], pb[0][0:64, 256:264].rearrange("p (h t) -> p h t", t=2)[:, :, 0], AF.Exp, rd=[P(0)], wr=["S_pc" + sl])
            kb.tt(q4[:, 0, :], w[:, 3, :], ex[:, 1, :], ALU.mult, rd=[rk_, "S_ex" + sl], wr=["S_q4" + sl])
            kb.tt(q4[:, 1, :], w[:, 0, :], ex[:, 0, :], ALU.mult, rd=[rk_, "S_ex" + sl], wr=["S_q4" + sl], eng="gpsimd")
            kb.tt(q4[:, 2, :], w[:, 4, :], ex[:, 2, :], ALU.mult, rd=[rk_, "S_ex" + sl], wr=["S_q4" + sl])
            kb.tt(q4[:, 3, :], w[:, 1, :], ex[:, 2, :], ALU.mult, rd=[rk_, "S_ex" + sl], wr=["S_q4" + sl], eng="gpsimd")
            for h in range(4):
                tb_ = 1 if h % 2 == 0 else 0
                for kd in range(4):
                    kb.tr(pb[tb_][0:64, kd * 128:(kd + 1) * 128], q4[:, kd, h * 64:(h + 1) * 64], ident[:], rd=["S_q4" + sl], wr=[P(tb_)])
                kb.cp(T4[h][:].rearrange("p k t -> p (k t)"), pb[tb_][0:64, :], rd=[P(tb_)], wr=["S_T4_%d" % h + sl], eng="scalar" if h % 2 else "vector")
            for h in range(4):
                t4 = T4[h]
                tk = "S_T4_%d" % h + sl
                gb_ = 2 if h % 2 == 0 else 7
                kb.mm(pb[gb_][:, 0:256], t4[:, 2, :], t4[:, 0:2, :], True, True, rd=[tk], wr=[P(gb_)])
                kb.mm(pb[gb_][:, 256:512], t4[:, 3, :], t4[:, 0:2, :], True, True, rd=[tk], wr=[P(gb_)])
                kb.mm(pb[3][:, h * 128:(h + 1) * 128], t4[:, 0, :], t4[:, 2, :], True, True, rd=[tk], wr=[P(3)])
                kb.tt(GM[h][:], pb[gb_][:].rearrange("p (a n) -> p a n", a=2), msk[:, d, 128:384].unsqueeze(1).to_broadcast([128, 2, 256]), ALU.mult,
                      rd=[P(gb_), "S_msk"], wr=["S_GM%d" % h + sl])
            X, XT, TT = Xs[0], XTs[0], TTs[0]
            XK = lambda c_, p_: ("S_X", c_, p_, d)
            XTK = lambda c_, p_: ("S_XT", c_, p_, d)
            TTK = lambda c_, p_: ("S_TT", c_, p_, d)
            kb.tt(X[:], pb[3][:].rearrange("p (h n) -> p h n", h=4), msk[:, d, 384:512].unsqueeze(1).to_broadcast([128, 4, 128]), ALU.mult,
                  rd=[P(3), "S_msk"], wr=[XK(0, 0), XK(0, 1)])
            for h in range(4):
                kb.cp(XT[:, h, :], GM[h][:, 0, 0:128], rd=["S_GM%d" % h + sl], wr=[XTK(0, h // 2)], eng="gpsimd")
                kb.tt(TT[:, h, :], ident[:], GM[h][:, 0, 0:128], ALU.subtract, rd=["ident", "S_GM%d" % h + sl], wr=[TTK(0, h // 2)])

        def back(d, n):
            i = orders[d][n]
            sl = "_s%d" % (d * 2 + n % 2)
            dl = "_d%d" % d
            w, ex, clx, q4, T4, GM, pc = rwt[d * 2 + n % 2], exs[d * 2 + n % 2], clxs[d * 2 + n % 2], q4s[d * 2 + n % 2], T4s[d * 2 + n % 2], GMs[d * 2 + n % 2], pcs[d * 2 + n % 2]
            H, Xs, XTs, TTs, R1, nU = Hs[d], Xss[d], XTss[d], TTss[d], R1s[d], nUs[d]
            rk_ = "S_rw" + sl
            lw = w[:, 5 + d, :]
            XK = lambda c_, p_: ("S_X", c_, p_, d)
            XTK = lambda c_, p_: ("S_XT", c_, p_, d)
            TTK = lambda c_, p_: ("S_TT", c_, p_, d)
            NB_ = ((4, 5, 6), (2, 7, 1))
            cur = 0
            for kq in range(6):
                nx = 1 - cur
                Xc, XTc, TTc = Xs[cur], XTs[cur], TTs[cur]
                Xn, XTn, TTn = Xs[nx], XTs[nx], TTs[nx]
                for p_ in range(2):
                    bX = NB_[p_][0]
                    for h in (2 * p_, 2 * p_ + 1):
                        kb.mm(pb[bX][:, (h % 2) * 128:(h % 2 + 1) * 128], XTc[:, h, :], Xc[:, h, :], True, True, rd=[XK(cur, p_), XTK(cur, p_)], wr=[P(bX)])
                    kb.cp(Xn[:, 2 * p_:2 * p_ + 2, :].rearrange("p h n -> p (h n)"), pb[bX][:, 0:256], rd=[P(bX)], wr=[XK(nx, p_)], eng="vector")
                if kq < 5:
                    for p_ in range(2):
                        bXT = NB_[p_][1]
                        for h in (2 * p_, 2 * p_ + 1):
                            kb.mm(pb[bXT][:, (h % 2) * 128:(h % 2 + 1) * 128], Xc[:, h, :], XTc[:, h, :], True, True, rd=[XK(cur, p_), XTK(cur, p_)], wr=[P(bXT)])
                        kb.cp(XTn[:, 2 * p_:2 * p_ + 2, :].rearrange("p h n -> p (h n)"), pb[bXT][:, 0:256], rd=[P(bXT)], wr=[XTK(nx, p_)], eng="scalar")
                for p_ in range(2):
                    bT = NB_[p_][2]
                    for h in (2 * p_, 2 * p_ + 1):
                        kb.mm(pb[bT][:, (h % 2) * 128:(h % 2 + 1) * 128], Xn[:, h, :], TTc[:, h, :], True, True, rd=[XK(nx, p_), TTK(cur, p_)], wr=[P(bT)])
                    kb.tt(TTn[:, 2 * p_:2 * p_ + 2, :].rearrange("p h n -> p (h n)"), pb[bT][:, 0:256],
                          TTc[:, 2 * p_:2 * p_ + 2, :].rearrange("p h n -> p (h n)"), ALU.add, rd=[P(bT), TTK(cur, p_)], wr=[TTK(nx, p_)])
                cur = nx
            TTf = TTs[cur]
            ktf = TTK(cur, 0)
            ktf1 = TTK(cur, 1)
            for h in range(4):
                kb.mm(pb[7][:, h * 64:(h + 1) * 64], T4[h][:, 0, :], H[:, h, :], True, False, rd=["S_T4_%d" % h + sl, "S_H" + dl], wr=[P(7)])
                kb.mm(pb[7][:, h * 64:(h + 1) * 64], GM[h][:, 1, 0:128], w[:, 2, h * 64:(h + 1) * 64], False, True, rd=["S_GM%d" % h + sl, rk_], wr=[P(7)])
            kb.cp(R1[:].rearrange("p h v -> p (h v)"), pb[7][:, 0:256], rd=[P(7)], wr=["S_R1" + dl], eng="vector")
            for h in range(4):
                kb.mm(pb[7][:, 256 + h * 64:256 + (h + 1) * 64], TTf[:, h, :], R1[:, h, :], True, True, rd=[ktf, ktf1, "S_R1" + dl], wr=[P(7)])
            kb.ts(nU[:].rearrange("p h v -> p (h v)"), pb[7][:, 256:512], -1.0, None, ALU.mult, None, rd=[P(7)], wr=["S_nU" + dl])
            want_y = (i < 16) or need_ctx
            if want_y:
                for h in range(4):
                    o = pb[3][:, h * 64:(h + 1) * 64]
                    kb.mm(o, T4[h][:, 1, :], H[:, h, :], True, False, rd=["S_T4_%d" % h + sl, "S_H" + dl], wr=[P(3)])
                    kb.mm(o, GM[h][:, 0, 128:256], nU[:, h, :], False, False, rd=["S_GM%d" % h + sl, "S_nU" + dl], wr=[P(3)])
                    kb.mm(o, GM[h][:, 1, 128:256], w[:, 2, h * 64:(h + 1) * 64], False, True, rd=["S_GM%d" % h + sl, rk_], wr=[P(3)])
                kb.tt(yacc[:, i, :], pb[3][:, 0:256], yacc[:, i, :], ALU.add, rd=[P(3), ("S_yacc", i)], wr=[("S_yacc", i)])
            for h in range(4):
                o = pb[2][0:64, h * 64:(h + 1) * 64]
                kb.mm(o, ident[0:64, 0:64], H[:, h, :], True, False, rd=["ident", "S_H" + dl], wr=[P(2)])
                kb.mm(o, q4[:, 2, h * 64:(h + 1) * 64], nU[:, h, :], False, False, rd=["S_q4" + sl, "S_nU" + dl], wr=[P(2)])
                kb.mm(o, q4[:, 3, h * 64:(h + 1) * 64], w[:, 2, h * 64:(h + 1) * 64], False, True, rd=["S_q4" + sl, rk_], wr=[P(2)])
            kb.tt(H[:], pb[2][0:64, 0:256].rearrange("p (h v) -> p h v", h=4), pc[:].unsqueeze(2).to_broadcast([64, 4, 64]), ALU.mult,
                  rd=[P(2), "S_pc" + sl], wr=["S_H" + dl])

        NCH = len(orders[0])
        front(0, 0)
        front(1, 0)
        for n in range(NCH):
            back(0, n)
            if n + 1 < NCH:
                front(0, n + 1)
            back(1, n)
            if n + 1 < NCH:
                front(1, n + 1)
        kb.S.barrier()
        ln = sb("S_ln", [128, 3, 256])
        kb.dma(ln[:, 0:2, :], C["b_lnx"][l:l + 1].to_broadcast([128, 2, 256]), rd=(), wr=["S_ln"])
        kb.dma(ln[:, 2, :], C["b_rk"][l:l + 1, :].to_broadcast([128, 256]), rd=(), wr=["S_ln"])
        NBR = 4
        st = [sb("S_st%d" % i, [128, 4]) for i in range(NBR)]
        st2 = [sb("S_st2%d" % i, [128, 4]) for i in range(NBR)]
        yc_ = [sb("S_yc%d" % i, [128, 256]) for i in range(NBR)]
        t1 = [sb("S_t1%d" % i, [128, 256]) for i in range(NBR)]
        t3 = [sb("S_t3%d" % i, [128, 256]) for i in range(NBR)]
        bo = [sb("S_bo%d" % i, [128, 4]) for i in range(NBR)]
        yo = [sb("S_yo%d" % i, [128, 256]) for i in range(NBR)]
        rwr = rwt
        v3 = lambda ap: ap.rearrange("p (h d) -> p h d", d=64)
        tiles = list(range(NT)) if need_ctx else list(range(16))

        def r0_(i):
            b = i % NBR
            rk_ = "S_rwr%d" % b
            w = rwr[b]
            kb.dma(w[:], RW[i * 128:(i + 1) * 128], rd=(), wr=[rk_])
            y = yacc[:, i, :]
            yk = ("S_yacc", i)
            kb.red(st[b][:], v3(y), ALU.add, rd=[yk], wr=["S_st%d" % b])
            kb.ts(st[b][:], st[b][:], 1.0 / 64, None, ALU.mult, None, rd=["S_st%d" % b], wr=["S_st%d" % b])
            kb.tt(v3(yc_[b][:]), v3(y), st[b][:].unsqueeze(2).to_broadcast([128, 4, 64]), ALU.subtract, rd=[yk, "S_st%d" % b], wr=["S_yc%d" % b])
            kb.tt(t1[b][:], yc_[b][:], yc_[b][:], ALU.mult, rd=["S_yc%d" % b], wr=["S_t1%d" % b], eng="gpsimd")

        def r1_(i):
            b = i % NBR
            rk_ = "S_rwr%d" % b
            w = rwr[b]
            kb.red(st2[b][:], v3(t1[b][:]), ALU.add, rd=["S_t1%d" % b], wr=["S_st2%d" % b])
            kb.ts(st2[b][:], st2[b][:], 1.0 / 64, 64e-5, ALU.mult, ALU.add, rd=["S_st2%d" % b], wr=["S_st2%d" % b])
            kb.act(st2[b][:], st2[b][:], AF.Sqrt, rd=["S_st2%d" % b], wr=["S_st2%d" % b])
            kb.tt(t3[b][:], w[:, 0, :], w[:, 1, :], ALU.mult, rd=[rk_], wr=["S_t3%d" % b], eng="gpsimd")
            kb.tt(t3[b][:], t3[b][:], ln[:, 2, :], ALU.mult, rd=["S_t3%d" % b, "S_ln"], wr=["S_t3%d" % b], eng="gpsimd")

        def r2_(i):
            b = i % NBR
            rk_ = "S_rwr%d" % b
            w = rwr[b]
            kb.recip(st2[b][:], st2[b][:], rd=["S_st2%d" % b], wr=["S_st2%d" % b])
            kb.tt(v3(yc_[b][:]), v3(yc_[b][:]), st2[b][:].unsqueeze(2).to_broadcast([128, 4, 64]), ALU.mult, rd=["S_yc%d" % b, "S_st2%d" % b], wr=["S_yc%d" % b])
            kb.tt(yc_[b][:], yc_[b][:], ln[:, 0, :], ALU.mult, rd=["S_yc%d" % b, "S_ln"], wr=["S_yc%d" % b], eng="gpsimd")
            kb.red(bo[b][:], v3(t3[b][:]), ALU.add, rd=["S_t3%d" % b], wr=["S_bo%d" % b])
            kb.tt(v3(t3[b][:]), v3(w[:, 2, :]), bo[b][:].unsqueeze(2).to_broadcast([128, 4, 64]), ALU.mult, rd=[rk_, "S_bo%d" % b, "S_t3%d" % b], wr=["S_t3%d" % b])

        def r3_(i):
            b = i % NBR
            rk_ = "S_rwr%d" % b
            w = rwr[b]
            kb.tt(t3[b][:], t3[b][:], ln[:, 1, :], ALU.add, rd=["S_t3%d" % b, "S_ln"], wr=["S_t3%d" % b], eng="gpsimd")
            kb.tt(yc_[b][:], yc_[b][:], t3[b][:], ALU.add, rd=["S_yc%d" % b, "S_t3%d" % b], wr=["S_yc%d" % b])
            kb.tt(yo[b][:], yc_[b][:], w[:, 7, :], ALU.mult, rd=["S_yc%d" % b, rk_], wr=["S_yo%d" % b])
            kb.dma(YB[i * 128:(i + 1) * 128, 256:512], yo[b][:], rd=["S_yo%d" % b], wr=[("YB", "S_yo%d" % b)], semres="S_yo%d" % b)

        pipeline([r0_, r1_, r2_, r3_], tiles)


def merge_phase(kb, l, need_ctx, x_in, ctx_in, X, Z, YB, H2T, MOD, ident, C, gate_all):
    nc = kb.nc
    with ExitStack() as es:
        sb = lambda n, s, dt=F32: es.enter_context(nc.sbuf_tensor(kb.un(n), s, dt))
        ps = lambda n, s, dt=F32: es.enter_context(nc.psum_tensor(kb.un(n), s, dt))
        wbr = sb("M_wbr", [128, 8, D], F32R)
        wout = sb("M_wout", [128, 8, D], F32R)
        kb.dma(wbr[:], C["w_branch"][l].rearrange("n (c p) d -> p (n c) d", p=128), rd=(), wr=["M_wbr"], q="gpsimd")
        kb.dma(wout[:], C["w_out"][l].rearrange("(c p) d -> p c d", p=128), rd=(), wr=["M_wout"], q="gpsimd")
        rwt = sb("M_rw", [128, 8, 16])
        kb.dma(rwt[:], C["router_w"].rearrange("(c p) e -> p c e", p=128), rd=(), wr=["M_rw"])
        rb = sb("M_rb", [128, 16])
        kb.dma(rb[:], C["router_b"].unsqueeze(0).to_broadcast([128, 16]), rd=(), wr=["M_rb"])
        gm = [sb("M_gm%d" % s, [128, D]) for s in range(2)]
        A2 = [sb("M_A2%d" % s, [128, D]) for s in range(2)]
        B2 = [sb("M_B2%d" % s, [128, D]) for s in range(2)]
        gbc = sb("M_gbc", [128, D])
        kb.dma(gbc[:], C["norm2"][l:l + 1, :].to_broadcast([128, D]), rd=(), wr=["M_gbc"])
        for s in range(2):
            kb.dma(gm[s][:], MOD[l, s:s + 1, 2 * D:3 * D].to_broadcast([128, D]), rd=(), wr=["M_gm%d" % s])
            kb.dma(A2[s][:], MOD[l, s:s + 1, 4 * D:5 * D].to_broadcast([128, D]), rd=(), wr=["M_A2%d" % s])
            kb.dma(B2[s][:], MOD[l, s:s + 1, 3 * D:4 * D].to_broadcast([128, D]), rd=(), wr=["M_B2%d" % s])
            kb.stt(A2[s][:], A2[s][:], 1.0, gbc[:], ALU.add, ALU.mult, rd=["M_A2%d" % s, "M_gbc"], wr=["M_A2%d" % s])
        yb_ = [sb("M_yb%d" % i_, [128, D]) for i_ in range(2)]
        ybT_ = [sb("M_ybT%d" % i_, [128, 8, 128], F32R) for i_ in range(2)]
        gt_ = [sb("M_gt%d" % i_, [128, 4 * D]) for i_ in range(2)]
        m_ = [sb("M_m%d" % i_, [128, D]) for i_ in range(2)]
        tmp_ = [sb("M_tmp%d" % i_, [128, 512]) for i_ in range(3)]
        mT_ = [sb("M_mT%d" % i_, [128, 8, 128], F32R) for i_ in range(2)]
        xt_ = [sb("M_xt%d" % i_, [128, D]) for i_ in range(2)]
        xn_ = [sb("M_xn%d" % i_, [128, D]) for i_ in range(2)]
        ss_ = [sb("M_ss%d" % i_, [128, 1]) for i_ in range(2)]
        h2_ = [sb("M_h2%d" % i_, [128, D]) for i_ in range(2)]
        h2T_ = [sb("M_h2T%d" % i_, [128, 8, 128]) for i_ in range(2)]
        sc = sb("M_sc", [128, NT, 16])
        bi = sb("M_bi", [128, NT, 16])
        r4 = sb("M_r4", [128, 8, NT, 4])
        msk = sb("M_msk", [128, NT, 16])
        r1 = sb("M_r1", [128, 4, NT])
        M_ptr = [ps("M_ptr%d" % i, [128, 4, 128]) for i in range(2)]
        M_pq = [ps("M_pq%d" % i, [128, 512]) for i in range(3)]
        M_pr = ps("M_pr", [128, 16])
        tiles = list(range(NT)) if need_ctx else list(range(16))
        kqc = [0]

        def transposes(src, srck, dst, dstk):
            for g in range(2):
                for j in range(4):
                    c = g * 4 + j
                    kb.tr(M_ptr[g][:, j, :], src[:, c * 128:(c + 1) * 128], ident[:], rd=srck, wr=["M_ptr%d" % g])
                kb.cp(dst[:, g * 4:(g + 1) * 4, :], M_ptr[g][:], rd=["M_ptr%d" % g], wr=[dstk], eng="scalar" if g == 0 else "vector")

        def s0(i):
            bb_ = i % 2
            kb.dma(yb_[bb_][:], YB[i * 128:(i + 1) * 128, :], rd=(), wr=["M_yb_%d" % bb_])

        def s1(i):
            bb_ = i % 2
            transposes(yb_[bb_], ["M_yb_%d" % bb_], ybT_[bb_], "M_ybT_%d" % bb_)
            kb.dma(gt_[bb_][:], Z[i * 128:(i + 1) * 128, O_G:O_G + 4 * D], rd=(), wr=[("M_gt", bb_, n_) for n_ in range(4)])

        def s2(i):
            bb_ = i % 2
            r0 = i * 128
            gt, m, ybT, xt = gt_[bb_], m_[bb_], ybT_[bb_], xt_[bb_]
            if l == 0:
                src = x_in[r0:r0 + 128, :] if i < 16 else ctx_in[r0 - SEQ:r0 - SEQ + 128, :]
            else:
                src = X[r0:r0 + 128, :]
            kb.dma(xt[:], src, rd=(), wr=["M_xt_%d" % bb_])
            for n in range(4):
                kb.act(gt[:, n * D:(n + 1) * D], gt[:, n * D:(n + 1) * D], AF.Sigmoid, rd=[("M_gt", bb_, n)], wr=[("M_gt", bb_, n)])
            for n in range(4):
                for hf in range(2):
                    b = kqc[0] % 3
                    kqc[0] += 1
                    pk = "M_pq%d" % b
                    for cc in range(2):
                        c = 2 * n + cc
                        kb.mm(M_pq[b][:], ybT[:, c, :], wbr[:, c, hf * 512:(hf + 1) * 512], cc == 0, cc == 1, rd=["M_ybT_%d" % bb_, "M_wbr"], wr=[pk])
                    gsl = gt[:, n * D + hf * 512:n * D + (hf + 1) * 512]
                    mk = ("M_m", hf, bb_)
                    if n == 0:
                        kb.tt(m[:, hf * 512:(hf + 1) * 512], M_pq[b][:], gsl, ALU.mult, rd=[pk, ("M_gt", bb_, n)], wr=[mk])
                    else:
                        tmp = tmp_[b]
                        kb.tt(tmp[:], M_pq[b][:], gsl, ALU.mult, rd=[pk, ("M_gt", bb_, n)], wr=["M_tmp%d" % b])
                        kb.tt(m[:, hf * 512:(hf + 1) * 512], m[:, hf * 512:(hf + 1) * 512], tmp[:], ALU.add, rd=[mk, "M_tmp%d" % b], wr=[mk], eng="gpsimd")
            transposes(m, [("M_m", 0, bb_), ("M_m", 1, bb_)], mT_[bb_], "M_mT_%d" % bb_)

        def s3(i):
            bb_ = i % 2
            s = 0 if i < 16 else 1
            r0 = i * 128
            mT, xt, xn, ss, h2 = mT_[bb_], xt_[bb_], xn_[bb_], ss_[bb_], h2_[bb_]
            xk = [("M_xnh", 0, bb_), ("M_xnh", 1, bb_)]
            for hf in range(2):
                b = kqc[0] % 3
                kqc[0] += 1
                pk = "M_pq%d" % b
                for c in range(8):
                    kb.mm(M_pq[b][:], mT[:, c, :], wout[:, c, hf * 512:(hf + 1) * 512], c == 0, c == 7, rd=["M_mT_%d" % bb_, "M_wout"], wr=[pk])
                tmp = tmp_[b]
                kb.tt(tmp[:], M_pq[b][:], gm[s][:, hf * 512:(hf + 1) * 512], ALU.mult, rd=[pk, "M_gm%d" % s], wr=["M_tmp%d" % b])
                kb.tt(xn[:, hf * 512:(hf + 1) * 512], tmp[:], xt[:, hf * 512:(hf + 1) * 512], ALU.add, rd=["M_tmp%d" % b, "M_xt_%d" % bb_], wr=[xk[hf]], eng="gpsimd")
            kb.dma(X[r0:r0 + 128, :], xn[:], rd=xk, wr=[("X", i)], semres="M_xn_%d" % bb_)
            hk = "M_h2_%d" % bb_
            sk = "M_ss_%d" % bb_
            kb.act(h2[:], xn[:], AF.Square, rd=xk, wr=[hk, sk], accum_out=ss[:])
            kb.rstd(ss[:], ss[:], D, EPS, rd=[sk], wr=[sk])
            kb.stt(h2[:], xn[:], ss[:, 0:1], A2[s][:], ALU.mult, ALU.mult, rd=xk + [sk, "M_A2%d" % s], wr=[hk])
            kb.tt(h2[:], h2[:], B2[s][:], ALU.add, rd=[hk, "M_B2%d" % s], wr=[hk])

        def s4(i):
            bb_ = i % 2
            r0 = i * 128
            h2, h2T = h2_[bb_], h2T_[bb_]
            tk = "M_h2T_%d" % bb_
            transposes(h2, ["M_h2_%d" % bb_], h2T, tk)
            kb.dma(H2T[:, :, r0:r0 + 128], h2T[:], rd=[tk], wr=[("H2T", i)], semres=tk)
            for c in range(8):
                kb.mm(M_pr[:], h2T[:, c, :], rwt[:, c, :], c == 0, c == 7, rd=[tk, "M_rw"], wr=["M_pr"])
            kb.act(sc[:, i, :], M_pr[:], AF.Sigmoid, rd=["M_pr"], wr=[("M_sc", i)])

        pipeline([s0, s1, s2, s3, s4], tiles)
        T_ = len(tiles)
        sck = [("M_sc", i_) for i_ in tiles]
        scv = sc[:, 0:T_, :]
        kb.tt(bi[:, 0:T_, :], scv, rb[:].unsqueeze(1).to_broadcast([128, T_, 16]), ALU.add, rd=sck + ["M_rb"], wr=["M_bi"])
        b4 = bi[:, 0:T_, :].rearrange("p t (g j) -> p t g j", j=4)
        K_ = ["M_r4"]
        R = lambda k_: r4[:, k_, 0:T_, :]
        kb.tt(R(0), b4[:, :, :, 0], b4[:, :, :, 1], ALU.max, rd=["M_bi"], wr=K_)
        kb.tt(R(1), b4[:, :, :, 0], b4[:, :, :, 1], ALU.min, rd=["M_bi"], wr=K_)
        kb.tt(R(2), b4[:, :, :, 2], b4[:, :, :, 3], ALU.max, rd=["M_bi"], wr=K_)
        kb.tt(R(3), b4[:, :, :, 2], b4[:, :, :, 3], ALU.min, rd=["M_bi"], wr=K_)
        kb.tt(R(4), R(0), R(2), ALU.max, rd=K_, wr=K_)
        kb.tt(R(5), R(0), R(2), ALU.min, rd=K_, wr=K_)
        kb.tt(R(6), R(1), R(3), ALU.max, rd=K_, wr=K_)
        kb.tt(R(5), R(5), R(6), ALU.max, rd=K_, wr=K_)
        kb.tt(R(4), R(4), R(5), ALU.add, rd=K_, wr=K_)
        kb.red(r1[:, 0, 0:T_], R(4), ALU.max, rd=K_, wr=["M_r1"])
        kb.tt(R(7), R(4), r1[:, 0, 0:T_].unsqueeze(2).to_broadcast([128, T_, 4]), ALU.is_equal, rd=K_ + ["M_r1"], wr=K_)
        kb.ts(R(6), R(7), 1e30, -1e30, ALU.mult, ALU.add, rd=K_, wr=K_)
        m4 = msk[:, 0:T_, :].rearrange("p t (g j) -> p t g j", j=4)
        kb.tt(m4, b4, R(7).unsqueeze(3).to_broadcast([128, T_, 4, 4]), ALU.mult, rd=["M_bi"] + K_, wr=["M_msk"])
        kb.tt(m4, m4, R(6).unsqueeze(3).to_broadcast([128, T_, 4, 4]), ALU.add, rd=["M_msk"] + K_, wr=["M_msk"])
        mv = msk[:, 0:T_, :]
        bv = bi[:, 0:T_, :]
        kb.red(r1[:, 1, 0:T_], mv, ALU.max, rd=["M_msk"], wr=["M_r1"])
        kb.tt(bv, mv, r1[:, 1, 0:T_].unsqueeze(2).to_broadcast([128, T_, 16]), ALU.is_equal, rd=["M_msk", "M_r1"], wr=["M_bi"])
        kb.stt(mv, bv, -1e30, mv, ALU.mult, ALU.add, rd=["M_bi", "M_msk"], wr=["M_msk"])
        kb.red(r1[:, 2, 0:T_], mv, ALU.max, rd=["M_msk"], wr=["M_r1"])
        kb.tt(mv, mv, r1[:, 2, 0:T_].unsqueeze(2).to_broadcast([128, T_, 16]), ALU.is_equal, rd=["M_msk", "M_r1"], wr=["M_msk"])
        kb.tt(bv, bv, mv, ALU.add, rd=["M_bi", "M_msk"], wr=["M_bi"])
        kb.tt(bv, bv, scv, ALU.mult, rd=["M_bi"] + sck, wr=["M_bi"])
        kb.red(r1[:, 3, 0:T_], bv, ALU.add, rd=["M_bi"], wr=["M_r1"])
        kb.recip(r1[:, 3, 0:T_], r1[:, 3, 0:T_], rd=["M_r1"], wr=["M_r1"])
        t0_ = tiles[0]
        kb.tt(gate_all[:, t0_:t0_ + T_, :], bv, r1[:, 3, 0:T_].unsqueeze(2).to_broadcast([128, T_, 16]), ALU.mult, rd=["M_bi", "M_r1"],
              wr=[("gate", i_) for i_ in tiles])


def moe_phase(kb, l, need_ctx, X, H2T, MOD, C, gate_all, out):
    nc = kb.nc
    last = (l == DEPTH - 1)
    tiles_all = list(range(NT)) if need_ctx else list(range(16))
    passes = [tiles_all[0:9], tiles_all[9:]]
    with ExitStack() as es:
        sb = lambda n, s, dt=F32: es.enter_context(nc.sbuf_tensor(kb.un(n), s, dt))
        ps = lambda n, s, dt=F32: es.enter_context(nc.psum_tensor(kb.un(n), s, dt))
        hT = sb("E_hT", [128, 8, 9 * 128], F32R)
        yacc = sb("E_yacc", [128, 9, D])
        Wg = [sb("E_Wg%d" % i, [128, 8, DE], F32R) for i in range(2)]
        Wu = [sb("E_Wu%d" % i, [128, 8, DE], F32R) for i in range(2)]
        Wd = [sb("E_Wd%d" % i, [128, 4, D], F32R) for i in range(2)]
        sg = [sb("E_sg%d" % i, [128, 512]) for i in range(2)]
        hid = sb("E_hid", [128, 4, 512], F32R)
        gmlp = [sb("E_gmlp%d" % s, [128, D]) for s in range(2)]
        xt = [sb("E_xt%d" % i, [128, D]) for i in range(2)]
        pg = [ps("E_pG%d" % i, [128, 512]) for i in range(2)]
        pu = [ps("E_pU%d" % i, [128, 512]) for i in range(2)]
        py = [ps("E_pY%d" % i, [128, 512]) for i in range(2)]
        for s in range(2):
            kb.dma(gmlp[s][:], MOD[l, s:s + 1, 5 * D:6 * D].to_broadcast([128, D]), rd=(), wr=["E_gmlp%d" % s])
        kg = 0
        ky = 0
        for pi, tl in enumerate(passes):
            ntl = len(tl)
            t0 = tl[0] * 128
            def load_w(e_):
                wb_ = e_ % 2
                kb.dma(Wg[wb_][:], C["e_gate"][l, e_].rearrange("(c p) n -> p c n", p=128), rd=(), wr=["E_Wg%d" % wb_], q="gpsimd")
                kb.dma(Wu[wb_][:], C["e_up"][l, e_].rearrange("(c p) n -> p c n", p=128), rd=(), wr=["E_Wu%d" % wb_], q="gpsimd")
                kb.dma(Wd[wb_][:], C["e_down"][l, e_].rearrange("(c p) n -> p c n", p=128), rd=(), wr=["E_Wd%d" % wb_], q="gpsimd")

            load_w(0)
            for c in range(8):
                kb.dma(hT[:, c, 0:ntl * 128], H2T[:, c, t0:t0 + ntl * 128], rd=(), wr=["E_hT"], q="gpsimd")
            load_w(1)
            groups = []
            ng_ = (ntl + 3) // 4
            a = 0
            for gi_ in range(ng_):
                n_ = (ntl - a + (ng_ - gi_) - 1) // (ng_ - gi_)
                groups.append((a, n_))
                a += n_
            for e in range(NE):
                wb = e % 2
                if e >= 2:
                    load_w(e)
                for (a, n_) in groups:
                    nt = n_ * 128
                    for hc in range(4):
                        b = kg % 2
                        kg += 1
                        for c in range(8):
                            kb.mm(pg[b][:, 0:nt], Wg[wb][:, c, hc * 128:(hc + 1) * 128], hT[:, c, a * 128:a * 128 + nt], c == 0, c == 7,
                                  rd=["E_Wg%d" % wb, "E_hT"], wr=["E_pG%d" % b])
                        for c in range(8):
                            kb.mm(pu[b][:, 0:nt], Wu[wb][:, c, hc * 128:(hc + 1) * 128], hT[:, c, a * 128:a * 128 + nt], c == 0, c == 7,
                                  rd=["E_Wu%d" % wb, "E_hT"], wr=["E_pU%d" % b])
                        kb.act(sg[b][:, 0:nt], pg[b][:, 0:nt], AF.Silu, rd=["E_pG%d" % b], wr=["E_sg%d" % b])
                        kb.tt(hid[:, hc, 0:nt], pu[b][:, 0:nt], sg[b][:, 0:nt], ALU.mult, rd=["E_pU%d" % b, "E_sg%d" % b], wr=[("E_hid", hc)])
                    for tt_ in range(n_):
                        ti = a + tt_
                        gi = tl[ti]
                        for hf in range(2):
                            b = ky % 2
                            ky += 1
                            for hc in range(4):
                                kb.mm(py[b][:], hid[:, hc, tt_ * 128:(tt_ + 1) * 128], Wd[wb][:, hc, hf * 512:(hf + 1) * 512], hc == 0, hc == 3,
                                      rd=[("E_hid", hc), "E_Wd%d" % wb], wr=["E_pY%d" % b])
                            ya = yacc[:, ti, hf * 512:(hf + 1) * 512]
                            yk = ("E_yacc", ti, hf)
                            gcol = gate_all[:, gi, e:e + 1]
                            if e == 0:
                                kb.ts(ya, py[b][:], gcol, None, ALU.mult, None, rd=["E_pY%d" % b, ("gate", gi)], wr=[yk])
                            else:
                                kb.stt(ya, py[b][:], gcol, ya, ALU.mult, ALU.add, rd=["E_pY%d" % b, ("gate", gi), yk], wr=[yk])
            for ti, gi in enumerate(tl):
                s = 0 if gi < 16 else 1
                b = ti % 2
                r0 = gi * 128
                kb.dma(xt[b][:], X[r0:r0 + 128, :], rd=[("X", gi)], wr=["E_xt%d" % b])
                kb.tt(yacc[:, ti, :], yacc[:, ti, :], gmlp[s][:], ALU.mult, rd=[("E_yacc", ti, 0), ("E_yacc", ti, 1), "E_gmlp%d" % s], wr=[("E_yacc", ti, 0), ("E_yacc", ti, 1)], eng="vector")
                kb.tt(xt[b][:], xt[b][:], yacc[:, ti, :], ALU.add, rd=["E_xt%d" % b, ("E_yacc", ti, 0), ("E_yacc", ti, 1)], wr=["E_xt%d" % b])
                if last:
                    kb.dma(out[r0:r0 + 128, :], xt[b][:], rd=["E_xt%d" % b], wr=[("out", gi)], semres="E_xt%d" % b, is_output=True)
                else:
                    kb.dma(X[r0:r0 + 128, :], xt[b][:], rd=["E_xt%d" % b], wr=[("X", gi)], semres="E_xt%d" % b)


def build(debug=(), stop_after=None):
    kb = KB(debug)
    nc, S = kb.nc, kb.S
    x_in = kb.inp("x", [SEQ, D])
    ctx_in = kb.inp("ctx", [CTX, D])
    cmat = kb.inp("cmat", [128, 8, 2])
    w_mod = kb.inp("w_mod", [DEPTH, D, 6 * D])
    b_mod = kb.inp("b_mod", [DEPTH, 6 * D])
    norm1 = kb.inp("norm1", [DEPTH, D])
    norm2 = kb.inp("norm2", [DEPTH, D])
    w_in = kb.inp("w_in", [DEPTH, D, D_IN])
    ident_in = kb.inp("ident", [128, 128])
    out = nc.dram_tensor("out", [SEQ, D], F32, kind="ExternalOutput").ap()
    X = kb.scratch("X", [NTOK, D])
    Z = kb.scratch("Z", [NTOK, D_IN])
    MOD = kb.scratch("MOD", [DEPTH, 2, 6 * D])
    YB = kb.scratch("YB", [NTOK, D])
    RW = kb.scratch("RW", [NTOK, 8, 256])
    H2T = kb.scratch("H2T", [128, 8, NTOK])
    C = {}
    for nm_, shp_ in (("b_shift", [DEPTH, 2, 896]), ("b_w0", [DEPTH, 2, 256]), ("b_w2", [DEPTH, 2, 32, 256]), ("b_a0", [DEPTH, 256]),
                      ("b_a2", [DEPTH, 32, 256]), ("b_g2", [DEPTH, 64, 256]), ("b_kk", [DEPTH, 256]), ("b_ka", [DEPTH, 256]),
                      ("b_rk", [DEPTH, 256]), ("b_lnx", [DEPTH, 2, 256]), ("rwkv_masks", [2, 128, 512])):
        C[nm_] = kb.inp(nm_, shp_)
    for nm_, shp_ in (("w_branch", [DEPTH, 4, 256, D]), ("w_out", [DEPTH, D, D]), ("router_w", [D, NE]), ("router_b", [NE]),
                      ("e_gate", [DEPTH, NE, D, DE]), ("e_up", [DEPTH, NE, D, DE]), ("e_down", [DEPTH, NE, DE, D])):
        C[nm_] = kb.inp(nm_, shp_)
    C["norm2"] = norm2
    C["a_qk_norm"] = kb.inp("a_qk_norm", [DEPTH, 2, 64])
    C["a_sink"] = kb.inp("a_sink", [DEPTH, 4])
    C["c_qk_norm"] = kb.inp("c_qk_norm", [DEPTH, 2, 32])
    C["c_lambda"] = kb.inp("c_lambda", [DEPTH, 4, 32])
    C["c_subln"] = kb.inp("c_subln", [DEPTH, 64])
    C["d_qk_norm"] = kb.inp("d_qk_norm", [DEPTH, 2, 64])
    C["ropeA"] = kb.inp("ropeA", [SEQ, 2, 384])
    C["ropeC"] = kb.inp("ropeC", [SEQ, 2, 512])
    C["bandmask"] = kb.inp("bandmask", [2, 128, 128])
    C["biasG"] = kb.inp("biasG", [DEPTH, 128, 8 * 4 * 4 * 64])
    C["maskD"] = kb.inp("maskD", [128, 64])

    with ExitStack() as es0:
        ident = es0.enter_context(nc.sbuf_tensor("ident_sb", [128, 128], F32))
        kb.dma(ident[:], ident_in, rd=(), wr=["ident"])
        gate_all = es0.enter_context(nc.sbuf_tensor("gate_all", [128, NT, 16], F32))
        kb.onez = es0.enter_context(nc.sbuf_tensor("onez", [128, 2], F32))
        kb.memset(kb.onez[:, 0:1], 1.0, wr=["onez"])
        kb.memset(kb.onez[:, 1:2], 0.0, wr=["onez"])

        for l in range(DEPTH):
            with ExitStack() as es:
                sb = lambda n, s, dt=F32: es.enter_context(nc.sbuf_tensor(kb.un(n), s, dt))
                ps = lambda n, s, dt=F32: es.enter_context(nc.psum_tensor(kb.un(n), s, dt))
                cT = sb("cT", [128, 8, 2])
                scT = sb("scT", [128, 8, 2], F32R)
                bm = sb("bm", [2, 6 * D])
                modsb = sb("modsb", [2, 6 * D])
                wm = [sb("wm%d" % i, [128, 8, 512], F32R) for i in range(2)]
                pm = [ps("pm%d" % i, [2, 512]) for i in range(2)]
                kb.dma(cT[:], cmat, rd=(), wr=["cT"])
                kb.dma(bm[:], b_mod[l:l + 1, :].to_broadcast([2, 6 * D]), rd=(), wr=["bm"])
                kb.act(scT[:], cT[:], AF.Silu, rd=["cT"], wr=["scT"])
                for cg in range(12):
                    w = wm[cg % 2]
                    wk = "wm%d" % (cg % 2)
                    pk = "pm%d" % (cg % 2)
                    kb.dma(w[:], w_mod[l, :, cg * 512:(cg + 1) * 512].rearrange("(c p) n -> p c n", p=128), rd=(), wr=[wk], q="gpsimd")
                    for c in range(8):
                        kb.mm(pm[cg % 2][:], scT[:, c, :], w[:, c, :], c == 0, c == 7, rd=["scT", wk], wr=[pk])
                    kb.tt(modsb[:, cg * 512:(cg + 1) * 512], pm[cg % 2][:], bm[:, cg * 512:(cg + 1) * 512], ALU.add, rd=[pk, "bm"], wr=["modsb"])
                kb.dma(MOD[l], modsb[:], rd=["modsb"], wr=[("MOD", l)])
            S.barrier()

            with ExitStack() as es:
                sb = lambda n, s, dt=F32: es.enter_context(nc.sbuf_tensor(kb.un(n), s, dt))
                ps = lambda n, s, dt=F32: es.enter_context(nc.psum_tensor(kb.un(n), s, dt))
                hT = sb("hT", [128, 8, NTOK], F32R)
                Am = [sb("Am%d" % s, [128, D]) for s in range(2)]
                Bm = [sb("Bm%d" % s, [128, D]) for s in range(2)]
                gbc = sb("gbc", [128, D])
                xt = [sb("xt%d" % i, [128, D]) for i in range(3)]
                sq = sb("sq", [128, D])
                ss = [sb("ss%d" % i, [128, 1]) for i in range(3)]
                hh = [sb("hh%d" % i, [128, D]) for i in range(3)]
                ptr = [ps("ptr%d" % i, [128, 4, 128]) for i in range(2)]
                kb.dma(gbc[:], norm1[l:l + 1, :].to_broadcast([128, D]), rd=(), wr=["gbc"])
                for s in range(2):
                    kb.dma(Am[s][:], MOD[l, s:s + 1, D:2 * D].to_broadcast([128, D]), rd=[("MOD", l)], wr=["Am%d" % s])
                    kb.dma(Bm[s][:], MOD[l, s:s + 1, 0:D].to_broadcast([128, D]), rd=[("MOD", l)], wr=["Bm%d" % s])
                    kb.stt(Am[s][:], Am[s][:], 1.0, gbc[:], ALU.add, ALU.mult, rd=["Am%d" % s, "gbc"], wr=["Am%d" % s])

                def h0(i):
                    b = i % 3
                    if l == 0:
                        src = x_in[i * 128:(i + 1) * 128, :] if i < 16 else ctx_in[(i - 16) * 128:(i - 15) * 128, :]
                        srck = ()
                    else:
                        src = X[i * 128:(i + 1) * 128, :]
                        srck = [("X", i)]
                    kb.dma(xt[b][:], src, rd=srck, wr=["xt%d" % b])
                    kb.act(sq[:], xt[b][:], AF.Square, rd=["xt%d" % b], wr=["sq", "ss%d" % b], accum_out=ss[b][:])
                    kb.ts(ss[b][:], ss[b][:], 1.0 / D, EPS, ALU.mult, ALU.add, rd=["ss%d" % b], wr=["ss%d" % b])
                    kb.act(ss[b][:], ss[b][:], AF.Sqrt, rd=["ss%d" % b], wr=["ss%d" % b])

                def h1(i):
                    b = i % 3
                    s = 0 if i < 16 else 1
                    kb.recip(ss[b][:], ss[b][:], rd=["ss%d" % b], wr=["ss%d" % b])
                    kb.stt(hh[b][:], xt[b][:], ss[b][:, 0:1], Am[s][:], ALU.mult, ALU.mult, rd=["xt%d" % b, "ss%d" % b, "Am%d" % s], wr=["hh%d" % b])
                    kb.tt(hh[b][:], hh[b][:], Bm[s][:], ALU.add, rd=["hh%d" % b, "Bm%d" % s], wr=["hh%d" % b], eng="gpsimd")

                def h2_(i):
                    b = i % 3
                    for g in range(2):
                        for j in range(4):
                            c = g * 4 + j
                            kb.tr(ptr[g][:, j, :], hh[b][:, c * 128:(c + 1) * 128], ident[:], rd=["hh%d" % b], wr=["ptr%d" % g])
                        kb.cp(hT[:, g * 4:(g + 1) * 4, i * 128:(i + 1) * 128], ptr[g][:], rd=["ptr%d" % g], wr=[("hT", i)],
                              eng="scalar" if g == 0 else "vector")

                pipeline([h0, h1, h2_], list(range(NT)))
                wz = [sb("wz%d" % i, [128, 8, 512], F32R) for i in range(2)]
                pz = [ps("pz%d" % i, [128, 512]) for i in range(4)]
                zs = [sb("zs%d" % i, [128, 512]) for i in range(4)]
                k = 0
                for cg in range(14):
                    n0 = cg * 512
                    ncol = min(512, D_IN - n0)
                    w = wz[cg % 2]
                    wk = "wz%d" % (cg % 2)
                    kb.dma(w[:, :, 0:ncol], w_in[l, :, n0:n0 + ncol].rearrange("(c p) n -> p c n", p=128), rd=(), wr=[wk], q="gpsimd")
                    for i in range(NT):
                        r = k % 4
                        k += 1
                        for c in range(8):
                            kb.mm(pz[r][:, 0:ncol], hT[:, c, i * 128:(i + 1) * 128], w[:, c, 0:ncol], c == 0, c == 7,
                                  rd=[("hT", i), wk], wr=["pz%d" % r])
                        kb.cp(zs[r][:, 0:ncol], pz[r][:, 0:ncol], rd=["pz%d" % r], wr=["zs%d" % r], eng="scalar" if r % 2 == 0 else "vector")
                        kb.dma(Z[i * 128:(i + 1) * 128, n0:n0 + ncol], zs[r][:, 0:ncol], rd=["zs%d" % r], wr=[("Z", i, cg)], semres="zs%d" % r)
            S.barrier()
            if stop_after == ("proj", l):
                break
            need_ctx = l < DEPTH - 1
            if stop_after != ("C", l):
                rwkv(kb, l, need_ctx, Z, YB, RW, ident, C)
                S.barrier()
            if stop_after == ("B", l):
                break
            attn_A(kb, l, need_ctx, Z, YB, ident, C)
            S.barrier()
            if stop_after == ("A", l):
                break
            attn_D(kb, l, need_ctx, Z, YB, ident, C)
            S.barrier()
            if stop_after == ("D", l):
                break
            attn_C(kb, l, need_ctx, Z, YB, ident, C)
            S.barrier()
            if stop_after == ("C", l):
                break
            merge_phase(kb, l, need_ctx, x_in, ctx_in, X, Z, YB, H2T, MOD, ident, C, gate_all)
            S.barrier()
            if stop_after == ("M", l):
                break
            moe_phase(kb, l, need_ctx, X, H2T, MOD, C, gate_all, out)
            S.barrier()
    S.barrier()
    cnt = S.finish()
    print("instr counts", cnt, "dma sems", S.n_dma_sems)
    return kb


_CONST_CACHE = {}


def host_constants():
    if _CONST_CACHE:
        return _CONST_CACHE
    t = np.arange(SEQ)
    rows, cols = (t // 64).astype(np.float32), (t % 64).astype(np.float32)

    def table(hd, G):
        h = hd // 2
        inv = (np.float32(10000.0) ** (-np.arange(0, h, 2, dtype=np.float32) / np.float32(h))).astype(np.float32)
        ar = rows[:, None] * inv[None, :]
        ac = cols[:, None] * inv[None, :]
        cos = np.concatenate([np.cos(ar), np.cos(ar), np.cos(ac), np.cos(ac)], -1).astype(np.float32)
        sin = np.concatenate([-np.sin(ar), np.sin(ar), -np.sin(ac), np.sin(ac)], -1).astype(np.float32)
        return np.ascontiguousarray(np.stack([np.tile(cos, (1, G)), np.tile(sin, (1, G))], 1))
    _CONST_CACHE["ropeA"] = table(64, 6)
    _CONST_CACHE["ropeC"] = table(32, 16)
    b = np.arange(128)[:, None]
    a = np.arange(128)[None, :]
    _CONST_CACHE["bandmask"] = np.stack([(a <= b), (b <= a)]).astype(np.float32)
    p = np.arange(128)
    kcol = p % 64
    q = np.arange(64)
    cstart = np.clip(q - 8, 0, 48)
    _CONST_CACHE["maskD"] = ((kcol[:, None] >= cstart[None, :]) & (kcol[:, None] < cstart[None, :] + 16)).astype(np.float32)
    _CONST_CACHE["ident"] = np.eye(128, dtype=np.float32)
    s_ = np.arange(128)[:, None]
    t_ = np.arange(128)[None, :]
    mk = []
    for d in range(2):
        le = (s_ <= t_) if d == 0 else (s_ >= t_)
        lt = (s_ < t_) if d == 0 else (s_ > t_)
        mk.append(np.concatenate([le, lt, le, lt.T], 1).astype(np.float32))
    _CONST_CACHE["rwkv_masks"] = np.stack(mk)
    return _CONST_CACHE


def gather_biasG(d_rpb):
    d_rpb = np.asarray(d_rpb, dtype=np.float32)
    p = np.arange(128)
    jl, kcol = p // 64, p % 64
    q = np.arange(64)
    dc = np.clip(kcol[:, None] - q[None, :] + 15, 0, 30)
    out = np.zeros((DEPTH, 128, 8, 4, 4, 64), np.float32)
    for cls in range(8):
        r = cls if cls < 4 else (4 if cls == 4 else 24 + cls)
        start = min(max(r - 4, 0), 24)
        for t in range(4):
            key_row = start + 2 * t + jl
            dr = key_row - r + 7
            for l in range(DEPTH):
                for h in range(4):
                    out[l, :, cls, h, t, :] = d_rpb[l, h][dr[:, None], dc]
    return np.ascontiguousarray(out.reshape(DEPTH, 128, 8 * 4 * 4 * 64))


def make_in_map(inp, b, kb, shared=None):
    f = lambda a: np.ascontiguousarray(np.asarray(a, dtype=np.float32))
    cm = np.stack([np.asarray(inp["c"][b]).reshape(8, 128).T, np.asarray(inp["c_ctx"]).reshape(8, 128).T], axis=-1)
    m = {"x": f(inp["x"][b]), "ctx": f(inp["ctx"][b]), "cmat": f(cm)}
    if shared is None:
        shared = {}
    if "biasG" not in shared:
        shared.update(host_constants())
        shared["biasG"] = gather_biasG(inp["d_rpb"])
    for k in kb.ins:
        if k in m:
            continue
        if k not in shared:
            shared[k] = f(inp[k])
        m[k] = shared[k]
    return m


_KB_CACHE = {}


def kernel(**inputs):
    inp = {k: np.asarray(v) for k, v in inputs.items()}
    if "kb" not in _KB_CACHE:
        _KB_CACHE["kb"] = build()
    kb = _KB_CACHE["kb"]
    shared = {}
    n = 8
    in_maps = [make_in_map(inp, b, kb, shared) for b in range(n)]
    res = run_bass_kernel_spmd(kb.nc, in_maps, core_ids=list(range(n)))
    return np.stack([np.asarray(res.results[b]["out"], dtype=np.float32) for b in range(n)], axis=0)
```
